# Optimizing a Trainium2 kernel written in Bass

```python
import jax, jax.numpy as jnp
from jax import lax
import numpy as np

D_MODEL = 2048
BATCH = 1
SEQ = 16384
DEPTH = 1

N_META = 16
EPS = 1e-6
GDN_QK_HEADS = 16
GDN_V_HEADS = 32
GDN_HEAD_DIM = 128
GDN_CONV = 4
GDN_CHUNK = 64
ATT_HEADS = 16
ATT_KV_HEADS = 2
ATT_HEAD_DIM = 128
IDX_HEADS = 16
IDX_HEAD_DIM = 128
TOPK_MAX = 256
Q_BLOCK = 128
NEG = -1e30
ROPE_THETA = 500000.0
ROPE_FRACTION = 4
D_FF = 3 * D_MODEL
FFN_CONV = 3

GDN_QK_W = GDN_QK_HEADS * GDN_HEAD_DIM
GDN_V_W = GDN_V_HEADS * GDN_HEAD_DIM
GDN_CONV_W = 2 * GDN_QK_W + GDN_V_W
ATT_Q_W = ATT_HEADS * ATT_HEAD_DIM
ATT_KV_W = ATT_KV_HEADS * ATT_HEAD_DIM
IDX_Q_W = IDX_HEADS * IDX_HEAD_DIM
IN_SPLITS = (GDN_QK_W, GDN_QK_W, GDN_V_W,
             GDN_V_W, GDN_V_HEADS, GDN_V_HEADS,
             ATT_Q_W, ATT_KV_W, ATT_KV_W,
             IDX_Q_W, IDX_HEAD_DIM, IDX_HEADS,
             D_MODEL, D_MODEL)
D_IN = sum(IN_SPLITS)

kernel_name = 'hybrid_gdn_dsa_gated_block'


def rmsnorm(x, g):
    xf = x.astype(jnp.float32)
    y = xf * lax.rsqrt(jnp.mean(xf * xf, axis=-1, keepdims=True) + EPS)
    return (y * g.astype(jnp.float32)).astype(x.dtype)


def l2norm(x):
    return x * lax.rsqrt(jnp.sum(x * x, axis=-1, keepdims=True) + EPS)


def causal_dwconv(x, w):
    K = w.shape[0]
    L = x.shape[1]
    xp = jnp.pad(x, ((0, 0), (K - 1, 0), (0, 0)))
    y = xp[:, 0:L] * w[0]
    for j in range(1, K):
        y = y + xp[:, j:j + L] * w[j]
    return y


def partial_rope(x, pos):
    r = x.shape[-1] // ROPE_FRACTION
    half = r // 2
    inv = ROPE_THETA ** (-jnp.arange(half, dtype=jnp.float32) / half)
    ang = pos.astype(jnp.float32)[:, None] * inv[None, :]
    cos = jnp.cos(ang)[None, :, None, :]
    sin = jnp.sin(ang)[None, :, None, :]
    xr = x[..., :r].astype(jnp.float32)
    x1, x2 = xr[..., :half], xr[..., half:]
    rot = jnp.concatenate([x1 * cos - x2 * sin, x2 * cos + x1 * sin], axis=-1)
    return jnp.concatenate([rot.astype(x.dtype), x[..., r:]], axis=-1)


def gated_deltanet(q, k, v, z, b, a, conv_w, a_log, dt_bias, norm_g):
    f32 = jnp.float32
    B, L, _ = q.shape
    H, Dh, C = GDN_V_HEADS, GDN_HEAD_DIM, GDN_CHUNK
    qkv = jax.nn.silu(causal_dwconv(jnp.concatenate([q, k, v], axis=-1), conv_w)).astype(f32)
    q, k, v = jnp.split(qkv, [GDN_QK_W, 2 * GDN_QK_W], axis=-1)
    rep = GDN_V_HEADS // GDN_QK_HEADS
    q = jnp.repeat(l2norm(q.reshape(B, L, GDN_QK_HEADS, Dh)), rep, axis=2) * (Dh ** -0.5)
    k = jnp.repeat(l2norm(k.reshape(B, L, GDN_QK_HEADS, Dh)), rep, axis=2)
    v = v.reshape(B, L, H, Dh)
    beta = jax.nn.sigmoid(b.astype(f32))
    g = -jnp.exp(a_log.astype(f32)) * jax.nn.softplus(a.astype(f32) + dt_bias.astype(f32))
    lead = (-N_META) % C
    tail = (-(lead + L)) % C
    n = (lead + L + tail) // C

    def to_chunks(t):
        t = jnp.pad(t, ((0, 0), (lead, tail)) + ((0, 0),) * (t.ndim - 2))
        t = t.reshape((B, n, C) + t.shape[2:])
        return jnp.moveaxis(t, (1, 3), (0, 2))

    incl = jnp.tril(jnp.ones((C, C), dtype=bool))
    strict = jnp.tril(jnp.ones((C, C), dtype=bool), -1)
    eye = jnp.eye(C, dtype=f32)

    def step(S, inp):
        qc, kc, vc, bc, gc = inp
        G = jnp.cumsum(gc, axis=-1)
        diff = G[..., :, None] - G[..., None, :]
        decay = jnp.where(incl, jnp.exp(jnp.where(incl, diff, 0.0)), 0.0)
        kk = jnp.einsum('bhid,bhjd->bhij', kc, kc)
        lower = jnp.where(strict, bc[..., :, None] * kk * decay, 0.0)
        rhs = jnp.concatenate([vc * bc[..., None], kc * (bc * jnp.exp(G))[..., None]], axis=-1)
        sol = lax.linalg.triangular_solve(eye + lower, rhs, left_side=True, lower=True)
        u, w = sol[..., :Dh], sol[..., Dh:]
        v_new = u - jnp.einsum('bhck,bhkv->bhcv', w, S)
        attn = jnp.where(incl, jnp.einsum('bhid,bhjd->bhij', qc, kc) * decay, 0.0)
        o = (jnp.einsum('bhck,bhkv->bhcv', qc * jnp.exp(G)[..., None], S)
             + jnp.einsum('bhij,bhjv->bhiv', attn, v_new))
        G_last = G[..., -1:]
        S = (S * jnp.exp(G_last)[..., None]
             + jnp.einsum('bhck,bhcv->bhkv', kc * jnp.exp(G_last - G)[..., None], v_new))
        return S, o

    S0 = jnp.zeros((B, H, Dh, Dh), f32)
    _, o = lax.scan(step, S0, (to_chunks(q), to_chunks(k), to_chunks(v),
                               to_chunks(beta), to_chunks(g)))
    o = jnp.moveaxis(o, (0, 2), (1, 3)).reshape(B, n * C, H, Dh)[:, lead:lead + L]
    o = rmsnorm(o, norm_g) * jax.nn.silu(z.astype(f32).reshape(B, L, H, Dh))
    return o.reshape(B, L, GDN_V_W).astype(z.dtype)


def dsa_attention(q, k, v, iq, ik, iw, pos):
    f32 = jnp.float32
    B, L, _ = q.shape
    topk = min(TOPK_MAX, L // 4)
    q = partial_rope(q.reshape(B, L, ATT_HEADS, ATT_HEAD_DIM), pos)
    k = partial_rope(k.reshape(B, L, ATT_KV_HEADS, ATT_HEAD_DIM), pos)
    v = v.reshape(B, L, ATT_KV_HEADS, ATT_HEAD_DIM)
    iq = partial_rope(iq.reshape(B, L, IDX_HEADS, IDX_HEAD_DIM), pos)
    ik = partial_rope(ik.reshape(B, L, 1, IDX_HEAD_DIM), pos)[:, :, 0]
    iw = iw.astype(f32) * ((IDX_HEADS ** -0.5) * (IDX_HEAD_DIM ** -0.5))
    nb = -(-L // Q_BLOCK)
    Lp = nb * Q_BLOCK
    group = ATT_HEADS // ATT_KV_HEADS
    key_pos = jnp.arange(L, dtype=jnp.int32)

    def to_blocks(t):
        t = jnp.pad(t, ((0, 0), (0, Lp - L)) + ((0, 0),) * (t.ndim - 2))
        t = t.reshape((B, nb, Q_BLOCK) + t.shape[2:])
        return jnp.moveaxis(t, 1, 0)

    def attend(inp):
        qb, iqb, iwb, start = inp
        t = start + jnp.arange(Q_BLOCK, dtype=jnp.int32)
        visible = key_pos[None, :] <= t[:, None]
        logits = jnp.einsum('bqhd,bsd->bqhs', iqb, ik, preferred_element_type=f32)
        score = jnp.einsum('bqhs,bqh->bqs', jax.nn.relu(logits), iwb)
        score = jnp.where(visible[None], score, NEG)
        _, idx = lax.top_k(score, topk)
        valid = idx <= t[None, :, None]
        ksel = jax.vmap(lambda kk, ii: kk[ii])(k, idx)
        vsel = jax.vmap(lambda vv, ii: vv[ii])(v, idx)
        qg = qb.reshape(B, Q_BLOCK, ATT_KV_HEADS, group, ATT_HEAD_DIM)
        s = jnp.einsum('bqgrd,bqkgd->bqgrk', qg, ksel, preferred_element_type=f32) * (ATT_HEAD_DIM ** -0.5)
        s = jnp.where(valid[:, :, None, None, :], s, NEG)
        p = jax.nn.softmax(s, axis=-1)
        o = jnp.einsum('bqgrk,bqkgd->bqgrd', p.astype(vsel.dtype), vsel)
        return o.reshape(B, Q_BLOCK, ATT_Q_W)

    starts = jnp.arange(nb, dtype=jnp.int32) * Q_BLOCK
    out = lax.map(attend, (to_blocks(q), to_blocks(iq), to_blocks(iw), starts))
    return jnp.moveaxis(out, 0, 1).reshape(B, Lp, ATT_Q_W)[:, :L]


def setup_inputs(seed: int = 0) -> dict:
    key = jax.random.key(seed)
    ks = jax.random.split(key, 18)
    f32 = jnp.float32

    def nrm(k, shape, scale):
        return jax.random.normal(k, shape, f32) * scale

    def gain(k, shape):
        return 1.0 + 0.05 * jax.random.normal(k, shape, f32)

    last_tap = (jnp.arange(FFN_CONV) == FFN_CONV - 1).astype(f32)[:, None]
    return {
        'x': nrm(ks[0], (BATCH, SEQ, D_MODEL), 1.0),
        'meta_tokens': nrm(ks[1], (N_META, D_MODEL), 1.0),
        'mix_pre_g': gain(ks[2], (DEPTH, D_MODEL)),
        'w_in': nrm(ks[3], (DEPTH, D_MODEL, D_IN), D_MODEL ** -0.5),
        'gdn_conv_w': nrm(ks[4], (DEPTH, GDN_CONV, GDN_CONV_W), GDN_CONV ** -0.5),
        'gdn_a_log': jnp.log(jax.random.uniform(ks[5], (DEPTH, GDN_V_HEADS), f32, 1.0, 16.0)),
        'gdn_dt_bias': nrm(ks[6], (DEPTH, GDN_V_HEADS), 0.1),
        'gdn_norm_g': gain(ks[7], (DEPTH, GDN_HEAD_DIM)),
        'w_branch_gdn': nrm(ks[8], (DEPTH, GDN_V_W, D_MODEL), GDN_V_W ** -0.5),
        'w_branch_att': nrm(ks[9], (DEPTH, ATT_Q_W, D_MODEL), ATT_Q_W ** -0.5),
        'w_out': nrm(ks[10], (DEPTH, D_MODEL, D_MODEL), D_MODEL ** -0.5),
        'mix_post_g': gain(ks[11], (DEPTH, D_MODEL)),
        'ffn_pre_g': gain(ks[12], (DEPTH, D_MODEL)),
        'w_up': nrm(ks[13], (DEPTH, D_MODEL, 2 * D_FF), D_MODEL ** -0.5),
        'ffn_conv_w': nrm(ks[14], (DEPTH, FFN_CONV, 2 * D_FF), 0.3) + last_tap,
        'ffn_conv_b': nrm(ks[15], (DEPTH, 2 * D_FF), 0.02),
        'w_down': nrm(ks[16], (DEPTH, D_FF, D_MODEL), D_FF ** -0.5),
        'ffn_post_g': gain(ks[17], (DEPTH, D_MODEL)),
    }


def reference(x, meta_tokens, mix_pre_g, w_in, gdn_conv_w, gdn_a_log, gdn_dt_bias, gdn_norm_g,
              w_branch_gdn, w_branch_att, w_out, mix_post_g, ffn_pre_g, w_up, ffn_conv_w,
              ffn_conv_b, w_down, ffn_post_g):
    B = x.shape[0]
    meta = jnp.broadcast_to(meta_tokens.astype(x.dtype)[None], (B, N_META, D_MODEL))
    h = jnp.concatenate([meta, x], axis=1)
    L = h.shape[1]
    pos = jnp.arange(L, dtype=jnp.int32)
    split_at = np.cumsum(IN_SPLITS)[:-1].tolist()
    for i in range(DEPTH):
        u = rmsnorm(h, mix_pre_g[i])
        (gq, gk, gv, gz, gb, ga, aq, ak, av, iq, ik, iw, gate_gdn, gate_att) = jnp.split(
            u @ w_in[i], split_at, axis=-1)
        y_gdn = gated_deltanet(gq, gk, gv, gz, gb, ga, gdn_conv_w[i], gdn_a_log[i],
                               gdn_dt_bias[i], gdn_norm_g[i]) @ w_branch_gdn[i]
        y_att = dsa_attention(aq, ak, av, iq, ik, iw, pos) @ w_branch_att[i]
        merged = jax.nn.sigmoid(gate_gdn) * y_gdn + jax.nn.sigmoid(gate_att) * y_att
        h = h + rmsnorm(merged @ w_out[i], mix_post_g[i])
        u = rmsnorm(h, ffn_pre_g[i])
        up = causal_dwconv(u @ w_up[i], ffn_conv_w[i]) + ffn_conv_b[i]
        gate, val = jnp.split(up, 2, axis=-1)
        h = h + rmsnorm((jax.nn.silu(gate) * val) @ w_down[i], ffn_post_g[i])
    return h[:, N_META:]
```

```python
import numpy as np
import concourse.bass as bass
import concourse.mybir as mybir
from concourse.bass_utils import run_bass_kernel_spmd
from contextlib import ExitStack

F32 = mybir.dt.float32; BF16 = mybir.dt.bfloat16; I32 = mybir.dt.int32
AF = mybir.ActivationFunctionType; ALU = mybir.AluOpType

D = 2048
NMETA = 16
PADF = 112
EPS = 1e-6
NCORE = 8
HPC = 4
NFF = 6144
ROPE_THETA = 500000.0


class Buf:
    __slots__ = ('w', 'r', 'multi')
    def __init__(self, multi=False):
        self.w = {}; self.r = {}; self.multi = multi


class Tl:
    def __init__(self, t, multi=False):
        self.t = t; self.b = Buf(multi)
    def __getitem__(self, k):
        return self.t[k]


class Prog:
    ENG = ('pe', 'act', 'dve', 'pool', 'sp')
    def __init__(self, nc, es, n_dma=12):
        self.nc = nc; self.es = es
        self.ops = {e: [] for e in self.ENG}
        self.cnt = {e: 0 for e in self.ENG}
        self.sem = {e: es.enter_context(nc.semaphore('s_' + e)) for e in ('pe', 'act', 'dve', 'pool')}
        self.seen = {e: {} for e in self.ENG}
        self.dsem = {q: [[es.enter_context(nc.semaphore(f'd_{q}{i}')), 0] for i in range(n_dma)] for q in ('sp', 'pool')}
        self.drr = {q: 0 for q in ('sp', 'pool')}
        self.nops = 0

    def op(self, eng, fn, reads=(), writes=(), dma=False):
        deps = {}
        def add(ev):
            s, v = ev
            k = id(s)
            if k not in deps or deps[k][1] < v: deps[k] = (s, v)
        for t in reads:
            b = t.b if isinstance(t, Tl) else t
            for ev in b.w.values(): add(ev)
        for t in writes:
            b = t.b if isinstance(t, Tl) else t
            if not b.multi:
                for ev in b.w.values(): add(ev)
                for ev in b.r.values(): add(ev)
        if dma:
            slots = self.dsem[eng]
            slot = slots[self.drr[eng] % len(slots)]; self.drr[eng] += 1
            if slot[1] > 0: add((slot[0], slot[1]))
            slot[1] += 16
            ev = (slot[0], slot[1]); inc = 16
        else:
            self.cnt[eng] += 1
            ev = (self.sem[eng], self.cnt[eng]); inc = 1
        waits = []
        seen = self.seen[eng]
        own = id(self.sem['pe']) if eng == 'pe' else None
        for k, (s, v) in deps.items():
            if k == own: continue
            if seen.get(k, 0) >= v: continue
            seen[k] = v; waits.append((s, v))
        for t in writes:
            b = t.b if isinstance(t, Tl) else t
            if b.multi:
                b.w[id(ev[0])] = ev
            else:
                b.w = {id(ev[0]): ev}; b.r = {}
        for t in reads:
            b = t.b if isinstance(t, Tl) else t
            if not b.multi:
                b.r[id(ev[0])] = ev
        self.ops[eng].append((waits, fn, ev, inc))
        self.nops += 1
        return ev

    def emit(self):
        nc = self.nc
        fin = []
        for e in ('pe', 'act', 'dve', 'pool'):
            if self.cnt[e]: fin.append((self.sem[e], self.cnt[e]))
        for q in self.dsem:
            for s, v in self.dsem[q]:
                if v: fin.append((s, v))
        bar = getattr(self, 'barrier', [])
        def run(name, e):
            for s, v in bar: e.wait_ge(s, v)
            for waits, fn, ev, inc in self.ops[name]:
                for (s, v) in waits: e.wait_ge(s, v)
                ins = fn(e)
                ins.then_inc(ev[0], inc)
            if name == 'sp':
                for s, v in fin: e.wait_ge(s, v)
            self.ops[name] = []
        self.barrier = fin
        with nc.Block() as block:
            @block.tensor
            def _(e): run('pe', e)
            @block.scalar
            def _(e): run('act', e)
            @block.vector
            def _(e): run('dve', e)
            @block.gpsimd
            def _(e): run('pool', e)
            @block.sync
            def _(e): run('sp', e)


class Ctx:
    def __init__(self, nc, es):
        self.nc = nc; self.es = es; self.P = Prog(nc, es)
        self._n = 0
    def scope(self):
        st = ExitStack()
        if not hasattr(self, '_stk'): self._stk = []
        self._stk.append(self.es); self.es = st
        return st
    def unscope(self):
        self.es = self._stk.pop()
    def sb(self, shape, dt, name=None):
        self._n += 1
        return Tl(self.es.enter_context(self.nc.sbuf_tensor(f"{name or 'sb'}_{self._n}", list(shape), dt)))
    def ps(self, shape, dt=F32, name=None):
        self._n += 1
        return Tl(self.es.enter_context(self.nc.psum_tensor(f"{name or 'ps'}_{self._n}", list(shape), dt)))
    def din(self, name, shape, dt=F32):
        return Tl(self.nc.dram_tensor(name, list(shape), dt, kind="ExternalInput").ap(), multi=True)
    def dout(self, name, shape, dt=F32):
        return Tl(self.nc.dram_tensor(name, list(shape), dt, kind="ExternalOutput").ap(), multi=True)
    def dtmp(self, name, shape, dt=BF16):
        return Tl(self.nc.dram_tensor(name, list(shape), dt, kind="Internal").ap(), multi=True)


CI_ID, CI_TRIU, CI_NEGM, CI_OFFD, CI_ONES = 0, 1, 2, 3, 4
def make_consts():
    c = np.zeros((128, 5, 128), np.float32)
    j = np.arange(128)[:, None]; i = np.arange(128)[None, :]
    c[:, CI_ID] = (i == j)
    c[:, CI_TRIU] = (j <= i)
    c[:, CI_NEGM] = np.where(i >= j, 0.0, -1e9)
    c[:, CI_OFFD] = (i != j)
    c[:, CI_ONES] = 1.0
    return c


def rope_table(pos):
    half = 16
    inv = ROPE_THETA ** (-np.arange(half, dtype=np.float32) / half)
    ang = pos.astype(np.float32)[:, None] * inv[None, :].astype(np.float32)
    return np.concatenate([np.cos(ang), np.sin(ang)], axis=1).astype(np.float32)


NA_FM = 1024
NA_TM = 512 + 8 + 256 + 256 + 128
NA = NA_FM + NA_TM


def emit_gdn_all(K, Lp, NG, hfull, w_a, gpre, cw, hp, ng, cst, rope, og_o, KT_o, V_o, ikT_o, scr, dbgt=None):
    P = K.P; nblk = Lp // 128; dbg = dbgt is not None
    if dbg: dbg_gb, dbg_q, dbg_k, dbg_v = dbgt
    if True:
        cf = K.sb([128, 5, 128], F32, "cf")
        P.op('sp', lambda e: e.dma_start(out=cf[:], in_=cst[:]), writes=[cf], dma=True)
        epsc = K.sb([128, 1], F32, "epsc")
        P.op('dve', lambda e: e.memset(epsc[:], EPS), writes=[epsc])
        cb = K.sb([128, 5, 128], BF16, "cb")
        P.op('dve', lambda e: e.tensor_copy(out=cb[:], in_=cf[:]), reads=[cf], writes=[cb])
        gpre_s = K.sb([128, 16], F32); cw_s = K.sb([128, 8, 4], F32); hp_s = K.sb([128, 3, HPC], F32); ng_s = K.sb([128, 128], F32)
        for dst, src in ((gpre_s, gpre), (ng_s, ng)):
            P.op('sp', lambda e, dst=dst, src=src: e.dma_start(out=dst[:], in_=src[:]), writes=[dst], dma=True)
        nea = K.sb([128, HPC], F32)
        ut_d = K.dtmp("ut_d", [128, 16, Lp]) if NG > 1 else None
        NPAR = min(2, NG)
        gb_alls = [K.sb([128, nblk, 8], F32, f"gb_all{i}") for i in range(NPAR)]
        pend = []

        for g in range(NG):
            gb_all = gb_alls[g % NPAR]
            qT_d, kT_d, k_d, v_d, sz_d = scr[g % NPAR]
            P.op('sp', lambda e, g=g: e.dma_start(out=cw_s[:], in_=cw[g]), writes=[cw_s], dma=True)
            P.op('sp', lambda e, g=g: e.dma_start(out=hp_s[:], in_=hp[g]), writes=[hp_s], dma=True)
            P.op('act', lambda e: e.activation(out=nea[:], in_=hp_s[:, 0, :], func=AF.Exp), reads=[hp_s], writes=[nea])
            P.op('dve', lambda e: e.tensor_scalar(out=nea[:], in0=nea[:], scalar1=-1.0, scalar2=None, op0=ALU.mult), reads=[nea], writes=[nea])
            scA = K.scope()
            wA = K.sb([128, 16, NA], BF16, "wA")
            wst = [K.sb([128, NA], F32, f"wst{i}") for i in range(1)]
            w_a_v = w_a.t[g].rearrange("(kc p) n -> p kc n", p=128)
            for kc in range(16):
                st = wst[0]
                P.op('sp', lambda e, st=st, kc=kc: e.dma_start(out=st[:], in_=w_a_v[:, kc, :]), writes=[st], dma=True)
                P.op('act', lambda e, st=st, kc=kc: e.activation(out=wA[:, kc, :], in_=st[:], func=AF.Copy, scale=gpre_s[:, kc:kc + 1]),
                     reads=[st, gpre_s], writes=[wA])

            TB = 512
            xf = [K.sb([128, D], F32, f"xf{i}") for i in range(2)]
            xs = [K.sb([128, D], BF16, f"xs{i}") for i in range(2)]
            ss = [K.sb([128, 2], F32, f"ss{i}") for i in range(2)]
            uTs = [K.sb([128, 16, TB], BF16, f"uT{i}") for i in range(2)]
            pre = K.sb([128, 8, 3 + TB], F32, "pre")
            pre_b = [Buf() for _ in range(8)]
            P.op('dve', lambda e: e.memset(pre[:], 0.0), writes=pre_b)
            cv = [K.sb([128, TB], F32, f"cv{i}") for i in range(2)]
            sl = [K.sb([128, TB], F32, f"sl{i}") for i in range(2)]
            sq = [K.sb([128, TB], BF16, f"sq{i}") for i in range(2)]
            rrs = [K.sb([128, TB], F32, f"rr{i}") for i in range(2)]
            fmTs = [K.sb([128, 8, TB], BF16, f"fmT{i}") for i in range(2)]
            tm_kv = [K.sb([128, 6, 128], BF16, f"tmkv{i}") for i in range(2)]
            ztm = [K.sb([128, 512], BF16, f"ztm{i}") for i in range(2)]
            ba = [K.sb([128, 8], F32, f"ba{i}") for i in range(2)]
            vst = [K.sb([128, 256], BF16, f"vst{i}") for i in range(2)]
            kif = [K.sb([128, 3, 128], F32, f"kif{i}") for i in range(2)]
            kib = [K.sb([128, 3, 128], BF16, f"kib{i}") for i in range(2)]
            rt = [K.sb([128, 32], F32, f"rt{i}") for i in range(2)]
            rtmp = [K.sb([128, 4, 3, 16], F32, f"rtmp{i}") for i in range(2)]
            kiT = [K.sb([128, 3, 128], BF16, f"kiT{i}") for i in range(2)]
            ps_tr = [K.ps([128, 4, 128], BF16, f"pstr{i}") for i in range(2)]
            ps_mm = [K.ps([128, 512], F32, f"psmm{i}") for i in range(3)]
            ps_n = K.ps([128, 512], F32, "psn")
            nmm = [0]
            def next_mm():
                nmm[0] += 1
                return ps_mm[nmm[0] % 3]
            ntr = [0]
            def next_tr():
                ntr[0] += 1
                return ps_tr[ntr[0] % 2]

            nsb = (Lp + TB - 1) // TB
            blk = 0
            for sbi in range(nsb):
                t0 = sbi * TB
                n_sub = min(4, (Lp - t0) // 128)
                TBn = n_sub * 128
                uT = uTs[sbi % 2]; fmT = fmTs[sbi % 2]
                if g > 0:
                    P.op('sp', lambda e, uT=uT, t0=t0, TBn=TBn: e.dma_start(out=uT[:, :, 0:TBn], in_=ut_d[:, :, t0:t0 + TBn]), reads=[ut_d], writes=[uT], dma=True)
                for j in range(n_sub if g == 0 else 0):
                    b = sbi * 4 + j
                    x_f = xf[b % 2]; x_s = xs[b % 2]; s_s = ss[b % 2]
                    P.op('sp', lambda e, x_f=x_f, b=b: e.dma_start(out=x_f[:], in_=hfull[b * 128:(b + 1) * 128, :]), writes=[x_f], dma=True)
                    P.op('act', lambda e, x_f=x_f, s_s=s_s, x_s=x_s: e.activation(out=x_s[:], in_=x_f[:], func=AF.Square, accum_out=s_s[:, 0:1]),
                         reads=[x_f], writes=[x_s, s_s])
                    P.op('act', lambda e, s_s=s_s: e.activation(out=s_s[:, 1:2], in_=s_s[:, 0:1], func=AF.Sqrt, scale=1.0 / D, bias=epsc[:, 0:1]),
                         reads=[s_s, epsc], writes=[s_s])
                    P.op('dve', lambda e, s_s=s_s: e.reciprocal(out=s_s[:, 1:2], in_=s_s[:, 1:2]), reads=[s_s], writes=[s_s])
                    P.op('dve', lambda e, x_f=x_f, x_s=x_s, s_s=s_s: e.tensor_scalar(out=x_s[:], in0=x_f[:], scalar1=s_s[:, 1:2], scalar2=None, op0=ALU.mult),
                         reads=[x_f, s_s], writes=[x_s])
                    for g4 in range(4):
                        pt = next_tr()
                        for q in range(4):
                            kc = g4 * 4 + q
                            P.op('pe', lambda e, pt=pt, q=q, kc=kc, x_s=x_s: e.transpose(out=pt[:, q, :], in_=x_s[:, kc * 128:(kc + 1) * 128], identity=cb[:, CI_ID, :]),
                                 reads=[x_s, cb], writes=[pt])
                        P.op('act' if g4 % 2 else 'dve',
                             (lambda e, uT=uT, pt=pt, g4=g4, j=j: e.activation(out=uT[:, g4 * 4:(g4 + 1) * 4, j * 128:(j + 1) * 128], in_=pt[:], func=AF.Copy)) if g4 % 2 else
                             (lambda e, uT=uT, pt=pt, g4=g4, j=j: e.tensor_copy(out=uT[:, g4 * 4:(g4 + 1) * 4, j * 128:(j + 1) * 128], in_=pt[:])),
                             reads=[pt], writes=[uT])
                if g == 0 and NG > 1:
                    P.op('sp', lambda e, uT=uT, t0=t0, TBn=TBn: e.dma_start(out=ut_d[:, :, t0:t0 + TBn], in_=uT[:, :, 0:TBn]), reads=[uT], writes=[ut_d], dma=True)
                pending = []
                for ch in range(8):
                    rr = rrs[ch % 2]; pre_c = pre_b[ch]
                    pm = next_mm()
                    for kc in range(16):
                        P.op('pe', lambda e, uT=uT, pm=pm, kc=kc, ch=ch, TBn=TBn: e.matmul(pm[:, 0:TBn], lhsT=wA[:, kc, ch * 128:(ch + 1) * 128], rhs=uT[:, kc, 0:TBn],
                                                                                  start=(kc == 0), stop=(kc == 15)),
                             reads=[wA, uT], writes=[pm])
                    P.op('act', lambda e, pm=pm, ch=ch, TBn=TBn: e.activation(out=pre[:, ch, 3:3 + TBn], in_=pm[:, 0:TBn], func=AF.Copy), reads=[pm], writes=[pre_c])
                    while pending: pending.pop(0)()
                    c_v = cv[ch % 2]; s_l = sl[ch % 2]; s_q = sq[ch % 2]
                    P.op('dve', lambda e, c_v=c_v, ch=ch, TBn=TBn: e.tensor_scalar(out=c_v[:, 0:TBn], in0=pre[:, ch, 0:TBn], scalar1=cw_s[:, ch, 0:1], scalar2=None, op0=ALU.mult),
                         reads=[pre_c, cw_s], writes=[c_v])
                    for tp in range(1, 4):
                        P.op('dve', lambda e, c_v=c_v, ch=ch, tp=tp, TBn=TBn: e.scalar_tensor_tensor(out=c_v[:, 0:TBn], in0=pre[:, ch, tp:tp + TBn], scalar=cw_s[:, ch, tp:tp + 1],
                                                                                                  in1=c_v[:, 0:TBn], op0=ALU.mult, op1=ALU.add),
                             reads=[pre_c, cw_s, c_v], writes=[c_v])
                    P.op('pool', lambda e, ch=ch, TBn=TBn: e.tensor_copy(out=pre[:, ch, 0:3], in_=pre[:, ch, TBn:TBn + 3]), reads=[pre_c], writes=[pre_c])
                    if ch >= 4:
                        P.op('act', lambda e, fmT=fmT, c_v=c_v, ch=ch, TBn=TBn: e.activation(out=fmT[:, ch, 0:TBn], in_=c_v[:, 0:TBn], func=AF.Silu), reads=[c_v], writes=[fmT])
                    else:
                        P.op('act', lambda e, c_v=c_v, s_l=s_l, TBn=TBn: e.activation(out=s_l[:, 0:TBn], in_=c_v[:, 0:TBn], func=AF.Silu), reads=[c_v], writes=[s_l])
                        def l2tail(ch=ch, s_l=s_l, s_q=s_q, TBn=TBn, rr=rr, fmT=fmT):
                            P.op('pool', lambda e, s_l=s_l, s_q=s_q, TBn=TBn: e.tensor_tensor(out=s_q[:, 0:TBn], in0=s_l[:, 0:TBn], in1=s_l[:, 0:TBn], op=ALU.mult),
                                 reads=[s_l], writes=[s_q])
                            P.op('pe', lambda e, s_q=s_q, TBn=TBn: e.matmul(ps_n[:, 0:TBn], lhsT=cb[:, CI_ONES, :], rhs=s_q[:, 0:TBn], start=True, stop=True),
                                 reads=[s_q, cb], writes=[ps_n])
                            P.op('act', lambda e, rr=rr, TBn=TBn: e.activation(out=rr[:, 0:TBn], in_=ps_n[:, 0:TBn], func=AF.Sqrt, bias=epsc[:, 0:1]), reads=[ps_n, epsc], writes=[rr])
                            P.op('dve', lambda e, rr=rr, TBn=TBn: e.reciprocal(out=rr[:, 0:TBn], in_=rr[:, 0:TBn]), reads=[rr], writes=[rr])
                            sc = (128 ** -0.5) if ch < 2 else 1.0
                            P.op('dve', lambda e, rr=rr, fmT=fmT, s_l=s_l, ch=ch, sc=sc, TBn=TBn: e.scalar_tensor_tensor(out=fmT[:, ch, 0:TBn], in0=s_l[:, 0:TBn], scalar=sc, in1=rr[:, 0:TBn],
                                                                                                      op0=ALU.mult, op1=ALU.mult),
                                 reads=[s_l, rr], writes=[fmT])
                        pending.append(l2tail)
                while pending: pending.pop(0)()
                for hq in range(2):
                    P.op('sp', lambda e, fmT=fmT, hq=hq, t0=t0, TBn=TBn: e.dma_start(out=qT_d[hq, :, t0:t0 + TBn], in_=fmT[:, hq, 0:TBn]), reads=[fmT], writes=[qT_d], dma=True)
                    P.op('sp', lambda e, fmT=fmT, hq=hq, t0=t0, TBn=TBn: e.dma_start(out=kT_d[hq, :, t0:t0 + TBn], in_=fmT[:, 2 + hq, 0:TBn]), reads=[fmT], writes=[kT_d], dma=True)
                if dbg:
                    for hq in range(2):
                        P.op('sp', lambda e, fmT=fmT, hq=hq, t0=t0, TBn=TBn: e.dma_start(out=dbg_q[hq, :, t0:t0 + TBn], in_=fmT[:, hq, 0:TBn]), reads=[fmT], writes=[dbg_q], dma=True)
                        P.op('sp', lambda e, fmT=fmT, hq=hq, t0=t0, TBn=TBn: e.dma_start(out=dbg_k[hq, :, t0:t0 + TBn], in_=fmT[:, 2 + hq, 0:TBn]), reads=[fmT], writes=[dbg_k], dma=True)
                for j in range(n_sub):
                    b = sbi * 4 + j
                    r0 = b * 128
                    pm = next_mm()
                    for kc in range(16):
                        P.op('pe', lambda e, uT=uT, pm=pm, kc=kc, j=j: e.matmul(pm[:, 0:512], lhsT=uT[:, kc, j * 128:(j + 1) * 128], rhs=wA[:, kc, NA_FM:NA_FM + 512],
                                                                       start=(kc == 0), stop=(kc == 15)), reads=[wA, uT], writes=[pm])
                    z_t = ztm[b % 2]
                    P.op('act', lambda e, pm=pm, z_t=z_t: e.activation(out=z_t[:], in_=pm[:, 0:512], func=AF.Silu), reads=[pm], writes=[z_t])
                    P.op('sp', lambda e, z_t=z_t, r0=r0: e.dma_start(out=sz_d[r0:r0 + 128, :], in_=z_t[:]), reads=[z_t], writes=[sz_d], dma=True)
                    pm = next_mm()
                    c0 = NA_FM + 512
                    nba = 264 if g == 0 else 8
                    for kc in range(16):
                        P.op('pe', lambda e, uT=uT, pm=pm, kc=kc, j=j, c0=c0, nba=nba: e.matmul(pm[:, 0:nba], lhsT=uT[:, kc, j * 128:(j + 1) * 128], rhs=wA[:, kc, c0:c0 + nba],
                                                                              start=(kc == 0), stop=(kc == 15)), reads=[wA, uT], writes=[pm])
                    b_a = ba[b % 2]; v_s = vst[b % 2]
                    P.op('act', lambda e, pm=pm, b_a=b_a: e.activation(out=b_a[:, 0:4], in_=pm[:, 0:4], func=AF.Exp, scale=-1.0), reads=[pm], writes=[b_a])
                    P.op('dve', lambda e, pm=pm, b_a=b_a: e.tensor_tensor(out=b_a[:, 4:8], in0=pm[:, 4:8], in1=hp_s[:, 1, :], op=ALU.add), reads=[pm, hp_s, b_a], writes=[b_a])
                    P.op('act', lambda e, b_a=b_a: e.activation(out=b_a[:, 4:8], in_=b_a[:, 4:8], func=AF.Exp), reads=[b_a], writes=[b_a])
                    P.op('dve', lambda e, b_a=b_a: e.tensor_scalar(out=b_a[:], in0=b_a[:], scalar1=1.0, scalar2=None, op0=ALU.add), reads=[b_a], writes=[b_a])
                    P.op('act', lambda e, b_a=b_a: e.activation(out=b_a[:, 4:8], in_=b_a[:, 4:8], func=AF.Ln), reads=[b_a], writes=[b_a])
                    P.op('dve', lambda e, b_a=b_a, b=b: e.reciprocal(out=gb_all[:, b, 0:4], in_=b_a[:, 0:4]), reads=[b_a], writes=[gb_all])
                    P.op('dve', lambda e, b_a=b_a, b=b: e.tensor_tensor(out=gb_all[:, b, 4:8], in0=b_a[:, 4:8], in1=nea[:], op=ALU.mult), reads=[b_a, nea, gb_all], writes=[gb_all])
                    if g > 0: continue
                    P.op('act', lambda e, pm=pm, v_s=v_s: e.activation(out=v_s[:], in_=pm[:, 8:264], func=AF.Copy), reads=[pm], writes=[v_s])
                    if g == 0: P.op('sp', lambda e, v_s=v_s, r0=r0: e.dma_start(out=V_o[r0:r0 + 128, :], in_=v_s[:]), reads=[v_s], writes=[V_o], dma=True)
                    pm = next_mm()
                    c0 = NA_FM + 512 + 264
                    for kc in range(16):
                        P.op('pe', lambda e, uT=uT, pm=pm, kc=kc, j=j, c0=c0: e.matmul(pm[:, 0:384], lhsT=uT[:, kc, j * 128:(j + 1) * 128], rhs=wA[:, kc, c0:c0 + 384],
                                                                              start=(kc == 0), stop=(kc == 15)), reads=[wA, uT], writes=[pm])
                    k_f = kif[b % 2]; k_b = kib[b % 2]; r_t = rt[b % 2]; r_m = rtmp[b % 2]; k_T = kiT[b % 2]
                    P.op('sp', lambda e, r_t=r_t, r0=r0: e.dma_start(out=r_t[:], in_=rope[r0:r0 + 128, :]), writes=[r_t], dma=True)
                    P.op('act', lambda e, pm=pm, k_f=k_f: e.activation(out=k_f[:], in_=pm[:, 0:384], func=AF.Copy), reads=[pm], writes=[k_f])
                    P.op('act', lambda e, k_f=k_f, k_b=k_b: e.activation(out=k_b[:], in_=k_f[:], func=AF.Copy), reads=[k_f], writes=[k_b])
                    emit_rope(P, k_f, k_b, r_t, r_m, 3)
                    pt = next_tr()
                    for q in range(3):
                        P.op('pe', lambda e, pt=pt, q=q, k_b=k_b: e.transpose(out=pt[:, q, :], in_=k_b[:, q, :], identity=cb[:, CI_ID, :]), reads=[k_b, cb], writes=[pt])
                    P.op('act', lambda e, pt=pt, k_T=k_T: e.activation(out=k_T[:], in_=pt[:, 0:3, :], func=AF.Copy), reads=[pt], writes=[k_T])
                    for q in range(2 if g == 0 else 0):
                        P.op('sp', lambda e, k_T=k_T, q=q, r0=r0: e.dma_start(out=KT_o[q, :, r0:r0 + 128], in_=k_T[:, q, :]), reads=[k_T], writes=[KT_o], dma=True)
                    if g == 0: P.op('sp', lambda e, k_T=k_T, r0=r0: e.dma_start(out=ikT_o[:, r0:r0 + 128], in_=k_T[:, 2, :]), reads=[k_T], writes=[ikT_o], dma=True)
                for j in range(n_sub):
                    b = sbi * 4 + j
                    r0 = b * 128
                    tk = tm_kv[b % 2]
                    pt = next_tr()
                    for q in range(4):
                        P.op('pe', lambda e, fmT=fmT, pt=pt, q=q, j=j: e.transpose(out=pt[:, q, :], in_=fmT[:, 4 + q, j * 128:(j + 1) * 128], identity=cb[:, CI_ID, :]),
                             reads=[fmT, cb], writes=[pt])
                    P.op('act', lambda e, pt=pt, tk=tk: e.activation(out=tk[:, 2:6, :], in_=pt[:], func=AF.Copy), reads=[pt], writes=[tk])
                    pt = next_tr()
                    for q in range(2):
                        P.op('pe', lambda e, fmT=fmT, pt=pt, q=q, j=j: e.transpose(out=pt[:, q, :], in_=fmT[:, 2 + q, j * 128:(j + 1) * 128], identity=cb[:, CI_ID, :]),
                             reads=[fmT, cb], writes=[pt])
                    P.op('dve', lambda e, pt=pt, tk=tk: e.tensor_copy(out=tk[:, 0:2, :], in_=pt[:, 0:2, :]), reads=[pt], writes=[tk])
                    P.op('sp', lambda e, tk=tk, r0=r0: e.dma_start(out=k_d[r0:r0 + 128, :], in_=tk[:, 0:2, :]), reads=[tk], writes=[k_d], dma=True)
                    P.op('sp', lambda e, tk=tk, r0=r0: e.dma_start(out=v_d[r0:r0 + 128, :], in_=tk[:, 2:6, :]), reads=[tk], writes=[v_d], dma=True)
                    if dbg:
                        P.op('sp', lambda e, tk=tk, r0=r0: e.dma_start(out=dbg_v[r0:r0 + 128, :], in_=tk[:, 2:6, :]), reads=[tk], writes=[dbg_v], dma=True)
            if dbg:
                P.op('sp', lambda e: e.dma_start(out=dbg_gb[:], in_=gb_all[:]), reads=[gb_all], writes=[dbg_gb], dma=True)

            P.emit()
            K.unscope(); scA.close()
            pend.append((gb_all, scr[g % NPAR], og_o, g * HPC * 128))
            if len(pend) == NPAR or g == NG - 1:
                scB = K.scope()
                emit_gdn_multi(K, nblk, cf, cb, epsc, ng_s, pend)
                P.emit()
                K.unscope(); scB.close()
                pend = []
    return cf, cb, epsc


def build_prog1(Lp, dbg=False, NG=1):
    nblk = Lp // 128
    nc = bass.Bass("TRN2", target_bir_lowering=False)
    es = ExitStack()
    with es:
        K = Ctx(nc, es); P = K.P
        hfull = K.din("hfull", [Lp, D])
        w_a = K.din("w_a", [NG, D, NA]); gpre = K.din("gpre", [128, 16]); cw = K.din("cw", [NG, 128, 8, 4]); hp = K.din("hp", [NG, 128, 3, HPC])
        ng = K.din("ng", [128, 128]); cst = K.din("cst", [128, 5, 128]); rope = K.din("rope", [Lp, 32])
        og_o = K.dout("og", [Lp, NG * HPC * 128], BF16)
        KT_o = K.dout("KT", [2, 128, Lp], BF16); V_o = K.dout("Vt", [Lp, 2 * 128], BF16); ikT_o = K.dout("ikT", [128, Lp], BF16)
        scr = gdn_scratch(K, Lp)
        dbgt = None
        if dbg:
            dbgt = (K.dout("dbg_gb", [128, nblk, 8]), K.dout("dbg_qT", [2, 128, Lp], BF16), K.dout("dbg_kT", [2, 128, Lp], BF16), K.dout("dbg_v", [Lp, 512], BF16))
        emit_gdn_all(K, Lp, NG, hfull, w_a, gpre, cw, hp, ng, cst, rope, og_o, KT_o, V_o, ikT_o, scr, dbgt)
    return nc


def gdn_scratch(K, Lp, n=2):
    return [(K.dtmp(f"qT_d{i}", [2, 128, Lp]), K.dtmp(f"kT_d{i}", [2, 128, Lp]), K.dtmp(f"k_d{i}", [Lp, 256]), K.dtmp(f"v_d{i}", [Lp, 512]), K.dtmp(f"sz_d{i}", [Lp, 512]))
            for i in range(n)]


def prep1(inp, Lp, core):
    f = np.float32
    x = inp['x'][0]; SEQ = x.shape[0]
    hfull = np.zeros((Lp, D), f)
    hfull[PADF:PADF + NMETA] = inp['meta_tokens']; hfull[PADF + NMETA:PADF + NMETA + SEQ] = x
    w_in = inp['w_in'][0]
    o = np.cumsum([0, 2048, 2048, 4096, 4096, 32, 32, 2048, 256, 256, 2048, 128, 16, 2048, 2048])
    gq, gk, gv, gz, gb, ga, aq, ak, av, iq, ik, iw, g1, g2 = [slice(o[i], o[i + 1]) for i in range(14)]
    c = core
    cols = np.concatenate([np.arange(o[0] + 256 * c, o[0] + 256 * c + 256), np.arange(o[1] + 256 * c, o[1] + 256 * c + 256),
                           np.arange(o[2] + 512 * c, o[2] + 512 * c + 512), np.arange(o[3] + 512 * c, o[3] + 512 * c + 512),
                           np.arange(o[4] + 4 * c, o[4] + 4 * c + 4), np.arange(o[5] + 4 * c, o[5] + 4 * c + 4),
                           np.arange(o[8], o[9]), np.arange(o[7], o[8]), np.arange(o[10], o[11])])
    w_a = np.ascontiguousarray(w_in[:, cols])
    gpre = np.ascontiguousarray(inp['mix_pre_g'][0].reshape(16, 128).T)
    cwf = inp['gdn_conv_w'][0]
    ccols = np.concatenate([np.arange(256 * c, 256 * c + 256), np.arange(2048 + 256 * c, 2048 + 256 * c + 256),
                            np.arange(4096 + 512 * c, 4096 + 512 * c + 512)])
    cw = np.ascontiguousarray(cwf[:, ccols].T.reshape(8, 128, 4).transpose(1, 0, 2))
    hp = np.zeros((128, 3, HPC), f)
    hp[:, 0, :] = inp['gdn_a_log'][0][4 * c:4 * c + 4][None, :]
    hp[:, 1, :] = inp['gdn_dt_bias'][0][4 * c:4 * c + 4][None, :]
    ng = np.ascontiguousarray(np.broadcast_to(inp['gdn_norm_g'][0][None, :], (128, 128))).astype(f)
    pos = np.maximum(np.arange(Lp) - PADF, 0)
    return {"hfull": hfull, "w_a": w_a[None], "gpre": gpre, "cw": cw[None], "hp": hp[None], "ng": ng, "cst": make_consts(), "rope": rope_table(pos)}


def emit_rope(P, xf, xb, r_t, r_m, nh):
    cosb = lambda: r_t[:, 0:16].unsqueeze(1).to_broadcast([128, nh, 16])
    sinb = lambda: r_t[:, 16:32].unsqueeze(1).to_broadcast([128, nh, 16])
    x1 = lambda: xf[:, 0:nh, 0:16]
    x2 = lambda: xf[:, 0:nh, 16:32]
    P.op('dve', lambda e: e.tensor_tensor(out=r_m[:, 0, 0:nh, :], in0=x1(), in1=cosb(), op=ALU.mult), reads=[xf, r_t], writes=[r_m])
    P.op('dve', lambda e: e.tensor_tensor(out=r_m[:, 1, 0:nh, :], in0=x2(), in1=sinb(), op=ALU.mult), reads=[xf, r_t, r_m], writes=[r_m])
    P.op('dve', lambda e: e.tensor_tensor(out=r_m[:, 2, 0:nh, :], in0=x2(), in1=cosb(), op=ALU.mult), reads=[xf, r_t, r_m], writes=[r_m])
    P.op('dve', lambda e: e.tensor_tensor(out=r_m[:, 3, 0:nh, :], in0=x1(), in1=sinb(), op=ALU.mult), reads=[xf, r_t, r_m], writes=[r_m])
    P.op('dve', lambda e: e.tensor_tensor(out=xb[:, 0:nh, 0:16], in0=r_m[:, 0, 0:nh, :], in1=r_m[:, 1, 0:nh, :], op=ALU.subtract), reads=[r_m, xb], writes=[xb])
    P.op('dve', lambda e: e.tensor_tensor(out=xb[:, 0:nh, 16:32], in0=r_m[:, 2, 0:nh, :], in1=r_m[:, 3, 0:nh, :], op=ALU.add), reads=[r_m, xb], writes=[xb])


def gdn_setup(K, nblk, cf, cb, epsc, psT, ps_s, gb_all, ng_s, qT_d, kT_d, k_d, v_d, sz_d, og_o, ogc0=0):
    P = K.P
    H = HPC
    S32 = K.sb([128, H, 128], F32, "S32"); Sb = K.sb([128, H, 128], BF16, "Sb")
    P.op('dve', lambda e: e.memset(S32[:], 0.0), writes=[S32])
    P.op('dve', lambda e: e.memset(Sb[:], 0.0), writes=[Sb])
    qT = [K.sb([128, 2, 128], BF16, f"g_qT{i}") for i in range(2)]
    kT = [K.sb([128, 2, 128], BF16, f"g_kT{i}") for i in range(2)]
    ktm = [K.sb([128, 2, 128], BF16, f"g_ktm{i}") for i in range(2)]
    vtm = [K.sb([128, H, 128], BF16, f"g_vtm{i}") for i in range(2)]
    szt = [K.sb([128, H, 128], BF16, f"g_sz{i}") for i in range(2)]
    sm = K.sb([128, 8, H], F32, "g_sm")
    gbc = K.sb([128, H, 128], F32, "g_gbc")
    Dt = K.sb([128, H, 128], F32, "g_Dt")
    grow = K.sb([128, H, 128], F32, "g_grow")
    ks = K.sb([128, H, 128], BF16, "g_ks")
    ksT = K.sb([128, H, 128], BF16, "g_ksT")
    Nn = [K.sb([128, H, 128], BF16, f"g_N{i}") for i in range(2)]
    NT = [K.sb([128, H, 128], BF16, f"g_NT{i}") for i in range(2)]
    Pb = K.sb([128, H, 128], BF16, "g_Pb")
    vs = K.sb([128, H, 128], F32, "g_vs")
    Rt = K.sb([128, H, 128], BF16, "g_Rt")
    vnew = K.sb([128, H, 128], BF16, "g_vnew")
    attnT = K.sb([128, H, 128], BF16, "g_attnT")
    qgT = K.sb([128, H, 128], BF16, "g_qgT")
    kd = K.sb([128, H, 128], BF16, "g_kd")
    gz = K.sb([128, H, 128], F32, "g_gz")
    og = [K.sb([128, H, 128], BF16, f"g_og{i}") for i in range(2)]
    junk = K.sb([128, 128], BF16, "g_junk")
    ssq = K.sb([128, 2, H], F32, "g_ssq")
    ident4 = K.sb([128, H, 128], F32, "g_id4")
    offd4 = K.sb([128, H, 128], F32, "g_offd4")
    for h in range(H):
        P.op('dve', lambda e, h=h: e.tensor_copy(out=ident4[:, h, :], in_=cf[:, CI_ID, :]), reads=[cf, ident4], writes=[ident4])
        P.op('dve', lambda e, h=h: e.tensor_copy(out=offd4[:, h, :], in_=cf[:, CI_OFFD, :]), reads=[cf, offd4], writes=[offd4])
    psA = K.ps([128, H, 128], F32, "g_psA"); psB = K.ps([128, H, 128], F32, "g_psB"); psC = K.ps([128, H, 128], F32, "g_psC")

    def chunk(c):
        r0 = c * 128
        q_T = qT[c % 2]; k_T = kT[c % 2]; k_t = ktm[c % 2]; v_t = vtm[c % 2]; s_z = szt[c % 2]; o_g = og[c % 2]
        P.op('sp', lambda e, q_T=q_T, r0=r0: e.dma_start(out=q_T[:], in_=qT_d.t[:, :, r0:r0 + 128].rearrange("h p t -> p h t")), reads=[qT_d], writes=[q_T], dma=True)
        P.op('sp', lambda e, k_T=k_T, r0=r0: e.dma_start(out=k_T[:], in_=kT_d.t[:, :, r0:r0 + 128].rearrange("h p t -> p h t")), reads=[kT_d], writes=[k_T], dma=True)
        P.op('sp', lambda e, k_t=k_t, r0=r0: e.dma_start(out=k_t[:], in_=k_d.t[r0:r0 + 128, :].rearrange("t (h d) -> t h d", h=2)), reads=[k_d], writes=[k_t], dma=True)
        P.op('sp', lambda e, v_t=v_t, r0=r0: e.dma_start(out=v_t[:], in_=v_d.t[r0:r0 + 128, :].rearrange("t (h d) -> t h d", h=H)), reads=[v_d], writes=[v_t], dma=True)
        P.op('sp', lambda e, s_z=s_z, r0=r0: e.dma_start(out=s_z[:], in_=sz_d.t[r0:r0 + 128, :].rearrange("t (h d) -> t h d", h=H)), reads=[sz_d], writes=[s_z], dma=True)
        beta = lambda: gb_all[:, c, 0:4]
        g = lambda: gb_all[:, c, 4:8]
        yield
        P.op('pe', lambda e, c=c: e.matmul(ps_s[:, 0, :], lhsT=cf[:, CI_TRIU, :], rhs=gb_all[:, c, 4:8], start=True, stop=True), reads=[cf, gb_all], writes=[ps_s])
        P.op('pe', lambda e, c=c: e.matmul(ps_s[:, 1, :], lhsT=cf[:, CI_ONES, :], rhs=gb_all[:, c, 4:8], start=True, stop=True), reads=[cf, gb_all, ps_s], writes=[ps_s])
        P.op('dve', lambda e: e.tensor_copy(out=sm[:, 0, :], in_=ps_s[:, 0, :]), reads=[ps_s, sm], writes=[sm])
        P.op('act', lambda e: e.activation(out=sm[:, 1, :], in_=ps_s[:, 0, :], func=AF.Exp), reads=[ps_s, sm], writes=[sm])
        P.op('act', lambda e: e.activation(out=sm[:, 4, :], in_=ps_s[:, 1, :], func=AF.Exp), reads=[ps_s, sm], writes=[sm])
        P.op('act', lambda e, c=c: e.activation(out=sm[:, 2, :], in_=gb_all[:, c, 0:4], func=AF.Sqrt), reads=[gb_all, sm], writes=[sm])
        P.op('dve', lambda e: e.scalar_tensor_tensor(out=sm[:, 3, :], in0=sm[:, 2, :], scalar=-1.0, in1=sm[:, 1, :], op0=ALU.mult, op1=ALU.mult), reads=[sm], writes=[sm])
        P.op('dve', lambda e: e.tensor_tensor(out=sm[:, 7, :], in0=ps_s[:, 1, :], in1=sm[:, 0, :], op=ALU.subtract), reads=[ps_s, sm], writes=[sm])
        P.op('act', lambda e: e.activation(out=sm[:, 5, :], in_=sm[:, 7, :], func=AF.Exp), reads=[sm], writes=[sm])
        P.op('dve', lambda e: e.tensor_scalar(out=sm[:, 6, :], in0=sm[:, 0, :], scalar1=-1.0, scalar2=None, op0=ALU.mult), reads=[sm], writes=[sm])
        yield
        yield
        for h in range(H):
            P.op('dve', lambda e, h=h, c=c: e.tensor_scalar(out=gbc[:, h, :], in0=cf[:, CI_ONES, :], scalar1=gb_all[:, c, 4 + h:5 + h], scalar2=None, op0=ALU.mult),
                 reads=[cf, gb_all, gbc], writes=[gbc])
        yield
        for h in range(H):
            P.op('pe', lambda e, h=h: e.matmul(psA[:, h, :], lhsT=gbc[:, h, :], rhs=cf[:, CI_TRIU, :], start=True, stop=True), reads=[gbc, cf, psA], writes=[psA])
            P.op('pe', lambda e, h=h: e.matmul(psB[:, h, :], lhsT=gbc[:, h, :], rhs=cf[:, CI_TRIU, :], start=True, stop=False), reads=[gbc, cf, psB], writes=[psB])
            P.op('pe', lambda e, h=h: e.matmul(psB[:, h, :], lhsT=cf[:, CI_ID, :], rhs=cf[:, CI_NEGM, :], start=False, stop=True), reads=[cf, psB], writes=[psB])
        P.op('act', lambda e: e.activation(out=grow[:], in_=psA[:], func=AF.Exp), reads=[psA], writes=[grow])
        yield
        for h in range(H):
            P.op('act', lambda e, h=h: e.activation(out=Dt[:, h, :], in_=psB[:, h, :], func=AF.Exp, bias=sm[:, 6, h:h + 1]), reads=[psB, sm, Dt], writes=[Dt])
        yield
        yield
        for h in range(H):
            P.op('dve', lambda e, h=h, k_t=k_t: e.tensor_scalar(out=ks[:, h, :], in0=k_t[:, h // 2, :], scalar1=sm[:, 2, h:h + 1], scalar2=None, op0=ALU.mult),
                 reads=[k_t, sm, ks], writes=[ks])
            P.op('act', lambda e, h=h, v_t=v_t: e.activation(out=vs[:, h, :], in_=v_t[:, h, :], func=AF.Copy, scale=sm[:, 2, h:h + 1]),
                 reads=[v_t, sm, vs], writes=[vs])
            P.op('act', lambda e, h=h, k_t=k_t: e.activation(out=kd[:, h, :], in_=k_t[:, h // 2, :], func=AF.Copy, scale=sm[:, 5, h:h + 1]),
                 reads=[k_t, sm, kd], writes=[kd])
            P.op('dve', lambda e, h=h, q_T=q_T: e.tensor_tensor(out=qgT[:, h, :], in0=q_T[:, h // 2, :], in1=grow[:, h, :], op=ALU.mult), reads=[q_T, grow, qgT], writes=[qgT])
            P.op('pool', lambda e, h=h, s_z=s_z: e.tensor_tensor(out=gz[:, h, :], in0=s_z[:, h, :], in1=ng_s[:], op=ALU.mult), reads=[s_z, ng_s, gz], writes=[gz])
        yield
        for h in range(H):
            P.op('pe', lambda e, h=h: e.transpose(out=psT[:, h, :], in_=ks[:, h, :], identity=cb[:, CI_ID, :]), reads=[ks, cb, psT], writes=[psT])
        P.op('act', lambda e: e.activation(out=ksT[:], in_=psT[:], func=AF.Copy), reads=[psT], writes=[ksT])
        yield
        yield
        for h in range(H):
            P.op('pe', lambda e, h=h: e.matmul(psA[:, h, :], lhsT=ksT[:, h, :], rhs=ksT[:, h, :], start=True, stop=True), reads=[ksT, psA], writes=[psA])
        yield
        for hq in range(2):
            P.op('pe', lambda e, hq=hq, k_T=k_T, q_T=q_T: e.matmul(psC[:, hq, :], lhsT=k_T[:, hq, :], rhs=q_T[:, hq, :], start=True, stop=True), reads=[k_T, q_T, psC], writes=[psC])
        yield
        for h in range(H):
            P.op('dve', lambda e, h=h: e.tensor_tensor(out=attnT[:, h, :], in0=psC[:, h // 2, :], in1=Dt[:, h, :], op=ALU.mult), reads=[psC, Dt, attnT], writes=[attnT])
        P.op('dve', lambda e: e.tensor_tensor(out=Dt[:], in0=Dt[:], in1=offd4[:], op=ALU.mult), reads=[Dt, offd4, attnT], writes=[Dt])
        N0 = Nn[0]; NT0 = NT[0]
        P.op('dve', lambda e: e.scalar_tensor_tensor(out=N0[:], in0=psA[:], scalar=-1.0, in1=Dt[:], op0=ALU.mult, op1=ALU.mult), reads=[psA, Dt], writes=[N0])
        yield
        for h in range(H):
            P.op('pe', lambda e, h=h: e.transpose(out=psT[:, h, :], in_=N0[:, h, :], identity=cb[:, CI_ID, :]), reads=[N0, cb, psT], writes=[psT])
        P.op('act', lambda e: e.activation(out=NT0[:], in_=psT[:], func=AF.Copy), reads=[psT], writes=[NT0])
        P.op('dve', lambda e: e.tensor_tensor(out=Pb[:], in0=N0[:], in1=ident4[:], op=ALU.add), reads=[N0, ident4], writes=[Pb])
        cur = 0
        yield
        for step in range(1, 7):
            Nc = Nn[cur]; NTc = NT[cur]; Nx = Nn[1 - cur]; NTx = NT[1 - cur]
            for h in range(H):
                P.op('pe', lambda e, h=h, Nc=Nc, NTc=NTc: e.matmul(psB[:, h, :], lhsT=Nc[:, h, :], rhs=NTc[:, h, :], start=True, stop=True), reads=[Nc, NTc, psB], writes=[psB])
            P.op('act', lambda e, NTx=NTx: e.activation(out=NTx[:], in_=psB[:], func=AF.Copy), reads=[psB], writes=[NTx])
            if step < 6:
                for h in range(H):
                    P.op('pe', lambda e, h=h, Nc=Nc, NTc=NTc: e.matmul(psA[:, h, :], lhsT=NTc[:, h, :], rhs=Nc[:, h, :], start=True, stop=True), reads=[Nc, NTc, psA], writes=[psA])
                P.op('dve', lambda e, Nx=Nx: e.tensor_copy(out=Nx[:], in_=psA[:]), reads=[psA], writes=[Nx])
            for h in range(H):
                P.op('pe', lambda e, h=h, NTx=NTx: e.matmul(psC[:, h, :], lhsT=NTx[:, h, :], rhs=Pb[:, h, :], start=True, stop=True), reads=[NTx, Pb, psC], writes=[psC])
            P.op('dve', lambda e: e.tensor_tensor(out=Pb[:], in0=Pb[:], in1=psC[:], op=ALU.add), reads=[Pb, psC], writes=[Pb])
            cur = 1 - cur
        yield
        yield
        for h in range(H):
            P.op('pe', lambda e, h=h, k_T=k_T: e.matmul(psA[:, h, :], lhsT=k_T[:, h // 2, :], rhs=Sb[:, h, :], start=True, stop=True), reads=[k_T, Sb, psA], writes=[psA])
        yield
        for h in range(H):
            P.op('dve', lambda e, h=h: e.scalar_tensor_tensor(out=Rt[:, h, :], in0=psA[:, h, :], scalar=sm[:, 3, h:h + 1], in1=vs[:, h, :], op0=ALU.mult, op1=ALU.add),
                 reads=[psA, sm, vs, Rt], writes=[Rt])
        yield
        for h in range(H):
            P.op('pe', lambda e, h=h: e.matmul(psB[:, h, :], lhsT=Pb[:, h, :], rhs=Rt[:, h, :], start=True, stop=True), reads=[Pb, Rt, psB], writes=[psB])
        yield
        for h in range(H):
            P.op('act', lambda e, h=h: e.activation(out=vnew[:, h, :], in_=psB[:, h, :], func=AF.Copy, scale=sm[:, 2, h:h + 1]), reads=[psB, sm, vnew], writes=[vnew])
        yield
        for h in range(H):
            P.op('pe', lambda e, h=h: e.matmul(psC[:, h, :], lhsT=qgT[:, h, :], rhs=Sb[:, h, :], start=True, stop=False), reads=[qgT, Sb, psC], writes=[psC])
            P.op('pe', lambda e, h=h: e.matmul(psC[:, h, :], lhsT=attnT[:, h, :], rhs=vnew[:, h, :], start=False, stop=True), reads=[attnT, vnew, psC], writes=[psC])
        yield
        for h in range(H):
            P.op('pe', lambda e, h=h: e.matmul(psA[:, h, :], lhsT=kd[:, h, :], rhs=vnew[:, h, :], start=True, stop=True), reads=[kd, vnew, psA], writes=[psA])
        yield
        for h in range(H):
            P.op('dve', lambda e, h=h: e.scalar_tensor_tensor(out=Sb[:, h, :], in0=S32[:, h, :], scalar=sm[:, 4, h:h + 1], in1=psA[:, h, :], op0=ALU.mult, op1=ALU.add),
                 reads=[S32, sm, psA, Sb], writes=[Sb])
        for h in range(H):
            P.op('dve', lambda e, h=h: e.scalar_tensor_tensor(out=S32[:, h, :], in0=S32[:, h, :], scalar=sm[:, 4, h:h + 1], in1=psA[:, h, :], op0=ALU.mult, op1=ALU.add),
                 reads=[S32, sm, psA], writes=[S32])
        yield
        yield
        for h in range(H):
            P.op('act', lambda e, h=h: e.activation(out=junk[:], in_=psC[:, h, :], func=AF.Square, accum_out=ssq[:, 0, h:h + 1]), reads=[psC, junk, ssq], writes=[junk, ssq])
        P.op('act', lambda e: e.activation(out=ssq[:, 1, :], in_=ssq[:, 0, :], func=AF.Sqrt, scale=1.0 / 128, bias=epsc[:, 0:1]), reads=[ssq, epsc], writes=[ssq])
        P.op('dve', lambda e: e.reciprocal(out=ssq[:, 1, :], in_=ssq[:, 1, :]), reads=[ssq], writes=[ssq])
        yield
        for h in range(H):
            P.op('dve', lambda e, h=h, o_g=o_g: e.scalar_tensor_tensor(out=o_g[:, h, :], in0=psC[:, h, :], scalar=ssq[:, 1, h:h + 1], in1=gz[:, h, :], op0=ALU.mult, op1=ALU.mult),
                 reads=[psC, ssq, gz, o_g], writes=[o_g])
        P.op('sp', lambda e, o_g=o_g, r0=r0: e.dma_start(out=og_o[r0:r0 + 128, ogc0:ogc0 + HPC * 128], in_=o_g[:]), reads=[o_g], writes=[og_o], dma=True)

    return chunk


def emit_gdn_multi(K, nblk, cf, cb, epsc, ng_s, groups):
    psT = K.ps([128, HPC, 128], BF16, "g_psT")
    ps_s = K.ps([128, 2, HPC], F32, "g_pss")
    fns = [gdn_setup(K, nblk, cf, cb, epsc, psT, ps_s, gb, ng_s, *scr, og_o, c0) for (gb, scr, og_o, c0) in groups]
    for c in range(nblk):
        gens = [f(c) for f in fns]
        while gens:
            nxt = []
            for gen in gens:
                try:
                    next(gen); nxt.append(gen)
                except StopIteration:
                    pass
            gens = nxt


NWC = 2048 + 2048 + 16 + 2048 + 2048
BSTR = 126
A0 = 126
NIOTA = 1408


def blocks_for(SEQ):
    nb = -(-SEQ // BSTR)
    NS = -(-nb // NCORE)
    return nb, NS


def slot_geom(j, nblk):
    a_min = A0 + BSTR * (NCORE * j)
    a_max = A0 + BSTR * (NCORE * j + NCORE - 1)
    kd = min(a_min // 128, nblk)
    kb_end = min((a_max + 127) // 128 + 1, nblk)
    kd = min(kd, kb_end)
    return kd, kb_end


def build_prog2(Lp, NS, topk, dbg=False, fused=False):
    nblk = Lp // 128
    nc = bass.Bass("TRN2", target_bir_lowering=False)
    es = ExitStack()
    with es:
        K = Ctx(nc, es); P = K.P
        R = NS * 128
        hown = K.din("hown", [R, D])
        if not fused:
            ogown = K.din("ogown", [R, 4096], BF16)
            KT_i = K.din("KT", [2, 128, Lp], BF16); V_i = K.din("Vt", [Lp, 256], BF16); ikT_i = K.din("ikT", [128, Lp], BF16)
            ogsrc = ('own', ogown)
        else:
            NG = NCORE
            hfull = K.din("hfull", [Lp, D])
            w_a = K.din("w_a", [NG, D, NA]); cw = K.din("cw", [NG, 128, 8, 4]); hp = K.din("hp", [NG, 128, 3, HPC])
            ng = K.din("ng", [128, 128]); rope = K.din("rope", [Lp, 32])
            sel_i = K.din("sel", [128, 8, 128], BF16)
            og_d = K.dtmp("og_d", [Lp, 4096]); KT_i = K.dtmp("KT_s", [2, 128, Lp]); V_i = K.dtmp("Vt_s", [Lp, 256]); ikT_i = K.dtmp("ikT_s", [128, Lp])
        ropeo = K.din("ropeo", [R, 32])
        qrel_i = K.din("qrel", [128, NS])
        iota_i = K.din("iota", [128, NIOTA])
        cst = K.din("cst", [128, 5, 128])
        gpre = K.din("gpre", [128, 16]); g2pre = K.din("g2pre", [128, 16])
        gpost = K.din("gpost", [128, D]); g2post = K.din("g2post", [128, D])
        fcw = K.din("fcw", [128, 96, 3]); fcb = K.din("fcb", [128, 96])
        w_c = K.din("w_c", [D, NWC]); wbg = K.din("wbg", [4096, D]); wba = K.din("wba", [D, D]); wout = K.din("wout", [D, D])
        wup = K.din("wup", [D, 2 * NFF]); wdn = K.din("wdn", [NFF, D])
        out_o = K.dout("out", [NS, BSTR, D])
        wc_b = K.dtmp("wc_b", [128, 17, 16, 512]); wbg_b = K.dtmp("wbg_b", [128, 4, 32, 512]); wba_b = K.dtmp("wba_b", [128, 4, 16, 512])
        wout_b = K.dtmp("wout_b", [128, 4, 16, 512]); wup_b = K.dtmp("wup_b", [128, 24, 16, 512]); wdn_b = K.dtmp("wdn_b", [128, 4, 48, 512])
        if dbg:
            dbg_oatt = K.dout("dbg_oatt", [R, D], BF16); dbg_thr = K.dout("dbg_thr", [128, NS]); dbg_h1 = K.dout("dbg_h1", [R, D])
            dbg_q = K.dout("dbg_q", [R, D], BF16)

        if fused:
            cf, cb, epsc = emit_gdn_all(K, Lp, NG, hfull, w_a, gpre, cw, hp, ng, cst, rope, og_d, KT_i, V_i, ikT_i, gdn_scratch(K, Lp))
            sel_s = K.sb([128, 8, 128], BF16, "sel_s")
            P.op('sp', lambda e: e.dma_start(out=sel_s[:], in_=sel_i[:]), writes=[sel_s], dma=True)
            ogsrc = ('sel', og_d, sel_s, Lp)
        else:
            cf = K.sb([128, 5, 128], F32, "cf"); cb = K.sb([128, 5, 128], BF16, "cb")
            P.op('sp', lambda e: e.dma_start(out=cf[:], in_=cst[:]), writes=[cf], dma=True)
            P.op('dve', lambda e: e.tensor_copy(out=cb[:], in_=cf[:]), reads=[cf], writes=[cb])
            epsc = K.sb([128, 1], F32, "epsc")
            P.op('dve', lambda e: e.memset(epsc[:], EPS), writes=[epsc])
        gpre_s = K.sb([128, 16], F32); g2pre_s = K.sb([128, 16], F32); qrel_s = K.sb([128, NS], F32)
        fcw_s = K.sb([128, 96, 3], F32); fcb_s = K.sb([128, 96], F32)
        for dst, src in ((gpre_s, gpre), (g2pre_s, g2pre), (qrel_s, qrel_i), (fcw_s, fcw), (fcb_s, fcb)):
            P.op('sp', lambda e, dst=dst, src=src: e.dma_start(out=dst[:], in_=src[:]), writes=[dst], dma=True)

        sc0 = K.scope()
        st = [K.sb([128, 2048], F32, f"w0s{i}") for i in range(2)]
        sbt = [K.sb([128, 2048], BF16, f"w0b{i}") for i in range(2)]
        it = [0]
        def cast_w(src, dst, KC, N, gain, up=False):
            sv = src.t.rearrange("(kc p) n -> p kc n", p=128)
            for kc in range(KC):
                for n0 in range(0, N, 2048):
                    n1 = min(N, n0 + 2048); w = n1 - n0
                    s_ = st[it[0] % 2]; b_ = sbt[it[0] % 2]; it[0] += 1
                    P.op('sp', lambda e, s_=s_, kc=kc, n0=n0, n1=n1, w=w: e.dma_start(out=s_[:, 0:w], in_=sv[:, kc, n0:n1]), writes=[s_], dma=True)
                    if gain is None:
                        P.op('act', lambda e, s_=s_, b_=b_, w=w: e.activation(out=b_[:, 0:w], in_=s_[:, 0:w], func=AF.Copy), reads=[s_], writes=[b_])
                    else:
                        P.op('act', lambda e, s_=s_, b_=b_, w=w, kc=kc: e.activation(out=b_[:, 0:w], in_=s_[:, 0:w], func=AF.Copy, scale=gain[:, kc:kc + 1]),
                             reads=[s_, gain], writes=[b_])
                    if up:
                        half = n0 // NFF; g0 = (n0 % NFF) // 256
                        P.op('pool', lambda e, b_=b_, kc=kc, g0=g0, half=half: e.dma_start(out=dst[:, g0:g0 + 8, kc, half * 256:(half + 1) * 256],
                                                                                          in_=b_[:, 0:2048].rearrange("p (g c) -> p g c", c=256)), reads=[b_], writes=[dst], dma=True)
                    else:
                        nt = w // 512; rem = w % 512; t0_ = n0 // 512
                        if nt:
                            P.op('pool', lambda e, b_=b_, kc=kc, nt=nt, t0_=t0_: e.dma_start(out=dst[:, t0_:t0_ + nt, kc, :], in_=b_[:, 0:nt * 512].rearrange("p (g c) -> p g c", c=512)),
                                 reads=[b_], writes=[dst], dma=True)
                        if rem:
                            P.op('pool', lambda e, b_=b_, kc=kc, nt=nt, t0_=t0_, rem=rem: e.dma_start(out=dst[:, t0_ + nt, kc, 0:rem], in_=b_[:, nt * 512:nt * 512 + rem]),
                                 reads=[b_], writes=[dst], dma=True)
        cast_w(w_c, wc_b, 16, NWC, gpre_s)
        cast_w(wbg, wbg_b, 32, D, None); cast_w(wba, wba_b, 16, D, None); cast_w(wout, wout_b, 16, D, None)
        cast_w(wup, wup_b, 16, 2 * NFF, g2pre_s, up=True); cast_w(wdn, wdn_b, 48, D, None)
        P.emit(); K.unscope(); sc0.close()

        xo = K.sb([128, D], F32, "xo")
        QT = K.sb([128, 16, 128], BF16, "QT"); iqT = K.sb([128, 16, 128], BF16, "iqT")
        sgn = K.sb([128, 16], F32, "sgn"); aiw = K.sb([128, 16], F32, "aiw")
        sig = K.sb([128, 2, D], BF16, "sig")
        oatt = K.sb([128, D], BF16, "oatt")
        thr = K.sb([128, NS], F32, "thr")

        for j in range(NS):
            r0 = j * 128
            kd, kb_end = slot_geom(j, nblk)
            nk = kb_end * 128
            scW = K.scope()
            W = make_wpool(K)
            xs = K.sb([128, D], BF16, "p_xs"); ss = K.sb([128, 2], F32, "p_ss")
            uT = K.sb([128, 16, 128], BF16, "p_uT")
            tq = K.sb([128, 16, 128], F32, "p_tq"); tqb = K.sb([128, 16, 128], BF16, "p_tqb")
            r_t = K.sb([128, 32], F32, "p_rt"); r_m = K.sb([128, 4, 16, 16], F32, "p_rm")
            iwt = K.sb([128, 16], F32, "p_iw")
            P.op('sp', lambda e, r0=r0: e.dma_start(out=xo[:], in_=hown[r0:r0 + 128, :]), writes=[xo], dma=True)
            P.op('sp', lambda e, r0=r0: e.dma_start(out=r_t[:], in_=ropeo[r0:r0 + 128, :]), writes=[r_t], dma=True)
            emit_norm_T(P, W, cb, epsc, xo, xs, ss, uT)
            for ct in range(17):
                n0 = ct * 512 if ct < 8 else (8192 if ct == 8 else 4096 + (ct - 9) * 512)
                ncol = 16 if ct == 8 else 512
                pm = W.linear(uT, wc_b, 16, n0, ncol)
                if ct < 8:
                    hh = (ct % 4) * 4
                    if ct == 4:
                        finish_q(P, W, cb, tq, tqb, r_t, r_m, QT, 128 ** -0.5, None)
                    P.op('act', lambda e, pm=pm, hh=hh: e.activation(out=tq[:, hh:hh + 4, :], in_=pm[:, 0:512], func=AF.Copy), reads=[pm], writes=[tq])
                elif ct == 8:
                    P.op('act', lambda e, pm=pm: e.activation(out=iwt[:], in_=pm[:, 0:16], func=AF.Copy), reads=[pm], writes=[iwt])
                    P.op('act', lambda e: e.activation(out=aiw[:], in_=iwt[:], func=AF.Abs, scale=(16 ** -0.5) * (128 ** -0.5)), reads=[iwt], writes=[aiw])
                    P.op('dve', lambda e: e.tensor_scalar(out=sgn[:], in0=iwt[:], scalar1=0.0, scalar2=2.0, op0=ALU.is_gt, op1=ALU.mult), reads=[iwt], writes=[sgn])
                    P.op('dve', lambda e: e.tensor_scalar(out=sgn[:], in0=sgn[:], scalar1=-1.0, scalar2=None, op0=ALU.add), reads=[sgn], writes=[sgn])
                    finish_q(P, W, cb, tq, tqb, r_t, r_m, iqT, None, aiw)
                else:
                    gi = (ct - 9) // 4; c0 = ((ct - 9) % 4) * 512
                    P.op('act', lambda e, pm=pm, gi=gi, c0=c0: e.activation(out=sig[:, gi, c0:c0 + 512], in_=pm[:, 0:512], func=AF.Sigmoid), reads=[pm], writes=[sig])
            if dbg:
                P.op('sp', lambda e, r0=r0: e.dma_start(out=dbg_q[r0:r0 + 128, :], in_=tqb[:]), reads=[tqb], writes=[dbg_q], dma=True)
            P.emit(); K.unscope(); scW.close()

            scA = K.scope()
            emit_attention(K, cb, cf, j, kd, kb_end, topk, QT, iqT, sgn, qrel_s, iota_i, KT_i, V_i, ikT_i, oatt, thr)
            K.unscope(); scA.close()
            if dbg:
                P.op('sp', lambda e, r0=r0: e.dma_start(out=dbg_oatt[r0:r0 + 128, :], in_=oatt[:]), reads=[oatt], writes=[dbg_oatt], dma=True)

            scW = K.scope()
            W = make_wpool(K)
            emit_merge_ffn(K, W, cb, epsc, j, xo, ogsrc, oatt, sig, gpost, g2post, fcw_s, fcb_s,
                           wbg_b, wba_b, wout_b, wup_b, wdn_b, out_o, dbg_h1 if dbg else None)
            P.emit(); K.unscope(); scW.close()
        if dbg:
            P.op('sp', lambda e: e.dma_start(out=dbg_thr[:], in_=thr[:]), reads=[thr], writes=[dbg_thr], dma=True)
        P.emit()
    return nc


class WPool:
    pass


def make_wpool(K):
    P = K.P
    W = WPool()
    W.wt = [K.sb([128, 16, 512], BF16, f"wt{i}") for i in range(3)]
    W.ps_mm = [K.ps([128, 512], F32, f"w_psmm{i}") for i in range(3)]
    W.ps_tr = [K.ps([128, 4, 128], BF16, f"w_pstr{i}") for i in range(2)]
    W.nw = 0; W.nm = 0; W.nt = 0
    def next_tr():
        W.nt += 1
        return W.ps_tr[W.nt % 2]
    W.next_tr = next_tr
    def linear(xT, wd, KC, n0, ncol, pm=None, xoff=0):
        if pm is None:
            W.nm += 1; pm = W.ps_mm[W.nm % 3]
        for part in range(KC // 16):
            wt = W.wt[W.nw % 3]; W.nw += 1
            P.op('sp', lambda e, wt=wt, part=part: e.dma_start(out=wt[:, :, 0:ncol], in_=wd[:, n0 // 512, part * 16:(part + 1) * 16, 0:ncol]), reads=[wd], writes=[wt], dma=True)
            for kc in range(16):
                kk = part * 16 + kc
                P.op('pe', lambda e, wt=wt, kc=kc, kk=kk: e.matmul(pm[:, 0:ncol], lhsT=xT[:, xoff + kk, :], rhs=wt[:, kc, 0:ncol], start=(kk == 0), stop=(kk == KC - 1)),
                     reads=[xT, wt], writes=[pm])
        return pm
    W.linear = linear
    return W


def emit_norm_T(P, W, cb, epsc, x, xs, ss, uT):
    P.op('act', lambda e: e.activation(out=xs[:], in_=x[:], func=AF.Square, accum_out=ss[:, 0:1]), reads=[x], writes=[xs, ss])
    P.op('act', lambda e: e.activation(out=ss[:, 1:2], in_=ss[:, 0:1], func=AF.Sqrt, scale=1.0 / D, bias=epsc[:, 0:1]), reads=[ss, epsc], writes=[ss])
    P.op('dve', lambda e: e.reciprocal(out=ss[:, 1:2], in_=ss[:, 1:2]), reads=[ss], writes=[ss])
    P.op('dve', lambda e: e.tensor_scalar(out=xs[:], in0=x[:], scalar1=ss[:, 1:2], scalar2=None, op0=ALU.mult), reads=[x, ss], writes=[xs])
    emit_T(P, W, cb, lambda kc: xs[:, kc * 128:(kc + 1) * 128], xs, uT, 16)


def emit_T(P, W, cb, src_ap, src_tl, dstT, n):
    for g4 in range(0, n, 4):
        pt = W.next_tr()
        m = min(4, n - g4)
        for q in range(m):
            P.op('pe', lambda e, pt=pt, q=q, i=g4 + q: e.transpose(out=pt[:, q, :], in_=src_ap(i), identity=cb[:, CI_ID, :]), reads=[src_tl, cb], writes=[pt])
        if (g4 // 4) % 2:
            P.op('act', lambda e, pt=pt, g4=g4, m=m: e.activation(out=dstT[:, g4:g4 + m, :], in_=pt[:, 0:m, :], func=AF.Copy), reads=[pt], writes=[dstT])
        else:
            P.op('dve', lambda e, pt=pt, g4=g4, m=m: e.tensor_copy(out=dstT[:, g4:g4 + m, :], in_=pt[:, 0:m, :]), reads=[pt], writes=[dstT])


def finish_q(P, W, cb, tq, tqb, r_t, r_m, dstT, const_scale, row_scale):
    if row_scale is not None:
        P.op('dve', lambda e: e.tensor_tensor(out=tq[:], in0=tq[:], in1=row_scale[:].unsqueeze(2).to_broadcast([128, 16, 128]), op=ALU.mult), reads=[tq, row_scale], writes=[tq])
    else:
        P.op('dve', lambda e: e.tensor_scalar(out=tq[:], in0=tq[:], scalar1=const_scale, scalar2=None, op0=ALU.mult), reads=[tq], writes=[tq])
    P.op('act', lambda e: e.activation(out=tqb[:], in_=tq[:], func=AF.Copy), reads=[tq], writes=[tqb])
    emit_rope(P, tq, tqb, r_t, r_m, 16)
    emit_T(P, W, cb, lambda h: tqb[:, h, :], tqb, dstT, 16)


def emit_attention(K, cb, cf, j, kd, kb_end, topk, QT, iqT, sgn, qrel_s, iota_i, KT_i, V_i, ikT_i, oatt, thr):
    P = K.P
    nk = kb_end * 128
    sc = K.sb([128, nk], F32, "a_sc")
    maskT = K.sb([128, kb_end, 128], BF16, "a_maskT")
    JW = 4096
    junk = K.sb([128, JW], BF16, "a_junk")
    rl = [K.sb([128, 512], F32, f"a_rl{i}") for i in range(2)]
    ikb = [K.sb([128, 512], BF16, f"a_ik{i}") for i in range(2)]
    iot = K.sb([128, NIOTA], F32, "a_iota")
    P.op('sp', lambda e: e.dma_start(out=iot[:], in_=iota_i[:]), writes=[iot], dma=True)
    scp = K.scope()
    ps_i = [K.ps([128, 512], F32, f"a_psi{i}") for i in range(3)]
    ps_t = [K.ps([128, 4, 128], BF16, f"a_pst{i}") for i in range(2)]
    ni = 0
    for u0 in range(0, kb_end, 4):
        nb_ = min(4, kb_end - u0); w = nb_ * 128; k0 = u0 * 128
        ik = ikb[(u0 // 4) % 2]
        P.op('sp', lambda e, ik=ik, k0=k0, w=w: e.dma_start(out=ik[:, 0:w], in_=ikT_i[:, k0:k0 + w]), reads=[ikT_i], writes=[ik], dma=True)
        for h in range(16):
            pi = ps_i[ni % 3]; r_ = rl[ni % 2]; ni += 1
            P.op('pe', lambda e, pi=pi, ik=ik, h=h, w=w: e.matmul(pi[:, 0:w], lhsT=iqT[:, h, :], rhs=ik[:, 0:w], start=True, stop=True), reads=[iqT, ik], writes=[pi])
            P.op('act', lambda e, pi=pi, r_=r_, w=w: e.activation(out=r_[:, 0:w], in_=pi[:, 0:w], func=AF.Relu), reads=[pi], writes=[r_])
            if h == 0:
                P.op('dve', lambda e, r_=r_, k0=k0, w=w: e.tensor_scalar(out=sc[:, k0:k0 + w], in0=r_[:, 0:w], scalar1=sgn[:, 0:1], scalar2=None, op0=ALU.mult), reads=[r_, sgn], writes=[sc])
            else:
                P.op('dve', lambda e, r_=r_, k0=k0, w=w, h=h: e.scalar_tensor_tensor(out=sc[:, k0:k0 + w], in0=r_[:, 0:w], scalar=sgn[:, h:h + 1], in1=sc[:, k0:k0 + w], op0=ALU.mult, op1=ALU.add),
                     reads=[r_, sgn, sc], writes=[sc])
    P.op('dve', lambda e: e.memset(sc[:, 0:PADF], -1e30), reads=[sc], writes=[sc])
    wc = (kb_end - kd) * 128
    if wc > 0:
        assert wc <= NIOTA
        P.op('dve', lambda e: e.tensor_scalar(out=iot[:, 0:wc], in0=iot[:, 0:wc], scalar1=qrel_s[:, j:j + 1], scalar2=-1e30, op0=ALU.is_gt, op1=ALU.mult), reads=[iot, qrel_s], writes=[iot])
        P.op('dve', lambda e: e.tensor_tensor(out=sc[:, kd * 128:nk], in0=sc[:, kd * 128:nk], in1=iot[:, 0:wc], op=ALU.add), reads=[sc, iot], writes=[sc])
    lo = K.sb([128, 1], F32, "a_lo"); mid = K.sb([128, 1], F32, "a_mid"); cnt = K.sb([128, 8], F32, "a_cnt"); dl = K.sb([128, 1], F32, "a_dl")
    LO0, RANGE, NIT = -16.0, 64.0, 26
    P.op('dve', lambda e: e.memset(lo[:], LO0), writes=[lo])
    npc = -(-nk // JW)
    for it in range(NIT):
        hk = RANGE / (2 ** (it + 1))
        P.op('dve', lambda e, hk=hk: e.tensor_scalar(out=mid[:], in0=lo[:], scalar1=hk, scalar2=None, op0=ALU.add), reads=[lo], writes=[mid])
        for pc in range(npc):
            c0 = pc * JW; c1 = min(nk, c0 + JW)
            P.op('dve', lambda e, c0=c0, c1=c1, pc=pc: e.tensor_scalar(out=junk[:, 0:c1 - c0], in0=sc[:, c0:c1], scalar1=mid[:, 0:1], scalar2=0.0, op0=ALU.is_ge, op1=ALU.add,
                                                                     accum_out=cnt[:, pc:pc + 1]), reads=[sc, mid, junk], writes=[junk, cnt])
        if npc > 1:
            P.op('dve', lambda e: e.tensor_reduce(out=cnt[:, 7:8], in_=cnt[:, 0:npc], axis=mybir.AxisListType.X, op=ALU.add), reads=[cnt], writes=[cnt])
            cc = 7
        else:
            cc = 0
        P.op('dve', lambda e, hk=hk, cc=cc: e.tensor_scalar(out=dl[:], in0=cnt[:, cc:cc + 1], scalar1=topk - 0.5, scalar2=hk, op0=ALU.is_gt, op1=ALU.mult), reads=[cnt], writes=[dl])
        P.op('dve', lambda e: e.tensor_tensor(out=lo[:], in0=lo[:], in1=dl[:], op=ALU.add), reads=[lo, dl], writes=[lo])
    P.op('dve', lambda e: e.tensor_copy(out=thr[:, j:j + 1], in_=lo[:]), reads=[lo, thr], writes=[thr])
    mk = [K.sb([128, 512], BF16, f"a_mk{i}") for i in range(2)]
    for u0 in range(0, kb_end, 4):
        nb_ = min(4, kb_end - u0); w = nb_ * 128; k0 = u0 * 128
        m_ = mk[(u0 // 4) % 2]; pt = ps_t[(u0 // 4) % 2]
        P.op('dve', lambda e, m_=m_, k0=k0, w=w: e.tensor_scalar(out=m_[:, 0:w], in0=sc[:, k0:k0 + w], scalar1=lo[:, 0:1], scalar2=None, op0=ALU.is_ge), reads=[sc, lo], writes=[m_])
        for q in range(nb_):
            P.op('pe', lambda e, pt=pt, q=q, m_=m_: e.transpose(out=pt[:, q, :], in_=m_[:, q * 128:(q + 1) * 128], identity=cb[:, CI_ID, :]), reads=[m_, cb], writes=[pt])
        P.op('act', lambda e, pt=pt, u0=u0, nb_=nb_: e.activation(out=maskT[:, u0:u0 + nb_, :], in_=pt[:, 0:nb_, :], func=AF.Copy), reads=[pt], writes=[maskT])
    P.emit(); K.unscope(); scp.close()
    scp = K.scope()
    psO = [K.ps([128, 512], F32, f"a_pso{i}") for i in range(6)]
    psS = [K.ps([128, 4, 128], F32, f"a_pss{i}") for i in range(2)]
    Kb = [K.sb([128, 2, 128], BF16, f"a_K{i}") for i in range(3)]
    Vb = [K.sb([128, 2, 129], BF16, f"a_V{i}") for i in range(3)]
    eb = [K.sb([128, 4, 128], BF16, f"a_e{i}") for i in range(2)]
    pb = [K.sb([128, 4, 128], BF16, f"a_p{i}") for i in range(3)]
    rc = K.sb([128, 16], F32, "a_rc")
    for v_ in Vb:
        P.op('dve', lambda e, v_=v_: e.memset(v_[:, :, 128:129], 1.0), writes=[v_])
    ns = 0
    for kb in range(kb_end):
        k_ = Kb[kb % 3]; v_ = Vb[kb % 3]; k0 = kb * 128
        P.op('sp', lambda e, k_=k_, k0=k0: e.dma_start(out=k_[:], in_=KT_i.t[:, :, k0:k0 + 128].rearrange("h p t -> p h t")), reads=[KT_i], writes=[k_], dma=True)
        P.op('sp', lambda e, v_=v_, k0=k0: e.dma_start(out=v_[:, :, 0:128], in_=V_i.t[k0:k0 + 128, :].rearrange("t (h d) -> t h d", h=2)), reads=[V_i, v_], writes=[v_], dma=True)
        for g in range(2):
            for half in range(2):
                h0 = g * 8 + half * 4
                pS = psS[ns % 2]; e_ = eb[ns % 2]; p_ = pb[ns % 3]; ns += 1
                P.op('pe', lambda e, pS=pS, k_=k_, g=g, h0=h0: e.matmul(pS[:], lhsT=k_[:, g, :], rhs=QT[:, h0:h0 + 4, :], start=True, stop=True), reads=[k_, QT], writes=[pS])
                P.op('act', lambda e, pS=pS, e_=e_: e.activation(out=e_[:], in_=pS[:], func=AF.Exp), reads=[pS], writes=[e_])
                P.op('dve', lambda e, e_=e_, p_=p_, kb=kb: e.tensor_tensor(out=p_[:], in0=e_[:], in1=maskT[:, kb, :].unsqueeze(1).to_broadcast([128, 4, 128]), op=ALU.mult),
                     reads=[e_, maskT], writes=[p_])
                for hh in range(4):
                    hd = h0 + hh
                    po = psO[hd // 3]
                    P.op('pe', lambda e, po=po, hd=hd, p_=p_, hh=hh, v_=v_, g=g, kb=kb: e.matmul(po[:, (hd % 3) * 129:(hd % 3) * 129 + 129], lhsT=p_[:, hh, :], rhs=v_[:, g, :], start=(kb == 0 and hd % 3 == 0), stop=(kb == kb_end - 1 and (hd % 3 == 2 or hd == 15))),
                         reads=[p_, v_], writes=[po])
    for hd in range(16):
        po = psO[hd // 3]
        P.op('dve', lambda e, po=po, hd=hd: e.reciprocal(out=rc[:, hd:hd + 1], in_=po[:, (hd % 3) * 129 + 128:(hd % 3) * 129 + 129]), reads=[po, rc], writes=[rc])
        P.op('act', lambda e, po=po, hd=hd: e.activation(out=oatt[:, hd * 128:(hd + 1) * 128], in_=po[:, (hd % 3) * 129:(hd % 3) * 129 + 128], func=AF.Copy, scale=rc[:, hd:hd + 1]), reads=[po, rc, oatt], writes=[oatt])
    P.emit(); K.unscope(); scp.close()


def emit_merge_ffn(K, W, cb, epsc, j, xo, ogsrc, oatt, sig, gpost, g2post, fcw_s, fcb_s,
                   wbg_b, wba_b, wout_b, wup_b, wdn_b, out_o, dbg_h1):
    P = K.P
    r0 = j * 128
    if ogsrc[0] == 'own':
        ogb = K.sb([128, 4096], BF16, "m_og")
    ogT = K.sb([128, 32, 128], BF16, "m_ogT"); oaT = K.sb([128, 16, 128], BF16, "m_oaT")
    mg = K.sb([128, D], F32, "m_mg"); t1 = K.sb([128, 512], F32, "m_t1"); mgb = K.sb([128, D], BF16, "m_mgb"); mgT = K.sb([128, 16, 128], BF16, "m_mgT")
    ysb = K.sb([128, D], F32, "m_y"); gp = K.sb([128, D], F32, "m_gp")
    ssq = K.sb([128, 8], F32, "m_ssq"); junk = K.sb([128, 512], BF16, "m_junk")
    xs = K.sb([128, D], BF16, "m_xs"); ss = K.sb([128, 2], F32, "m_ss"); u2T = K.sb([128, 16, 128], BF16, "m_u2T")
    actT = K.sb([128, 48, 128], BF16, "m_actT")
    cv = [K.sb([128, 4, 126], F32, f"m_cv{i}") for i in range(2)]
    sg = [K.sb([128, 2, 126], F32, f"m_sg{i}") for i in range(2)]
    if ogsrc[0] == 'own':
        ogown = ogsrc[1]
        P.op('sp', lambda e: e.dma_start(out=ogb[:], in_=ogown[r0:r0 + 128, :]), writes=[ogb], dma=True)
    P.op('sp', lambda e: e.dma_start(out=gp[:], in_=gpost[:]), writes=[gp], dma=True)
    if ogsrc[0] == 'own':
        emit_T(P, W, cb, lambda i: ogb[:, i * 128:(i + 1) * 128], ogb, ogT, 32)
    else:
        _, og_d, sel_s, Lp_ = ogsrc
        win0 = A0 + BSTR * (NCORE * j)
        ogw = [K.sb([128, 8, 1024], BF16, f"m_ogw{i}") for i in range(2)]
        psg = W.ps_mm
        for fq in range(4):
            t_ = ogw[fq % 2]
            valid = []
            P.op('pool', lambda e, t_=t_: e.memset(t_[:], 0.0), writes=[t_])
            for kc in range(8):
                rlo = win0 + kc * 128
                nv = max(0, min(128, Lp_ - rlo))
                if nv == 0: continue
                valid.append(kc)
                P.op('sp', lambda e, t_=t_, kc=kc, rlo=rlo, nv=nv, fq=fq: e.dma_start(out=t_[0:nv, kc, :], in_=og_d[rlo:rlo + nv, fq * 1024:(fq + 1) * 1024]),
                     reads=[og_d, t_], writes=[t_], dma=True)
            for half in range(2):
                W.nm += 1; pm = psg[W.nm % 3]
                for q in range(4):
                    fc = half * 4 + q
                    for kc in valid:
                        P.op('pe', lambda e, pm=pm, q=q, fc=fc, kc=kc, t_=t_: e.matmul(pm[:, q * 128:(q + 1) * 128], lhsT=t_[:, kc, fc * 128:(fc + 1) * 128], rhs=sel_s[:, kc, :],
                                                                                  start=(kc == valid[0]), stop=(kc == valid[-1])), reads=[t_, sel_s], writes=[pm])
                i0 = fq * 8 + half * 4
                if valid:
                    P.op('act', lambda e, pm=pm, i0=i0: e.activation(out=ogT[:, i0:i0 + 4, :], in_=pm[:, 0:512], func=AF.Copy), reads=[pm], writes=[ogT])
                else:
                    P.op('pool', lambda e, i0=i0: e.memset(ogT[:, i0:i0 + 4, :], 0.0), writes=[ogT])
    emit_T(P, W, cb, lambda i: oatt[:, i * 128:(i + 1) * 128], oatt, oaT, 16)
    for ct in range(4):
        c0 = ct * 512
        pm = W.linear(ogT, wbg_b, 32, c0, 512)
        P.op('dve', lambda e, pm=pm, c0=c0: e.tensor_tensor(out=mg[:, c0:c0 + 512], in0=pm[:, 0:512], in1=sig[:, 0, c0:c0 + 512], op=ALU.mult), reads=[pm, sig, mg], writes=[mg])
        pm = W.linear(oaT, wba_b, 16, c0, 512)
        P.op('dve', lambda e, pm=pm, c0=c0: e.tensor_tensor(out=t1[:], in0=pm[:, 0:512], in1=sig[:, 1, c0:c0 + 512], op=ALU.mult), reads=[pm, sig], writes=[t1])
        P.op('pool', lambda e, c0=c0: e.tensor_tensor(out=mgb[:, c0:c0 + 512], in0=mg[:, c0:c0 + 512], in1=t1[:], op=ALU.add), reads=[mg, t1, mgb], writes=[mgb])
    emit_T(P, W, cb, lambda i: mgb[:, i * 128:(i + 1) * 128], mgb, mgT, 16)

    def post_norm_residual(src_T, wd, KC, gain_tl, res_in, res_out):
        for ct in range(4):
            c0 = ct * 512
            pm = W.linear(src_T, wd, KC, c0, 512)
            P.op('act', lambda e, pm=pm, c0=c0: e.activation(out=ysb[:, c0:c0 + 512], in_=pm[:, 0:512], func=AF.Copy), reads=[pm, ysb], writes=[ysb])
            P.op('act', lambda e, pm=pm, ct=ct: e.activation(out=junk[:], in_=pm[:, 0:512], func=AF.Square, accum_out=ssq[:, ct:ct + 1]), reads=[pm, junk, ssq], writes=[junk, ssq])
        P.op('dve', lambda e: e.tensor_reduce(out=ssq[:, 4:5], in_=ssq[:, 0:4], axis=mybir.AxisListType.X, op=ALU.add), reads=[ssq], writes=[ssq])
        P.op('act', lambda e: e.activation(out=ssq[:, 5:6], in_=ssq[:, 4:5], func=AF.Sqrt, scale=1.0 / D, bias=epsc[:, 0:1]), reads=[ssq, epsc], writes=[ssq])
        P.op('dve', lambda e: e.reciprocal(out=ssq[:, 5:6], in_=ssq[:, 5:6]), reads=[ssq], writes=[ssq])
        P.op('dve', lambda e: e.scalar_tensor_tensor(out=ysb[:], in0=ysb[:], scalar=ssq[:, 5:6], in1=gain_tl[:], op0=ALU.mult, op1=ALU.mult), reads=[ysb, ssq, gain_tl], writes=[ysb])
        P.op('pool', lambda e: e.tensor_tensor(out=res_out[:], in0=res_in[:], in1=ysb[:], op=ALU.add), reads=[res_in, ysb], writes=[res_out])

    post_norm_residual(mgT, wout_b, 16, gp, xo, xo)
    if dbg_h1 is not None:
        P.op('sp', lambda e: e.dma_start(out=dbg_h1[r0:r0 + 128, :], in_=xo[:]), reads=[xo], writes=[dbg_h1], dma=True)
    P.op('sp', lambda e: e.dma_start(out=gp[:], in_=g2post[:]), reads=[gp], writes=[gp], dma=True)
    emit_norm_T(P, W, cb, epsc, xo, xs, ss, u2T)
    P.op('pool', lambda e: e.memset(actT[:], 0.0), writes=[actT])
    psu = W.ps_mm
    nu = 0
    for g in range(24):
        wt = W.wt[W.nw % 3]; W.nw += 1
        P.op('sp', lambda e, wt=wt, g=g: e.dma_start(out=wt[:], in_=wup_b[:, g, :, :]), reads=[wup_b], writes=[wt], dma=True)
        pm = psu[nu % 3]; c_v = cv[nu % 2]; s_g = sg[nu % 2]; nu += 1
        for cc in range(4):
            for kc in range(16):
                P.op('pe', lambda e, pm=pm, wt=wt, cc=cc, kc=kc: e.matmul(pm[:, cc * 128:(cc + 1) * 128], lhsT=wt[:, kc, cc * 128:(cc + 1) * 128], rhs=u2T[:, kc, :], start=(kc == 0), stop=(kc == 15)),
                     reads=[wt, u2T], writes=[pm])
        for cc in range(4):
            ch = (2 * g + cc) if cc < 2 else (48 + 2 * g + cc - 2)
            P.op('dve', lambda e, pm=pm, c_v=c_v, cc=cc, ch=ch: e.tensor_scalar(out=c_v[:, cc, :], in0=pm[:, cc * 128:cc * 128 + 126], scalar1=fcw_s[:, ch, 0:1], scalar2=fcb_s[:, ch:ch + 1], op0=ALU.mult, op1=ALU.add),
                 reads=[pm, fcw_s, fcb_s, c_v], writes=[c_v])
            for tp in (1, 2):
                P.op('dve', lambda e, pm=pm, c_v=c_v, cc=cc, ch=ch, tp=tp: e.scalar_tensor_tensor(out=c_v[:, cc, :], in0=pm[:, cc * 128 + tp:cc * 128 + tp + 126], scalar=fcw_s[:, ch, tp:tp + 1], in1=c_v[:, cc, :], op0=ALU.mult, op1=ALU.add),
                     reads=[pm, fcw_s, c_v], writes=[c_v])
        P.op('act', lambda e, c_v=c_v, s_g=s_g: e.activation(out=s_g[:], in_=c_v[:, 0:2, :], func=AF.Silu), reads=[c_v], writes=[s_g])
        P.op('pool', lambda e, c_v=c_v, s_g=s_g, g=g: e.tensor_tensor(out=actT[:, 2 * g:2 * g + 2, 2:128], in0=s_g[:], in1=c_v[:, 2:4, :], op=ALU.mult), reads=[c_v, s_g, actT], writes=[actT])
    post_norm_residual(actT, wdn_b, 48, gp, xo, ysb)
    P.op('sp', lambda e: e.dma_start(out=out_o[j, :, :], in_=ysb[2:128, :]), reads=[ysb], writes=[out_o], dma=True)


def own_rows(core, NS):
    rows = np.zeros(NS * 128, np.int64)
    for j in range(NS):
        s_ = NCORE * j + core
        rows[j * 128:(j + 1) * 128] = A0 + BSTR * s_ + np.arange(128)
    return rows


def prep2_common(inp):
    f = np.float32
    o = np.cumsum([0, 2048, 2048, 4096, 4096, 32, 32, 2048, 256, 256, 2048, 128, 16, 2048, 2048])
    w_in = inp['w_in'][0]
    cols = np.concatenate([np.arange(o[6], o[7]), np.arange(o[9], o[10]), np.arange(o[12], o[13]), np.arange(o[13], o[14]), np.arange(o[11], o[12])])
    cm = {
        "w_c": np.ascontiguousarray(w_in[:, cols]),
        "wbg": np.ascontiguousarray(inp['w_branch_gdn'][0]), "wba": np.ascontiguousarray(inp['w_branch_att'][0]),
        "wout": np.ascontiguousarray(inp['w_out'][0]), "wup": np.ascontiguousarray(inp['w_up'][0]), "wdn": np.ascontiguousarray(inp['w_down'][0]),
        "gpre": np.ascontiguousarray(inp['mix_pre_g'][0].reshape(16, 128).T), "g2pre": np.ascontiguousarray(inp['ffn_pre_g'][0].reshape(16, 128).T),
        "gpost": np.ascontiguousarray(np.broadcast_to(inp['mix_post_g'][0][None, :], (128, D))).astype(f),
        "g2post": np.ascontiguousarray(np.broadcast_to(inp['ffn_post_g'][0][None, :], (128, D))).astype(f),
        "fcw": np.ascontiguousarray(inp['ffn_conv_w'][0].T.reshape(96, 128, 3).transpose(1, 0, 2)),
        "fcb": np.ascontiguousarray(inp['ffn_conv_b'][0].reshape(96, 128).T),
        "cst": make_consts(),
        "iota": np.ascontiguousarray(np.broadcast_to(np.arange(NIOTA, dtype=f)[None, :], (128, NIOTA))),
    }
    return cm


def prep2(cm, hfull, og_all, KT, Vt, ikT, Lp, NS, core):
    f = np.float32
    nblk = Lp // 128
    rows = own_rows(core, NS)
    ok = rows < Lp
    rc = np.minimum(rows, Lp - 1)
    hown = np.where(ok[:, None], hfull[rc], 0).astype(f)
    ogown = None
    if og_all is not None:
        ogown = og_all[rc].copy(); ogown[~ok] = 0
    pos = np.maximum(rows - PADF, 0)
    qrel = np.zeros((128, NS), f)
    for j in range(NS):
        kd, kb_end = slot_geom(j, nblk)
        qrel[:, j] = rows[j * 128:(j + 1) * 128] - kd * 128
    m = dict(cm)
    m.update({"hown": hown, "ogown": ogown, "KT": KT, "Vt": Vt, "ikT": ikT, "ropeo": rope_table(pos), "qrel": qrel})
    return m


def kernel(**inputs):
    inp = {k: np.asarray(v) for k, v in inputs.items()}
    return kernel_fused(inp)


def kernel_unfused(**inputs):
    inp = {k: np.asarray(v) for k, v in inputs.items()}
    x = inp['x']
    SEQ = x.shape[1]
    Lp = PADF + NMETA + SEQ
    assert Lp % 128 == 0
    L = NMETA + SEQ
    topk = min(256, L // 4)
    nb, NS = blocks_for(SEQ)
    cores = list(range(NCORE))
    nc1 = build_prog1(Lp)
    ims = [prep1(inp, Lp, c) for c in cores]
    hfull = ims[0]["hfull"]
    for m in ims[1:]:
        m["hfull"] = hfull
    r1 = run_bass_kernel_spmd(nc1, ims, core_ids=cores).results
    og_all = np.concatenate([np.asarray(r1[c]["og"]) for c in cores], axis=1)
    KT = np.asarray(r1[0]["KT"]); Vt = np.asarray(r1[0]["Vt"]); ikT = np.asarray(r1[0]["ikT"])
    del ims, r1
    nc2 = build_prog2(Lp, NS, topk)
    cm = prep2_common(inp)
    ims2 = [prep2(cm, hfull, og_all, KT, Vt, ikT, Lp, NS, c) for c in cores]
    r2 = run_bass_kernel_spmd(nc2, ims2, core_ids=cores).results
    out = np.zeros((1, SEQ, D), np.float32)
    for c in cores:
        o = np.asarray(r2[c]["out"])
        for j in range(NS):
            s_ = NCORE * j + c
            t_lo = BSTR * s_; t_hi = min(SEQ, t_lo + BSTR)
            if t_hi > t_lo:
                out[0, t_lo:t_hi] = o[j, :t_hi - t_lo]
    return out


def make_sel(core):
    import ml_dtypes
    sel = np.zeros((128, 8, 128), np.float32)
    for r in range(128):
        w = BSTR * core + r
        sel[w % 128, w // 128, r] = 1.0
    return sel.astype(ml_dtypes.bfloat16)


def kernel_fused(inp):
    x = inp['x']; SEQ = x.shape[1]
    Lp = PADF + NMETA + SEQ
    L = NMETA + SEQ
    topk = min(256, L // 4)
    nb, NS = blocks_for(SEQ)
    cores = list(range(NCORE))
    p1 = [prep1(inp, Lp, c) for c in cores]
    hfull = p1[0]["hfull"]
    w_a = np.concatenate([p["w_a"] for p in p1], 0); cw = np.concatenate([p["cw"] for p in p1], 0); hp = np.concatenate([p["hp"] for p in p1], 0)
    cm = prep2_common(inp)
    cm.update({"hfull": hfull, "w_a": w_a, "cw": cw, "hp": hp, "ng": p1[0]["ng"], "rope": p1[0]["rope"]})
    del p1
    ims = []
    for c in cores:
        m = prep2(cm, hfull, None, None, None, None, Lp, NS, c)
        for k in ("ogown", "KT", "Vt", "ikT"): m.pop(k)
        m["sel"] = make_sel(c)
        ims.append(m)
    nc = build_prog2(Lp, NS, topk, fused=True)
    r2 = run_bass_kernel_spmd(nc, ims, core_ids=cores).results
    out = np.zeros((1, SEQ, D), np.float32)
    for c in cores:
        o = np.asarray(r2[c]["out"])
        for j in range(NS):
            s_ = NCORE * j + c
            t_lo = BSTR * s_; t_hi = min(SEQ, t_lo + BSTR)
            if t_hi > t_lo:
                out[0, t_lo:t_hi] = o[j, :t_hi - t_lo]
    return out
```

```python
import numpy as np
import concourse.bass as bass
import concourse.mybir as mybir
from concourse.bass_utils import run_bass_kernel_spmd
from contextlib import ExitStack

F32 = mybir.dt.float32; BF16 = mybir.dt.bfloat16; I32 = mybir.dt.int32
AF = mybir.ActivationFunctionType; ALU = mybir.AluOpType

D = 2048
NMETA = 16
PADF = 112
EPS = 1e-6
NCORE = 8
HPC = 4
NFF = 6144
ROPE_THETA = 500000.0


class Buf:
    __slots__ = ('w', 'r', 'multi')
    def __init__(self, multi=False):
        self.w = {}; self.r = {}; self.multi = multi


class Tl:
    def __init__(self, t, multi=False):
        self.t = t; self.b = Buf(multi)
    def __getitem__(self, k):
        return self.t[k]


class Prog:
    ENG = ('pe', 'act', 'dve', 'pool', 'sp')
    def __init__(self, nc, es, n_dma=12):
        self.nc = nc; self.es = es
        self.ops = {e: [] for e in self.ENG}
        self.cnt = {e: 0 for e in self.ENG}
        self.sem = {e: es.enter_context(nc.semaphore('s_' + e)) for e in ('pe', 'act', 'dve', 'pool')}
        self.seen = {e: {} for e in self.ENG}
        self.dsem = {q: [[es.enter_context(nc.semaphore(f'd_{q}{i}')), 0] for i in range(n_dma)] for q in ('sp', 'pool')}
        self.drr = {q: 0 for q in ('sp', 'pool')}
        self.nops = 0

    def op(self, eng, fn, reads=(), writes=(), dma=False):
        deps = {}
        def add(ev):
            s, v = ev
            k = id(s)
            if k not in deps or deps[k][1] < v: deps[k] = (s, v)
        for t in reads:
            b = t.b if isinstance(t, Tl) else t
            for ev in b.w.values(): add(ev)
        for t in writes:
            b = t.b if isinstance(t, Tl) else t
            if not b.multi:
                for ev in b.w.values(): add(ev)
                for ev in b.r.values(): add(ev)
        if dma:
            slots = self.dsem[eng]
            slot = slots[self.drr[eng] % len(slots)]; self.drr[eng] += 1
            if slot[1] > 0: add((slot[0], slot[1]))
            slot[1] += 16
            ev = (slot[0], slot[1]); inc = 16
        else:
            self.cnt[eng] += 1
            ev = (self.sem[eng], self.cnt[eng]); inc = 1
        waits = []
        seen = self.seen[eng]
        own = id(self.sem['pe']) if eng == 'pe' else None
        for k, (s, v) in deps.items():
            if k == own: continue
            if seen.get(k, 0) >= v: continue
            seen[k] = v; waits.append((s, v))
        for t in writes:
            b = t.b if isinstance(t, Tl) else t
            if b.multi:
                b.w[id(ev[0])] = ev
            else:
                b.w = {id(ev[0]): ev}; b.r = {}
        for t in reads:
            b = t.b if isinstance(t, Tl) else t
            if not b.multi:
                b.r[id(ev[0])] = ev
        self.ops[eng].append((waits, fn, ev, inc))
        self.nops += 1
        return ev

    def emit(self):
        nc = self.nc
        fin = []
        for e in ('pe', 'act', 'dve', 'pool'):
            if self.cnt[e]: fin.append((self.sem[e], self.cnt[e]))
        for q in self.dsem:
            for s, v in self.dsem[q]:
                if v: fin.append((s, v))
        bar = getattr(self, 'barrier', [])
        def run(name, e):
            for s, v in bar: e.wait_ge(s, v)
            for waits, fn, ev, inc in self.ops[name]:
                for (s, v) in waits: e.wait_ge(s, v)
                ins = fn(e)
                ins.then_inc(ev[0], inc)
            if name == 'sp':
                for s, v in fin: e.wait_ge(s, v)
            self.ops[name] = []
        self.barrier = fin
        with nc.Block() as block:
            @block.tensor
            def _(e): run('pe', e)
            @block.scalar
            def _(e): run('act', e)
            @block.vector
            def _(e): run('dve', e)
            @block.gpsimd
            def _(e): run('pool', e)
            @block.sync
            def _(e): run('sp', e)


class Ctx:
    def __init__(self, nc, es):
        self.nc = nc; self.es = es; self.P = Prog(nc, es)
        self._n = 0
    def scope(self):
        st = ExitStack()
        if not hasattr(self, '_stk'): self._stk = []
        self._stk.append(self.es); self.es = st
        return st
    def unscope(self):
        self.es = self._stk.pop()
    def sb(self, shape, dt, name=None):
        self._n += 1
        return Tl(self.es.enter_context(self.nc.sbuf_tensor(f"{name or 'sb'}_{self._n}", list(shape), dt)))
    def ps(self, shape, dt=F32, name=None):
        self._n += 1
        return Tl(self.es.enter_context(self.nc.psum_tensor(f"{name or 'ps'}_{self._n}", list(shape), dt)))
    def din(self, name, shape, dt=F32):
        return Tl(self.nc.dram_tensor(name, list(shape), dt, kind="ExternalInput").ap(), multi=True)
    def dout(self, name, shape, dt=F32):
        return Tl(self.nc.dram_tensor(name, list(shape), dt, kind="ExternalOutput").ap(), multi=True)
    def dtmp(self, name, shape, dt=BF16):
        return Tl(self.nc.dram_tensor(name, list(shape), dt, kind="Internal").ap(), multi=True)


CI_ID, CI_TRIU, CI_NEGM, CI_OFFD, CI_ONES = 0, 1, 2, 3, 4
def make_consts():
    c = np.zeros((128, 5, 128), np.float32)
    j = np.arange(128)[:, None]; i = np.arange(128)[None, :]
    c[:, CI_ID] = (i == j)
    c[:, CI_TRIU] = (j <= i)
    c[:, CI_NEGM] = np.where(i >= j, 0.0, -1e9)
    c[:, CI_OFFD] = (i != j)
    c[:, CI_ONES] = 1.0
    return c


def rope_table(pos):
    half = 16
    inv = ROPE_THETA ** (-np.arange(half, dtype=np.float32) / half)
    ang = pos.astype(np.float32)[:, None] * inv[None, :].astype(np.float32)
    return np.concatenate([np.cos(ang), np.sin(ang)], axis=1).astype(np.float32)


NA_FM = 1024
NA_TM = 512 + 8 + 256 + 256 + 128
NA = NA_FM + NA_TM


def emit_gdn_all(K, Lp, NG, hfull, w_a, gpre, cw, hp, ng, cst, rope, og_o, KT_o, V_o, ikT_o, scr, dbgt=None):
    P = K.P; nblk = Lp // 128; dbg = dbgt is not None
    if dbg: dbg_gb, dbg_q, dbg_k, dbg_v = dbgt
    if True:
        cf = K.sb([128, 5, 128], F32, "cf")
        P.op('sp', lambda e: e.dma_start(out=cf[:], in_=cst[:]), writes=[cf], dma=True)
        epsc = K.sb([128, 1], F32, "epsc")
        P.op('dve', lambda e: e.memset(epsc[:], EPS), writes=[epsc])
        cb = K.sb([128, 5, 128], BF16, "cb")
        P.op('dve', lambda e: e.tensor_copy(out=cb[:], in_=cf[:]), reads=[cf], writes=[cb])
        gpre_s = K.sb([128, 16], F32); cw_s = K.sb([128, 8, 4], F32); hp_s = K.sb([128, 3, HPC], F32); ng_s = K.sb([128, 128], F32)
        for dst, src in ((gpre_s, gpre), (ng_s, ng)):
            P.op('sp', lambda e, dst=dst, src=src: e.dma_start(out=dst[:], in_=src[:]), writes=[dst], dma=True)
        nea = K.sb([128, HPC], F32)
        ut_d = K.dtmp("ut_d", [128, 16, Lp]) if NG > 1 else None
        NPAR = min(2, NG)
        gb_alls = [K.sb([128, nblk, 8], F32, f"gb_all{i}") for i in range(NPAR)]
        pend = []

        for g in range(NG):
            gb_all = gb_alls[g % NPAR]
            qT_d, kT_d, k_d, v_d, sz_d = scr[g % NPAR]
            P.op('sp', lambda e, g=g: e.dma_start(out=cw_s[:], in_=cw[g]), writes=[cw_s], dma=True)
            P.op('sp', lambda e, g=g: e.dma_start(out=hp_s[:], in_=hp[g]), writes=[hp_s], dma=True)
            P.op('act', lambda e: e.activation(out=nea[:], in_=hp_s[:, 0, :], func=AF.Exp), reads=[hp_s], writes=[nea])
            P.op('dve', lambda e: e.tensor_scalar(out=nea[:], in0=nea[:], scalar1=-1.0, scalar2=None, op0=ALU.mult), reads=[nea], writes=[nea])
            scA = K.scope()
            wA = K.sb([128, 16, NA], BF16, "wA")
            wst = [K.sb([128, NA], F32, f"wst{i}") for i in range(1)]
            w_a_v = w_a.t[g].rearrange("(kc p) n -> p kc n", p=128)
            for kc in range(16):
                st = wst[0]
                P.op('sp', lambda e, st=st, kc=kc: e.dma_start(out=st[:], in_=w_a_v[:, kc, :]), writes=[st], dma=True)
                P.op('act', lambda e, st=st, kc=kc: e.activation(out=wA[:, kc, :], in_=st[:], func=AF.Copy, scale=gpre_s[:, kc:kc + 1]),
                     reads=[st, gpre_s], writes=[wA])

            TB = 512
            xf = [K.sb([128, D], F32, f"xf{i}") for i in range(2)]
            xs = [K.sb([128, D], BF16, f"xs{i}") for i in range(2)]
            ss = [K.sb([128, 2], F32, f"ss{i}") for i in range(2)]
            uTs = [K.sb([128, 16, TB], BF16, f"uT{i}") for i in range(2)]
            pre = K.sb([128, 8, 3 + TB], F32, "pre")
            pre_b = [Buf() for _ in range(8)]
            P.op('dve', lambda e: e.memset(pre[:], 0.0), writes=pre_b)
            cv = [K.sb([128, TB], F32, f"cv{i}") for i in range(2)]
            sl = [K.sb([128, TB], F32, f"sl{i}") for i in range(2)]
            sq = [K.sb([128, TB], BF16, f"sq{i}") for i in range(2)]
            rrs = [K.sb([128, TB], F32, f"rr{i}") for i in range(2)]
            fmTs = [K.sb([128, 8, TB], BF16, f"fmT{i}") for i in range(2)]
            tm_kv = [K.sb([128, 6, 128], BF16, f"tmkv{i}") for i in range(2)]
            ztm = [K.sb([128, 512], BF16, f"ztm{i}") for i in range(2)]
            bars = [K.sb([128, 4, 8], F32, f"bar{i}") for i in range(2)]
            vst = [K.sb([128, 256], BF16, f"vst{i}") for i in range(2)]
            kif = [K.sb([128, 3, 128], F32, f"kif{i}") for i in range(2)]
            kib = [K.sb([128, 3, 128], BF16, f"kib{i}") for i in range(2)]
            rt = [K.sb([128, 32], F32, f"rt{i}") for i in range(2)]
            rtmp = [K.sb([128, 4, 3, 16], F32, f"rtmp{i}") for i in range(2)]
            kiT = [K.sb([128, 3, 128], BF16, f"kiT{i}") for i in range(2)]
            ps_tr = [K.ps([128, 4, 128], BF16, f"pstr{i}") for i in range(2)]
            ps_mm = [K.ps([128, 512], F32, f"psmm{i}") for i in range(3)]
            ps_n = K.ps([128, 512], F32, "psn")
            nmm = [0]
            def next_mm():
                nmm[0] += 1
                return ps_mm[nmm[0] % 3]
            ntr = [0]
            def next_tr():
                ntr[0] += 1
                return ps_tr[ntr[0] % 2]

            nsb = (Lp + TB - 1) // TB
            blk = 0
            for sbi in range(nsb):
                t0 = sbi * TB
                n_sub = min(4, (Lp - t0) // 128)
                TBn = n_sub * 128
                uT = uTs[sbi % 2]; fmT = fmTs[sbi % 2]; bar = bars[sbi % 2]
                if g > 0:
                    if sbi == 0:
                        P.op('sp', lambda e, uT=uT, t0=t0, TBn=TBn: e.dma_start(out=uT[:, :, 0:TBn], in_=ut_d[:, :, t0:t0 + TBn]), reads=[ut_d], writes=[uT], dma=True)
                    if sbi + 1 < nsb:
                        t1_ = (sbi + 1) * TB; TB1 = min(4, (Lp - t1_) // 128) * 128; uT1 = uTs[(sbi + 1) % 2]
                        P.op('sp', lambda e, uT1=uT1, t1_=t1_, TB1=TB1: e.dma_start(out=uT1[:, :, 0:TB1], in_=ut_d[:, :, t1_:t1_ + TB1]), reads=[ut_d], writes=[uT1], dma=True)
                for j in range(n_sub if g == 0 else 0):
                    b = sbi * 4 + j
                    x_f = xf[b % 2]; x_s = xs[b % 2]; s_s = ss[b % 2]
                    P.op('sp', lambda e, x_f=x_f, b=b: e.dma_start(out=x_f[:], in_=hfull[b * 128:(b + 1) * 128, :]), writes=[x_f], dma=True)
                    P.op('act', lambda e, x_f=x_f, s_s=s_s, x_s=x_s: e.activation(out=x_s[:], in_=x_f[:], func=AF.Square, accum_out=s_s[:, 0:1]),
                         reads=[x_f], writes=[x_s, s_s])
                    P.op('act', lambda e, s_s=s_s: e.activation(out=s_s[:, 1:2], in_=s_s[:, 0:1], func=AF.Sqrt, scale=1.0 / D, bias=epsc[:, 0:1]),
                         reads=[s_s, epsc], writes=[s_s])
                    P.op('dve', lambda e, s_s=s_s: e.reciprocal(out=s_s[:, 1:2], in_=s_s[:, 1:2]), reads=[s_s], writes=[s_s])
                    P.op('dve', lambda e, x_f=x_f, x_s=x_s, s_s=s_s: e.tensor_scalar(out=x_s[:], in0=x_f[:], scalar1=s_s[:, 1:2], scalar2=None, op0=ALU.mult),
                         reads=[x_f, s_s], writes=[x_s])
                    for g4 in range(4):
                        pt = next_tr()
                        for q in range(4):
                            kc = g4 * 4 + q
                            P.op('pe', lambda e, pt=pt, q=q, kc=kc, x_s=x_s: e.transpose(out=pt[:, q, :], in_=x_s[:, kc * 128:(kc + 1) * 128], identity=cb[:, CI_ID, :]),
                                 reads=[x_s, cb], writes=[pt])
                        P.op('act' if g4 % 2 else 'dve',
                             (lambda e, uT=uT, pt=pt, g4=g4, j=j: e.activation(out=uT[:, g4 * 4:(g4 + 1) * 4, j * 128:(j + 1) * 128], in_=pt[:], func=AF.Copy)) if g4 % 2 else
                             (lambda e, uT=uT, pt=pt, g4=g4, j=j: e.tensor_copy(out=uT[:, g4 * 4:(g4 + 1) * 4, j * 128:(j + 1) * 128], in_=pt[:])),
                             reads=[pt], writes=[uT])
                if g == 0 and NG > 1:
                    P.op('sp', lambda e, uT=uT, t0=t0, TBn=TBn: e.dma_start(out=ut_d[:, :, t0:t0 + TBn], in_=uT[:, :, 0:TBn]), reads=[uT], writes=[ut_d], dma=True)
                pending = []
                for ch in range(8):
                    rr = rrs[ch % 2]; pre_c = pre_b[ch]
                    pm = next_mm()
                    for kc in range(16):
                        P.op('pe', lambda e, uT=uT, pm=pm, kc=kc, ch=ch, TBn=TBn: e.matmul(pm[:, 0:TBn], lhsT=wA[:, kc, ch * 128:(ch + 1) * 128], rhs=uT[:, kc, 0:TBn],
                                                                                  start=(kc == 0), stop=(kc == 15)),
                             reads=[wA, uT], writes=[pm])
                    P.op('act', lambda e, pm=pm, ch=ch, TBn=TBn: e.activation(out=pre[:, ch, 3:3 + TBn], in_=pm[:, 0:TBn], func=AF.Copy), reads=[pm], writes=[pre_c])
                    while pending: pending.pop(0)()
                    c_v = cv[ch % 2]; s_l = sl[ch % 2]; s_q = sq[ch % 2]
                    P.op('dve', lambda e, c_v=c_v, ch=ch, TBn=TBn: e.tensor_scalar(out=c_v[:, 0:TBn], in0=pre[:, ch, 0:TBn], scalar1=cw_s[:, ch, 0:1], scalar2=None, op0=ALU.mult),
                         reads=[pre_c, cw_s], writes=[c_v])
                    for tp in range(1, 4):
                        P.op('dve', lambda e, c_v=c_v, ch=ch, tp=tp, TBn=TBn: e.scalar_tensor_tensor(out=c_v[:, 0:TBn], in0=pre[:, ch, tp:tp + TBn], scalar=cw_s[:, ch, tp:tp + 1],
                                                                                                  in1=c_v[:, 0:TBn], op0=ALU.mult, op1=ALU.add),
                             reads=[pre_c, cw_s, c_v], writes=[c_v])
                    P.op('pool', lambda e, ch=ch, TBn=TBn: e.tensor_copy(out=pre[:, ch, 0:3], in_=pre[:, ch, TBn:TBn + 3]), reads=[pre_c], writes=[pre_c])
                    if ch >= 4:
                        P.op('act', lambda e, fmT=fmT, c_v=c_v, ch=ch, TBn=TBn: e.activation(out=fmT[:, ch, 0:TBn], in_=c_v[:, 0:TBn], func=AF.Silu), reads=[c_v], writes=[fmT])
                    else:
                        P.op('act', lambda e, c_v=c_v, s_l=s_l, TBn=TBn: e.activation(out=s_l[:, 0:TBn], in_=c_v[:, 0:TBn], func=AF.Silu), reads=[c_v], writes=[s_l])
                        def l2tail(ch=ch, s_l=s_l, s_q=s_q, TBn=TBn, rr=rr, fmT=fmT):
                            P.op('pool', lambda e, s_l=s_l, s_q=s_q, TBn=TBn: e.tensor_tensor(out=s_q[:, 0:TBn], in0=s_l[:, 0:TBn], in1=s_l[:, 0:TBn], op=ALU.mult),
                                 reads=[s_l], writes=[s_q])
                            P.op('pe', lambda e, s_q=s_q, TBn=TBn: e.matmul(ps_n[:, 0:TBn], lhsT=cb[:, CI_ONES, :], rhs=s_q[:, 0:TBn], start=True, stop=True),
                                 reads=[s_q, cb], writes=[ps_n])
                            P.op('act', lambda e, rr=rr, TBn=TBn: e.activation(out=rr[:, 0:TBn], in_=ps_n[:, 0:TBn], func=AF.Sqrt, bias=epsc[:, 0:1]), reads=[ps_n, epsc], writes=[rr])
                            P.op('dve', lambda e, rr=rr, TBn=TBn: e.reciprocal(out=rr[:, 0:TBn], in_=rr[:, 0:TBn]), reads=[rr], writes=[rr])
                            sc = (128 ** -0.5) if ch < 2 else 1.0
                            P.op('dve', lambda e, rr=rr, fmT=fmT, s_l=s_l, ch=ch, sc=sc, TBn=TBn: e.scalar_tensor_tensor(out=fmT[:, ch, 0:TBn], in0=s_l[:, 0:TBn], scalar=sc, in1=rr[:, 0:TBn],
                                                                                                      op0=ALU.mult, op1=ALU.mult),
                                 reads=[s_l, rr], writes=[fmT])
                        pending.append(l2tail)
                while pending: pending.pop(0)()
                for hq in range(2):
                    P.op('sp', lambda e, fmT=fmT, hq=hq, t0=t0, TBn=TBn: e.dma_start(out=qT_d[hq, :, t0:t0 + TBn], in_=fmT[:, hq, 0:TBn]), reads=[fmT], writes=[qT_d], dma=True)
                    P.op('sp', lambda e, fmT=fmT, hq=hq, t0=t0, TBn=TBn: e.dma_start(out=kT_d[hq, :, t0:t0 + TBn], in_=fmT[:, 2 + hq, 0:TBn]), reads=[fmT], writes=[kT_d], dma=True)
                if dbg:
                    for hq in range(2):
                        P.op('sp', lambda e, fmT=fmT, hq=hq, t0=t0, TBn=TBn: e.dma_start(out=dbg_q[hq, :, t0:t0 + TBn], in_=fmT[:, hq, 0:TBn]), reads=[fmT], writes=[dbg_q], dma=True)
                        P.op('sp', lambda e, fmT=fmT, hq=hq, t0=t0, TBn=TBn: e.dma_start(out=dbg_k[hq, :, t0:t0 + TBn], in_=fmT[:, 2 + hq, 0:TBn]), reads=[fmT], writes=[dbg_k], dma=True)
                for j in range(n_sub):
                    b = sbi * 4 + j
                    r0 = b * 128
                    pm = next_mm()
                    for kc in range(16):
                        P.op('pe', lambda e, uT=uT, pm=pm, kc=kc, j=j: e.matmul(pm[:, 0:512], lhsT=uT[:, kc, j * 128:(j + 1) * 128], rhs=wA[:, kc, NA_FM:NA_FM + 512],
                                                                       start=(kc == 0), stop=(kc == 15)), reads=[wA, uT], writes=[pm])
                    z_t = ztm[b % 2]
                    P.op('act', lambda e, pm=pm, z_t=z_t: e.activation(out=z_t[:], in_=pm[:, 0:512], func=AF.Silu), reads=[pm], writes=[z_t])
                    P.op('sp', lambda e, z_t=z_t, r0=r0: e.dma_start(out=sz_d[r0:r0 + 128, :], in_=z_t[:]), reads=[z_t], writes=[sz_d], dma=True)
                    pm = next_mm()
                    c0 = NA_FM + 512
                    nba = 264 if g == 0 else 8
                    for kc in range(16):
                        P.op('pe', lambda e, uT=uT, pm=pm, kc=kc, j=j, c0=c0, nba=nba: e.matmul(pm[:, 0:nba], lhsT=uT[:, kc, j * 128:(j + 1) * 128], rhs=wA[:, kc, c0:c0 + nba],
                                                                              start=(kc == 0), stop=(kc == 15)), reads=[wA, uT], writes=[pm])
                    v_s = vst[b % 2]
                    P.op('dve', lambda e, pm=pm, j=j, bar=bar: e.tensor_copy(out=bar[:, j, :], in_=pm[:, 0:8]), reads=[pm, bar], writes=[bar])
                    if g > 0: continue
                    P.op('act', lambda e, pm=pm, v_s=v_s: e.activation(out=v_s[:], in_=pm[:, 8:264], func=AF.Copy), reads=[pm], writes=[v_s])
                    if g == 0: P.op('sp', lambda e, v_s=v_s, r0=r0: e.dma_start(out=V_o[r0:r0 + 128, :], in_=v_s[:]), reads=[v_s], writes=[V_o], dma=True)
                    pm = next_mm()
                    c0 = NA_FM + 512 + 264
                    for kc in range(16):
                        P.op('pe', lambda e, uT=uT, pm=pm, kc=kc, j=j, c0=c0: e.matmul(pm[:, 0:384], lhsT=uT[:, kc, j * 128:(j + 1) * 128], rhs=wA[:, kc, c0:c0 + 384],
                                                                              start=(kc == 0), stop=(kc == 15)), reads=[wA, uT], writes=[pm])
                    k_f = kif[b % 2]; k_b = kib[b % 2]; r_t = rt[b % 2]; r_m = rtmp[b % 2]; k_T = kiT[b % 2]
                    P.op('sp', lambda e, r_t=r_t, r0=r0: e.dma_start(out=r_t[:], in_=rope[r0:r0 + 128, :]), writes=[r_t], dma=True)
                    P.op('act', lambda e, pm=pm, k_f=k_f: e.activation(out=k_f[:], in_=pm[:, 0:384], func=AF.Copy), reads=[pm], writes=[k_f])
                    P.op('act', lambda e, k_f=k_f, k_b=k_b: e.activation(out=k_b[:], in_=k_f[:], func=AF.Copy), reads=[k_f], writes=[k_b])
                    emit_rope(P, k_f, k_b, r_t, r_m, 3)
                    pt = next_tr()
                    for q in range(3):
                        P.op('pe', lambda e, pt=pt, q=q, k_b=k_b: e.transpose(out=pt[:, q, :], in_=k_b[:, q, :], identity=cb[:, CI_ID, :]), reads=[k_b, cb], writes=[pt])
                    P.op('act', lambda e, pt=pt, k_T=k_T: e.activation(out=k_T[:], in_=pt[:, 0:3, :], func=AF.Copy), reads=[pt], writes=[k_T])
                    for q in range(2 if g == 0 else 0):
                        P.op('sp', lambda e, k_T=k_T, q=q, r0=r0: e.dma_start(out=KT_o[q, :, r0:r0 + 128], in_=k_T[:, q, :]), reads=[k_T], writes=[KT_o], dma=True)
                    if g == 0: P.op('sp', lambda e, k_T=k_T, r0=r0: e.dma_start(out=ikT_o[:, r0:r0 + 128], in_=k_T[:, 2, :]), reads=[k_T], writes=[ikT_o], dma=True)
                b0 = sbi * 4
                P.op('act', lambda e, bar=bar, n_sub=n_sub: e.activation(out=bar[:, 0:n_sub, 0:4], in_=bar[:, 0:n_sub, 0:4], func=AF.Exp, scale=-1.0), reads=[bar], writes=[bar])
                P.op('dve', lambda e, bar=bar, n_sub=n_sub: e.tensor_tensor(out=bar[:, 0:n_sub, 4:8], in0=bar[:, 0:n_sub, 4:8], in1=hp_s[:, 1, :].unsqueeze(1).to_broadcast([128, n_sub, 4]), op=ALU.add),
                     reads=[bar, hp_s], writes=[bar])
                P.op('act', lambda e, bar=bar, n_sub=n_sub: e.activation(out=bar[:, 0:n_sub, 4:8], in_=bar[:, 0:n_sub, 4:8], func=AF.Exp), reads=[bar], writes=[bar])
                P.op('dve', lambda e, bar=bar, n_sub=n_sub: e.tensor_scalar(out=bar[:, 0:n_sub, :], in0=bar[:, 0:n_sub, :], scalar1=1.0, scalar2=None, op0=ALU.add), reads=[bar], writes=[bar])
                P.op('act', lambda e, bar=bar, n_sub=n_sub: e.activation(out=bar[:, 0:n_sub, 4:8], in_=bar[:, 0:n_sub, 4:8], func=AF.Ln), reads=[bar], writes=[bar])
                P.op('dve', lambda e, bar=bar, n_sub=n_sub, b0=b0: e.reciprocal(out=gb_all[:, b0:b0 + n_sub, 0:4], in_=bar[:, 0:n_sub, 0:4]), reads=[bar], writes=[gb_all])
                P.op('dve', lambda e, bar=bar, n_sub=n_sub, b0=b0: e.tensor_tensor(out=gb_all[:, b0:b0 + n_sub, 4:8], in0=bar[:, 0:n_sub, 4:8], in1=nea[:].unsqueeze(1).to_broadcast([128, n_sub, 4]), op=ALU.mult),
                     reads=[bar, nea, gb_all], writes=[gb_all])
                for j in range(n_sub):
                    b = sbi * 4 + j
                    r0 = b * 128
                    tk = tm_kv[b % 2]
                    pt = next_tr()
                    for q in range(4):
                        P.op('pe', lambda e, fmT=fmT, pt=pt, q=q, j=j: e.transpose(out=pt[:, q, :], in_=fmT[:, 4 + q, j * 128:(j + 1) * 128], identity=cb[:, CI_ID, :]),
                             reads=[fmT, cb], writes=[pt])
                    P.op('act', lambda e, pt=pt, tk=tk: e.activation(out=tk[:, 2:6, :], in_=pt[:], func=AF.Copy), reads=[pt], writes=[tk])
                    pt = next_tr()
                    for q in range(2):
                        P.op('pe', lambda e, fmT=fmT, pt=pt, q=q, j=j: e.transpose(out=pt[:, q, :], in_=fmT[:, 2 + q, j * 128:(j + 1) * 128], identity=cb[:, CI_ID, :]),
                             reads=[fmT, cb], writes=[pt])
                    P.op('dve', lambda e, pt=pt, tk=tk: e.tensor_copy(out=tk[:, 0:2, :], in_=pt[:, 0:2, :]), reads=[pt], writes=[tk])
                    P.op('sp', lambda e, tk=tk, r0=r0: e.dma_start(out=k_d[r0:r0 + 128, :], in_=tk[:, 0:2, :]), reads=[tk], writes=[k_d], dma=True)
                    P.op('sp', lambda e, tk=tk, r0=r0: e.dma_start(out=v_d[r0:r0 + 128, :], in_=tk[:, 2:6, :]), reads=[tk], writes=[v_d], dma=True)
                    if dbg:
                        P.op('sp', lambda e, tk=tk, r0=r0: e.dma_start(out=dbg_v[r0:r0 + 128, :], in_=tk[:, 2:6, :]), reads=[tk], writes=[dbg_v], dma=True)
            if dbg:
                P.op('sp', lambda e: e.dma_start(out=dbg_gb[:], in_=gb_all[:]), reads=[gb_all], writes=[dbg_gb], dma=True)

            P.emit()
            K.unscope(); scA.close()
            pend.append((gb_all, scr[g % NPAR], og_o, g * HPC * 128))
            if len(pend) == NPAR or g == NG - 1:
                scB = K.scope()
                emit_gdn_multi(K, nblk, cf, cb, epsc, ng_s, pend)
                P.emit()
                K.unscope(); scB.close()
                pend = []
    return cf, cb, epsc


def build_prog1(Lp, dbg=False, NG=1):
    nblk = Lp // 128
    nc = bass.Bass("TRN2", target_bir_lowering=False)
    es = ExitStack()
    with es:
        K = Ctx(nc, es); P = K.P
        hfull = K.din("hfull", [Lp, D])
        w_a = K.din("w_a", [NG, D, NA]); gpre = K.din("gpre", [128, 16]); cw = K.din("cw", [NG, 128, 8, 4]); hp = K.din("hp", [NG, 128, 3, HPC])
        ng = K.din("ng", [128, 128]); cst = K.din("cst", [128, 5, 128]); rope = K.din("rope", [Lp, 32])
        og_o = K.dout("og", [Lp, NG * HPC * 128], BF16)
        KT_o = K.dout("KT", [2, 128, Lp], BF16); V_o = K.dout("Vt", [Lp, 2 * 128], BF16); ikT_o = K.dout("ikT", [128, Lp], BF16)
        scr = gdn_scratch(K, Lp)
        dbgt = None
        if dbg:
            dbgt = (K.dout("dbg_gb", [128, nblk, 8]), K.dout("dbg_qT", [2, 128, Lp], BF16), K.dout("dbg_kT", [2, 128, Lp], BF16), K.dout("dbg_v", [Lp, 512], BF16))
        emit_gdn_all(K, Lp, NG, hfull, w_a, gpre, cw, hp, ng, cst, rope, og_o, KT_o, V_o, ikT_o, scr, dbgt)
    return nc


def gdn_scratch(K, Lp, n=2):
    return [(K.dtmp(f"qT_d{i}", [2, 128, Lp]), K.dtmp(f"kT_d{i}", [2, 128, Lp]), K.dtmp(f"k_d{i}", [Lp, 256]), K.dtmp(f"v_d{i}", [Lp, 512]), K.dtmp(f"sz_d{i}", [Lp, 512]))
            for i in range(n)]


def prep1(inp, Lp, core):
    f = np.float32
    x = inp['x'][0]; SEQ = x.shape[0]
    hfull = np.zeros((Lp, D), f)
    hfull[PADF:PADF + NMETA] = inp['meta_tokens']; hfull[PADF + NMETA:PADF + NMETA + SEQ] = x
    w_in = inp['w_in'][0]
    o = np.cumsum([0, 2048, 2048, 4096, 4096, 32, 32, 2048, 256, 256, 2048, 128, 16, 2048, 2048])
    gq, gk, gv, gz, gb, ga, aq, ak, av, iq, ik, iw, g1, g2 = [slice(o[i], o[i + 1]) for i in range(14)]
    c = core
    cols = np.concatenate([np.arange(o[0] + 256 * c, o[0] + 256 * c + 256), np.arange(o[1] + 256 * c, o[1] + 256 * c + 256),
                           np.arange(o[2] + 512 * c, o[2] + 512 * c + 512), np.arange(o[3] + 512 * c, o[3] + 512 * c + 512),
                           np.arange(o[4] + 4 * c, o[4] + 4 * c + 4), np.arange(o[5] + 4 * c, o[5] + 4 * c + 4),
                           np.arange(o[8], o[9]), np.arange(o[7], o[8]), np.arange(o[10], o[11])])
    w_a = np.ascontiguousarray(w_in[:, cols])
    gpre = np.ascontiguousarray(inp['mix_pre_g'][0].reshape(16, 128).T)
    cwf = inp['gdn_conv_w'][0]
    ccols = np.concatenate([np.arange(256 * c, 256 * c + 256), np.arange(2048 + 256 * c, 2048 + 256 * c + 256),
                            np.arange(4096 + 512 * c, 4096 + 512 * c + 512)])
    cw = np.ascontiguousarray(cwf[:, ccols].T.reshape(8, 128, 4).transpose(1, 0, 2))
    hp = np.zeros((128, 3, HPC), f)
    hp[:, 0, :] = inp['gdn_a_log'][0][4 * c:4 * c + 4][None, :]
    hp[:, 1, :] = inp['gdn_dt_bias'][0][4 * c:4 * c + 4][None, :]
    ng = np.ascontiguousarray(np.broadcast_to(inp['gdn_norm_g'][0][None, :], (128, 128))).astype(f)
    pos = np.maximum(np.arange(Lp) - PADF, 0)
    return {"hfull": hfull, "w_a": w_a[None], "gpre": gpre, "cw": cw[None], "hp": hp[None], "ng": ng, "cst": make_consts(), "rope": rope_table(pos)}


def emit_rope(P, xf, xb, r_t, r_m, nh):
    cosb = lambda: r_t[:, 0:16].unsqueeze(1).to_broadcast([128, nh, 16])
    sinb = lambda: r_t[:, 16:32].unsqueeze(1).to_broadcast([128, nh, 16])
    x1 = lambda: xf[:, 0:nh, 0:16]
    x2 = lambda: xf[:, 0:nh, 16:32]
    P.op('dve', lambda e: e.tensor_tensor(out=r_m[:, 0, 0:nh, :], in0=x1(), in1=cosb(), op=ALU.mult), reads=[xf, r_t], writes=[r_m])
    P.op('dve', lambda e: e.tensor_tensor(out=r_m[:, 1, 0:nh, :], in0=x2(), in1=sinb(), op=ALU.mult), reads=[xf, r_t, r_m], writes=[r_m])
    P.op('dve', lambda e: e.tensor_tensor(out=r_m[:, 2, 0:nh, :], in0=x2(), in1=cosb(), op=ALU.mult), reads=[xf, r_t, r_m], writes=[r_m])
    P.op('dve', lambda e: e.tensor_tensor(out=r_m[:, 3, 0:nh, :], in0=x1(), in1=sinb(), op=ALU.mult), reads=[xf, r_t, r_m], writes=[r_m])
    P.op('dve', lambda e: e.tensor_tensor(out=xb[:, 0:nh, 0:16], in0=r_m[:, 0, 0:nh, :], in1=r_m[:, 1, 0:nh, :], op=ALU.subtract), reads=[r_m, xb], writes=[xb])
    P.op('dve', lambda e: e.tensor_tensor(out=xb[:, 0:nh, 16:32], in0=r_m[:, 2, 0:nh, :], in1=r_m[:, 3, 0:nh, :], op=ALU.add), reads=[r_m, xb], writes=[xb])


def gdn_setup(K, nblk, cf, cb, epsc, psT, ps_s, gb_all, ng_s, qT_d, kT_d, k_d, v_d, sz_d, og_o, ogc0=0):
    P = K.P
    H = HPC
    S32 = K.sb([128, H, 128], F32, "S32"); Sb = K.sb([128, H, 128], BF16, "Sb")
    P.op('dve', lambda e: e.memset(S32[:], 0.0), writes=[S32])
    P.op('dve', lambda e: e.memset(Sb[:], 0.0), writes=[Sb])
    qT = [K.sb([128, 2, 128], BF16, f"g_qT{i}") for i in range(2)]
    kT = [K.sb([128, 2, 128], BF16, f"g_kT{i}") for i in range(2)]
    ktm = [K.sb([128, 2, 128], BF16, f"g_ktm{i}") for i in range(2)]
    vtm = [K.sb([128, H, 128], BF16, f"g_vtm{i}") for i in range(2)]
    szt = [K.sb([128, H, 128], BF16, f"g_sz{i}") for i in range(2)]
    sm = K.sb([128, 8, H], F32, "g_sm")
    gbc = K.sb([128, H, 128], F32, "g_gbc")
    Dt = K.sb([128, H, 128], F32, "g_Dt")
    grow = K.sb([128, H, 128], F32, "g_grow")
    ks = K.sb([128, H, 128], BF16, "g_ks")
    ksT = K.sb([128, H, 128], BF16, "g_ksT")
    Nn = [K.sb([128, H, 128], BF16, f"g_N{i}") for i in range(2)]
    NT = [K.sb([128, H, 128], BF16, f"g_NT{i}") for i in range(2)]
    Pb = K.sb([128, H, 128], BF16, "g_Pb")
    vs = K.sb([128, H, 128], F32, "g_vs")
    Rt = K.sb([128, H, 128], BF16, "g_Rt")
    vnew = K.sb([128, H, 128], BF16, "g_vnew")
    attnT = K.sb([128, H, 128], BF16, "g_attnT")
    qgT = K.sb([128, H, 128], BF16, "g_qgT")
    kd = K.sb([128, H, 128], BF16, "g_kd")
    gz = K.sb([128, H, 128], F32, "g_gz")
    og = [K.sb([128, H, 128], BF16, f"g_og{i}") for i in range(2)]
    junk = K.sb([128, 128], BF16, "g_junk")
    ssq = K.sb([128, 2, H], F32, "g_ssq")
    ident4 = K.sb([128, H, 128], F32, "g_id4")
    offd4 = K.sb([128, H, 128], F32, "g_offd4")
    for h in range(H):
        P.op('dve', lambda e, h=h: e.tensor_copy(out=ident4[:, h, :], in_=cf[:, CI_ID, :]), reads=[cf, ident4], writes=[ident4])
        P.op('dve', lambda e, h=h: e.tensor_copy(out=offd4[:, h, :], in_=cf[:, CI_OFFD, :]), reads=[cf, offd4], writes=[offd4])
    psA = K.ps([128, H, 128], F32, "g_psA"); psB = K.ps([128, H, 128], F32, "g_psB"); psC = K.ps([128, H, 128], F32, "g_psC")

    def chunk(c):
        r0 = c * 128
        q_T = qT[c % 2]; k_T = kT[c % 2]; k_t = ktm[c % 2]; v_t = vtm[c % 2]; s_z = szt[c % 2]; o_g = og[c % 2]
        P.op('sp', lambda e, q_T=q_T, r0=r0: e.dma_start(out=q_T[:], in_=qT_d.t[:, :, r0:r0 + 128].rearrange("h p t -> p h t")), reads=[qT_d], writes=[q_T], dma=True)
        P.op('sp', lambda e, k_T=k_T, r0=r0: e.dma_start(out=k_T[:], in_=kT_d.t[:, :, r0:r0 + 128].rearrange("h p t -> p h t")), reads=[kT_d], writes=[k_T], dma=True)
        P.op('sp', lambda e, k_t=k_t, r0=r0: e.dma_start(out=k_t[:], in_=k_d.t[r0:r0 + 128, :].rearrange("t (h d) -> t h d", h=2)), reads=[k_d], writes=[k_t], dma=True)
        P.op('sp', lambda e, v_t=v_t, r0=r0: e.dma_start(out=v_t[:], in_=v_d.t[r0:r0 + 128, :].rearrange("t (h d) -> t h d", h=H)), reads=[v_d], writes=[v_t], dma=True)
        P.op('sp', lambda e, s_z=s_z, r0=r0: e.dma_start(out=s_z[:], in_=sz_d.t[r0:r0 + 128, :].rearrange("t (h d) -> t h d", h=H)), reads=[sz_d], writes=[s_z], dma=True)
        beta = lambda: gb_all[:, c, 0:4]
        g = lambda: gb_all[:, c, 4:8]
        yield
        P.op('pe', lambda e, c=c: e.matmul(ps_s[:, 0, :], lhsT=cf[:, CI_TRIU, :], rhs=gb_all[:, c, 4:8], start=True, stop=True), reads=[cf, gb_all], writes=[ps_s])
        P.op('pe', lambda e, c=c: e.matmul(ps_s[:, 1, :], lhsT=cf[:, CI_ONES, :], rhs=gb_all[:, c, 4:8], start=True, stop=True), reads=[cf, gb_all, ps_s], writes=[ps_s])
        P.op('dve', lambda e: e.tensor_copy(out=sm[:, 0, :], in_=ps_s[:, 0, :]), reads=[ps_s, sm], writes=[sm])
        P.op('act', lambda e: e.activation(out=sm[:, 1, :], in_=ps_s[:, 0, :], func=AF.Exp), reads=[ps_s, sm], writes=[sm])
        P.op('act', lambda e: e.activation(out=sm[:, 4, :], in_=ps_s[:, 1, :], func=AF.Exp), reads=[ps_s, sm], writes=[sm])
        P.op('act', lambda e, c=c: e.activation(out=sm[:, 2, :], in_=gb_all[:, c, 0:4], func=AF.Sqrt), reads=[gb_all, sm], writes=[sm])
        P.op('dve', lambda e: e.scalar_tensor_tensor(out=sm[:, 3, :], in0=sm[:, 2, :], scalar=-1.0, in1=sm[:, 1, :], op0=ALU.mult, op1=ALU.mult), reads=[sm], writes=[sm])
        P.op('dve', lambda e: e.tensor_tensor(out=sm[:, 7, :], in0=ps_s[:, 1, :], in1=sm[:, 0, :], op=ALU.subtract), reads=[ps_s, sm], writes=[sm])
        P.op('act', lambda e: e.activation(out=sm[:, 5, :], in_=sm[:, 7, :], func=AF.Exp), reads=[sm], writes=[sm])
        P.op('dve', lambda e: e.tensor_scalar(out=sm[:, 6, :], in0=sm[:, 0, :], scalar1=-1.0, scalar2=None, op0=ALU.mult), reads=[sm], writes=[sm])
        yield
        yield
        for h in range(H):
            P.op('dve', lambda e, h=h, c=c: e.tensor_scalar(out=gbc[:, h, :], in0=cf[:, CI_ONES, :], scalar1=gb_all[:, c, 4 + h:5 + h], scalar2=None, op0=ALU.mult),
                 reads=[cf, gb_all, gbc], writes=[gbc])
        yield
        for h in range(H):
            P.op('pe', lambda e, h=h: e.matmul(psA[:, h, :], lhsT=gbc[:, h, :], rhs=cf[:, CI_TRIU, :], start=True, stop=True), reads=[gbc, cf, psA], writes=[psA])
            P.op('pe', lambda e, h=h: e.matmul(psB[:, h, :], lhsT=gbc[:, h, :], rhs=cf[:, CI_TRIU, :], start=True, stop=False), reads=[gbc, cf, psB], writes=[psB])
            P.op('pe', lambda e, h=h: e.matmul(psB[:, h, :], lhsT=cf[:, CI_ID, :], rhs=cf[:, CI_NEGM, :], start=False, stop=True), reads=[cf, psB], writes=[psB])
        P.op('act', lambda e: e.activation(out=grow[:], in_=psA[:], func=AF.Exp), reads=[psA], writes=[grow])
        yield
        for h in range(H):
            P.op('act', lambda e, h=h: e.activation(out=Dt[:, h, :], in_=psB[:, h, :], func=AF.Exp, bias=sm[:, 6, h:h + 1]), reads=[psB, sm, Dt], writes=[Dt])
        yield
        yield
        for h in range(H):
            P.op('dve', lambda e, h=h, k_t=k_t: e.tensor_scalar(out=ks[:, h, :], in0=k_t[:, h // 2, :], scalar1=sm[:, 2, h:h + 1], scalar2=None, op0=ALU.mult),
                 reads=[k_t, sm, ks], writes=[ks])
            P.op('act', lambda e, h=h, v_t=v_t: e.activation(out=vs[:, h, :], in_=v_t[:, h, :], func=AF.Copy, scale=sm[:, 2, h:h + 1]),
                 reads=[v_t, sm, vs], writes=[vs])
            P.op('act', lambda e, h=h, k_t=k_t: e.activation(out=kd[:, h, :], in_=k_t[:, h // 2, :], func=AF.Copy, scale=sm[:, 5, h:h + 1]),
                 reads=[k_t, sm, kd], writes=[kd])
            P.op('dve', lambda e, h=h, q_T=q_T: e.tensor_tensor(out=qgT[:, h, :], in0=q_T[:, h // 2, :], in1=grow[:, h, :], op=ALU.mult), reads=[q_T, grow, qgT], writes=[qgT])
            P.op('pool', lambda e, h=h, s_z=s_z: e.tensor_tensor(out=gz[:, h, :], in0=s_z[:, h, :], in1=ng_s[:], op=ALU.mult), reads=[s_z, ng_s, gz], writes=[gz])
        yield
        for h in range(H):
            P.op('pe', lambda e, h=h: e.transpose(out=psT[:, h, :], in_=ks[:, h, :], identity=cb[:, CI_ID, :]), reads=[ks, cb, psT], writes=[psT])
        P.op('act', lambda e: e.activation(out=ksT[:], in_=psT[:], func=AF.Copy), reads=[psT], writes=[ksT])
        yield
        yield
        for h in range(H):
            P.op('pe', lambda e, h=h: e.matmul(psA[:, h, :], lhsT=ksT[:, h, :], rhs=ksT[:, h, :], start=True, stop=True), reads=[ksT, psA], writes=[psA])
        yield
        for hq in range(2):
            P.op('pe', lambda e, hq=hq, k_T=k_T, q_T=q_T: e.matmul(psC[:, hq, :], lhsT=k_T[:, hq, :], rhs=q_T[:, hq, :], start=True, stop=True), reads=[k_T, q_T, psC], writes=[psC])
        yield
        for h in range(H):
            P.op('dve', lambda e, h=h: e.tensor_tensor(out=attnT[:, h, :], in0=psC[:, h // 2, :], in1=Dt[:, h, :], op=ALU.mult), reads=[psC, Dt, attnT], writes=[attnT])
        P.op('dve', lambda e: e.tensor_tensor(out=Dt[:], in0=Dt[:], in1=offd4[:], op=ALU.mult), reads=[Dt, offd4, attnT], writes=[Dt])
        N0 = Nn[0]; NT0 = NT[0]
        P.op('dve', lambda e: e.scalar_tensor_tensor(out=N0[:], in0=psA[:], scalar=-1.0, in1=Dt[:], op0=ALU.mult, op1=ALU.mult), reads=[psA, Dt], writes=[N0])
        yield
        for h in range(H):
            P.op('pe', lambda e, h=h: e.transpose(out=psT[:, h, :], in_=N0[:, h, :], identity=cb[:, CI_ID, :]), reads=[N0, cb, psT], writes=[psT])
        P.op('act', lambda e: e.activation(out=NT0[:], in_=psT[:], func=AF.Copy), reads=[psT], writes=[NT0])
        P.op('dve', lambda e: e.tensor_tensor(out=Pb[:], in0=N0[:], in1=ident4[:], op=ALU.add), reads=[N0, ident4], writes=[Pb])
        cur = 0
        yield
        for step in range(1, 7):
            Nc = Nn[cur]; NTc = NT[cur]; Nx = Nn[1 - cur]; NTx = NT[1 - cur]
            for h in range(H):
                P.op('pe', lambda e, h=h, Nc=Nc, NTc=NTc: e.matmul(psB[:, h, :], lhsT=Nc[:, h, :], rhs=NTc[:, h, :], start=True, stop=True), reads=[Nc, NTc, psB], writes=[psB])
            P.op('act', lambda e, NTx=NTx: e.activation(out=NTx[:], in_=psB[:], func=AF.Copy), reads=[psB], writes=[NTx])
            if step < 6:
                for h in range(H):
                    P.op('pe', lambda e, h=h, Nc=Nc, NTc=NTc: e.matmul(psA[:, h, :], lhsT=NTc[:, h, :], rhs=Nc[:, h, :], start=True, stop=True), reads=[Nc, NTc, psA], writes=[psA])
                P.op('dve', lambda e, Nx=Nx: e.tensor_copy(out=Nx[:], in_=psA[:]), reads=[psA], writes=[Nx])
            for h in range(H):
                P.op('pe', lambda e, h=h, NTx=NTx: e.matmul(psC[:, h, :], lhsT=NTx[:, h, :], rhs=Pb[:, h, :], start=True, stop=True), reads=[NTx, Pb, psC], writes=[psC])
            P.op('dve', lambda e: e.tensor_tensor(out=Pb[:], in0=Pb[:], in1=psC[:], op=ALU.add), reads=[Pb, psC], writes=[Pb])
            cur = 1 - cur
        yield
        yield
        for h in range(H):
            P.op('pe', lambda e, h=h, k_T=k_T: e.matmul(psA[:, h, :], lhsT=k_T[:, h // 2, :], rhs=Sb[:, h, :], start=True, stop=True), reads=[k_T, Sb, psA], writes=[psA])
        yield
        for h in range(H):
            P.op('dve', lambda e, h=h: e.scalar_tensor_tensor(out=Rt[:, h, :], in0=psA[:, h, :], scalar=sm[:, 3, h:h + 1], in1=vs[:, h, :], op0=ALU.mult, op1=ALU.add),
                 reads=[psA, sm, vs, Rt], writes=[Rt])
        yield
        for h in range(H):
            P.op('pe', lambda e, h=h: e.matmul(psB[:, h, :], lhsT=Pb[:, h, :], rhs=Rt[:, h, :], start=True, stop=True), reads=[Pb, Rt, psB], writes=[psB])
        yield
        for h in range(H):
            P.op('act', lambda e, h=h: e.activation(out=vnew[:, h, :], in_=psB[:, h, :], func=AF.Copy, scale=sm[:, 2, h:h + 1]), reads=[psB, sm, vnew], writes=[vnew])
        yield
        for h in range(H):
            P.op('pe', lambda e, h=h: e.matmul(psC[:, h, :], lhsT=qgT[:, h, :], rhs=Sb[:, h, :], start=True, stop=False), reads=[qgT, Sb, psC], writes=[psC])
            P.op('pe', lambda e, h=h: e.matmul(psC[:, h, :], lhsT=attnT[:, h, :], rhs=vnew[:, h, :], start=False, stop=True), reads=[attnT, vnew, psC], writes=[psC])
        yield
        for h in range(H):
            P.op('pe', lambda e, h=h: e.matmul(psA[:, h, :], lhsT=kd[:, h, :], rhs=vnew[:, h, :], start=True, stop=True), reads=[kd, vnew, psA], writes=[psA])
        yield
        for h in range(H):
            P.op('dve', lambda e, h=h: e.scalar_tensor_tensor(out=Sb[:, h, :], in0=S32[:, h, :], scalar=sm[:, 4, h:h + 1], in1=psA[:, h, :], op0=ALU.mult, op1=ALU.add),
                 reads=[S32, sm, psA, Sb], writes=[Sb])
        for h in range(H):
            P.op('dve', lambda e, h=h: e.scalar_tensor_tensor(out=S32[:, h, :], in0=S32[:, h, :], scalar=sm[:, 4, h:h + 1], in1=psA[:, h, :], op0=ALU.mult, op1=ALU.add),
                 reads=[S32, sm, psA], writes=[S32])
        yield
        yield
        for h in range(H):
            P.op('act', lambda e, h=h: e.activation(out=junk[:], in_=psC[:, h, :], func=AF.Square, accum_out=ssq[:, 0, h:h + 1]), reads=[psC, junk, ssq], writes=[junk, ssq])
        P.op('act', lambda e: e.activation(out=ssq[:, 1, :], in_=ssq[:, 0, :], func=AF.Sqrt, scale=1.0 / 128, bias=epsc[:, 0:1]), reads=[ssq, epsc], writes=[ssq])
        P.op('dve', lambda e: e.reciprocal(out=ssq[:, 1, :], in_=ssq[:, 1, :]), reads=[ssq], writes=[ssq])
        yield
        for h in range(H):
            P.op('dve', lambda e, h=h, o_g=o_g: e.scalar_tensor_tensor(out=o_g[:, h, :], in0=psC[:, h, :], scalar=ssq[:, 1, h:h + 1], in1=gz[:, h, :], op0=ALU.mult, op1=ALU.mult),
                 reads=[psC, ssq, gz, o_g], writes=[o_g])
        P.op('sp', lambda e, o_g=o_g, r0=r0: e.dma_start(out=og_o[r0:r0 + 128, ogc0:ogc0 + HPC * 128], in_=o_g[:]), reads=[o_g], writes=[og_o], dma=True)

    return chunk


def emit_gdn_multi(K, nblk, cf, cb, epsc, ng_s, groups):
    psT = K.ps([128, HPC, 128], BF16, "g_psT")
    ps_s = K.ps([128, 2, HPC], F32, "g_pss")
    fns = [gdn_setup(K, nblk, cf, cb, epsc, psT, ps_s, gb, ng_s, *scr, og_o, c0) for (gb, scr, og_o, c0) in groups]
    for c in range(nblk):
        gens = [f(c) for f in fns]
        while gens:
            nxt = []
            for gen in gens:
                try:
                    next(gen); nxt.append(gen)
                except StopIteration:
                    pass
            gens = nxt


NWC = 2048 + 2048 + 16 + 2048 + 2048
BSTR = 126
A0 = 126
NIOTA = 1408


def blocks_for(SEQ):
    nb = -(-SEQ // BSTR)
    NS = -(-nb // NCORE)
    return nb, NS


def slot_geom(j, nblk):
    a_min = A0 + BSTR * (NCORE * j)
    a_max = A0 + BSTR * (NCORE * j + NCORE - 1)
    kd = min(a_min // 128, nblk)
    kb_end = min((a_max + 127) // 128 + 1, nblk)
    kd = min(kd, kb_end)
    return kd, kb_end


def build_prog2(Lp, NS, topk, dbg=False, fused=False):
    nblk = Lp // 128
    nc = bass.Bass("TRN2", target_bir_lowering=False)
    es = ExitStack()
    with es:
        K = Ctx(nc, es); P = K.P
        R = NS * 128
        hown = K.din("hown", [R, D])
        if not fused:
            ogown = K.din("ogown", [R, 4096], BF16)
            KT_i = K.din("KT", [2, 128, Lp], BF16); V_i = K.din("Vt", [Lp, 256], BF16); ikT_i = K.din("ikT", [128, Lp], BF16)
            ogsrc = ('own', ogown)
        else:
            NG = NCORE
            hfull = K.din("hfull", [Lp, D])
            w_a = K.din("w_a", [NG, D, NA]); cw = K.din("cw", [NG, 128, 8, 4]); hp = K.din("hp", [NG, 128, 3, HPC])
            ng = K.din("ng", [128, 128]); rope = K.din("rope", [Lp, 32])
            sel_i = K.din("sel", [128, 8, 128], BF16)
            og_d = K.dtmp("og_d", [Lp, 4096]); KT_i = K.dtmp("KT_s", [2, 128, Lp]); V_i = K.dtmp("Vt_s", [Lp, 256]); ikT_i = K.dtmp("ikT_s", [128, Lp])
        ropeo = K.din("ropeo", [R, 32])
        qrel_i = K.din("qrel", [128, NS])
        iota_i = K.din("iota", [128, NIOTA])
        cst = K.din("cst", [128, 5, 128])
        gpre = K.din("gpre", [128, 16]); g2pre = K.din("g2pre", [128, 16])
        gpost = K.din("gpost", [128, D]); g2post = K.din("g2post", [128, D])
        fcw = K.din("fcw", [128, 96, 3]); fcb = K.din("fcb", [128, 96])
        w_c = K.din("w_c", [D, NWC]); wbg = K.din("wbg", [4096, D]); wba = K.din("wba", [D, D]); wout = K.din("wout", [D, D])
        wup = K.din("wup", [D, 2 * NFF]); wdn = K.din("wdn", [NFF, D])
        out_o = K.dout("out", [NS, BSTR, D])
        wc_b = K.dtmp("wc_b", [128, 17, 16, 512]); wbg_b = K.dtmp("wbg_b", [128, 4, 32, 512]); wba_b = K.dtmp("wba_b", [128, 4, 16, 512])
        wout_b = K.dtmp("wout_b", [128, 4, 16, 512]); wup_b = K.dtmp("wup_b", [128, 24, 16, 512]); wdn_b = K.dtmp("wdn_b", [128, 4, 48, 512])
        if dbg:
            dbg_oatt = K.dout("dbg_oatt", [R, D], BF16); dbg_thr = K.dout("dbg_thr", [128, NS]); dbg_h1 = K.dout("dbg_h1", [R, D])
            dbg_q = K.dout("dbg_q", [R, D], BF16)

        if fused:
            cf, cb, epsc = emit_gdn_all(K, Lp, NG, hfull, w_a, gpre, cw, hp, ng, cst, rope, og_d, KT_i, V_i, ikT_i, gdn_scratch(K, Lp))
            sel_s = K.sb([128, 8, 128], BF16, "sel_s")
            P.op('sp', lambda e: e.dma_start(out=sel_s[:], in_=sel_i[:]), writes=[sel_s], dma=True)
            ogsrc = ('sel', og_d, sel_s, Lp)
        else:
            cf = K.sb([128, 5, 128], F32, "cf"); cb = K.sb([128, 5, 128], BF16, "cb")
            P.op('sp', lambda e: e.dma_start(out=cf[:], in_=cst[:]), writes=[cf], dma=True)
            P.op('dve', lambda e: e.tensor_copy(out=cb[:], in_=cf[:]), reads=[cf], writes=[cb])
            epsc = K.sb([128, 1], F32, "epsc")
            P.op('dve', lambda e: e.memset(epsc[:], EPS), writes=[epsc])
        gpre_s = K.sb([128, 16], F32); g2pre_s = K.sb([128, 16], F32); qrel_s = K.sb([128, NS], F32)
        fcw_s = K.sb([128, 96, 3], F32); fcb_s = K.sb([128, 96], F32)
        for dst, src in ((gpre_s, gpre), (g2pre_s, g2pre), (qrel_s, qrel_i), (fcw_s, fcw), (fcb_s, fcb)):
            P.op('sp', lambda e, dst=dst, src=src: e.dma_start(out=dst[:], in_=src[:]), writes=[dst], dma=True)

        sc0 = K.scope()
        st = [K.sb([128, 2048], F32, f"w0s{i}") for i in range(2)]
        sbt = [K.sb([128, 2048], BF16, f"w0b{i}") for i in range(2)]
        it = [0]
        def cast_w(src, dst, KC, N, gain, up=False):
            sv = src.t.rearrange("(kc p) n -> p kc n", p=128)
            for kc in range(KC):
                for n0 in range(0, N, 2048):
                    n1 = min(N, n0 + 2048); w = n1 - n0
                    s_ = st[it[0] % 2]; b_ = sbt[it[0] % 2]; it[0] += 1
                    P.op('sp', lambda e, s_=s_, kc=kc, n0=n0, n1=n1, w=w: e.dma_start(out=s_[:, 0:w], in_=sv[:, kc, n0:n1]), writes=[s_], dma=True)
                    if gain is None:
                        P.op('act', lambda e, s_=s_, b_=b_, w=w: e.activation(out=b_[:, 0:w], in_=s_[:, 0:w], func=AF.Copy), reads=[s_], writes=[b_])
                    else:
                        P.op('act', lambda e, s_=s_, b_=b_, w=w, kc=kc: e.activation(out=b_[:, 0:w], in_=s_[:, 0:w], func=AF.Copy, scale=gain[:, kc:kc + 1]),
                             reads=[s_, gain], writes=[b_])
                    if up:
                        half = n0 // NFF; g0 = (n0 % NFF) // 256
                        P.op('pool', lambda e, b_=b_, kc=kc, g0=g0, half=half: e.dma_start(out=dst[:, g0:g0 + 8, kc, half * 256:(half + 1) * 256],
                                                                                          in_=b_[:, 0:2048].rearrange("p (g c) -> p g c", c=256)), reads=[b_], writes=[dst], dma=True)
                    else:
                        nt = w // 512; rem = w % 512; t0_ = n0 // 512
                        if nt:
                            P.op('pool', lambda e, b_=b_, kc=kc, nt=nt, t0_=t0_: e.dma_start(out=dst[:, t0_:t0_ + nt, kc, :], in_=b_[:, 0:nt * 512].rearrange("p (g c) -> p g c", c=512)),
                                 reads=[b_], writes=[dst], dma=True)
                        if rem:
                            P.op('pool', lambda e, b_=b_, kc=kc, nt=nt, t0_=t0_, rem=rem: e.dma_start(out=dst[:, t0_ + nt, kc, 0:rem], in_=b_[:, nt * 512:nt * 512 + rem]),
                                 reads=[b_], writes=[dst], dma=True)
        cast_w(w_c, wc_b, 16, NWC, gpre_s)
        cast_w(wbg, wbg_b, 32, D, None); cast_w(wba, wba_b, 16, D, None); cast_w(wout, wout_b, 16, D, None)
        cast_w(wup, wup_b, 16, 2 * NFF, g2pre_s, up=True); cast_w(wdn, wdn_b, 48, D, None)
        P.emit(); K.unscope(); sc0.close()

        xo = K.sb([128, D], F32, "xo")
        QT = K.sb([128, 16, 128], BF16, "QT"); iqT = K.sb([128, 16, 128], BF16, "iqT")
        sgn = K.sb([128, 16], F32, "sgn"); aiw = K.sb([128, 16], F32, "aiw")
        sig = K.sb([128, 2, D], BF16, "sig")
        oatt = K.sb([128, D], BF16, "oatt")
        thr = K.sb([128, NS], F32, "thr")

        for j in range(NS):
            r0 = j * 128
            kd, kb_end = slot_geom(j, nblk)
            nk = kb_end * 128
            scW = K.scope()
            W = make_wpool(K)
            xs = K.sb([128, D], BF16, "p_xs"); ss = K.sb([128, 2], F32, "p_ss")
            uT = K.sb([128, 16, 128], BF16, "p_uT")
            tq = K.sb([128, 16, 128], F32, "p_tq"); tqb = K.sb([128, 16, 128], BF16, "p_tqb")
            r_t = K.sb([128, 32], F32, "p_rt"); r_m = K.sb([128, 4, 16, 16], F32, "p_rm")
            iwt = K.sb([128, 16], F32, "p_iw")
            P.op('sp', lambda e, r0=r0: e.dma_start(out=xo[:], in_=hown[r0:r0 + 128, :]), writes=[xo], dma=True)
            P.op('sp', lambda e, r0=r0: e.dma_start(out=r_t[:], in_=ropeo[r0:r0 + 128, :]), writes=[r_t], dma=True)
            emit_norm_T(P, W, cb, epsc, xo, xs, ss, uT)
            for ct in range(17):
                n0 = ct * 512 if ct < 8 else (8192 if ct == 8 else 4096 + (ct - 9) * 512)
                ncol = 16 if ct == 8 else 512
                pm = W.linear(uT, wc_b, 16, n0, ncol)
                if ct < 8:
                    hh = (ct % 4) * 4
                    if ct == 4:
                        finish_q(P, W, cb, tq, tqb, r_t, r_m, QT, 128 ** -0.5, None)
                    P.op('act', lambda e, pm=pm, hh=hh: e.activation(out=tq[:, hh:hh + 4, :], in_=pm[:, 0:512], func=AF.Copy), reads=[pm], writes=[tq])
                elif ct == 8:
                    P.op('act', lambda e, pm=pm: e.activation(out=iwt[:], in_=pm[:, 0:16], func=AF.Copy), reads=[pm], writes=[iwt])
                    P.op('act', lambda e: e.activation(out=aiw[:], in_=iwt[:], func=AF.Abs, scale=(16 ** -0.5) * (128 ** -0.5)), reads=[iwt], writes=[aiw])
                    P.op('dve', lambda e: e.tensor_scalar(out=sgn[:], in0=iwt[:], scalar1=0.0, scalar2=2.0, op0=ALU.is_gt, op1=ALU.mult), reads=[iwt], writes=[sgn])
                    P.op('dve', lambda e: e.tensor_scalar(out=sgn[:], in0=sgn[:], scalar1=-1.0, scalar2=None, op0=ALU.add), reads=[sgn], writes=[sgn])
                    finish_q(P, W, cb, tq, tqb, r_t, r_m, iqT, None, aiw)
                else:
                    gi = (ct - 9) // 4; c0 = ((ct - 9) % 4) * 512
                    P.op('act', lambda e, pm=pm, gi=gi, c0=c0: e.activation(out=sig[:, gi, c0:c0 + 512], in_=pm[:, 0:512], func=AF.Sigmoid), reads=[pm], writes=[sig])
            if dbg:
                P.op('sp', lambda e, r0=r0: e.dma_start(out=dbg_q[r0:r0 + 128, :], in_=tqb[:]), reads=[tqb], writes=[dbg_q], dma=True)
            P.emit(); K.unscope(); scW.close()

            scA = K.scope()
            emit_attention(K, cb, cf, j, kd, kb_end, topk, QT, iqT, sgn, qrel_s, iota_i, KT_i, V_i, ikT_i, oatt, thr)
            K.unscope(); scA.close()
            if dbg:
                P.op('sp', lambda e, r0=r0: e.dma_start(out=dbg_oatt[r0:r0 + 128, :], in_=oatt[:]), reads=[oatt], writes=[dbg_oatt], dma=True)

            scW = K.scope()
            W = make_wpool(K)
            emit_merge_ffn(K, W, cb, epsc, j, xo, ogsrc, oatt, sig, gpost, g2post, fcw_s, fcb_s,
                           wbg_b, wba_b, wout_b, wup_b, wdn_b, out_o, dbg_h1 if dbg else None)
            P.emit(); K.unscope(); scW.close()
        if dbg:
            P.op('sp', lambda e: e.dma_start(out=dbg_thr[:], in_=thr[:]), reads=[thr], writes=[dbg_thr], dma=True)
        P.emit()
    return nc


class WPool:
    pass


def make_wpool(K):
    P = K.P
    W = WPool()
    W.wt = [K.sb([128, 16, 512], BF16, f"wt{i}") for i in range(3)]
    W.ps_mm = [K.ps([128, 512], F32, f"w_psmm{i}") for i in range(3)]
    W.ps_tr = [K.ps([128, 4, 128], BF16, f"w_pstr{i}") for i in range(2)]
    W.nw = 0; W.nm = 0; W.nt = 0
    def next_tr():
        W.nt += 1
        return W.ps_tr[W.nt % 2]
    W.next_tr = next_tr
    def linear(xT, wd, KC, n0, ncol, pm=None, xoff=0):
        if pm is None:
            W.nm += 1; pm = W.ps_mm[W.nm % 3]
        for part in range(KC // 16):
            wt = W.wt[W.nw % 3]; W.nw += 1
            P.op('sp', lambda e, wt=wt, part=part: e.dma_start(out=wt[:, :, 0:ncol], in_=wd[:, n0 // 512, part * 16:(part + 1) * 16, 0:ncol]), reads=[wd], writes=[wt], dma=True)
            for kc in range(16):
                kk = part * 16 + kc
                P.op('pe', lambda e, wt=wt, kc=kc, kk=kk: e.matmul(pm[:, 0:ncol], lhsT=xT[:, xoff + kk, :], rhs=wt[:, kc, 0:ncol], start=(kk == 0), stop=(kk == KC - 1)),
                     reads=[xT, wt], writes=[pm])
        return pm
    W.linear = linear
    return W


def emit_norm_T(P, W, cb, epsc, x, xs, ss, uT):
    P.op('act', lambda e: e.activation(out=xs[:], in_=x[:], func=AF.Square, accum_out=ss[:, 0:1]), reads=[x], writes=[xs, ss])
    P.op('act', lambda e: e.activation(out=ss[:, 1:2], in_=ss[:, 0:1], func=AF.Sqrt, scale=1.0 / D, bias=epsc[:, 0:1]), reads=[ss, epsc], writes=[ss])
    P.op('dve', lambda e: e.reciprocal(out=ss[:, 1:2], in_=ss[:, 1:2]), reads=[ss], writes=[ss])
    P.op('dve', lambda e: e.tensor_scalar(out=xs[:], in0=x[:], scalar1=ss[:, 1:2], scalar2=None, op0=ALU.mult), reads=[x, ss], writes=[xs])
    emit_T(P, W, cb, lambda kc: xs[:, kc * 128:(kc + 1) * 128], xs, uT, 16)


def emit_T(P, W, cb, src_ap, src_tl, dstT, n):
    for g4 in range(0, n, 4):
        pt = W.next_tr()
        m = min(4, n - g4)
        for q in range(m):
            P.op('pe', lambda e, pt=pt, q=q, i=g4 + q: e.transpose(out=pt[:, q, :], in_=src_ap(i), identity=cb[:, CI_ID, :]), reads=[src_tl, cb], writes=[pt])
        if (g4 // 4) % 2:
            P.op('act', lambda e, pt=pt, g4=g4, m=m: e.activation(out=dstT[:, g4:g4 + m, :], in_=pt[:, 0:m, :], func=AF.Copy), reads=[pt], writes=[dstT])
        else:
            P.op('dve', lambda e, pt=pt, g4=g4, m=m: e.tensor_copy(out=dstT[:, g4:g4 + m, :], in_=pt[:, 0:m, :]), reads=[pt], writes=[dstT])


def finish_q(P, W, cb, tq, tqb, r_t, r_m, dstT, const_scale, row_scale):
    if row_scale is not None:
        P.op('dve', lambda e: e.tensor_tensor(out=tq[:], in0=tq[:], in1=row_scale[:].unsqueeze(2).to_broadcast([128, 16, 128]), op=ALU.mult), reads=[tq, row_scale], writes=[tq])
    else:
        P.op('dve', lambda e: e.tensor_scalar(out=tq[:], in0=tq[:], scalar1=const_scale, scalar2=None, op0=ALU.mult), reads=[tq], writes=[tq])
    P.op('act', lambda e: e.activation(out=tqb[:], in_=tq[:], func=AF.Copy), reads=[tq], writes=[tqb])
    emit_rope(P, tq, tqb, r_t, r_m, 16)
    emit_T(P, W, cb, lambda h: tqb[:, h, :], tqb, dstT, 16)


def emit_attention(K, cb, cf, j, kd, kb_end, topk, QT, iqT, sgn, qrel_s, iota_i, KT_i, V_i, ikT_i, oatt, thr):
    P = K.P
    nk = kb_end * 128
    sc = K.sb([128, nk], F32, "a_sc")
    maskT = K.sb([128, kb_end, 128], BF16, "a_maskT")
    JW = 4096
    junk = K.sb([128, JW], BF16, "a_junk")
    rl = [K.sb([128, 512], F32, f"a_rl{i}") for i in range(2)]
    ikb = [K.sb([128, 512], BF16, f"a_ik{i}") for i in range(2)]
    iot = K.sb([128, NIOTA], F32, "a_iota")
    P.op('sp', lambda e: e.dma_start(out=iot[:], in_=iota_i[:]), writes=[iot], dma=True)
    scp = K.scope()
    ps_i = [K.ps([128, 512], F32, f"a_psi{i}") for i in range(3)]
    ps_t = [K.ps([128, 4, 128], BF16, f"a_pst{i}") for i in range(2)]
    ni = 0
    for u0 in range(0, kb_end, 4):
        nb_ = min(4, kb_end - u0); w = nb_ * 128; k0 = u0 * 128
        ik = ikb[(u0 // 4) % 2]
        P.op('sp', lambda e, ik=ik, k0=k0, w=w: e.dma_start(out=ik[:, 0:w], in_=ikT_i[:, k0:k0 + w]), reads=[ikT_i], writes=[ik], dma=True)
        for h in range(16):
            pi = ps_i[ni % 3]; r_ = rl[ni % 2]; ni += 1
            P.op('pe', lambda e, pi=pi, ik=ik, h=h, w=w: e.matmul(pi[:, 0:w], lhsT=iqT[:, h, :], rhs=ik[:, 0:w], start=True, stop=True), reads=[iqT, ik], writes=[pi])
            P.op('act', lambda e, pi=pi, r_=r_, w=w: e.activation(out=r_[:, 0:w], in_=pi[:, 0:w], func=AF.Relu), reads=[pi], writes=[r_])
            if h == 0:
                P.op('dve', lambda e, r_=r_, k0=k0, w=w: e.tensor_scalar(out=sc[:, k0:k0 + w], in0=r_[:, 0:w], scalar1=sgn[:, 0:1], scalar2=None, op0=ALU.mult), reads=[r_, sgn], writes=[sc])
            else:
                P.op('dve', lambda e, r_=r_, k0=k0, w=w, h=h: e.scalar_tensor_tensor(out=sc[:, k0:k0 + w], in0=r_[:, 0:w], scalar=sgn[:, h:h + 1], in1=sc[:, k0:k0 + w], op0=ALU.mult, op1=ALU.add),
                     reads=[r_, sgn, sc], writes=[sc])
    P.op('dve', lambda e: e.memset(sc[:, 0:PADF], -1e30), reads=[sc], writes=[sc])
    wc = (kb_end - kd) * 128
    if wc > 0:
        assert wc <= NIOTA
        P.op('dve', lambda e: e.tensor_scalar(out=iot[:, 0:wc], in0=iot[:, 0:wc], scalar1=qrel_s[:, j:j + 1], scalar2=-1e30, op0=ALU.is_gt, op1=ALU.mult), reads=[iot, qrel_s], writes=[iot])
        P.op('dve', lambda e: e.tensor_tensor(out=sc[:, kd * 128:nk], in0=sc[:, kd * 128:nk], in1=iot[:, 0:wc], op=ALU.add), reads=[sc, iot], writes=[sc])
    lo = K.sb([128, 1], F32, "a_lo"); mid = K.sb([128, 1], F32, "a_mid"); cnt = K.sb([128, 8], F32, "a_cnt"); dl = K.sb([128, 1], F32, "a_dl")
    LO0, RANGE, NIT = -16.0, 64.0, 26
    P.op('dve', lambda e: e.memset(lo[:], LO0), writes=[lo])
    npc = -(-nk // JW)
    for it in range(NIT):
        hk = RANGE / (2 ** (it + 1))
        P.op('dve', lambda e, hk=hk: e.tensor_scalar(out=mid[:], in0=lo[:], scalar1=hk, scalar2=None, op0=ALU.add), reads=[lo], writes=[mid])
        for pc in range(npc):
            c0 = pc * JW; c1 = min(nk, c0 + JW)
            P.op('dve', lambda e, c0=c0, c1=c1, pc=pc: e.tensor_scalar(out=junk[:, 0:c1 - c0], in0=sc[:, c0:c1], scalar1=mid[:, 0:1], scalar2=0.0, op0=ALU.is_ge, op1=ALU.add,
                                                                     accum_out=cnt[:, pc:pc + 1]), reads=[sc, mid, junk], writes=[junk, cnt])
        if npc > 1:
            P.op('dve', lambda e: e.tensor_reduce(out=cnt[:, 7:8], in_=cnt[:, 0:npc], axis=mybir.AxisListType.X, op=ALU.add), reads=[cnt], writes=[cnt])
            cc = 7
        else:
            cc = 0
        P.op('dve', lambda e, hk=hk, cc=cc: e.tensor_scalar(out=dl[:], in0=cnt[:, cc:cc + 1], scalar1=topk - 0.5, scalar2=hk, op0=ALU.is_gt, op1=ALU.mult), reads=[cnt], writes=[dl])
        P.op('dve', lambda e: e.tensor_tensor(out=lo[:], in0=lo[:], in1=dl[:], op=ALU.add), reads=[lo, dl], writes=[lo])
    P.op('dve', lambda e: e.tensor_copy(out=thr[:, j:j + 1], in_=lo[:]), reads=[lo, thr], writes=[thr])
    mk = [K.sb([128, 512], BF16, f"a_mk{i}") for i in range(2)]
    for u0 in range(0, kb_end, 4):
        nb_ = min(4, kb_end - u0); w = nb_ * 128; k0 = u0 * 128
        m_ = mk[(u0 // 4) % 2]; pt = ps_t[(u0 // 4) % 2]
        P.op('dve', lambda e, m_=m_, k0=k0, w=w: e.tensor_scalar(out=m_[:, 0:w], in0=sc[:, k0:k0 + w], scalar1=lo[:, 0:1], scalar2=None, op0=ALU.is_ge), reads=[sc, lo], writes=[m_])
        for q in range(nb_):
            P.op('pe', lambda e, pt=pt, q=q, m_=m_: e.transpose(out=pt[:, q, :], in_=m_[:, q * 128:(q + 1) * 128], identity=cb[:, CI_ID, :]), reads=[m_, cb], writes=[pt])
        P.op('act', lambda e, pt=pt, u0=u0, nb_=nb_: e.activation(out=maskT[:, u0:u0 + nb_, :], in_=pt[:, 0:nb_, :], func=AF.Copy), reads=[pt], writes=[maskT])
    P.emit(); K.unscope(); scp.close()
    scp = K.scope()
    psO = [K.ps([128, 512], F32, f"a_pso{i}") for i in range(6)]
    psS = [K.ps([128, 4, 128], F32, f"a_pss{i}") for i in range(2)]
    Kb = [K.sb([128, 2, 128], BF16, f"a_K{i}") for i in range(3)]
    Vb = [K.sb([128, 2, 129], BF16, f"a_V{i}") for i in range(3)]
    eb = [K.sb([128, 4, 128], BF16, f"a_e{i}") for i in range(2)]
    pb = [K.sb([128, 4, 128], BF16, f"a_p{i}") for i in range(3)]
    rc = K.sb([128, 16], F32, "a_rc")
    for v_ in Vb:
        P.op('dve', lambda e, v_=v_: e.memset(v_[:, :, 128:129], 1.0), writes=[v_])
    ns = 0
    for kb in range(kb_end):
        k_ = Kb[kb % 3]; v_ = Vb[kb % 3]; k0 = kb * 128
        P.op('sp', lambda e, k_=k_, k0=k0: e.dma_start(out=k_[:], in_=KT_i.t[:, :, k0:k0 + 128].rearrange("h p t -> p h t")), reads=[KT_i], writes=[k_], dma=True)
        P.op('sp', lambda e, v_=v_, k0=k0: e.dma_start(out=v_[:, :, 0:128], in_=V_i.t[k0:k0 + 128, :].rearrange("t (h d) -> t h d", h=2)), reads=[V_i, v_], writes=[v_], dma=True)
        for g in range(2):
            for half in range(2):
                h0 = g * 8 + half * 4
                pS = psS[ns % 2]; e_ = eb[ns % 2]; p_ = pb[ns % 3]; ns += 1
                P.op('pe', lambda e, pS=pS, k_=k_, g=g, h0=h0: e.matmul(pS[:], lhsT=k_[:, g, :], rhs=QT[:, h0:h0 + 4, :], start=True, stop=True), reads=[k_, QT], writes=[pS])
                P.op('act', lambda e, pS=pS, e_=e_: e.activation(out=e_[:], in_=pS[:], func=AF.Exp), reads=[pS], writes=[e_])
                P.op('dve', lambda e, e_=e_, p_=p_, kb=kb: e.tensor_tensor(out=p_[:], in0=e_[:], in1=maskT[:, kb, :].unsqueeze(1).to_broadcast([128, 4, 128]), op=ALU.mult),
                     reads=[e_, maskT], writes=[p_])
                for hh in range(4):
                    hd = h0 + hh
                    po = psO[hd // 3]
                    P.op('pe', lambda e, po=po, hd=hd, p_=p_, hh=hh, v_=v_, g=g, kb=kb: e.matmul(po[:, (hd % 3) * 129:(hd % 3) * 129 + 129], lhsT=p_[:, hh, :], rhs=v_[:, g, :], start=(kb == 0 and hd % 3 == 0), stop=(kb == kb_end - 1 and (hd % 3 == 2 or hd == 15))),
                         reads=[p_, v_], writes=[po])
    for hd in range(16):
        po = psO[hd // 3]
        P.op('dve', lambda e, po=po, hd=hd: e.reciprocal(out=rc[:, hd:hd + 1], in_=po[:, (hd % 3) * 129 + 128:(hd % 3) * 129 + 129]), reads=[po, rc], writes=[rc])
        P.op('act', lambda e, po=po, hd=hd: e.activation(out=oatt[:, hd * 128:(hd + 1) * 128], in_=po[:, (hd % 3) * 129:(hd % 3) * 129 + 128], func=AF.Copy, scale=rc[:, hd:hd + 1]), reads=[po, rc, oatt], writes=[oatt])
    P.emit(); K.unscope(); scp.close()


def emit_merge_ffn(K, W, cb, epsc, j, xo, ogsrc, oatt, sig, gpost, g2post, fcw_s, fcb_s,
                   wbg_b, wba_b, wout_b, wup_b, wdn_b, out_o, dbg_h1):
    P = K.P
    r0 = j * 128
    if ogsrc[0] == 'own':
        ogb = K.sb([128, 4096], BF16, "m_og")
    ogT = K.sb([128, 32, 128], BF16, "m_ogT"); oaT = K.sb([128, 16, 128], BF16, "m_oaT")
    mg = K.sb([128, D], F32, "m_mg"); t1 = K.sb([128, 512], F32, "m_t1"); mgb = K.sb([128, D], BF16, "m_mgb"); mgT = K.sb([128, 16, 128], BF16, "m_mgT")
    ysb = K.sb([128, D], F32, "m_y"); gp = K.sb([128, D], F32, "m_gp")
    ssq = K.sb([128, 8], F32, "m_ssq"); junk = K.sb([128, 512], BF16, "m_junk")
    xs = K.sb([128, D], BF16, "m_xs"); ss = K.sb([128, 2], F32, "m_ss"); u2T = K.sb([128, 16, 128], BF16, "m_u2T")
    actT = K.sb([128, 48, 128], BF16, "m_actT")
    cv = [K.sb([128, 4, 126], F32, f"m_cv{i}") for i in range(2)]
    sg = [K.sb([128, 2, 126], F32, f"m_sg{i}") for i in range(2)]
    if ogsrc[0] == 'own':
        ogown = ogsrc[1]
        P.op('sp', lambda e: e.dma_start(out=ogb[:], in_=ogown[r0:r0 + 128, :]), writes=[ogb], dma=True)
    P.op('sp', lambda e: e.dma_start(out=gp[:], in_=gpost[:]), writes=[gp], dma=True)
    if ogsrc[0] == 'own':
        emit_T(P, W, cb, lambda i: ogb[:, i * 128:(i + 1) * 128], ogb, ogT, 32)
    else:
        _, og_d, sel_s, Lp_ = ogsrc
        win0 = A0 + BSTR * (NCORE * j)
        ogw = [K.sb([128, 8, 1024], BF16, f"m_ogw{i}") for i in range(2)]
        psg = W.ps_mm
        for fq in range(4):
            t_ = ogw[fq % 2]
            valid = []
            P.op('pool', lambda e, t_=t_: e.memset(t_[:], 0.0), writes=[t_])
            for kc in range(8):
                rlo = win0 + kc * 128
                nv = max(0, min(128, Lp_ - rlo))
                if nv == 0: continue
                valid.append(kc)
                P.op('sp', lambda e, t_=t_, kc=kc, rlo=rlo, nv=nv, fq=fq: e.dma_start(out=t_[0:nv, kc, :], in_=og_d[rlo:rlo + nv, fq * 1024:(fq + 1) * 1024]),
                     reads=[og_d, t_], writes=[t_], dma=True)
            for half in range(2):
                W.nm += 1; pm = psg[W.nm % 3]
                for q in range(4):
                    fc = half * 4 + q
                    for kc in valid:
                        P.op('pe', lambda e, pm=pm, q=q, fc=fc, kc=kc, t_=t_: e.matmul(pm[:, q * 128:(q + 1) * 128], lhsT=t_[:, kc, fc * 128:(fc + 1) * 128], rhs=sel_s[:, kc, :],
                                                                                  start=(kc == valid[0]), stop=(kc == valid[-1])), reads=[t_, sel_s], writes=[pm])
                i0 = fq * 8 + half * 4
                if valid:
                    P.op('act', lambda e, pm=pm, i0=i0: e.activation(out=ogT[:, i0:i0 + 4, :], in_=pm[:, 0:512], func=AF.Copy), reads=[pm], writes=[ogT])
                else:
                    P.op('pool', lambda e, i0=i0: e.memset(ogT[:, i0:i0 + 4, :], 0.0), writes=[ogT])
    emit_T(P, W, cb, lambda i: oatt[:, i * 128:(i + 1) * 128], oatt, oaT, 16)
    for ct in range(4):
        c0 = ct * 512
        pm = W.linear(ogT, wbg_b, 32, c0, 512)
        P.op('dve', lambda e, pm=pm, c0=c0: e.tensor_tensor(out=mg[:, c0:c0 + 512], in0=pm[:, 0:512], in1=sig[:, 0, c0:c0 + 512], op=ALU.mult), reads=[pm, sig, mg], writes=[mg])
        pm = W.linear(oaT, wba_b, 16, c0, 512)
        P.op('dve', lambda e, pm=pm, c0=c0: e.tensor_tensor(out=t1[:], in0=pm[:, 0:512], in1=sig[:, 1, c0:c0 + 512], op=ALU.mult), reads=[pm, sig], writes=[t1])
        P.op('pool', lambda e, c0=c0: e.tensor_tensor(out=mgb[:, c0:c0 + 512], in0=mg[:, c0:c0 + 512], in1=t1[:], op=ALU.add), reads=[mg, t1, mgb], writes=[mgb])
    emit_T(P, W, cb, lambda i: mgb[:, i * 128:(i + 1) * 128], mgb, mgT, 16)

    def post_norm_residual(src_T, wd, KC, gain_tl, res_in, res_out):
        for ct in range(4):
            c0 = ct * 512
            pm = W.linear(src_T, wd, KC, c0, 512)
            P.op('act', lambda e, pm=pm, c0=c0: e.activation(out=ysb[:, c0:c0 + 512], in_=pm[:, 0:512], func=AF.Copy), reads=[pm, ysb], writes=[ysb])
            P.op('act', lambda e, pm=pm, ct=ct: e.activation(out=junk[:], in_=pm[:, 0:512], func=AF.Square, accum_out=ssq[:, ct:ct + 1]), reads=[pm, junk, ssq], writes=[junk, ssq])
        P.op('dve', lambda e: e.tensor_reduce(out=ssq[:, 4:5], in_=ssq[:, 0:4], axis=mybir.AxisListType.X, op=ALU.add), reads=[ssq], writes=[ssq])
        P.op('act', lambda e: e.activation(out=ssq[:, 5:6], in_=ssq[:, 4:5], func=AF.Sqrt, scale=1.0 / D, bias=epsc[:, 0:1]), reads=[ssq, epsc], writes=[ssq])
        P.op('dve', lambda e: e.reciprocal(out=ssq[:, 5:6], in_=ssq[:, 5:6]), reads=[ssq], writes=[ssq])
        P.op('dve', lambda e: e.scalar_tensor_tensor(out=ysb[:], in0=ysb[:], scalar=ssq[:, 5:6], in1=gain_tl[:], op0=ALU.mult, op1=ALU.mult), reads=[ysb, ssq, gain_tl], writes=[ysb])
        P.op('pool', lambda e: e.tensor_tensor(out=res_out[:], in0=res_in[:], in1=ysb[:], op=ALU.add), reads=[res_in, ysb], writes=[res_out])

    post_norm_residual(mgT, wout_b, 16, gp, xo, xo)
    if dbg_h1 is not None:
        P.op('sp', lambda e: e.dma_start(out=dbg_h1[r0:r0 + 128, :], in_=xo[:]), reads=[xo], writes=[dbg_h1], dma=True)
    P.op('sp', lambda e: e.dma_start(out=gp[:], in_=g2post[:]), reads=[gp], writes=[gp], dma=True)
    emit_norm_T(P, W, cb, epsc, xo, xs, ss, u2T)
    P.op('pool', lambda e: e.memset(actT[:], 0.0), writes=[actT])
    psu = W.ps_mm
    nu = 0
    for g in range(24):
        wt = W.wt[W.nw % 3]; W.nw += 1
        P.op('sp', lambda e, wt=wt, g=g: e.dma_start(out=wt[:], in_=wup_b[:, g, :, :]), reads=[wup_b], writes=[wt], dma=True)
        pm = psu[nu % 3]; c_v = cv[nu % 2]; s_g = sg[nu % 2]; nu += 1
        for cc in range(4):
            for kc in range(16):
                P.op('pe', lambda e, pm=pm, wt=wt, cc=cc, kc=kc: e.matmul(pm[:, cc * 128:(cc + 1) * 128], lhsT=wt[:, kc, cc * 128:(cc + 1) * 128], rhs=u2T[:, kc, :], start=(kc == 0), stop=(kc == 15)),
                     reads=[wt, u2T], writes=[pm])
        for cc in range(4):
            ch = (2 * g + cc) if cc < 2 else (48 + 2 * g + cc - 2)
            P.op('dve', lambda e, pm=pm, c_v=c_v, cc=cc, ch=ch: e.tensor_scalar(out=c_v[:, cc, :], in0=pm[:, cc * 128:cc * 128 + 126], scalar1=fcw_s[:, ch, 0:1], scalar2=fcb_s[:, ch:ch + 1], op0=ALU.mult, op1=ALU.add),
                 reads=[pm, fcw_s, fcb_s, c_v], writes=[c_v])
            for tp in (1, 2):
                P.op('dve', lambda e, pm=pm, c_v=c_v, cc=cc, ch=ch, tp=tp: e.scalar_tensor_tensor(out=c_v[:, cc, :], in0=pm[:, cc * 128 + tp:cc * 128 + tp + 126], scalar=fcw_s[:, ch, tp:tp + 1], in1=c_v[:, cc, :], op0=ALU.mult, op1=ALU.add),
                     reads=[pm, fcw_s, c_v], writes=[c_v])
        P.op('act', lambda e, c_v=c_v, s_g=s_g: e.activation(out=s_g[:], in_=c_v[:, 0:2, :], func=AF.Silu), reads=[c_v], writes=[s_g])
        P.op('pool', lambda e, c_v=c_v, s_g=s_g, g=g: e.tensor_tensor(out=actT[:, 2 * g:2 * g + 2, 2:128], in0=s_g[:], in1=c_v[:, 2:4, :], op=ALU.mult), reads=[c_v, s_g, actT], writes=[actT])
    post_norm_residual(actT, wdn_b, 48, gp, xo, ysb)
    P.op('sp', lambda e: e.dma_start(out=out_o[j, :, :], in_=ysb[2:128, :]), reads=[ysb], writes=[out_o], dma=True)


def own_rows(core, NS):
    rows = np.zeros(NS * 128, np.int64)
    for j in range(NS):
        s_ = NCORE * j + core
        rows[j * 128:(j + 1) * 128] = A0 + BSTR * s_ + np.arange(128)
    return rows


def prep2_common(inp):
    f = np.float32
    o = np.cumsum([0, 2048, 2048, 4096, 4096, 32, 32, 2048, 256, 256, 2048, 128, 16, 2048, 2048])
    w_in = inp['w_in'][0]
    cols = np.concatenate([np.arange(o[6], o[7]), np.arange(o[9], o[10]), np.arange(o[12], o[13]), np.arange(o[13], o[14]), np.arange(o[11], o[12])])
    cm = {
        "w_c": np.ascontiguousarray(w_in[:, cols]),
        "wbg": np.ascontiguousarray(inp['w_branch_gdn'][0]), "wba": np.ascontiguousarray(inp['w_branch_att'][0]),
        "wout": np.ascontiguousarray(inp['w_out'][0]), "wup": np.ascontiguousarray(inp['w_up'][0]), "wdn": np.ascontiguousarray(inp['w_down'][0]),
        "gpre": np.ascontiguousarray(inp['mix_pre_g'][0].reshape(16, 128).T), "g2pre": np.ascontiguousarray(inp['ffn_pre_g'][0].reshape(16, 128).T),
        "gpost": np.ascontiguousarray(np.broadcast_to(inp['mix_post_g'][0][None, :], (128, D))).astype(f),
        "g2post": np.ascontiguousarray(np.broadcast_to(inp['ffn_post_g'][0][None, :], (128, D))).astype(f),
        "fcw": np.ascontiguousarray(inp['ffn_conv_w'][0].T.reshape(96, 128, 3).transpose(1, 0, 2)),
        "fcb": np.ascontiguousarray(inp['ffn_conv_b'][0].reshape(96, 128).T),
        "cst": make_consts(),
        "iota": np.ascontiguousarray(np.broadcast_to(np.arange(NIOTA, dtype=f)[None, :], (128, NIOTA))),
    }
    return cm


def prep2(cm, hfull, og_all, KT, Vt, ikT, Lp, NS, core):
    f = np.float32
    nblk = Lp // 128
    rows = own_rows(core, NS)
    ok = rows < Lp
    rc = np.minimum(rows, Lp - 1)
    hown = np.where(ok[:, None], hfull[rc], 0).astype(f)
    ogown = None
    if og_all is not None:
        ogown = og_all[rc].copy(); ogown[~ok] = 0
    pos = np.maximum(rows - PADF, 0)
    qrel = np.zeros((128, NS), f)
    for j in range(NS):
        kd, kb_end = slot_geom(j, nblk)
        qrel[:, j] = rows[j * 128:(j + 1) * 128] - kd * 128
    m = dict(cm)
    m.update({"hown": hown, "ogown": ogown, "KT": KT, "Vt": Vt, "ikT": ikT, "ropeo": rope_table(pos), "qrel": qrel})
    return m


def kernel(**inputs):
    inp = {k: np.asarray(v) for k, v in inputs.items()}
    return kernel_fused(inp)


def kernel_unfused(**inputs):
    inp = {k: np.asarray(v) for k, v in inputs.items()}
    x = inp['x']
    SEQ = x.shape[1]
    Lp = PADF + NMETA + SEQ
    assert Lp % 128 == 0
    L = NMETA + SEQ
    topk = min(256, L // 4)
    nb, NS = blocks_for(SEQ)
    cores = list(range(NCORE))
    nc1 = build_prog1(Lp)
    ims = [prep1(inp, Lp, c) for c in cores]
    hfull = ims[0]["hfull"]
    for m in ims[1:]:
        m["hfull"] = hfull
    r1 = run_bass_kernel_spmd(nc1, ims, core_ids=cores).results
    og_all = np.concatenate([np.asarray(r1[c]["og"]) for c in cores], axis=1)
    KT = np.asarray(r1[0]["KT"]); Vt = np.asarray(r1[0]["Vt"]); ikT = np.asarray(r1[0]["ikT"])
    del ims, r1
    nc2 = build_prog2(Lp, NS, topk)
    cm = prep2_common(inp)
    ims2 = [prep2(cm, hfull, og_all, KT, Vt, ikT, Lp, NS, c) for c in cores]
    r2 = run_bass_kernel_spmd(nc2, ims2, core_ids=cores).results
    out = np.zeros((1, SEQ, D), np.float32)
    for c in cores:
        o = np.asarray(r2[c]["out"])
        for j in range(NS):
            s_ = NCORE * j + c
            t_lo = BSTR * s_; t_hi = min(SEQ, t_lo + BSTR)
            if t_hi > t_lo:
                out[0, t_lo:t_hi] = o[j, :t_hi - t_lo]
    return out


def make_sel(core):
    import ml_dtypes
    sel = np.zeros((128, 8, 128), np.float32)
    for r in range(128):
        w = BSTR * core + r
        sel[w % 128, w // 128, r] = 1.0
    return sel.astype(ml_dtypes.bfloat16)


def kernel_fused(inp):
    x = inp['x']; SEQ = x.shape[1]
    Lp = PADF + NMETA + SEQ
    L = NMETA + SEQ
    topk = min(256, L // 4)
    nb, NS = blocks_for(SEQ)
    cores = list(range(NCORE))
    p1 = [prep1(inp, Lp, c) for c in cores]
    hfull = p1[0]["hfull"]
    w_a = np.concatenate([p["w_a"] for p in p1], 0); cw = np.concatenate([p["cw"] for p in p1], 0); hp = np.concatenate([p["hp"] for p in p1], 0)
    cm = prep2_common(inp)
    cm.update({"hfull": hfull, "w_a": w_a, "cw": cw, "hp": hp, "ng": p1[0]["ng"], "rope": p1[0]["rope"]})
    del p1
    ims = []
    for c in cores:
        m = prep2(cm, hfull, None, None, None, None, Lp, NS, c)
        for k in ("ogown", "KT", "Vt", "ikT"): m.pop(k)
        m["sel"] = make_sel(c)
        ims.append(m)
    nc = build_prog2(Lp, NS, topk, fused=True)
    r2 = run_bass_kernel_spmd(nc, ims, core_ids=cores).results
    out = np.zeros((1, SEQ, D), np.float32)
    for c in cores:
        o = np.asarray(r2[c]["out"])
        for j in range(NS):
            s_ = NCORE * j + c
            t_lo = BSTR * s_; t_hi = min(SEQ, t_lo + BSTR)
            if t_hi > t_lo:
                out[0, t_lo:t_hi] = o[j, :t_hi - t_lo]
    return out
```

```python
import numpy as np
import concourse.bass as bass
import concourse.mybir as mybir
from concourse.bass_utils import run_bass_kernel_spmd
from contextlib import ExitStack

F32 = mybir.dt.float32; BF16 = mybir.dt.bfloat16; I32 = mybir.dt.int32
AF = mybir.ActivationFunctionType; ALU = mybir.AluOpType

D = 2048
NMETA = 16
PADF = 112
EPS = 1e-6
NCORE = 8
HPC = 4
NFF = 6144
ROPE_THETA = 500000.0


class Buf:
    __slots__ = ('w', 'r', 'multi')
    def __init__(self, multi=False):
        self.w = {}; self.r = {}; self.multi = multi


class Tl:
    def __init__(self, t, multi=False):
        self.t = t; self.b = Buf(multi)
    def __getitem__(self, k):
        return self.t[k]


class SMView(Tl):
    def __init__(self, T, c):
        self.t = T.t; self.b = T.b; self.c = c
    def __getitem__(self, k):
        return self.t[(k[0], self.c) + tuple(k[1:])]


class Prog:
    ENG = ('pe', 'act', 'dve', 'pool', 'sp')
    def __init__(self, nc, es, n_dma=12):
        self.nc = nc; self.es = es
        self.ops = {e: [] for e in self.ENG}
        self.cnt = {e: 0 for e in self.ENG}
        self.sem = {e: es.enter_context(nc.semaphore('s_' + e)) for e in ('pe', 'act', 'dve', 'pool')}
        self.seen = {e: {} for e in self.ENG}
        self.dsem = {q: [[es.enter_context(nc.semaphore(f'd_{q}{i}')), 0] for i in range(n_dma)] for q in ('sp', 'pool')}
        self.drr = {q: 0 for q in ('sp', 'pool')}
        self.nops = 0

    def op(self, eng, fn, reads=(), writes=(), dma=False):
        deps = {}
        def add(ev):
            s, v = ev
            k = id(s)
            if k not in deps or deps[k][1] < v: deps[k] = (s, v)
        for t in reads:
            b = t.b if isinstance(t, Tl) else t
            for ev in b.w.values(): add(ev)
        for t in writes:
            b = t.b if isinstance(t, Tl) else t
            if not b.multi:
                for ev in b.w.values(): add(ev)
                for ev in b.r.values(): add(ev)
        if dma:
            slots = self.dsem[eng]
            slot = slots[self.drr[eng] % len(slots)]; self.drr[eng] += 1
            if slot[1] > 0: add((slot[0], slot[1]))
            slot[1] += 16
            ev = (slot[0], slot[1]); inc = 16
        else:
            self.cnt[eng] += 1
            ev = (self.sem[eng], self.cnt[eng]); inc = 1
        waits = []
        seen = self.seen[eng]
        own = id(self.sem['pe']) if eng == 'pe' else None
        for k, (s, v) in deps.items():
            if k == own: continue
            if seen.get(k, 0) >= v: continue
            seen[k] = v; waits.append((s, v))
        for t in writes:
            b = t.b if isinstance(t, Tl) else t
            if b.multi:
                b.w[id(ev[0])] = ev
            else:
                b.w = {id(ev[0]): ev}; b.r = {}
        for t in reads:
            b = t.b if isinstance(t, Tl) else t
            if not b.multi:
                b.r[id(ev[0])] = ev
        self.ops[eng].append((waits, fn, ev, inc))
        self.nops += 1
        return ev

    def emit(self):
        nc = self.nc
        fin = []
        for e in ('pe', 'act', 'dve', 'pool'):
            if self.cnt[e]: fin.append((self.sem[e], self.cnt[e]))
        for q in self.dsem:
            for s, v in self.dsem[q]:
                if v: fin.append((s, v))
        bar = getattr(self, 'barrier', [])
        def run(name, e):
            for s, v in bar: e.wait_ge(s, v)
            for waits, fn, ev, inc in self.ops[name]:
                for (s, v) in waits: e.wait_ge(s, v)
                ins = fn(e)
                ins.then_inc(ev[0], inc)
            if name == 'sp':
                for s, v in fin: e.wait_ge(s, v)
            self.ops[name] = []
        self.barrier = fin
        with nc.Block() as block:
            @block.tensor
            def _(e): run('pe', e)
            @block.scalar
            def _(e): run('act', e)
            @block.vector
            def _(e): run('dve', e)
            @block.gpsimd
            def _(e): run('pool', e)
            @block.sync
            def _(e): run('sp', e)


class Ctx:
    def __init__(self, nc, es):
        self.nc = nc; self.es = es; self.P = Prog(nc, es)
        self._n = 0
    def scope(self):
        st = ExitStack()
        if not hasattr(self, '_stk'): self._stk = []
        self._stk.append(self.es); self.es = st
        return st
    def unscope(self):
        self.es = self._stk.pop()
    def sb(self, shape, dt, name=None):
        self._n += 1
        return Tl(self.es.enter_context(self.nc.sbuf_tensor(f"{name or 'sb'}_{self._n}", list(shape), dt)))
    def ps(self, shape, dt=F32, name=None):
        self._n += 1
        return Tl(self.es.enter_context(self.nc.psum_tensor(f"{name or 'ps'}_{self._n}", list(shape), dt)))
    def din(self, name, shape, dt=F32):
        return Tl(self.nc.dram_tensor(name, list(shape), dt, kind="ExternalInput").ap(), multi=True)
    def dout(self, name, shape, dt=F32):
        return Tl(self.nc.dram_tensor(name, list(shape), dt, kind="ExternalOutput").ap(), multi=True)
    def dtmp(self, name, shape, dt=BF16):
        return Tl(self.nc.dram_tensor(name, list(shape), dt, kind="Internal").ap(), multi=True)


CI_ID, CI_TRIU, CI_NEGM, CI_OFFD, CI_ONES = 0, 1, 2, 3, 4
def make_consts():
    c = np.zeros((128, 5, 128), np.float32)
    j = np.arange(128)[:, None]; i = np.arange(128)[None, :]
    c[:, CI_ID] = (i == j)
    c[:, CI_TRIU] = (j <= i)
    c[:, CI_NEGM] = np.where(i >= j, 0.0, -1e9)
    c[:, CI_OFFD] = (i != j)
    c[:, CI_ONES] = 1.0
    return c


def rope_table(pos):
    half = 16
    inv = ROPE_THETA ** (-np.arange(half, dtype=np.float32) / half)
    ang = pos.astype(np.float32)[:, None] * inv[None, :].astype(np.float32)
    return np.concatenate([np.cos(ang), np.sin(ang)], axis=1).astype(np.float32)


NA_FM = 1024
NA_TM = 512 + 8 + 256 + 256 + 128
NA = NA_FM + NA_TM


def emit_gdn_all(K, Lp, NG, hfull, w_a, gpre, cw, hp, ng, cst, rope, og_o, KT_o, V_o, ikT_o, scr, dbgt=None):
    P = K.P; nblk = Lp // 128; dbg = dbgt is not None
    if dbg: dbg_gb, dbg_q, dbg_k, dbg_v = dbgt
    if True:
        cf = K.sb([128, 5, 128], F32, "cf")
        P.op('sp', lambda e: e.dma_start(out=cf[:], in_=cst[:]), writes=[cf], dma=True)
        epsc = K.sb([128, 1], F32, "epsc")
        P.op('dve', lambda e: e.memset(epsc[:], EPS), writes=[epsc])
        cb = K.sb([128, 5, 128], BF16, "cb")
        P.op('dve', lambda e: e.tensor_copy(out=cb[:], in_=cf[:]), reads=[cf], writes=[cb])
        gpre_s = K.sb([128, 16], F32); cw_s = K.sb([128, 8, 4], F32); hp_s = K.sb([128, 3, HPC], F32); ng_s = K.sb([128, 128], F32)
        for dst, src in ((gpre_s, gpre), (ng_s, ng)):
            P.op('sp', lambda e, dst=dst, src=src: e.dma_start(out=dst[:], in_=src[:]), writes=[dst], dma=True)
        nea = K.sb([128, HPC], F32)
        ut_d = K.dtmp("ut_d", [128, 16, Lp]) if NG > 1 else None
        NPAR = min(2, NG)
        gb_alls = [K.sb([128, nblk, 8], F32, f"gb_all{i}") for i in range(NPAR)]
        pend = []

        for g in range(NG):
            gb_all = gb_alls[g % NPAR]
            qT_d, kT_d, k_d, v_d, sz_d = scr[g % NPAR]
            P.op('sp', lambda e, g=g: e.dma_start(out=cw_s[:], in_=cw[g]), writes=[cw_s], dma=True)
            P.op('sp', lambda e, g=g: e.dma_start(out=hp_s[:], in_=hp[g]), writes=[hp_s], dma=True)
            P.op('act', lambda e: e.activation(out=nea[:], in_=hp_s[:, 0, :], func=AF.Exp), reads=[hp_s], writes=[nea])
            P.op('dve', lambda e: e.tensor_scalar(out=nea[:], in0=nea[:], scalar1=-1.0, scalar2=None, op0=ALU.mult), reads=[nea], writes=[nea])
            scA = K.scope()
            wA = K.sb([128, 16, NA], BF16, "wA")
            wst = [K.sb([128, NA], F32, f"wst{i}") for i in range(1)]
            w_a_v = w_a.t[g].rearrange("(kc p) n -> p kc n", p=128)
            for kc in range(16):
                st = wst[0]
                P.op('sp', lambda e, st=st, kc=kc: e.dma_start(out=st[:], in_=w_a_v[:, kc, :]), writes=[st], dma=True)
                P.op('act', lambda e, st=st, kc=kc: e.activation(out=wA[:, kc, :], in_=st[:], func=AF.Copy, scale=gpre_s[:, kc:kc + 1]),
                     reads=[st, gpre_s], writes=[wA])

            TB = 512
            xf = [K.sb([128, D], F32, f"xf{i}") for i in range(2)]
            xs = [K.sb([128, D], BF16, f"xs{i}") for i in range(2)]
            ss = [K.sb([128, 2], F32, f"ss{i}") for i in range(2)]
            uTs = [K.sb([128, 16, TB], BF16, f"uT{i}") for i in range(2)]
            pre = K.sb([128, 8, 3 + TB], F32, "pre")
            pre_b = [Buf() for _ in range(8)]
            P.op('dve', lambda e: e.memset(pre[:], 0.0), writes=pre_b)
            cv = [K.sb([128, TB], F32, f"cv{i}") for i in range(2)]
            sl = [K.sb([128, TB], F32, f"sl{i}") for i in range(2)]
            sq = [K.sb([128, TB], BF16, f"sq{i}") for i in range(2)]
            rrs = [K.sb([128, TB], F32, f"rr{i}") for i in range(2)]
            fmTs = [K.sb([128, 8, TB], BF16, f"fmT{i}") for i in range(2)]
            tm_kv = [K.sb([128, 6, 128], BF16, f"tmkv{i}") for i in range(2)]
            ztm = [K.sb([128, 512], BF16, f"ztm{i}") for i in range(2)]
            bars = [K.sb([128, 4, 8], F32, f"bar{i}") for i in range(2)]
            vst = [K.sb([128, 256], BF16, f"vst{i}") for i in range(2)]
            kif = [K.sb([128, 3, 128], F32, f"kif{i}") for i in range(2)]
            kib = [K.sb([128, 3, 128], BF16, f"kib{i}") for i in range(2)]
            rt = [K.sb([128, 32], F32, f"rt{i}") for i in range(2)]
            rtmp = [K.sb([128, 4, 3, 16], F32, f"rtmp{i}") for i in range(2)]
            kiT = [K.sb([128, 3, 128], BF16, f"kiT{i}") for i in range(2)]
            ps_tr = [K.ps([128, 4, 128], BF16, f"pstr{i}") for i in range(2)]
            ps_mm = [K.ps([128, 512], F32, f"psmm{i}") for i in range(3)]
            ps_n = K.ps([128, 512], F32, "psn")
            nmm = [0]
            def next_mm():
                nmm[0] += 1
                return ps_mm[nmm[0] % 3]
            ntr = [0]
            def next_tr():
                ntr[0] += 1
                return ps_tr[ntr[0] % 2]

            nsb = (Lp + TB - 1) // TB
            blk = 0
            for sbi in range(nsb):
                t0 = sbi * TB
                n_sub = min(4, (Lp - t0) // 128)
                TBn = n_sub * 128
                uT = uTs[sbi % 2]; fmT = fmTs[sbi % 2]; bar = bars[sbi % 2]
                if g > 0:
                    if sbi == 0:
                        P.op('sp', lambda e, uT=uT, t0=t0, TBn=TBn: e.dma_start(out=uT[:, :, 0:TBn], in_=ut_d[:, :, t0:t0 + TBn]), reads=[ut_d], writes=[uT], dma=True)
                    if sbi + 1 < nsb:
                        t1_ = (sbi + 1) * TB; TB1 = min(4, (Lp - t1_) // 128) * 128; uT1 = uTs[(sbi + 1) % 2]
                        P.op('sp', lambda e, uT1=uT1, t1_=t1_, TB1=TB1: e.dma_start(out=uT1[:, :, 0:TB1], in_=ut_d[:, :, t1_:t1_ + TB1]), reads=[ut_d], writes=[uT1], dma=True)
                for j in range(n_sub if g == 0 else 0):
                    b = sbi * 4 + j
                    x_f = xf[b % 2]; x_s = xs[b % 2]; s_s = ss[b % 2]
                    P.op('sp', lambda e, x_f=x_f, b=b: e.dma_start(out=x_f[:], in_=hfull[b * 128:(b + 1) * 128, :]), writes=[x_f], dma=True)
                    P.op('act', lambda e, x_f=x_f, s_s=s_s, x_s=x_s: e.activation(out=x_s[:], in_=x_f[:], func=AF.Square, accum_out=s_s[:, 0:1]),
                         reads=[x_f], writes=[x_s, s_s])
                    P.op('act', lambda e, s_s=s_s: e.activation(out=s_s[:, 1:2], in_=s_s[:, 0:1], func=AF.Sqrt, scale=1.0 / D, bias=epsc[:, 0:1]),
                         reads=[s_s, epsc], writes=[s_s])
                    P.op('dve', lambda e, s_s=s_s: e.reciprocal(out=s_s[:, 1:2], in_=s_s[:, 1:2]), reads=[s_s], writes=[s_s])
                    P.op('dve', lambda e, x_f=x_f, x_s=x_s, s_s=s_s: e.tensor_scalar(out=x_s[:], in0=x_f[:], scalar1=s_s[:, 1:2], scalar2=None, op0=ALU.mult),
                         reads=[x_f, s_s], writes=[x_s])
                    for g4 in range(4):
                        pt = next_tr()
                        for q in range(4):
                            kc = g4 * 4 + q
                            P.op('pe', lambda e, pt=pt, q=q, kc=kc, x_s=x_s: e.transpose(out=pt[:, q, :], in_=x_s[:, kc * 128:(kc + 1) * 128], identity=cb[:, CI_ID, :]),
                                 reads=[x_s, cb], writes=[pt])
                        P.op('act' if g4 % 2 else 'dve',
                             (lambda e, uT=uT, pt=pt, g4=g4, j=j: e.activation(out=uT[:, g4 * 4:(g4 + 1) * 4, j * 128:(j + 1) * 128], in_=pt[:], func=AF.Copy)) if g4 % 2 else
                             (lambda e, uT=uT, pt=pt, g4=g4, j=j: e.tensor_copy(out=uT[:, g4 * 4:(g4 + 1) * 4, j * 128:(j + 1) * 128], in_=pt[:])),
                             reads=[pt], writes=[uT])
                if g == 0 and NG > 1:
                    P.op('sp', lambda e, uT=uT, t0=t0, TBn=TBn: e.dma_start(out=ut_d[:, :, t0:t0 + TBn], in_=uT[:, :, 0:TBn]), reads=[uT], writes=[ut_d], dma=True)
                pending = []
                for ch in range(8):
                    rr = rrs[ch % 2]; pre_c = pre_b[ch]
                    pm = next_mm()
                    for kc in range(16):
                        P.op('pe', lambda e, uT=uT, pm=pm, kc=kc, ch=ch, TBn=TBn: e.matmul(pm[:, 0:TBn], lhsT=wA[:, kc, ch * 128:(ch + 1) * 128], rhs=uT[:, kc, 0:TBn],
                                                                                  start=(kc == 0), stop=(kc == 15)),
                             reads=[wA, uT], writes=[pm])
                    P.op('act', lambda e, pm=pm, ch=ch, TBn=TBn: e.activation(out=pre[:, ch, 3:3 + TBn], in_=pm[:, 0:TBn], func=AF.Copy), reads=[pm], writes=[pre_c])
                    while pending: pending.pop(0)()
                    c_v = cv[ch % 2]; s_l = sl[ch % 2]; s_q = sq[ch % 2]
                    P.op('dve', lambda e, c_v=c_v, ch=ch, TBn=TBn: e.tensor_scalar(out=c_v[:, 0:TBn], in0=pre[:, ch, 0:TBn], scalar1=cw_s[:, ch, 0:1], scalar2=None, op0=ALU.mult),
                         reads=[pre_c, cw_s], writes=[c_v])
                    for tp in range(1, 4):
                        P.op('dve', lambda e, c_v=c_v, ch=ch, tp=tp, TBn=TBn: e.scalar_tensor_tensor(out=c_v[:, 0:TBn], in0=pre[:, ch, tp:tp + TBn], scalar=cw_s[:, ch, tp:tp + 1],
                                                                                                  in1=c_v[:, 0:TBn], op0=ALU.mult, op1=ALU.add),
                             reads=[pre_c, cw_s, c_v], writes=[c_v])
                    P.op('pool', lambda e, ch=ch, TBn=TBn: e.tensor_copy(out=pre[:, ch, 0:3], in_=pre[:, ch, TBn:TBn + 3]), reads=[pre_c], writes=[pre_c])
                    if ch >= 4:
                        P.op('act', lambda e, fmT=fmT, c_v=c_v, ch=ch, TBn=TBn: e.activation(out=fmT[:, ch, 0:TBn], in_=c_v[:, 0:TBn], func=AF.Silu), reads=[c_v], writes=[fmT])
                    else:
                        P.op('act', lambda e, c_v=c_v, s_l=s_l, TBn=TBn: e.activation(out=s_l[:, 0:TBn], in_=c_v[:, 0:TBn], func=AF.Silu), reads=[c_v], writes=[s_l])
                        def l2tail(ch=ch, s_l=s_l, s_q=s_q, TBn=TBn, rr=rr, fmT=fmT):
                            P.op('pool', lambda e, s_l=s_l, s_q=s_q, TBn=TBn: e.tensor_tensor(out=s_q[:, 0:TBn], in0=s_l[:, 0:TBn], in1=s_l[:, 0:TBn], op=ALU.mult),
                                 reads=[s_l], writes=[s_q])
                            P.op('pe', lambda e, s_q=s_q, TBn=TBn: e.matmul(ps_n[:, 0:TBn], lhsT=cb[:, CI_ONES, :], rhs=s_q[:, 0:TBn], start=True, stop=True),
                                 reads=[s_q, cb], writes=[ps_n])
                            P.op('act', lambda e, rr=rr, TBn=TBn: e.activation(out=rr[:, 0:TBn], in_=ps_n[:, 0:TBn], func=AF.Sqrt, bias=epsc[:, 0:1]), reads=[ps_n, epsc], writes=[rr])
                            P.op('dve', lambda e, rr=rr, TBn=TBn: e.reciprocal(out=rr[:, 0:TBn], in_=rr[:, 0:TBn]), reads=[rr], writes=[rr])
                            sc = (128 ** -0.5) if ch < 2 else 1.0
                            P.op('dve', lambda e, rr=rr, fmT=fmT, s_l=s_l, ch=ch, sc=sc, TBn=TBn: e.scalar_tensor_tensor(out=fmT[:, ch, 0:TBn], in0=s_l[:, 0:TBn], scalar=sc, in1=rr[:, 0:TBn],
                                                                                                      op0=ALU.mult, op1=ALU.mult),
                                 reads=[s_l, rr], writes=[fmT])
                        pending.append(l2tail)
                while pending: pending.pop(0)()
                for hq in range(2):
                    P.op('sp', lambda e, fmT=fmT, hq=hq, t0=t0, TBn=TBn: e.dma_start(out=qT_d[hq, :, t0:t0 + TBn], in_=fmT[:, hq, 0:TBn]), reads=[fmT], writes=[qT_d], dma=True)
                    P.op('sp', lambda e, fmT=fmT, hq=hq, t0=t0, TBn=TBn: e.dma_start(out=kT_d[hq, :, t0:t0 + TBn], in_=fmT[:, 2 + hq, 0:TBn]), reads=[fmT], writes=[kT_d], dma=True)
                if dbg:
                    for hq in range(2):
                        P.op('sp', lambda e, fmT=fmT, hq=hq, t0=t0, TBn=TBn: e.dma_start(out=dbg_q[hq, :, t0:t0 + TBn], in_=fmT[:, hq, 0:TBn]), reads=[fmT], writes=[dbg_q], dma=True)
                        P.op('sp', lambda e, fmT=fmT, hq=hq, t0=t0, TBn=TBn: e.dma_start(out=dbg_k[hq, :, t0:t0 + TBn], in_=fmT[:, 2 + hq, 0:TBn]), reads=[fmT], writes=[dbg_k], dma=True)
                for j in range(n_sub):
                    b = sbi * 4 + j
                    r0 = b * 128
                    pm = next_mm()
                    for kc in range(16):
                        P.op('pe', lambda e, uT=uT, pm=pm, kc=kc, j=j: e.matmul(pm[:, 0:512], lhsT=uT[:, kc, j * 128:(j + 1) * 128], rhs=wA[:, kc, NA_FM:NA_FM + 512],
                                                                       start=(kc == 0), stop=(kc == 15)), reads=[wA, uT], writes=[pm])
                    z_t = ztm[b % 2]
                    P.op('act', lambda e, pm=pm, z_t=z_t: e.activation(out=z_t[:], in_=pm[:, 0:512], func=AF.Silu), reads=[pm], writes=[z_t])
                    P.op('sp', lambda e, z_t=z_t, r0=r0: e.dma_start(out=sz_d[r0:r0 + 128, :], in_=z_t[:]), reads=[z_t], writes=[sz_d], dma=True)
                    pm = next_mm()
                    c0 = NA_FM + 512
                    nba = 264 if g == 0 else 8
                    for kc in range(16):
                        P.op('pe', lambda e, uT=uT, pm=pm, kc=kc, j=j, c0=c0, nba=nba: e.matmul(pm[:, 0:nba], lhsT=uT[:, kc, j * 128:(j + 1) * 128], rhs=wA[:, kc, c0:c0 + nba],
                                                                              start=(kc == 0), stop=(kc == 15)), reads=[wA, uT], writes=[pm])
                    v_s = vst[b % 2]
                    P.op('dve', lambda e, pm=pm, j=j, bar=bar: e.tensor_copy(out=bar[:, j, :], in_=pm[:, 0:8]), reads=[pm, bar], writes=[bar])
                    if g > 0: continue
                    P.op('act', lambda e, pm=pm, v_s=v_s: e.activation(out=v_s[:], in_=pm[:, 8:264], func=AF.Copy), reads=[pm], writes=[v_s])
                    if g == 0: P.op('sp', lambda e, v_s=v_s, r0=r0: e.dma_start(out=V_o[r0:r0 + 128, :], in_=v_s[:]), reads=[v_s], writes=[V_o], dma=True)
                    pm = next_mm()
                    c0 = NA_FM + 512 + 264
                    for kc in range(16):
                        P.op('pe', lambda e, uT=uT, pm=pm, kc=kc, j=j, c0=c0: e.matmul(pm[:, 0:384], lhsT=uT[:, kc, j * 128:(j + 1) * 128], rhs=wA[:, kc, c0:c0 + 384],
                                                                              start=(kc == 0), stop=(kc == 15)), reads=[wA, uT], writes=[pm])
                    k_f = kif[b % 2]; k_b = kib[b % 2]; r_t = rt[b % 2]; r_m = rtmp[b % 2]; k_T = kiT[b % 2]
                    P.op('sp', lambda e, r_t=r_t, r0=r0: e.dma_start(out=r_t[:], in_=rope[r0:r0 + 128, :]), writes=[r_t], dma=True)
                    P.op('act', lambda e, pm=pm, k_f=k_f: e.activation(out=k_f[:], in_=pm[:, 0:384], func=AF.Copy), reads=[pm], writes=[k_f])
                    P.op('act', lambda e, k_f=k_f, k_b=k_b: e.activation(out=k_b[:], in_=k_f[:], func=AF.Copy), reads=[k_f], writes=[k_b])
                    emit_rope(P, k_f, k_b, r_t, r_m, 3)
                    pt = next_tr()
                    for q in range(3):
                        P.op('pe', lambda e, pt=pt, q=q, k_b=k_b: e.transpose(out=pt[:, q, :], in_=k_b[:, q, :], identity=cb[:, CI_ID, :]), reads=[k_b, cb], writes=[pt])
                    P.op('act', lambda e, pt=pt, k_T=k_T: e.activation(out=k_T[:], in_=pt[:, 0:3, :], func=AF.Copy), reads=[pt], writes=[k_T])
                    for q in range(2 if g == 0 else 0):
                        P.op('sp', lambda e, k_T=k_T, q=q, r0=r0: e.dma_start(out=KT_o[q, :, r0:r0 + 128], in_=k_T[:, q, :]), reads=[k_T], writes=[KT_o], dma=True)
                    if g == 0: P.op('sp', lambda e, k_T=k_T, r0=r0: e.dma_start(out=ikT_o[:, r0:r0 + 128], in_=k_T[:, 2, :]), reads=[k_T], writes=[ikT_o], dma=True)
                b0 = sbi * 4
                P.op('act', lambda e, bar=bar, n_sub=n_sub: e.activation(out=bar[:, 0:n_sub, 0:4], in_=bar[:, 0:n_sub, 0:4], func=AF.Exp, scale=-1.0), reads=[bar], writes=[bar])
                P.op('dve', lambda e, bar=bar, n_sub=n_sub: e.tensor_tensor(out=bar[:, 0:n_sub, 4:8], in0=bar[:, 0:n_sub, 4:8], in1=hp_s[:, 1, :].unsqueeze(1).to_broadcast([128, n_sub, 4]), op=ALU.add),
                     reads=[bar, hp_s], writes=[bar])
                P.op('act', lambda e, bar=bar, n_sub=n_sub: e.activation(out=bar[:, 0:n_sub, 4:8], in_=bar[:, 0:n_sub, 4:8], func=AF.Exp), reads=[bar], writes=[bar])
                P.op('dve', lambda e, bar=bar, n_sub=n_sub: e.tensor_scalar(out=bar[:, 0:n_sub, :], in0=bar[:, 0:n_sub, :], scalar1=1.0, scalar2=None, op0=ALU.add), reads=[bar], writes=[bar])
                P.op('act', lambda e, bar=bar, n_sub=n_sub: e.activation(out=bar[:, 0:n_sub, 4:8], in_=bar[:, 0:n_sub, 4:8], func=AF.Ln), reads=[bar], writes=[bar])
                P.op('dve', lambda e, bar=bar, n_sub=n_sub, b0=b0: e.reciprocal(out=gb_all[:, b0:b0 + n_sub, 0:4], in_=bar[:, 0:n_sub, 0:4]), reads=[bar], writes=[gb_all])
                P.op('dve', lambda e, bar=bar, n_sub=n_sub, b0=b0: e.tensor_tensor(out=gb_all[:, b0:b0 + n_sub, 4:8], in0=bar[:, 0:n_sub, 4:8], in1=nea[:].unsqueeze(1).to_broadcast([128, n_sub, 4]), op=ALU.mult),
                     reads=[bar, nea, gb_all], writes=[gb_all])
                for j in range(n_sub):
                    b = sbi * 4 + j
                    r0 = b * 128
                    tk = tm_kv[b % 2]
                    pt = next_tr()
                    for q in range(4):
                        P.op('pe', lambda e, fmT=fmT, pt=pt, q=q, j=j: e.transpose(out=pt[:, q, :], in_=fmT[:, 4 + q, j * 128:(j + 1) * 128], identity=cb[:, CI_ID, :]),
                             reads=[fmT, cb], writes=[pt])
                    P.op('act', lambda e, pt=pt, tk=tk: e.activation(out=tk[:, 2:6, :], in_=pt[:], func=AF.Copy), reads=[pt], writes=[tk])
                    pt = next_tr()
                    for q in range(2):
                        P.op('pe', lambda e, fmT=fmT, pt=pt, q=q, j=j: e.transpose(out=pt[:, q, :], in_=fmT[:, 2 + q, j * 128:(j + 1) * 128], identity=cb[:, CI_ID, :]),
                             reads=[fmT, cb], writes=[pt])
                    P.op('dve', lambda e, pt=pt, tk=tk: e.tensor_copy(out=tk[:, 0:2, :], in_=pt[:, 0:2, :]), reads=[pt], writes=[tk])
                    P.op('sp', lambda e, tk=tk, r0=r0: e.dma_start(out=k_d[r0:r0 + 128, :], in_=tk[:, 0:2, :]), reads=[tk], writes=[k_d], dma=True)
                    P.op('sp', lambda e, tk=tk, r0=r0: e.dma_start(out=v_d[r0:r0 + 128, :], in_=tk[:, 2:6, :]), reads=[tk], writes=[v_d], dma=True)
                    if dbg:
                        P.op('sp', lambda e, tk=tk, r0=r0: e.dma_start(out=dbg_v[r0:r0 + 128, :], in_=tk[:, 2:6, :]), reads=[tk], writes=[dbg_v], dma=True)
            if dbg:
                P.op('sp', lambda e: e.dma_start(out=dbg_gb[:], in_=gb_all[:]), reads=[gb_all], writes=[dbg_gb], dma=True)

            P.emit()
            K.unscope(); scA.close()
            pend.append((gb_all, scr[g % NPAR], og_o, g * HPC * 128))
            if len(pend) == NPAR or g == NG - 1:
                scB = K.scope()
                emit_gdn_multi(K, nblk, cf, cb, epsc, ng_s, pend)
                P.emit()
                K.unscope(); scB.close()
                pend = []
    return cf, cb, epsc


def build_prog1(Lp, dbg=False, NG=1):
    nblk = Lp // 128
    nc = bass.Bass("TRN2", target_bir_lowering=False)
    es = ExitStack()
    with es:
        K = Ctx(nc, es); P = K.P
        hfull = K.din("hfull", [Lp, D])
        w_a = K.din("w_a", [NG, D, NA]); gpre = K.din("gpre", [128, 16]); cw = K.din("cw", [NG, 128, 8, 4]); hp = K.din("hp", [NG, 128, 3, HPC])
        ng = K.din("ng", [128, 128]); cst = K.din("cst", [128, 5, 128]); rope = K.din("rope", [Lp, 32])
        og_o = K.dout("og", [Lp, NG * HPC * 128], BF16)
        KT_o = K.dout("KT", [2, 128, Lp], BF16); V_o = K.dout("Vt", [Lp, 2 * 128], BF16); ikT_o = K.dout("ikT", [128, Lp], BF16)
        scr = gdn_scratch(K, Lp)
        dbgt = None
        if dbg:
            dbgt = (K.dout("dbg_gb", [128, nblk, 8]), K.dout("dbg_qT", [2, 128, Lp], BF16), K.dout("dbg_kT", [2, 128, Lp], BF16), K.dout("dbg_v", [Lp, 512], BF16))
        emit_gdn_all(K, Lp, NG, hfull, w_a, gpre, cw, hp, ng, cst, rope, og_o, KT_o, V_o, ikT_o, scr, dbgt)
    return nc


def gdn_scratch(K, Lp, n=2):
    return [(K.dtmp(f"qT_d{i}", [2, 128, Lp]), K.dtmp(f"kT_d{i}", [2, 128, Lp]), K.dtmp(f"k_d{i}", [Lp, 256]), K.dtmp(f"v_d{i}", [Lp, 512]), K.dtmp(f"sz_d{i}", [Lp, 512]))
            for i in range(n)]


def prep1(inp, Lp, core):
    f = np.float32
    x = inp['x'][0]; SEQ = x.shape[0]
    hfull = np.zeros((Lp, D), f)
    hfull[PADF:PADF + NMETA] = inp['meta_tokens']; hfull[PADF + NMETA:PADF + NMETA + SEQ] = x
    w_in = inp['w_in'][0]
    o = np.cumsum([0, 2048, 2048, 4096, 4096, 32, 32, 2048, 256, 256, 2048, 128, 16, 2048, 2048])
    gq, gk, gv, gz, gb, ga, aq, ak, av, iq, ik, iw, g1, g2 = [slice(o[i], o[i + 1]) for i in range(14)]
    c = core
    cols = np.concatenate([np.arange(o[0] + 256 * c, o[0] + 256 * c + 256), np.arange(o[1] + 256 * c, o[1] + 256 * c + 256),
                           np.arange(o[2] + 512 * c, o[2] + 512 * c + 512), np.arange(o[3] + 512 * c, o[3] + 512 * c + 512),
                           np.arange(o[4] + 4 * c, o[4] + 4 * c + 4), np.arange(o[5] + 4 * c, o[5] + 4 * c + 4),
                           np.arange(o[8], o[9]), np.arange(o[7], o[8]), np.arange(o[10], o[11])])
    w_a = np.ascontiguousarray(w_in[:, cols])
    gpre = np.ascontiguousarray(inp['mix_pre_g'][0].reshape(16, 128).T)
    cwf = inp['gdn_conv_w'][0]
    ccols = np.concatenate([np.arange(256 * c, 256 * c + 256), np.arange(2048 + 256 * c, 2048 + 256 * c + 256),
                            np.arange(4096 + 512 * c, 4096 + 512 * c + 512)])
    cw = np.ascontiguousarray(cwf[:, ccols].T.reshape(8, 128, 4).transpose(1, 0, 2))
    hp = np.zeros((128, 3, HPC), f)
    hp[:, 0, :] = inp['gdn_a_log'][0][4 * c:4 * c + 4][None, :]
    hp[:, 1, :] = inp['gdn_dt_bias'][0][4 * c:4 * c + 4][None, :]
    ng = np.ascontiguousarray(np.broadcast_to(inp['gdn_norm_g'][0][None, :], (128, 128))).astype(f)
    pos = np.maximum(np.arange(Lp) - PADF, 0)
    return {"hfull": hfull, "w_a": w_a[None], "gpre": gpre, "cw": cw[None], "hp": hp[None], "ng": ng, "cst": make_consts(), "rope": rope_table(pos)}


def emit_rope(P, xf, xb, r_t, r_m, nh):
    cosb = lambda: r_t[:, 0:16].unsqueeze(1).to_broadcast([128, nh, 16])
    sinb = lambda: r_t[:, 16:32].unsqueeze(1).to_broadcast([128, nh, 16])
    x1 = lambda: xf[:, 0:nh, 0:16]
    x2 = lambda: xf[:, 0:nh, 16:32]
    P.op('dve', lambda e: e.tensor_tensor(out=r_m[:, 0, 0:nh, :], in0=x1(), in1=cosb(), op=ALU.mult), reads=[xf, r_t], writes=[r_m])
    P.op('dve', lambda e: e.tensor_tensor(out=r_m[:, 1, 0:nh, :], in0=x2(), in1=sinb(), op=ALU.mult), reads=[xf, r_t, r_m], writes=[r_m])
    P.op('dve', lambda e: e.tensor_tensor(out=r_m[:, 2, 0:nh, :], in0=x2(), in1=cosb(), op=ALU.mult), reads=[xf, r_t, r_m], writes=[r_m])
    P.op('dve', lambda e: e.tensor_tensor(out=r_m[:, 3, 0:nh, :], in0=x1(), in1=sinb(), op=ALU.mult), reads=[xf, r_t, r_m], writes=[r_m])
    P.op('dve', lambda e: e.tensor_tensor(out=xb[:, 0:nh, 0:16], in0=r_m[:, 0, 0:nh, :], in1=r_m[:, 1, 0:nh, :], op=ALU.subtract), reads=[r_m, xb], writes=[xb])
    P.op('dve', lambda e: e.tensor_tensor(out=xb[:, 0:nh, 16:32], in0=r_m[:, 2, 0:nh, :], in1=r_m[:, 3, 0:nh, :], op=ALU.add), reads=[r_m, xb], writes=[xb])


def gdn_setup(K, nblk, cf, cb, epsc, psT, ps_s, gb_all, ng_s, qT_d, kT_d, k_d, v_d, sz_d, og_o, ogc0=0):
    P = K.P
    H = HPC
    S32 = K.sb([128, H, 128], F32, "S32"); Sb = K.sb([128, H, 128], BF16, "Sb")
    P.op('dve', lambda e: e.memset(S32[:], 0.0), writes=[S32])
    P.op('dve', lambda e: e.memset(Sb[:], 0.0), writes=[Sb])
    qT = [K.sb([128, 2, 128], BF16, f"g_qT{i}") for i in range(2)]
    kT = [K.sb([128, 2, 128], BF16, f"g_kT{i}") for i in range(2)]
    ktm = [K.sb([128, 2, 128], BF16, f"g_ktm{i}") for i in range(2)]
    vtm = [K.sb([128, H, 128], BF16, f"g_vtm{i}") for i in range(2)]
    szt = [K.sb([128, H, 128], BF16, f"g_sz{i}") for i in range(2)]
    gbc = K.sb([128, H, 128], F32, "g_gbc")
    Dt = K.sb([128, H, 128], F32, "g_Dt")
    grow = K.sb([128, H, 128], F32, "g_grow")
    ks = K.sb([128, H, 128], BF16, "g_ks")
    ksT = K.sb([128, H, 128], BF16, "g_ksT")
    Nn = [K.sb([128, H, 128], BF16, f"g_N{i}") for i in range(2)]
    NT = [K.sb([128, H, 128], BF16, f"g_NT{i}") for i in range(2)]
    Pb = K.sb([128, H, 128], BF16, "g_Pb")
    vs = K.sb([128, H, 128], F32, "g_vs")
    Rt = K.sb([128, H, 128], BF16, "g_Rt")
    vnew = K.sb([128, H, 128], BF16, "g_vnew")
    attnT = K.sb([128, H, 128], BF16, "g_attnT")
    qgT = K.sb([128, H, 128], BF16, "g_qgT")
    kd = K.sb([128, H, 128], BF16, "g_kd")
    gz = K.sb([128, H, 128], F32, "g_gz")
    og = [K.sb([128, H, 128], BF16, f"g_og{i}") for i in range(2)]
    junk = K.sb([128, 128], BF16, "g_junk")
    ssq = K.sb([128, 2, H], F32, "g_ssq")
    ident4 = K.sb([128, H, 128], F32, "g_id4")
    offd4 = K.sb([128, H, 128], F32, "g_offd4")
    for h in range(H):
        P.op('dve', lambda e, h=h: e.tensor_copy(out=ident4[:, h, :], in_=cf[:, CI_ID, :]), reads=[cf, ident4], writes=[ident4])
        P.op('dve', lambda e, h=h: e.tensor_copy(out=offd4[:, h, :], in_=cf[:, CI_OFFD, :]), reads=[cf, offd4], writes=[offd4])
    psA = K.ps([128, H, 128], F32, "g_psA"); psB = K.ps([128, H, 128], F32, "g_psB"); psC = K.ps([128, H, 128], F32, "g_psC")

    SM = K.sb([128, nblk, 8, H], F32, "g_SM")
    gcn = K.sb([128, nblk, H], F32, "g_gcn")
    P.op('dve', lambda e: e.tensor_copy(out=gcn[:], in_=gb_all[:, :, 4:8]), reads=[gb_all], writes=[gcn])
    for b0 in range(0, nblk, 128):
        nb = min(128, nblk - b0)
        pav = lambda nb=nb: psA[:].rearrange("p h d -> p (h d)")[:, 0:nb * H].rearrange("p (b f) -> p b f", f=H)
        pbv = lambda nb=nb: psB[:].rearrange("p h d -> p (h d)")[:, 0:nb * H].rearrange("p (b f) -> p b f", f=H)
        smr = lambda r, b0=b0, nb=nb: SM[:, b0:b0 + nb, r, :]
        P.op('pe', lambda e, b0=b0, nb=nb: e.matmul(psA[:].rearrange("p h d -> p (h d)")[:, 0:nb * H], lhsT=cf[:, CI_TRIU, :],
                                                   rhs=gcn[:, b0:b0 + nb, :].rearrange("p b f -> p (b f)"), start=True, stop=True), reads=[cf, gcn], writes=[psA])
        P.op('pe', lambda e, b0=b0, nb=nb: e.matmul(psB[:].rearrange("p h d -> p (h d)")[:, 0:nb * H], lhsT=cf[:, CI_ONES, :],
                                                   rhs=gcn[:, b0:b0 + nb, :].rearrange("p b f -> p (b f)"), start=True, stop=True), reads=[cf, gcn], writes=[psB])
        P.op('dve', lambda e, pav=pav, smr=smr: e.tensor_copy(out=smr(0), in_=pav()), reads=[psA, SM], writes=[SM])
        P.op('act', lambda e, pav=pav, smr=smr: e.activation(out=smr(1), in_=pav(), func=AF.Exp), reads=[psA, SM], writes=[SM])
        P.op('act', lambda e, pbv=pbv, smr=smr: e.activation(out=smr(4), in_=pbv(), func=AF.Exp), reads=[psB, SM], writes=[SM])
        P.op('act', lambda e, smr=smr, b0=b0, nb=nb: e.activation(out=smr(2), in_=gb_all[:, b0:b0 + nb, 0:4], func=AF.Sqrt), reads=[gb_all, SM], writes=[SM])
        P.op('dve', lambda e, smr=smr: e.scalar_tensor_tensor(out=smr(3), in0=smr(2), scalar=-1.0, in1=smr(1), op0=ALU.mult, op1=ALU.mult), reads=[SM], writes=[SM])
        P.op('dve', lambda e, pbv=pbv, smr=smr: e.tensor_tensor(out=smr(7), in0=pbv(), in1=smr(0), op=ALU.subtract), reads=[psB, SM], writes=[SM])
        P.op('act', lambda e, smr=smr: e.activation(out=smr(5), in_=smr(7), func=AF.Exp), reads=[SM], writes=[SM])
        P.op('dve', lambda e, smr=smr: e.tensor_scalar(out=smr(6), in0=smr(0), scalar1=-1.0, scalar2=None, op0=ALU.mult), reads=[SM], writes=[SM])
    def chunk(c):
        r0 = c * 128
        q_T = qT[c % 2]; k_T = kT[c % 2]; k_t = ktm[c % 2]; v_t = vtm[c % 2]; s_z = szt[c % 2]; o_g = og[c % 2]
        P.op('sp', lambda e, q_T=q_T, r0=r0: e.dma_start(out=q_T[:], in_=qT_d.t[:, :, r0:r0 + 128].rearrange("h p t -> p h t")), reads=[qT_d], writes=[q_T], dma=True)
        P.op('sp', lambda e, k_T=k_T, r0=r0: e.dma_start(out=k_T[:], in_=kT_d.t[:, :, r0:r0 + 128].rearrange("h p t -> p h t")), reads=[kT_d], writes=[k_T], dma=True)
        P.op('sp', lambda e, k_t=k_t, r0=r0: e.dma_start(out=k_t[:], in_=k_d.t[r0:r0 + 128, :].rearrange("t (h d) -> t h d", h=2)), reads=[k_d], writes=[k_t], dma=True)
        P.op('sp', lambda e, v_t=v_t, r0=r0: e.dma_start(out=v_t[:], in_=v_d.t[r0:r0 + 128, :].rearrange("t (h d) -> t h d", h=H)), reads=[v_d], writes=[v_t], dma=True)
        P.op('sp', lambda e, s_z=s_z, r0=r0: e.dma_start(out=s_z[:], in_=sz_d.t[r0:r0 + 128, :].rearrange("t (h d) -> t h d", h=H)), reads=[sz_d], writes=[s_z], dma=True)
        beta = lambda: gb_all[:, c, 0:4]
        g = lambda: gb_all[:, c, 4:8]
        yield
        sm = SMView(SM, c)
        yield
        yield
        for h in range(H):
            P.op('dve', lambda e, h=h, c=c: e.tensor_scalar(out=gbc[:, h, :], in0=cf[:, CI_ONES, :], scalar1=gb_all[:, c, 4 + h:5 + h], scalar2=None, op0=ALU.mult),
                 reads=[cf, gb_all, gbc], writes=[gbc])
        yield
        for h in range(H):
            P.op('pe', lambda e, h=h: e.matmul(psA[:, h, :], lhsT=gbc[:, h, :], rhs=cf[:, CI_TRIU, :], start=True, stop=True), reads=[gbc, cf, psA], writes=[psA])
            P.op('pe', lambda e, h=h: e.matmul(psB[:, h, :], lhsT=gbc[:, h, :], rhs=cf[:, CI_TRIU, :], start=True, stop=False), reads=[gbc, cf, psB], writes=[psB])
            P.op('pe', lambda e, h=h: e.matmul(psB[:, h, :], lhsT=cf[:, CI_ID, :], rhs=cf[:, CI_NEGM, :], start=False, stop=True), reads=[cf, psB], writes=[psB])
        P.op('act', lambda e: e.activation(out=grow[:], in_=psA[:], func=AF.Exp), reads=[psA], writes=[grow])
        yield
        for h in range(H):
            P.op('act', lambda e, h=h: e.activation(out=Dt[:, h, :], in_=psB[:, h, :], func=AF.Exp, bias=sm[:, 6, h:h + 1]), reads=[psB, sm, Dt], writes=[Dt])
        yield
        yield
        for h in range(H):
            P.op('dve', lambda e, h=h, k_t=k_t: e.tensor_scalar(out=ks[:, h, :], in0=k_t[:, h // 2, :], scalar1=sm[:, 2, h:h + 1], scalar2=None, op0=ALU.mult),
                 reads=[k_t, sm, ks], writes=[ks])
            P.op('act', lambda e, h=h, v_t=v_t: e.activation(out=vs[:, h, :], in_=v_t[:, h, :], func=AF.Copy, scale=sm[:, 2, h:h + 1]),
                 reads=[v_t, sm, vs], writes=[vs])
            P.op('act', lambda e, h=h, k_t=k_t: e.activation(out=kd[:, h, :], in_=k_t[:, h // 2, :], func=AF.Copy, scale=sm[:, 5, h:h + 1]),
                 reads=[k_t, sm, kd], writes=[kd])
            P.op('dve', lambda e, h=h, q_T=q_T: e.tensor_tensor(out=qgT[:, h, :], in0=q_T[:, h // 2, :], in1=grow[:, h, :], op=ALU.mult), reads=[q_T, grow, qgT], writes=[qgT])
            P.op('pool', lambda e, h=h, s_z=s_z: e.tensor_tensor(out=gz[:, h, :], in0=s_z[:, h, :], in1=ng_s[:], op=ALU.mult), reads=[s_z, ng_s, gz], writes=[gz])
        yield
        for h in range(H):
            P.op('pe', lambda e, h=h: e.transpose(out=psT[:, h, :], in_=ks[:, h, :], identity=cb[:, CI_ID, :]), reads=[ks, cb, psT], writes=[psT])
        P.op('act', lambda e: e.activation(out=ksT[:], in_=psT[:], func=AF.Copy), reads=[psT], writes=[ksT])
        yield
        yield
        for h in range(H):
            P.op('pe', lambda e, h=h: e.matmul(psA[:, h, :], lhsT=ksT[:, h, :], rhs=ksT[:, h, :], start=True, stop=True), reads=[ksT, psA], writes=[psA])
        yield
        for hq in range(2):
            P.op('pe', lambda e, hq=hq, k_T=k_T, q_T=q_T: e.matmul(psC[:, hq, :], lhsT=k_T[:, hq, :], rhs=q_T[:, hq, :], start=True, stop=True), reads=[k_T, q_T, psC], writes=[psC])
        yield
        for h in range(H):
            P.op('dve', lambda e, h=h: e.tensor_tensor(out=attnT[:, h, :], in0=psC[:, h // 2, :], in1=Dt[:, h, :], op=ALU.mult), reads=[psC, Dt, attnT], writes=[attnT])
        P.op('dve', lambda e: e.tensor_tensor(out=Dt[:], in0=Dt[:], in1=offd4[:], op=ALU.mult), reads=[Dt, offd4, attnT], writes=[Dt])
        N0 = Nn[0]; NT0 = NT[0]
        P.op('dve', lambda e: e.scalar_tensor_tensor(out=N0[:], in0=psA[:], scalar=-1.0, in1=Dt[:], op0=ALU.mult, op1=ALU.mult), reads=[psA, Dt], writes=[N0])
        yield
        for h in range(H):
            P.op('pe', lambda e, h=h: e.transpose(out=psT[:, h, :], in_=N0[:, h, :], identity=cb[:, CI_ID, :]), reads=[N0, cb, psT], writes=[psT])
        P.op('act', lambda e: e.activation(out=NT0[:], in_=psT[:], func=AF.Copy), reads=[psT], writes=[NT0])
        P.op('dve', lambda e: e.tensor_tensor(out=Pb[:], in0=N0[:], in1=ident4[:], op=ALU.add), reads=[N0, ident4], writes=[Pb])
        cur = 0
        yield
        for step in range(1, 7):
            Nc = Nn[cur]; NTc = NT[cur]; Nx = Nn[1 - cur]; NTx = NT[1 - cur]
            for h in range(H):
                P.op('pe', lambda e, h=h, Nc=Nc, NTc=NTc: e.matmul(psB[:, h, :], lhsT=Nc[:, h, :], rhs=NTc[:, h, :], start=True, stop=True), reads=[Nc, NTc, psB], writes=[psB])
            P.op('act', lambda e, NTx=NTx: e.activation(out=NTx[:], in_=psB[:], func=AF.Copy), reads=[psB], writes=[NTx])
            if step < 6:
                for h in range(H):
                    P.op('pe', lambda e, h=h, Nc=Nc, NTc=NTc: e.matmul(psA[:, h, :], lhsT=NTc[:, h, :], rhs=Nc[:, h, :], start=True, stop=True), reads=[Nc, NTc, psA], writes=[psA])
                P.op('dve', lambda e, Nx=Nx: e.tensor_copy(out=Nx[:], in_=psA[:]), reads=[psA], writes=[Nx])
            for h in range(H):
                P.op('pe', lambda e, h=h, NTx=NTx: e.matmul(psC[:, h, :], lhsT=NTx[:, h, :], rhs=Pb[:, h, :], start=True, stop=True), reads=[NTx, Pb, psC], writes=[psC])
            P.op('dve', lambda e: e.tensor_tensor(out=Pb[:], in0=Pb[:], in1=psC[:], op=ALU.add), reads=[Pb, psC], writes=[Pb])
            cur = 1 - cur
        yield
        yield
        for h in range(H):
            P.op('pe', lambda e, h=h, k_T=k_T: e.matmul(psA[:, h, :], lhsT=k_T[:, h // 2, :], rhs=Sb[:, h, :], start=True, stop=True), reads=[k_T, Sb, psA], writes=[psA])
        yield
        for h in range(H):
            P.op('dve', lambda e, h=h: e.scalar_tensor_tensor(out=Rt[:, h, :], in0=psA[:, h, :], scalar=sm[:, 3, h:h + 1], in1=vs[:, h, :], op0=ALU.mult, op1=ALU.add),
                 reads=[psA, sm, vs, Rt], writes=[Rt])
        yield
        for h in range(H):
            P.op('pe', lambda e, h=h: e.matmul(psB[:, h, :], lhsT=Pb[:, h, :], rhs=Rt[:, h, :], start=True, stop=True), reads=[Pb, Rt, psB], writes=[psB])
        yield
        for h in range(H):
            P.op('act', lambda e, h=h: e.activation(out=vnew[:, h, :], in_=psB[:, h, :], func=AF.Copy, scale=sm[:, 2, h:h + 1]), reads=[psB, sm, vnew], writes=[vnew])
        yield
        for h in range(H):
            P.op('pe', lambda e, h=h: e.matmul(psC[:, h, :], lhsT=qgT[:, h, :], rhs=Sb[:, h, :], start=True, stop=False), reads=[qgT, Sb, psC], writes=[psC])
            P.op('pe', lambda e, h=h: e.matmul(psC[:, h, :], lhsT=attnT[:, h, :], rhs=vnew[:, h, :], start=False, stop=True), reads=[attnT, vnew, psC], writes=[psC])
        yield
        for h in range(H):
            P.op('pe', lambda e, h=h: e.matmul(psA[:, h, :], lhsT=kd[:, h, :], rhs=vnew[:, h, :], start=True, stop=True), reads=[kd, vnew, psA], writes=[psA])
        yield
        for h in range(H):
            P.op('dve', lambda e, h=h: e.scalar_tensor_tensor(out=Sb[:, h, :], in0=S32[:, h, :], scalar=sm[:, 4, h:h + 1], in1=psA[:, h, :], op0=ALU.mult, op1=ALU.add),
                 reads=[S32, sm, psA, Sb], writes=[Sb])
        for h in range(H):
            P.op('dve', lambda e, h=h: e.scalar_tensor_tensor(out=S32[:, h, :], in0=S32[:, h, :], scalar=sm[:, 4, h:h + 1], in1=psA[:, h, :], op0=ALU.mult, op1=ALU.add),
                 reads=[S32, sm, psA], writes=[S32])
        yield
        yield
        for h in range(H):
            P.op('act', lambda e, h=h: e.activation(out=junk[:], in_=psC[:, h, :], func=AF.Square, accum_out=ssq[:, 0, h:h + 1]), reads=[psC, junk, ssq], writes=[junk, ssq])
        P.op('act', lambda e: e.activation(out=ssq[:, 1, :], in_=ssq[:, 0, :], func=AF.Sqrt, scale=1.0 / 128, bias=epsc[:, 0:1]), reads=[ssq, epsc], writes=[ssq])
        P.op('dve', lambda e: e.reciprocal(out=ssq[:, 1, :], in_=ssq[:, 1, :]), reads=[ssq], writes=[ssq])
        yield
        for h in range(H):
            P.op('dve', lambda e, h=h, o_g=o_g: e.scalar_tensor_tensor(out=o_g[:, h, :], in0=psC[:, h, :], scalar=ssq[:, 1, h:h + 1], in1=gz[:, h, :], op0=ALU.mult, op1=ALU.mult),
                 reads=[psC, ssq, gz, o_g], writes=[o_g])
        P.op('sp', lambda e, o_g=o_g, r0=r0: e.dma_start(out=og_o[r0:r0 + 128, ogc0:ogc0 + HPC * 128], in_=o_g[:]), reads=[o_g], writes=[og_o], dma=True)

    return chunk


def emit_gdn_multi(K, nblk, cf, cb, epsc, ng_s, groups):
    psT = K.ps([128, HPC, 128], BF16, "g_psT")
    ps_s = K.ps([128, 2, HPC], F32, "g_pss")
    fns = [gdn_setup(K, nblk, cf, cb, epsc, psT, ps_s, gb, ng_s, *scr, og_o, c0) for (gb, scr, og_o, c0) in groups]
    for c in range(nblk):
        gens = [f(c) for f in fns]
        while gens:
            nxt = []
            for gen in gens:
                try:
                    next(gen); nxt.append(gen)
                except StopIteration:
                    pass
            gens = nxt


NWC = 2048 + 2048 + 16 + 2048 + 2048
BSTR = 126
A0 = 126
NIOTA = 1408


def blocks_for(SEQ):
    nb = -(-SEQ // BSTR)
    NS = -(-nb // NCORE)
    return nb, NS


def slot_geom(j, nblk):
    a_min = A0 + BSTR * (NCORE * j)
    a_max = A0 + BSTR * (NCORE * j + NCORE - 1)
    kd = min(a_min // 128, nblk)
    kb_end = min((a_max + 127) // 128 + 1, nblk)
    kd = min(kd, kb_end)
    return kd, kb_end


def build_prog2(Lp, NS, topk, dbg=False, fused=False):
    nblk = Lp // 128
    nc = bass.Bass("TRN2", target_bir_lowering=False)
    es = ExitStack()
    with es:
        K = Ctx(nc, es); P = K.P
        R = NS * 128
        hown = K.din("hown", [R, D])
        if not fused:
            ogown = K.din("ogown", [R, 4096], BF16)
            KT_i = K.din("KT", [2, 128, Lp], BF16); V_i = K.din("Vt", [Lp, 256], BF16); ikT_i = K.din("ikT", [128, Lp], BF16)
            ogsrc = ('own', ogown)
        else:
            NG = NCORE
            hfull = K.din("hfull", [Lp, D])
            w_a = K.din("w_a", [NG, D, NA]); cw = K.din("cw", [NG, 128, 8, 4]); hp = K.din("hp", [NG, 128, 3, HPC])
            ng = K.din("ng", [128, 128]); rope = K.din("rope", [Lp, 32])
            sel_i = K.din("sel", [128, 8, 128], BF16)
            og_d = K.dtmp("og_d", [Lp, 4096]); KT_i = K.dtmp("KT_s", [2, 128, Lp]); V_i = K.dtmp("Vt_s", [Lp, 256]); ikT_i = K.dtmp("ikT_s", [128, Lp])
        ropeo = K.din("ropeo", [R, 32])
        qrel_i = K.din("qrel", [128, NS])
        iota_i = K.din("iota", [128, NIOTA])
        cst = K.din("cst", [128, 5, 128])
        gpre = K.din("gpre", [128, 16]); g2pre = K.din("g2pre", [128, 16])
        gpost = K.din("gpost", [128, D]); g2post = K.din("g2post", [128, D])
        fcw = K.din("fcw", [128, 96, 3]); fcb = K.din("fcb", [128, 96])
        w_c = K.din("w_c", [D, NWC]); wbg = K.din("wbg", [4096, D]); wba = K.din("wba", [D, D]); wout = K.din("wout", [D, D])
        wup = K.din("wup", [D, 2 * NFF]); wdn = K.din("wdn", [NFF, D])
        out_o = K.dout("out", [NS, BSTR, D])
        wc_b = K.dtmp("wc_b", [128, 17, 16, 512]); wbg_b = K.dtmp("wbg_b", [128, 4, 32, 512]); wba_b = K.dtmp("wba_b", [128, 4, 16, 512])
        wout_b = K.dtmp("wout_b", [128, 4, 16, 512]); wup_b = K.dtmp("wup_b", [128, 24, 16, 512]); wdn_b = K.dtmp("wdn_b", [128, 4, 48, 512])
        if dbg:
            dbg_oatt = K.dout("dbg_oatt", [R, D], BF16); dbg_thr = K.dout("dbg_thr", [128, NS]); dbg_h1 = K.dout("dbg_h1", [R, D])
            dbg_q = K.dout("dbg_q", [R, D], BF16)

        if fused:
            cf, cb, epsc = emit_gdn_all(K, Lp, NG, hfull, w_a, gpre, cw, hp, ng, cst, rope, og_d, KT_i, V_i, ikT_i, gdn_scratch(K, Lp))
            sel_s = K.sb([128, 8, 128], BF16, "sel_s")
            P.op('sp', lambda e: e.dma_start(out=sel_s[:], in_=sel_i[:]), writes=[sel_s], dma=True)
            ogsrc = ('sel', og_d, sel_s, Lp)
        else:
            cf = K.sb([128, 5, 128], F32, "cf"); cb = K.sb([128, 5, 128], BF16, "cb")
            P.op('sp', lambda e: e.dma_start(out=cf[:], in_=cst[:]), writes=[cf], dma=True)
            P.op('dve', lambda e: e.tensor_copy(out=cb[:], in_=cf[:]), reads=[cf], writes=[cb])
            epsc = K.sb([128, 1], F32, "epsc")
            P.op('dve', lambda e: e.memset(epsc[:], EPS), writes=[epsc])
        gpre_s = K.sb([128, 16], F32); g2pre_s = K.sb([128, 16], F32); qrel_s = K.sb([128, NS], F32)
        fcw_s = K.sb([128, 96, 3], F32); fcb_s = K.sb([128, 96], F32)
        for dst, src in ((gpre_s, gpre), (g2pre_s, g2pre), (qrel_s, qrel_i), (fcw_s, fcw), (fcb_s, fcb)):
            P.op('sp', lambda e, dst=dst, src=src: e.dma_start(out=dst[:], in_=src[:]), writes=[dst], dma=True)

        sc0 = K.scope()
        st = [K.sb([128, 2048], F32, f"w0s{i}") for i in range(2)]
        sbt = [K.sb([128, 2048], BF16, f"w0b{i}") for i in range(2)]
        it = [0]
        def cast_w(src, dst, KC, N, gain, up=False):
            sv = src.t.rearrange("(kc p) n -> p kc n", p=128)
            for kc in range(KC):
                for n0 in range(0, N, 2048):
                    n1 = min(N, n0 + 2048); w = n1 - n0
                    s_ = st[it[0] % 2]; b_ = sbt[it[0] % 2]; it[0] += 1
                    P.op('sp', lambda e, s_=s_, kc=kc, n0=n0, n1=n1, w=w: e.dma_start(out=s_[:, 0:w], in_=sv[:, kc, n0:n1]), writes=[s_], dma=True)
                    if gain is None:
                        P.op('act', lambda e, s_=s_, b_=b_, w=w: e.activation(out=b_[:, 0:w], in_=s_[:, 0:w], func=AF.Copy), reads=[s_], writes=[b_])
                    else:
                        P.op('act', lambda e, s_=s_, b_=b_, w=w, kc=kc: e.activation(out=b_[:, 0:w], in_=s_[:, 0:w], func=AF.Copy, scale=gain[:, kc:kc + 1]),
                             reads=[s_, gain], writes=[b_])
                    if up:
                        half = n0 // NFF; g0 = (n0 % NFF) // 256
                        P.op('pool', lambda e, b_=b_, kc=kc, g0=g0, half=half: e.dma_start(out=dst[:, g0:g0 + 8, kc, half * 256:(half + 1) * 256],
                                                                                          in_=b_[:, 0:2048].rearrange("p (g c) -> p g c", c=256)), reads=[b_], writes=[dst], dma=True)
                    else:
                        nt = w // 512; rem = w % 512; t0_ = n0 // 512
                        if nt:
                            P.op('pool', lambda e, b_=b_, kc=kc, nt=nt, t0_=t0_: e.dma_start(out=dst[:, t0_:t0_ + nt, kc, :], in_=b_[:, 0:nt * 512].rearrange("p (g c) -> p g c", c=512)),
                                 reads=[b_], writes=[dst], dma=True)
                        if rem:
                            P.op('pool', lambda e, b_=b_, kc=kc, nt=nt, t0_=t0_, rem=rem: e.dma_start(out=dst[:, t0_ + nt, kc, 0:rem], in_=b_[:, nt * 512:nt * 512 + rem]),
                                 reads=[b_], writes=[dst], dma=True)
        cast_w(w_c, wc_b, 16, NWC, gpre_s)
        cast_w(wbg, wbg_b, 32, D, None); cast_w(wba, wba_b, 16, D, None); cast_w(wout, wout_b, 16, D, None)
        cast_w(wup, wup_b, 16, 2 * NFF, g2pre_s, up=True); cast_w(wdn, wdn_b, 48, D, None)
        P.emit(); K.unscope(); sc0.close()

        xo = K.sb([128, D], F32, "xo")
        QT = K.sb([128, 16, 128], BF16, "QT"); iqT = K.sb([128, 16, 128], BF16, "iqT")
        sgn = K.sb([128, 16], F32, "sgn"); aiw = K.sb([128, 16], F32, "aiw")
        sig = K.sb([128, 2, D], BF16, "sig")
        oatt = K.sb([128, D], BF16, "oatt")
        thr = K.sb([128, NS], F32, "thr")

        for j in range(NS):
            r0 = j * 128
            kd, kb_end = slot_geom(j, nblk)
            nk = kb_end * 128
            scW = K.scope()
            W = make_wpool(K)
            xs = K.sb([128, D], BF16, "p_xs"); ss = K.sb([128, 2], F32, "p_ss")
            uT = K.sb([128, 16, 128], BF16, "p_uT")
            tq = K.sb([128, 16, 128], F32, "p_tq"); tqb = K.sb([128, 16, 128], BF16, "p_tqb")
            r_t = K.sb([128, 32], F32, "p_rt"); r_m = K.sb([128, 4, 16, 16], F32, "p_rm")
            iwt = K.sb([128, 16], F32, "p_iw")
            P.op('sp', lambda e, r0=r0: e.dma_start(out=xo[:], in_=hown[r0:r0 + 128, :]), writes=[xo], dma=True)
            P.op('sp', lambda e, r0=r0: e.dma_start(out=r_t[:], in_=ropeo[r0:r0 + 128, :]), writes=[r_t], dma=True)
            emit_norm_T(P, W, cb, epsc, xo, xs, ss, uT)
            for ct in range(17):
                n0 = ct * 512 if ct < 8 else (8192 if ct == 8 else 4096 + (ct - 9) * 512)
                ncol = 16 if ct == 8 else 512
                pm = W.linear(uT, wc_b, 16, n0, ncol)
                if ct < 8:
                    hh = (ct % 4) * 4
                    if ct == 4:
                        finish_q(P, W, cb, tq, tqb, r_t, r_m, QT, 128 ** -0.5, None)
                    P.op('act', lambda e, pm=pm, hh=hh: e.activation(out=tq[:, hh:hh + 4, :], in_=pm[:, 0:512], func=AF.Copy), reads=[pm], writes=[tq])
                elif ct == 8:
                    P.op('act', lambda e, pm=pm: e.activation(out=iwt[:], in_=pm[:, 0:16], func=AF.Copy), reads=[pm], writes=[iwt])
                    P.op('act', lambda e: e.activation(out=aiw[:], in_=iwt[:], func=AF.Abs, scale=(16 ** -0.5) * (128 ** -0.5)), reads=[iwt], writes=[aiw])
                    P.op('dve', lambda e: e.tensor_scalar(out=sgn[:], in0=iwt[:], scalar1=0.0, scalar2=2.0, op0=ALU.is_gt, op1=ALU.mult), reads=[iwt], writes=[sgn])
                    P.op('dve', lambda e: e.tensor_scalar(out=sgn[:], in0=sgn[:], scalar1=-1.0, scalar2=None, op0=ALU.add), reads=[sgn], writes=[sgn])
                    finish_q(P, W, cb, tq, tqb, r_t, r_m, iqT, None, aiw)
                else:
                    gi = (ct - 9) // 4; c0 = ((ct - 9) % 4) * 512
                    P.op('act', lambda e, pm=pm, gi=gi, c0=c0: e.activation(out=sig[:, gi, c0:c0 + 512], in_=pm[:, 0:512], func=AF.Sigmoid), reads=[pm], writes=[sig])
            if dbg:
                P.op('sp', lambda e, r0=r0: e.dma_start(out=dbg_q[r0:r0 + 128, :], in_=tqb[:]), reads=[tqb], writes=[dbg_q], dma=True)
            P.emit(); K.unscope(); scW.close()

            scA = K.scope()
            emit_attention(K, cb, cf, j, kd, kb_end, topk, QT, iqT, sgn, qrel_s, iota_i, KT_i, V_i, ikT_i, oatt, thr)
            K.unscope(); scA.close()
            if dbg:
                P.op('sp', lambda e, r0=r0: e.dma_start(out=dbg_oatt[r0:r0 + 128, :], in_=oatt[:]), reads=[oatt], writes=[dbg_oatt], dma=True)

            scW = K.scope()
            W = make_wpool(K)
            emit_merge_ffn(K, W, cb, epsc, j, xo, ogsrc, oatt, sig, gpost, g2post, fcw_s, fcb_s,
                           wbg_b, wba_b, wout_b, wup_b, wdn_b, out_o, dbg_h1 if dbg else None)
            P.emit(); K.unscope(); scW.close()
        if dbg:
            P.op('sp', lambda e: e.dma_start(out=dbg_thr[:], in_=thr[:]), reads=[thr], writes=[dbg_thr], dma=True)
        P.emit()
    return nc


class WPool:
    pass


def make_wpool(K):
    P = K.P
    W = WPool()
    W.wt = [K.sb([128, 16, 512], BF16, f"wt{i}") for i in range(3)]
    W.ps_mm = [K.ps([128, 512], F32, f"w_psmm{i}") for i in range(3)]
    W.ps_tr = [K.ps([128, 4, 128], BF16, f"w_pstr{i}") for i in range(2)]
    W.nw = 0; W.nm = 0; W.nt = 0
    def next_tr():
        W.nt += 1
        return W.ps_tr[W.nt % 2]
    W.next_tr = next_tr
    def linear(xT, wd, KC, n0, ncol, pm=None, xoff=0):
        if pm is None:
            W.nm += 1; pm = W.ps_mm[W.nm % 3]
        for part in range(KC // 16):
            wt = W.wt[W.nw % 3]; W.nw += 1
            P.op('sp', lambda e, wt=wt, part=part: e.dma_start(out=wt[:, :, 0:ncol], in_=wd[:, n0 // 512, part * 16:(part + 1) * 16, 0:ncol]), reads=[wd], writes=[wt], dma=True)
            for kc in range(16):
                kk = part * 16 + kc
                P.op('pe', lambda e, wt=wt, kc=kc, kk=kk: e.matmul(pm[:, 0:ncol], lhsT=xT[:, xoff + kk, :], rhs=wt[:, kc, 0:ncol], start=(kk == 0), stop=(kk == KC - 1)),
                     reads=[xT, wt], writes=[pm])
        return pm
    W.linear = linear
    return W


def emit_norm_T(P, W, cb, epsc, x, xs, ss, uT):
    P.op('act', lambda e: e.activation(out=xs[:], in_=x[:], func=AF.Square, accum_out=ss[:, 0:1]), reads=[x], writes=[xs, ss])
    P.op('act', lambda e: e.activation(out=ss[:, 1:2], in_=ss[:, 0:1], func=AF.Sqrt, scale=1.0 / D, bias=epsc[:, 0:1]), reads=[ss, epsc], writes=[ss])
    P.op('dve', lambda e: e.reciprocal(out=ss[:, 1:2], in_=ss[:, 1:2]), reads=[ss], writes=[ss])
    P.op('dve', lambda e: e.tensor_scalar(out=xs[:], in0=x[:], scalar1=ss[:, 1:2], scalar2=None, op0=ALU.mult), reads=[x, ss], writes=[xs])
    emit_T(P, W, cb, lambda kc: xs[:, kc * 128:(kc + 1) * 128], xs, uT, 16)


def emit_T(P, W, cb, src_ap, src_tl, dstT, n):
    for g4 in range(0, n, 4):
        pt = W.next_tr()
        m = min(4, n - g4)
        for q in range(m):
            P.op('pe', lambda e, pt=pt, q=q, i=g4 + q: e.transpose(out=pt[:, q, :], in_=src_ap(i), identity=cb[:, CI_ID, :]), reads=[src_tl, cb], writes=[pt])
        if (g4 // 4) % 2:
            P.op('act', lambda e, pt=pt, g4=g4, m=m: e.activation(out=dstT[:, g4:g4 + m, :], in_=pt[:, 0:m, :], func=AF.Copy), reads=[pt], writes=[dstT])
        else:
            P.op('dve', lambda e, pt=pt, g4=g4, m=m: e.tensor_copy(out=dstT[:, g4:g4 + m, :], in_=pt[:, 0:m, :]), reads=[pt], writes=[dstT])


def finish_q(P, W, cb, tq, tqb, r_t, r_m, dstT, const_scale, row_scale):
    if row_scale is not None:
        P.op('dve', lambda e: e.tensor_tensor(out=tq[:], in0=tq[:], in1=row_scale[:].unsqueeze(2).to_broadcast([128, 16, 128]), op=ALU.mult), reads=[tq, row_scale], writes=[tq])
    else:
        P.op('dve', lambda e: e.tensor_scalar(out=tq[:], in0=tq[:], scalar1=const_scale, scalar2=None, op0=ALU.mult), reads=[tq], writes=[tq])
    P.op('act', lambda e: e.activation(out=tqb[:], in_=tq[:], func=AF.Copy), reads=[tq], writes=[tqb])
    emit_rope(P, tq, tqb, r_t, r_m, 16)
    emit_T(P, W, cb, lambda h: tqb[:, h, :], tqb, dstT, 16)


def emit_attention(K, cb, cf, j, kd, kb_end, topk, QT, iqT, sgn, qrel_s, iota_i, KT_i, V_i, ikT_i, oatt, thr):
    P = K.P
    nk = kb_end * 128
    sc = K.sb([128, nk], F32, "a_sc")
    maskT = K.sb([128, kb_end, 128], BF16, "a_maskT")
    JW = 4096
    junk = K.sb([128, JW], BF16, "a_junk")
    rl = [K.sb([128, 512], F32, f"a_rl{i}") for i in range(2)]
    ikb = [K.sb([128, 512], BF16, f"a_ik{i}") for i in range(2)]
    iot = K.sb([128, NIOTA], F32, "a_iota")
    P.op('sp', lambda e: e.dma_start(out=iot[:], in_=iota_i[:]), writes=[iot], dma=True)
    scp = K.scope()
    ps_i = [K.ps([128, 512], F32, f"a_psi{i}") for i in range(3)]
    ps_t = [K.ps([128, 4, 128], BF16, f"a_pst{i}") for i in range(2)]
    ni = 0
    for u0 in range(0, kb_end, 4):
        nb_ = min(4, kb_end - u0); w = nb_ * 128; k0 = u0 * 128
        ik = ikb[(u0 // 4) % 2]
        P.op('sp', lambda e, ik=ik, k0=k0, w=w: e.dma_start(out=ik[:, 0:w], in_=ikT_i[:, k0:k0 + w]), reads=[ikT_i], writes=[ik], dma=True)
        for h in range(16):
            pi = ps_i[ni % 3]; r_ = rl[ni % 2]; ni += 1
            P.op('pe', lambda e, pi=pi, ik=ik, h=h, w=w: e.matmul(pi[:, 0:w], lhsT=iqT[:, h, :], rhs=ik[:, 0:w], start=True, stop=True), reads=[iqT, ik], writes=[pi])
            P.op('act', lambda e, pi=pi, r_=r_, w=w: e.activation(out=r_[:, 0:w], in_=pi[:, 0:w], func=AF.Relu), reads=[pi], writes=[r_])
            if h == 0:
                P.op('dve', lambda e, r_=r_, k0=k0, w=w: e.tensor_scalar(out=sc[:, k0:k0 + w], in0=r_[:, 0:w], scalar1=sgn[:, 0:1], scalar2=None, op0=ALU.mult), reads=[r_, sgn], writes=[sc])
            else:
                P.op('dve', lambda e, r_=r_, k0=k0, w=w, h=h: e.scalar_tensor_tensor(out=sc[:, k0:k0 + w], in0=r_[:, 0:w], scalar=sgn[:, h:h + 1], in1=sc[:, k0:k0 + w], op0=ALU.mult, op1=ALU.add),
                     reads=[r_, sgn, sc], writes=[sc])
    P.op('dve', lambda e: e.memset(sc[:, 0:PADF], -1e30), reads=[sc], writes=[sc])
    wc = (kb_end - kd) * 128
    if wc > 0:
        assert wc <= NIOTA
        P.op('dve', lambda e: e.tensor_scalar(out=iot[:, 0:wc], in0=iot[:, 0:wc], scalar1=qrel_s[:, j:j + 1], scalar2=-1e30, op0=ALU.is_gt, op1=ALU.mult), reads=[iot, qrel_s], writes=[iot])
        P.op('dve', lambda e: e.tensor_tensor(out=sc[:, kd * 128:nk], in0=sc[:, kd * 128:nk], in1=iot[:, 0:wc], op=ALU.add), reads=[sc, iot], writes=[sc])
    lo = K.sb([128, 1], F32, "a_lo"); mid = K.sb([128, 1], F32, "a_mid"); cnt = K.sb([128, 8], F32, "a_cnt"); dl = K.sb([128, 1], F32, "a_dl")
    LO0, RANGE, NIT = -16.0, 64.0, 26
    P.op('dve', lambda e: e.memset(lo[:], LO0), writes=[lo])
    npc = -(-nk // JW)
    for it in range(NIT):
        hk = RANGE / (2 ** (it + 1))
        P.op('dve', lambda e, hk=hk: e.tensor_scalar(out=mid[:], in0=lo[:], scalar1=hk, scalar2=None, op0=ALU.add), reads=[lo], writes=[mid])
        for pc in range(npc):
            c0 = pc * JW; c1 = min(nk, c0 + JW)
            P.op('dve', lambda e, c0=c0, c1=c1, pc=pc: e.tensor_scalar(out=junk[:, 0:c1 - c0], in0=sc[:, c0:c1], scalar1=mid[:, 0:1], scalar2=0.0, op0=ALU.is_ge, op1=ALU.add,
                                                                     accum_out=cnt[:, pc:pc + 1]), reads=[sc, mid, junk], writes=[junk, cnt])
        if npc > 1:
            P.op('dve', lambda e: e.tensor_reduce(out=cnt[:, 7:8], in_=cnt[:, 0:npc], axis=mybir.AxisListType.X, op=ALU.add), reads=[cnt], writes=[cnt])
            cc = 7
        else:
            cc = 0
        P.op('dve', lambda e, hk=hk, cc=cc: e.tensor_scalar(out=dl[:], in0=cnt[:, cc:cc + 1], scalar1=topk - 0.5, scalar2=hk, op0=ALU.is_gt, op1=ALU.mult), reads=[cnt], writes=[dl])
        P.op('dve', lambda e: e.tensor_tensor(out=lo[:], in0=lo[:], in1=dl[:], op=ALU.add), reads=[lo, dl], writes=[lo])
    P.op('dve', lambda e: e.tensor_copy(out=thr[:, j:j + 1], in_=lo[:]), reads=[lo, thr], writes=[thr])
    mk = [K.sb([128, 512], BF16, f"a_mk{i}") for i in range(2)]
    for u0 in range(0, kb_end, 4):
        nb_ = min(4, kb_end - u0); w = nb_ * 128; k0 = u0 * 128
        m_ = mk[(u0 // 4) % 2]; pt = ps_t[(u0 // 4) % 2]
        P.op('dve', lambda e, m_=m_, k0=k0, w=w: e.tensor_scalar(out=m_[:, 0:w], in0=sc[:, k0:k0 + w], scalar1=lo[:, 0:1], scalar2=None, op0=ALU.is_ge), reads=[sc, lo], writes=[m_])
        for q in range(nb_):
            P.op('pe', lambda e, pt=pt, q=q, m_=m_: e.transpose(out=pt[:, q, :], in_=m_[:, q * 128:(q + 1) * 128], identity=cb[:, CI_ID, :]), reads=[m_, cb], writes=[pt])
        P.op('act', lambda e, pt=pt, u0=u0, nb_=nb_: e.activation(out=maskT[:, u0:u0 + nb_, :], in_=pt[:, 0:nb_, :], func=AF.Copy), reads=[pt], writes=[maskT])
    P.emit(); K.unscope(); scp.close()
    scp = K.scope()
    psO = [K.ps([128, 512], F32, f"a_pso{i}") for i in range(6)]
    psS = [K.ps([128, 4, 128], F32, f"a_pss{i}") for i in range(2)]
    Kb = [K.sb([128, 2, 128], BF16, f"a_K{i}") for i in range(3)]
    Vb = [K.sb([128, 2, 129], BF16, f"a_V{i}") for i in range(3)]
    eb = [K.sb([128, 4, 128], BF16, f"a_e{i}") for i in range(2)]
    pb = [K.sb([128, 4, 128], BF16, f"a_p{i}") for i in range(3)]
    rc = K.sb([128, 16], F32, "a_rc")
    for v_ in Vb:
        P.op('dve', lambda e, v_=v_: e.memset(v_[:, :, 128:129], 1.0), writes=[v_])
    ns = 0
    for kb in range(kb_end):
        k_ = Kb[kb % 3]; v_ = Vb[kb % 3]; k0 = kb * 128
        P.op('sp', lambda e, k_=k_, k0=k0: e.dma_start(out=k_[:], in_=KT_i.t[:, :, k0:k0 + 128].rearrange("h p t -> p h t")), reads=[KT_i], writes=[k_], dma=True)
        P.op('sp', lambda e, v_=v_, k0=k0: e.dma_start(out=v_[:, :, 0:128], in_=V_i.t[k0:k0 + 128, :].rearrange("t (h d) -> t h d", h=2)), reads=[V_i, v_], writes=[v_], dma=True)
        for g in range(2):
            for half in range(2):
                h0 = g * 8 + half * 4
                pS = psS[ns % 2]; e_ = eb[ns % 2]; p_ = pb[ns % 3]; ns += 1
                P.op('pe', lambda e, pS=pS, k_=k_, g=g, h0=h0: e.matmul(pS[:], lhsT=k_[:, g, :], rhs=QT[:, h0:h0 + 4, :], start=True, stop=True), reads=[k_, QT], writes=[pS])
                P.op('act', lambda e, pS=pS, e_=e_: e.activation(out=e_[:], in_=pS[:], func=AF.Exp), reads=[pS], writes=[e_])
                P.op('dve', lambda e, e_=e_, p_=p_, kb=kb: e.tensor_tensor(out=p_[:], in0=e_[:], in1=maskT[:, kb, :].unsqueeze(1).to_broadcast([128, 4, 128]), op=ALU.mult),
                     reads=[e_, maskT], writes=[p_])
                for hh in range(4):
                    hd = h0 + hh
                    po = psO[hd // 3]
                    P.op('pe', lambda e, po=po, hd=hd, p_=p_, hh=hh, v_=v_, g=g, kb=kb: e.matmul(po[:, (hd % 3) * 129:(hd % 3) * 129 + 129], lhsT=p_[:, hh, :], rhs=v_[:, g, :], start=(kb == 0 and hd % 3 == 0), stop=(kb == kb_end - 1 and (hd % 3 == 2 or hd == 15))),
                         reads=[p_, v_], writes=[po])
    for hd in range(16):
        po = psO[hd // 3]
        P.op('dve', lambda e, po=po, hd=hd: e.reciprocal(out=rc[:, hd:hd + 1], in_=po[:, (hd % 3) * 129 + 128:(hd % 3) * 129 + 129]), reads=[po, rc], writes=[rc])
        P.op('act', lambda e, po=po, hd=hd: e.activation(out=oatt[:, hd * 128:(hd + 1) * 128], in_=po[:, (hd % 3) * 129:(hd % 3) * 129 + 128], func=AF.Copy, scale=rc[:, hd:hd + 1]), reads=[po, rc, oatt], writes=[oatt])
    P.emit(); K.unscope(); scp.close()


def emit_merge_ffn(K, W, cb, epsc, j, xo, ogsrc, oatt, sig, gpost, g2post, fcw_s, fcb_s,
                   wbg_b, wba_b, wout_b, wup_b, wdn_b, out_o, dbg_h1):
    P = K.P
    r0 = j * 128
    if ogsrc[0] == 'own':
        ogb = K.sb([128, 4096], BF16, "m_og")
    ogT = K.sb([128, 32, 128], BF16, "m_ogT"); oaT = K.sb([128, 16, 128], BF16, "m_oaT")
    mg = K.sb([128, D], F32, "m_mg"); t1 = K.sb([128, 512], F32, "m_t1"); mgb = K.sb([128, D], BF16, "m_mgb"); mgT = K.sb([128, 16, 128], BF16, "m_mgT")
    ysb = K.sb([128, D], F32, "m_y"); gp = K.sb([128, D], F32, "m_gp")
    ssq = K.sb([128, 8], F32, "m_ssq"); junk = K.sb([128, 512], BF16, "m_junk")
    xs = K.sb([128, D], BF16, "m_xs"); ss = K.sb([128, 2], F32, "m_ss"); u2T = K.sb([128, 16, 128], BF16, "m_u2T")
    actT = K.sb([128, 48, 128], BF16, "m_actT")
    cv = [K.sb([128, 4, 126], F32, f"m_cv{i}") for i in range(2)]
    sg = [K.sb([128, 2, 126], F32, f"m_sg{i}") for i in range(2)]
    if ogsrc[0] == 'own':
        ogown = ogsrc[1]
        P.op('sp', lambda e: e.dma_start(out=ogb[:], in_=ogown[r0:r0 + 128, :]), writes=[ogb], dma=True)
    P.op('sp', lambda e: e.dma_start(out=gp[:], in_=gpost[:]), writes=[gp], dma=True)
    if ogsrc[0] == 'own':
        emit_T(P, W, cb, lambda i: ogb[:, i * 128:(i + 1) * 128], ogb, ogT, 32)
    else:
        _, og_d, sel_s, Lp_ = ogsrc
        win0 = A0 + BSTR * (NCORE * j)
        ogw = [K.sb([128, 8, 1024], BF16, f"m_ogw{i}") for i in range(2)]
        psg = W.ps_mm
        for fq in range(4):
            t_ = ogw[fq % 2]
            valid = []
            P.op('pool', lambda e, t_=t_: e.memset(t_[:], 0.0), writes=[t_])
            for kc in range(8):
                rlo = win0 + kc * 128
                nv = max(0, min(128, Lp_ - rlo))
                if nv == 0: continue
                valid.append(kc)
                P.op('sp', lambda e, t_=t_, kc=kc, rlo=rlo, nv=nv, fq=fq: e.dma_start(out=t_[0:nv, kc, :], in_=og_d[rlo:rlo + nv, fq * 1024:(fq + 1) * 1024]),
                     reads=[og_d, t_], writes=[t_], dma=True)
            for half in range(2):
                W.nm += 1; pm = psg[W.nm % 3]
                for q in range(4):
                    fc = half * 4 + q
                    for kc in valid:
                        P.op('pe', lambda e, pm=pm, q=q, fc=fc, kc=kc, t_=t_: e.matmul(pm[:, q * 128:(q + 1) * 128], lhsT=t_[:, kc, fc * 128:(fc + 1) * 128], rhs=sel_s[:, kc, :],
                                                                                  start=(kc == valid[0]), stop=(kc == valid[-1])), reads=[t_, sel_s], writes=[pm])
                i0 = fq * 8 + half * 4
                if valid:
                    P.op('act', lambda e, pm=pm, i0=i0: e.activation(out=ogT[:, i0:i0 + 4, :], in_=pm[:, 0:512], func=AF.Copy), reads=[pm], writes=[ogT])
                else:
                    P.op('pool', lambda e, i0=i0: e.memset(ogT[:, i0:i0 + 4, :], 0.0), writes=[ogT])
    emit_T(P, W, cb, lambda i: oatt[:, i * 128:(i + 1) * 128], oatt, oaT, 16)
    for ct in range(4):
        c0 = ct * 512
        pm = W.linear(ogT, wbg_b, 32, c0, 512)
        P.op('dve', lambda e, pm=pm, c0=c0: e.tensor_tensor(out=mg[:, c0:c0 + 512], in0=pm[:, 0:512], in1=sig[:, 0, c0:c0 + 512], op=ALU.mult), reads=[pm, sig, mg], writes=[mg])
        pm = W.linear(oaT, wba_b, 16, c0, 512)
        P.op('dve', lambda e, pm=pm, c0=c0: e.tensor_tensor(out=t1[:], in0=pm[:, 0:512], in1=sig[:, 1, c0:c0 + 512], op=ALU.mult), reads=[pm, sig], writes=[t1])
        P.op('pool', lambda e, c0=c0: e.tensor_tensor(out=mgb[:, c0:c0 + 512], in0=mg[:, c0:c0 + 512], in1=t1[:], op=ALU.add), reads=[mg, t1, mgb], writes=[mgb])
    emit_T(P, W, cb, lambda i: mgb[:, i * 128:(i + 1) * 128], mgb, mgT, 16)

    def post_norm_residual(src_T, wd, KC, gain_tl, res_in, res_out):
        for ct in range(4):
            c0 = ct * 512
            pm = W.linear(src_T, wd, KC, c0, 512)
            P.op('act', lambda e, pm=pm, c0=c0: e.activation(out=ysb[:, c0:c0 + 512], in_=pm[:, 0:512], func=AF.Copy), reads=[pm, ysb], writes=[ysb])
            P.op('act', lambda e, pm=pm, ct=ct: e.activation(out=junk[:], in_=pm[:, 0:512], func=AF.Square, accum_out=ssq[:, ct:ct + 1]), reads=[pm, junk, ssq], writes=[junk, ssq])
        P.op('dve', lambda e: e.tensor_reduce(out=ssq[:, 4:5], in_=ssq[:, 0:4], axis=mybir.AxisListType.X, op=ALU.add), reads=[ssq], writes=[ssq])
        P.op('act', lambda e: e.activation(out=ssq[:, 5:6], in_=ssq[:, 4:5], func=AF.Sqrt, scale=1.0 / D, bias=epsc[:, 0:1]), reads=[ssq, epsc], writes=[ssq])
        P.op('dve', lambda e: e.reciprocal(out=ssq[:, 5:6], in_=ssq[:, 5:6]), reads=[ssq], writes=[ssq])
        P.op('dve', lambda e: e.scalar_tensor_tensor(out=ysb[:], in0=ysb[:], scalar=ssq[:, 5:6], in1=gain_tl[:], op0=ALU.mult, op1=ALU.mult), reads=[ysb, ssq, gain_tl], writes=[ysb])
        P.op('pool', lambda e: e.tensor_tensor(out=res_out[:], in0=res_in[:], in1=ysb[:], op=ALU.add), reads=[res_in, ysb], writes=[res_out])

    post_norm_residual(mgT, wout_b, 16, gp, xo, xo)
    if dbg_h1 is not None:
        P.op('sp', lambda e: e.dma_start(out=dbg_h1[r0:r0 + 128, :], in_=xo[:]), reads=[xo], writes=[dbg_h1], dma=True)
    P.op('sp', lambda e: e.dma_start(out=gp[:], in_=g2post[:]), reads=[gp], writes=[gp], dma=True)
    emit_norm_T(P, W, cb, epsc, xo, xs, ss, u2T)
    P.op('pool', lambda e: e.memset(actT[:], 0.0), writes=[actT])
    psu = W.ps_mm
    nu = 0
    for g in range(24):
        wt = W.wt[W.nw % 3]; W.nw += 1
        P.op('sp', lambda e, wt=wt, g=g: e.dma_start(out=wt[:], in_=wup_b[:, g, :, :]), reads=[wup_b], writes=[wt], dma=True)
        pm = psu[nu % 3]; c_v = cv[nu % 2]; s_g = sg[nu % 2]; nu += 1
        for cc in range(4):
            for kc in range(16):
                P.op('pe', lambda e, pm=pm, wt=wt, cc=cc, kc=kc: e.matmul(pm[:, cc * 128:(cc + 1) * 128], lhsT=wt[:, kc, cc * 128:(cc + 1) * 128], rhs=u2T[:, kc, :], start=(kc == 0), stop=(kc == 15)),
                     reads=[wt, u2T], writes=[pm])
        for cc in range(4):
            ch = (2 * g + cc) if cc < 2 else (48 + 2 * g + cc - 2)
            P.op('dve', lambda e, pm=pm, c_v=c_v, cc=cc, ch=ch: e.tensor_scalar(out=c_v[:, cc, :], in0=pm[:, cc * 128:cc * 128 + 126], scalar1=fcw_s[:, ch, 0:1], scalar2=fcb_s[:, ch:ch + 1], op0=ALU.mult, op1=ALU.add),
                 reads=[pm, fcw_s, fcb_s, c_v], writes=[c_v])
            for tp in (1, 2):
                P.op('dve', lambda e, pm=pm, c_v=c_v, cc=cc, ch=ch, tp=tp: e.scalar_tensor_tensor(out=c_v[:, cc, :], in0=pm[:, cc * 128 + tp:cc * 128 + tp + 126], scalar=fcw_s[:, ch, tp:tp + 1], in1=c_v[:, cc, :], op0=ALU.mult, op1=ALU.add),
                     reads=[pm, fcw_s, c_v], writes=[c_v])
        P.op('act', lambda e, c_v=c_v, s_g=s_g: e.activation(out=s_g[:], in_=c_v[:, 0:2, :], func=AF.Silu), reads=[c_v], writes=[s_g])
        P.op('pool', lambda e, c_v=c_v, s_g=s_g, g=g: e.tensor_tensor(out=actT[:, 2 * g:2 * g + 2, 2:128], in0=s_g[:], in1=c_v[:, 2:4, :], op=ALU.mult), reads=[c_v, s_g, actT], writes=[actT])
    post_norm_residual(actT, wdn_b, 48, gp, xo, ysb)
    P.op('sp', lambda e: e.dma_start(out=out_o[j, :, :], in_=ysb[2:128, :]), reads=[ysb], writes=[out_o], dma=True)


def own_rows(core, NS):
    rows = np.zeros(NS * 128, np.int64)
    for j in range(NS):
        s_ = NCORE * j + core
        rows[j * 128:(j + 1) * 128] = A0 + BSTR * s_ + np.arange(128)
    return rows


def prep2_common(inp):
    f = np.float32
    o = np.cumsum([0, 2048, 2048, 4096, 4096, 32, 32, 2048, 256, 256, 2048, 128, 16, 2048, 2048])
    w_in = inp['w_in'][0]
    cols = np.concatenate([np.arange(o[6], o[7]), np.arange(o[9], o[10]), np.arange(o[12], o[13]), np.arange(o[13], o[14]), np.arange(o[11], o[12])])
    cm = {
        "w_c": np.ascontiguousarray(w_in[:, cols]),
        "wbg": np.ascontiguousarray(inp['w_branch_gdn'][0]), "wba": np.ascontiguousarray(inp['w_branch_att'][0]),
        "wout": np.ascontiguousarray(inp['w_out'][0]), "wup": np.ascontiguousarray(inp['w_up'][0]), "wdn": np.ascontiguousarray(inp['w_down'][0]),
        "gpre": np.ascontiguousarray(inp['mix_pre_g'][0].reshape(16, 128).T), "g2pre": np.ascontiguousarray(inp['ffn_pre_g'][0].reshape(16, 128).T),
        "gpost": np.ascontiguousarray(np.broadcast_to(inp['mix_post_g'][0][None, :], (128, D))).astype(f),
        "g2post": np.ascontiguousarray(np.broadcast_to(inp['ffn_post_g'][0][None, :], (128, D))).astype(f),
        "fcw": np.ascontiguousarray(inp['ffn_conv_w'][0].T.reshape(96, 128, 3).transpose(1, 0, 2)),
        "fcb": np.ascontiguousarray(inp['ffn_conv_b'][0].reshape(96, 128).T),
        "cst": make_consts(),
        "iota": np.ascontiguousarray(np.broadcast_to(np.arange(NIOTA, dtype=f)[None, :], (128, NIOTA))),
    }
    return cm


def prep2(cm, hfull, og_all, KT, Vt, ikT, Lp, NS, core):
    f = np.float32
    nblk = Lp // 128
    rows = own_rows(core, NS)
    ok = rows < Lp
    rc = np.minimum(rows, Lp - 1)
    hown = np.where(ok[:, None], hfull[rc], 0).astype(f)
    ogown = None
    if og_all is not None:
        ogown = og_all[rc].copy(); ogown[~ok] = 0
    pos = np.maximum(rows - PADF, 0)
    qrel = np.zeros((128, NS), f)
    for j in range(NS):
        kd, kb_end = slot_geom(j, nblk)
        qrel[:, j] = rows[j * 128:(j + 1) * 128] - kd * 128
    m = dict(cm)
    m.update({"hown": hown, "ogown": ogown, "KT": KT, "Vt": Vt, "ikT": ikT, "ropeo": rope_table(pos), "qrel": qrel})
    return m


def kernel(**inputs):
    inp = {k: np.asarray(v) for k, v in inputs.items()}
    return kernel_fused(inp)


def kernel_unfused(**inputs):
    inp = {k: np.asarray(v) for k, v in inputs.items()}
    x = inp['x']
    SEQ = x.shape[1]
    Lp = PADF + NMETA + SEQ
    assert Lp % 128 == 0
    L = NMETA + SEQ
    topk = min(256, L // 4)
    nb, NS = blocks_for(SEQ)
    cores = list(range(NCORE))
    nc1 = build_prog1(Lp)
    ims = [prep1(inp, Lp, c) for c in cores]
    hfull = ims[0]["hfull"]
    for m in ims[1:]:
        m["hfull"] = hfull
    r1 = run_bass_kernel_spmd(nc1, ims, core_ids=cores).results
    og_all = np.concatenate([np.asarray(r1[c]["og"]) for c in cores], axis=1)
    KT = np.asarray(r1[0]["KT"]); Vt = np.asarray(r1[0]["Vt"]); ikT = np.asarray(r1[0]["ikT"])
    del ims, r1
    nc2 = build_prog2(Lp, NS, topk)
    cm = prep2_common(inp)
    ims2 = [prep2(cm, hfull, og_all, KT, Vt, ikT, Lp, NS, c) for c in cores]
    r2 = run_bass_kernel_spmd(nc2, ims2, core_ids=cores).results
    out = np.zeros((1, SEQ, D), np.float32)
    for c in cores:
        o = np.asarray(r2[c]["out"])
        for j in range(NS):
            s_ = NCORE * j + c
            t_lo = BSTR * s_; t_hi = min(SEQ, t_lo + BSTR)
            if t_hi > t_lo:
                out[0, t_lo:t_hi] = o[j, :t_hi - t_lo]
    return out


def make_sel(core):
    import ml_dtypes
    sel = np.zeros((128, 8, 128), np.float32)
    for r in range(128):
        w = BSTR * core + r
        sel[w % 128, w // 128, r] = 1.0
    return sel.astype(ml_dtypes.bfloat16)


def kernel_fused(inp):
    x = inp['x']; SEQ = x.shape[1]
    Lp = PADF + NMETA + SEQ
    L = NMETA + SEQ
    topk = min(256, L // 4)
    nb, NS = blocks_for(SEQ)
    cores = list(range(NCORE))
    p1 = [prep1(inp, Lp, c) for c in cores]
    hfull = p1[0]["hfull"]
    w_a = np.concatenate([p["w_a"] for p in p1], 0); cw = np.concatenate([p["cw"] for p in p1], 0); hp = np.concatenate([p["hp"] for p in p1], 0)
    cm = prep2_common(inp)
    cm.update({"hfull": hfull, "w_a": w_a, "cw": cw, "hp": hp, "ng": p1[0]["ng"], "rope": p1[0]["rope"]})
    del p1
    ims = []
    for c in cores:
        m = prep2(cm, hfull, None, None, None, None, Lp, NS, c)
        for k in ("ogown", "KT", "Vt", "ikT"): m.pop(k)
        m["sel"] = make_sel(c)
        ims.append(m)
    nc = build_prog2(Lp, NS, topk, fused=True)
    r2 = run_bass_kernel_spmd(nc, ims, core_ids=cores).results
    out = np.zeros((1, SEQ, D), np.float32)
    for c in cores:
        o = np.asarray(r2[c]["out"])
        for j in range(NS):
            s_ = NCORE * j + c
            t_lo = BSTR * s_; t_hi = min(SEQ, t_lo + BSTR)
            if t_hi > t_lo:
                out[0, t_lo:t_hi] = o[j, :t_hi - t_lo]
    return out
```

```python
import numpy as np
import concourse.bass as bass
import concourse.mybir as mybir
from concourse.bass_utils import run_bass_kernel_spmd
from contextlib import ExitStack

F32 = mybir.dt.float32; BF16 = mybir.dt.bfloat16; I32 = mybir.dt.int32
AF = mybir.ActivationFunctionType; ALU = mybir.AluOpType

D = 2048
NMETA = 16
PADF = 112
EPS = 1e-6
NCORE = 8
HPC = 4
NFF = 6144
ROPE_THETA = 500000.0


class Buf:
    __slots__ = ('w', 'r', 'multi')
    def __init__(self, multi=False):
        self.w = {}; self.r = {}; self.multi = multi


class Tl:
    def __init__(self, t, multi=False):
        self.t = t; self.b = Buf(multi)
    def __getitem__(self, k):
        return self.t[k]


class SMView(Tl):
    def __init__(self, T, c):
        self.t = T.t; self.b = T.b; self.c = c
    def __getitem__(self, k):
        return self.t[(k[0], self.c) + tuple(k[1:])]


class Prog:
    ENG = ('pe', 'act', 'dve', 'pool', 'sp')
    def __init__(self, nc, es, n_dma=12):
        self.nc = nc; self.es = es
        self.ops = {e: [] for e in self.ENG}
        self.cnt = {e: 0 for e in self.ENG}
        self.sem = {e: es.enter_context(nc.semaphore('s_' + e)) for e in ('pe', 'act', 'dve', 'pool')}
        self.seen = {e: {} for e in self.ENG}
        self.dsem = {q: [[es.enter_context(nc.semaphore(f'd_{q}{i}')), 0] for i in range(n_dma)] for q in ('sp', 'pool')}
        self.drr = {q: 0 for q in ('sp', 'pool')}
        self.nops = 0

    def op(self, eng, fn, reads=(), writes=(), dma=False):
        deps = {}
        def add(ev):
            s, v = ev
            k = id(s)
            if k not in deps or deps[k][1] < v: deps[k] = (s, v)
        for t in reads:
            b = t.b if isinstance(t, Tl) else t
            for ev in b.w.values(): add(ev)
        for t in writes:
            b = t.b if isinstance(t, Tl) else t
            if not b.multi:
                for ev in b.w.values(): add(ev)
                for ev in b.r.values(): add(ev)
        if dma:
            slots = self.dsem[eng]
            slot = slots[self.drr[eng] % len(slots)]; self.drr[eng] += 1
            if slot[1] > 0: add((slot[0], slot[1]))
            slot[1] += 16
            ev = (slot[0], slot[1]); inc = 16
        else:
            self.cnt[eng] += 1
            ev = (self.sem[eng], self.cnt[eng]); inc = 1
        waits = []
        seen = self.seen[eng]
        own = id(self.sem['pe']) if eng == 'pe' else None
        for k, (s, v) in deps.items():
            if k == own: continue
            if seen.get(k, 0) >= v: continue
            seen[k] = v; waits.append((s, v))
        for t in writes:
            b = t.b if isinstance(t, Tl) else t
            if b.multi:
                b.w[id(ev[0])] = ev
            else:
                b.w = {id(ev[0]): ev}; b.r = {}
        for t in reads:
            b = t.b if isinstance(t, Tl) else t
            if not b.multi:
                b.r[id(ev[0])] = ev
        self.ops[eng].append((waits, fn, ev, inc))
        self.nops += 1
        return ev

    def emit(self):
        nc = self.nc
        fin = []
        for e in ('pe', 'act', 'dve', 'pool'):
            if self.cnt[e]: fin.append((self.sem[e], self.cnt[e]))
        for q in self.dsem:
            for s, v in self.dsem[q]:
                if v: fin.append((s, v))
        bar = getattr(self, 'barrier', [])
        def run(name, e):
            for s, v in bar: e.wait_ge(s, v)
            for waits, fn, ev, inc in self.ops[name]:
                for (s, v) in waits: e.wait_ge(s, v)
                ins = fn(e)
                ins.then_inc(ev[0], inc)
            if name == 'sp':
                for s, v in fin: e.wait_ge(s, v)
            self.ops[name] = []
        self.barrier = fin
        with nc.Block() as block:
            @block.tensor
            def _(e): run('pe', e)
            @block.scalar
            def _(e): run('act', e)
            @block.vector
            def _(e): run('dve', e)
            @block.gpsimd
            def _(e): run('pool', e)
            @block.sync
            def _(e): run('sp', e)


class Ctx:
    def __init__(self, nc, es):
        self.nc = nc; self.es = es; self.P = Prog(nc, es)
        self._n = 0
    def scope(self):
        st = ExitStack()
        if not hasattr(self, '_stk'): self._stk = []
        self._stk.append(self.es); self.es = st
        return st
    def unscope(self):
        self.es = self._stk.pop()
    def sb(self, shape, dt, name=None):
        self._n += 1
        return Tl(self.es.enter_context(self.nc.sbuf_tensor(f"{name or 'sb'}_{self._n}", list(shape), dt)))
    def ps(self, shape, dt=F32, name=None):
        self._n += 1
        return Tl(self.es.enter_context(self.nc.psum_tensor(f"{name or 'ps'}_{self._n}", list(shape), dt)))
    def din(self, name, shape, dt=F32):
        return Tl(self.nc.dram_tensor(name, list(shape), dt, kind="ExternalInput").ap(), multi=True)
    def dout(self, name, shape, dt=F32):
        return Tl(self.nc.dram_tensor(name, list(shape), dt, kind="ExternalOutput").ap(), multi=True)
    def dtmp(self, name, shape, dt=BF16):
        return Tl(self.nc.dram_tensor(name, list(shape), dt, kind="Internal").ap(), multi=True)


CI_ID, CI_TRIU, CI_NEGM, CI_OFFD, CI_ONES = 0, 1, 2, 3, 4
def make_consts():
    c = np.zeros((128, 5, 128), np.float32)
    j = np.arange(128)[:, None]; i = np.arange(128)[None, :]
    c[:, CI_ID] = (i == j)
    c[:, CI_TRIU] = (j <= i)
    c[:, CI_NEGM] = np.where(i >= j, 0.0, -1e9)
    c[:, CI_OFFD] = (i != j)
    c[:, CI_ONES] = 1.0
    return c


def rope_table(pos):
    half = 16
    inv = ROPE_THETA ** (-np.arange(half, dtype=np.float32) / half)
    ang = pos.astype(np.float32)[:, None] * inv[None, :].astype(np.float32)
    return np.concatenate([np.cos(ang), np.sin(ang)], axis=1).astype(np.float32)


NA_FM = 1024
NA_TM = 512 + 8 + 256 + 256 + 128
NA = NA_FM + NA_TM


def emit_gdn_all(K, Lp, NG, hfull, w_a, gpre, cw, hp, ng, cst, rope, og_o, KT_o, V_o, ikT_o, scr, dbgt=None):
    P = K.P; nblk = Lp // 128; dbg = dbgt is not None
    if dbg: dbg_gb, dbg_q, dbg_k, dbg_v = dbgt
    if True:
        cf = K.sb([128, 5, 128], F32, "cf")
        P.op('sp', lambda e: e.dma_start(out=cf[:], in_=cst[:]), writes=[cf], dma=True)
        epsc = K.sb([128, 1], F32, "epsc")
        P.op('dve', lambda e: e.memset(epsc[:], EPS), writes=[epsc])
        cb = K.sb([128, 5, 128], BF16, "cb")
        P.op('dve', lambda e: e.tensor_copy(out=cb[:], in_=cf[:]), reads=[cf], writes=[cb])
        gpre_s = K.sb([128, 16], F32); cw_s = K.sb([128, 8, 4], F32); hp_s = K.sb([128, 3, HPC], F32); ng_s = K.sb([128, 128], F32)
        for dst, src in ((gpre_s, gpre), (ng_s, ng)):
            P.op('sp', lambda e, dst=dst, src=src: e.dma_start(out=dst[:], in_=src[:]), writes=[dst], dma=True)
        nea = K.sb([128, HPC], F32)
        ut_d = K.dtmp("ut_d", [128, 16, Lp]) if NG > 1 else None
        NPAR = min(2, NG)
        gb_alls = [K.sb([128, nblk, 8], F32, f"gb_all{i}") for i in range(NPAR)]
        pend = []

        for g in range(NG):
            gb_all = gb_alls[g % NPAR]
            qT_d, kT_d, k_d, v_d, sz_d = scr[g % NPAR]
            P.op('sp', lambda e, g=g: e.dma_start(out=cw_s[:], in_=cw[g]), writes=[cw_s], dma=True)
            P.op('sp', lambda e, g=g: e.dma_start(out=hp_s[:], in_=hp[g]), writes=[hp_s], dma=True)
            P.op('act', lambda e: e.activation(out=nea[:], in_=hp_s[:, 0, :], func=AF.Exp), reads=[hp_s], writes=[nea])
            P.op('dve', lambda e: e.tensor_scalar(out=nea[:], in0=nea[:], scalar1=-1.0, scalar2=None, op0=ALU.mult), reads=[nea], writes=[nea])
            scA = K.scope()
            wA = K.sb([128, 16, NA], BF16, "wA")
            wst = [K.sb([128, NA], F32, f"wst{i}") for i in range(1)]
            w_a_v = w_a.t[g].rearrange("(kc p) n -> p kc n", p=128)
            for kc in range(16):
                st = wst[0]
                P.op('sp', lambda e, st=st, kc=kc: e.dma_start(out=st[:], in_=w_a_v[:, kc, :]), writes=[st], dma=True)
                P.op('act', lambda e, st=st, kc=kc: e.activation(out=wA[:, kc, :], in_=st[:], func=AF.Copy, scale=gpre_s[:, kc:kc + 1]),
                     reads=[st, gpre_s], writes=[wA])

            TB = 512
            xf = [K.sb([128, D], F32, f"xf{i}") for i in range(2)]
            xs = [K.sb([128, D], BF16, f"xs{i}") for i in range(2)]
            ss = [K.sb([128, 2], F32, f"ss{i}") for i in range(2)]
            uTs = [K.sb([128, 16, TB], BF16, f"uT{i}") for i in range(2)]
            pre = K.sb([128, 8, 3 + TB], F32, "pre")
            pre_b = [Buf() for _ in range(8)]
            P.op('dve', lambda e: e.memset(pre[:], 0.0), writes=pre_b)
            cv = [K.sb([128, TB], F32, f"cv{i}") for i in range(2)]
            sl = [K.sb([128, TB], F32, f"sl{i}") for i in range(2)]
            sq = [K.sb([128, TB], BF16, f"sq{i}") for i in range(2)]
            rrs = [K.sb([128, TB], F32, f"rr{i}") for i in range(2)]
            fmTs = [K.sb([128, 8, TB], BF16, f"fmT{i}") for i in range(2)]
            tm_kv = [K.sb([128, 6, 128], BF16, f"tmkv{i}") for i in range(2)]
            ztm = [K.sb([128, 512], BF16, f"ztm{i}") for i in range(2)]
            bars = [K.sb([128, 4, 8], F32, f"bar{i}") for i in range(2)]
            vst = [K.sb([128, 256], BF16, f"vst{i}") for i in range(2)]
            kif = [K.sb([128, 3, 128], F32, f"kif{i}") for i in range(2)]
            kib = [K.sb([128, 3, 128], BF16, f"kib{i}") for i in range(2)]
            rt = [K.sb([128, 32], F32, f"rt{i}") for i in range(2)]
            rtmp = [K.sb([128, 4, 3, 16], F32, f"rtmp{i}") for i in range(2)]
            kiT = [K.sb([128, 3, 128], BF16, f"kiT{i}") for i in range(2)]
            ps_tr = [K.ps([128, 4, 128], BF16, f"pstr{i}") for i in range(2)]
            ps_mm = [K.ps([128, 512], F32, f"psmm{i}") for i in range(3)]
            ps_n = K.ps([128, 512], F32, "psn")
            nmm = [0]
            def next_mm():
                nmm[0] += 1
                return ps_mm[nmm[0] % 3]
            ntr = [0]
            def next_tr():
                ntr[0] += 1
                return ps_tr[ntr[0] % 2]

            nsb = (Lp + TB - 1) // TB
            blk = 0
            for sbi in range(nsb):
                t0 = sbi * TB
                n_sub = min(4, (Lp - t0) // 128)
                TBn = n_sub * 128
                uT = uTs[sbi % 2]; fmT = fmTs[sbi % 2]; bar = bars[sbi % 2]
                if g > 0:
                    if sbi == 0:
                        P.op('sp', lambda e, uT=uT, t0=t0, TBn=TBn: e.dma_start(out=uT[:, :, 0:TBn], in_=ut_d[:, :, t0:t0 + TBn]), reads=[ut_d], writes=[uT], dma=True)
                    if sbi + 1 < nsb:
                        t1_ = (sbi + 1) * TB; TB1 = min(4, (Lp - t1_) // 128) * 128; uT1 = uTs[(sbi + 1) % 2]
                        P.op('sp', lambda e, uT1=uT1, t1_=t1_, TB1=TB1: e.dma_start(out=uT1[:, :, 0:TB1], in_=ut_d[:, :, t1_:t1_ + TB1]), reads=[ut_d], writes=[uT1], dma=True)
                for j in range(n_sub if g == 0 else 0):
                    b = sbi * 4 + j
                    x_f = xf[b % 2]; x_s = xs[b % 2]; s_s = ss[b % 2]
                    P.op('sp', lambda e, x_f=x_f, b=b: e.dma_start(out=x_f[:], in_=hfull[b * 128:(b + 1) * 128, :]), writes=[x_f], dma=True)
                    P.op('act', lambda e, x_f=x_f, s_s=s_s, x_s=x_s: e.activation(out=x_s[:], in_=x_f[:], func=AF.Square, accum_out=s_s[:, 0:1]),
                         reads=[x_f], writes=[x_s, s_s])
                    P.op('act', lambda e, s_s=s_s: e.activation(out=s_s[:, 1:2], in_=s_s[:, 0:1], func=AF.Sqrt, scale=1.0 / D, bias=epsc[:, 0:1]),
                         reads=[s_s, epsc], writes=[s_s])
                    P.op('dve', lambda e, s_s=s_s: e.reciprocal(out=s_s[:, 1:2], in_=s_s[:, 1:2]), reads=[s_s], writes=[s_s])
                    P.op('dve', lambda e, x_f=x_f, x_s=x_s, s_s=s_s: e.tensor_scalar(out=x_s[:], in0=x_f[:], scalar1=s_s[:, 1:2], scalar2=None, op0=ALU.mult),
                         reads=[x_f, s_s], writes=[x_s])
                    for g4 in range(4):
                        pt = next_tr()
                        for q in range(4):
                            kc = g4 * 4 + q
                            P.op('pe', lambda e, pt=pt, q=q, kc=kc, x_s=x_s: e.transpose(out=pt[:, q, :], in_=x_s[:, kc * 128:(kc + 1) * 128], identity=cb[:, CI_ID, :]),
                                 reads=[x_s, cb], writes=[pt])
                        P.op('act' if g4 % 2 else 'dve',
                             (lambda e, uT=uT, pt=pt, g4=g4, j=j: e.activation(out=uT[:, g4 * 4:(g4 + 1) * 4, j * 128:(j + 1) * 128], in_=pt[:], func=AF.Copy)) if g4 % 2 else
                             (lambda e, uT=uT, pt=pt, g4=g4, j=j: e.tensor_copy(out=uT[:, g4 * 4:(g4 + 1) * 4, j * 128:(j + 1) * 128], in_=pt[:])),
                             reads=[pt], writes=[uT])
                if g == 0 and NG > 1:
                    P.op('sp', lambda e, uT=uT, t0=t0, TBn=TBn: e.dma_start(out=ut_d[:, :, t0:t0 + TBn], in_=uT[:, :, 0:TBn]), reads=[uT], writes=[ut_d], dma=True)
                pending = []
                for ch in range(8):
                    rr = rrs[ch % 2]; pre_c = pre_b[ch]
                    pm = next_mm()
                    for kc in range(16):
                        P.op('pe', lambda e, uT=uT, pm=pm, kc=kc, ch=ch, TBn=TBn: e.matmul(pm[:, 0:TBn], lhsT=wA[:, kc, ch * 128:(ch + 1) * 128], rhs=uT[:, kc, 0:TBn],
                                                                                  start=(kc == 0), stop=(kc == 15)),
                             reads=[wA, uT], writes=[pm])
                    P.op('act', lambda e, pm=pm, ch=ch, TBn=TBn: e.activation(out=pre[:, ch, 3:3 + TBn], in_=pm[:, 0:TBn], func=AF.Copy), reads=[pm], writes=[pre_c])
                    while pending: pending.pop(0)()
                    c_v = cv[ch % 2]; s_l = sl[ch % 2]; s_q = sq[ch % 2]
                    P.op('dve', lambda e, c_v=c_v, ch=ch, TBn=TBn: e.tensor_scalar(out=c_v[:, 0:TBn], in0=pre[:, ch, 0:TBn], scalar1=cw_s[:, ch, 0:1], scalar2=None, op0=ALU.mult),
                         reads=[pre_c, cw_s], writes=[c_v])
                    for tp in range(1, 4):
                        P.op('dve', lambda e, c_v=c_v, ch=ch, tp=tp, TBn=TBn: e.scalar_tensor_tensor(out=c_v[:, 0:TBn], in0=pre[:, ch, tp:tp + TBn], scalar=cw_s[:, ch, tp:tp + 1],
                                                                                                  in1=c_v[:, 0:TBn], op0=ALU.mult, op1=ALU.add),
                             reads=[pre_c, cw_s, c_v], writes=[c_v])
                    P.op('pool', lambda e, ch=ch, TBn=TBn: e.tensor_copy(out=pre[:, ch, 0:3], in_=pre[:, ch, TBn:TBn + 3]), reads=[pre_c], writes=[pre_c])
                    if ch >= 4:
                        P.op('act', lambda e, fmT=fmT, c_v=c_v, ch=ch, TBn=TBn: e.activation(out=fmT[:, ch, 0:TBn], in_=c_v[:, 0:TBn], func=AF.Silu), reads=[c_v], writes=[fmT])
                    else:
                        P.op('act', lambda e, c_v=c_v, s_l=s_l, TBn=TBn: e.activation(out=s_l[:, 0:TBn], in_=c_v[:, 0:TBn], func=AF.Silu), reads=[c_v], writes=[s_l])
                        def l2tail(ch=ch, s_l=s_l, s_q=s_q, TBn=TBn, rr=rr, fmT=fmT):
                            P.op('pool', lambda e, s_l=s_l, s_q=s_q, TBn=TBn: e.tensor_tensor(out=s_q[:, 0:TBn], in0=s_l[:, 0:TBn], in1=s_l[:, 0:TBn], op=ALU.mult),
                                 reads=[s_l], writes=[s_q])
                            P.op('pe', lambda e, s_q=s_q, TBn=TBn: e.matmul(ps_n[:, 0:TBn], lhsT=cb[:, CI_ONES, :], rhs=s_q[:, 0:TBn], start=True, stop=True),
                                 reads=[s_q, cb], writes=[ps_n])
                            P.op('act', lambda e, rr=rr, TBn=TBn: e.activation(out=rr[:, 0:TBn], in_=ps_n[:, 0:TBn], func=AF.Sqrt, bias=epsc[:, 0:1]), reads=[ps_n, epsc], writes=[rr])
                            P.op('dve', lambda e, rr=rr, TBn=TBn: e.reciprocal(out=rr[:, 0:TBn], in_=rr[:, 0:TBn]), reads=[rr], writes=[rr])
                            sc = (128 ** -0.5) if ch < 2 else 1.0
                            P.op('dve', lambda e, rr=rr, fmT=fmT, s_l=s_l, ch=ch, sc=sc, TBn=TBn: e.scalar_tensor_tensor(out=fmT[:, ch, 0:TBn], in0=s_l[:, 0:TBn], scalar=sc, in1=rr[:, 0:TBn],
                                                                                                      op0=ALU.mult, op1=ALU.mult),
                                 reads=[s_l, rr], writes=[fmT])
                        pending.append(l2tail)
                while pending: pending.pop(0)()
                for hq in range(2):
                    P.op('sp', lambda e, fmT=fmT, hq=hq, t0=t0, TBn=TBn: e.dma_start(out=qT_d[hq, :, t0:t0 + TBn], in_=fmT[:, hq, 0:TBn]), reads=[fmT], writes=[qT_d], dma=True)
                    P.op('sp', lambda e, fmT=fmT, hq=hq, t0=t0, TBn=TBn: e.dma_start(out=kT_d[hq, :, t0:t0 + TBn], in_=fmT[:, 2 + hq, 0:TBn]), reads=[fmT], writes=[kT_d], dma=True)
                if dbg:
                    for hq in range(2):
                        P.op('sp', lambda e, fmT=fmT, hq=hq, t0=t0, TBn=TBn: e.dma_start(out=dbg_q[hq, :, t0:t0 + TBn], in_=fmT[:, hq, 0:TBn]), reads=[fmT], writes=[dbg_q], dma=True)
                        P.op('sp', lambda e, fmT=fmT, hq=hq, t0=t0, TBn=TBn: e.dma_start(out=dbg_k[hq, :, t0:t0 + TBn], in_=fmT[:, 2 + hq, 0:TBn]), reads=[fmT], writes=[dbg_k], dma=True)
                for j in range(n_sub):
                    b = sbi * 4 + j
                    r0 = b * 128
                    pm = next_mm()
                    for kc in range(16):
                        P.op('pe', lambda e, uT=uT, pm=pm, kc=kc, j=j: e.matmul(pm[:, 0:512], lhsT=uT[:, kc, j * 128:(j + 1) * 128], rhs=wA[:, kc, NA_FM:NA_FM + 512],
                                                                       start=(kc == 0), stop=(kc == 15)), reads=[wA, uT], writes=[pm])
                    z_t = ztm[b % 2]
                    P.op('act', lambda e, pm=pm, z_t=z_t: e.activation(out=z_t[:], in_=pm[:, 0:512], func=AF.Silu), reads=[pm], writes=[z_t])
                    P.op('sp', lambda e, z_t=z_t, r0=r0: e.dma_start(out=sz_d[r0:r0 + 128, :], in_=z_t[:]), reads=[z_t], writes=[sz_d], dma=True)
                    pm = next_mm()
                    c0 = NA_FM + 512
                    nba = 264 if g == 0 else 8
                    for kc in range(16):
                        P.op('pe', lambda e, uT=uT, pm=pm, kc=kc, j=j, c0=c0, nba=nba: e.matmul(pm[:, 0:nba], lhsT=uT[:, kc, j * 128:(j + 1) * 128], rhs=wA[:, kc, c0:c0 + nba],
                                                                              start=(kc == 0), stop=(kc == 15)), reads=[wA, uT], writes=[pm])
                    v_s = vst[b % 2]
                    P.op('dve', lambda e, pm=pm, j=j, bar=bar: e.tensor_copy(out=bar[:, j, :], in_=pm[:, 0:8]), reads=[pm, bar], writes=[bar])
                    if g > 0: continue
                    P.op('act', lambda e, pm=pm, v_s=v_s: e.activation(out=v_s[:], in_=pm[:, 8:264], func=AF.Copy), reads=[pm], writes=[v_s])
                    if g == 0: P.op('sp', lambda e, v_s=v_s, r0=r0: e.dma_start(out=V_o[r0:r0 + 128, :], in_=v_s[:]), reads=[v_s], writes=[V_o], dma=True)
                    pm = next_mm()
                    c0 = NA_FM + 512 + 264
                    for kc in range(16):
                        P.op('pe', lambda e, uT=uT, pm=pm, kc=kc, j=j, c0=c0: e.matmul(pm[:, 0:384], lhsT=uT[:, kc, j * 128:(j + 1) * 128], rhs=wA[:, kc, c0:c0 + 384],
                                                                              start=(kc == 0), stop=(kc == 15)), reads=[wA, uT], writes=[pm])
                    k_f = kif[b % 2]; k_b = kib[b % 2]; r_t = rt[b % 2]; r_m = rtmp[b % 2]; k_T = kiT[b % 2]
                    P.op('sp', lambda e, r_t=r_t, r0=r0: e.dma_start(out=r_t[:], in_=rope[r0:r0 + 128, :]), writes=[r_t], dma=True)
                    P.op('act', lambda e, pm=pm, k_f=k_f: e.activation(out=k_f[:], in_=pm[:, 0:384], func=AF.Copy), reads=[pm], writes=[k_f])
                    P.op('act', lambda e, k_f=k_f, k_b=k_b: e.activation(out=k_b[:], in_=k_f[:], func=AF.Copy), reads=[k_f], writes=[k_b])
                    emit_rope(P, k_f, k_b, r_t, r_m, 3)
                    pt = next_tr()
                    for q in range(3):
                        P.op('pe', lambda e, pt=pt, q=q, k_b=k_b: e.transpose(out=pt[:, q, :], in_=k_b[:, q, :], identity=cb[:, CI_ID, :]), reads=[k_b, cb], writes=[pt])
                    P.op('act', lambda e, pt=pt, k_T=k_T: e.activation(out=k_T[:], in_=pt[:, 0:3, :], func=AF.Copy), reads=[pt], writes=[k_T])
                    for q in range(2 if g == 0 else 0):
                        P.op('sp', lambda e, k_T=k_T, q=q, r0=r0: e.dma_start(out=KT_o[q, :, r0:r0 + 128], in_=k_T[:, q, :]), reads=[k_T], writes=[KT_o], dma=True)
                    if g == 0: P.op('sp', lambda e, k_T=k_T, r0=r0: e.dma_start(out=ikT_o[:, r0:r0 + 128], in_=k_T[:, 2, :]), reads=[k_T], writes=[ikT_o], dma=True)
                b0 = sbi * 4
                P.op('act', lambda e, bar=bar, n_sub=n_sub: e.activation(out=bar[:, 0:n_sub, 0:4], in_=bar[:, 0:n_sub, 0:4], func=AF.Exp, scale=-1.0), reads=[bar], writes=[bar])
                P.op('dve', lambda e, bar=bar, n_sub=n_sub: e.tensor_tensor(out=bar[:, 0:n_sub, 4:8], in0=bar[:, 0:n_sub, 4:8], in1=hp_s[:, 1, :].unsqueeze(1).to_broadcast([128, n_sub, 4]), op=ALU.add),
                     reads=[bar, hp_s], writes=[bar])
                P.op('act', lambda e, bar=bar, n_sub=n_sub: e.activation(out=bar[:, 0:n_sub, 4:8], in_=bar[:, 0:n_sub, 4:8], func=AF.Exp), reads=[bar], writes=[bar])
                P.op('dve', lambda e, bar=bar, n_sub=n_sub: e.tensor_scalar(out=bar[:, 0:n_sub, :], in0=bar[:, 0:n_sub, :], scalar1=1.0, scalar2=None, op0=ALU.add), reads=[bar], writes=[bar])
                P.op('act', lambda e, bar=bar, n_sub=n_sub: e.activation(out=bar[:, 0:n_sub, 4:8], in_=bar[:, 0:n_sub, 4:8], func=AF.Ln), reads=[bar], writes=[bar])
                P.op('dve', lambda e, bar=bar, n_sub=n_sub, b0=b0: e.reciprocal(out=gb_all[:, b0:b0 + n_sub, 0:4], in_=bar[:, 0:n_sub, 0:4]), reads=[bar], writes=[gb_all])
                P.op('dve', lambda e, bar=bar, n_sub=n_sub, b0=b0: e.tensor_tensor(out=gb_all[:, b0:b0 + n_sub, 4:8], in0=bar[:, 0:n_sub, 4:8], in1=nea[:].unsqueeze(1).to_broadcast([128, n_sub, 4]), op=ALU.mult),
                     reads=[bar, nea, gb_all], writes=[gb_all])
                for j in range(n_sub):
                    b = sbi * 4 + j
                    r0 = b * 128
                    tk = tm_kv[b % 2]
                    pt = next_tr()
                    for q in range(4):
                        P.op('pe', lambda e, fmT=fmT, pt=pt, q=q, j=j: e.transpose(out=pt[:, q, :], in_=fmT[:, 4 + q, j * 128:(j + 1) * 128], identity=cb[:, CI_ID, :]),
                             reads=[fmT, cb], writes=[pt])
                    P.op('act', lambda e, pt=pt, tk=tk: e.activation(out=tk[:, 2:6, :], in_=pt[:], func=AF.Copy), reads=[pt], writes=[tk])
                    pt = next_tr()
                    for q in range(2):
                        P.op('pe', lambda e, fmT=fmT, pt=pt, q=q, j=j: e.transpose(out=pt[:, q, :], in_=fmT[:, 2 + q, j * 128:(j + 1) * 128], identity=cb[:, CI_ID, :]),
                             reads=[fmT, cb], writes=[pt])
                    P.op('dve', lambda e, pt=pt, tk=tk: e.tensor_copy(out=tk[:, 0:2, :], in_=pt[:, 0:2, :]), reads=[pt], writes=[tk])
                    P.op('sp', lambda e, tk=tk, r0=r0: e.dma_start(out=k_d[r0:r0 + 128, :], in_=tk[:, 0:2, :]), reads=[tk], writes=[k_d], dma=True)
                    P.op('sp', lambda e, tk=tk, r0=r0: e.dma_start(out=v_d[r0:r0 + 128, :], in_=tk[:, 2:6, :]), reads=[tk], writes=[v_d], dma=True)
                    if dbg:
                        P.op('sp', lambda e, tk=tk, r0=r0: e.dma_start(out=dbg_v[r0:r0 + 128, :], in_=tk[:, 2:6, :]), reads=[tk], writes=[dbg_v], dma=True)
            if dbg:
                P.op('sp', lambda e: e.dma_start(out=dbg_gb[:], in_=gb_all[:]), reads=[gb_all], writes=[dbg_gb], dma=True)

            P.emit()
            K.unscope(); scA.close()
            pend.append((gb_all, scr[g % NPAR], og_o, g * HPC * 128))
            if len(pend) == NPAR or g == NG - 1:
                scB = K.scope()
                emit_gdn_multi(K, nblk, cf, cb, epsc, ng_s, pend)
                P.emit()
                K.unscope(); scB.close()
                pend = []
    return cf, cb, epsc


def build_prog1(Lp, dbg=False, NG=1):
    nblk = Lp // 128
    nc = bass.Bass("TRN2", target_bir_lowering=False)
    es = ExitStack()
    with es:
        K = Ctx(nc, es); P = K.P
        hfull = K.din("hfull", [Lp, D])
        w_a = K.din("w_a", [NG, D, NA]); gpre = K.din("gpre", [128, 16]); cw = K.din("cw", [NG, 128, 8, 4]); hp = K.din("hp", [NG, 128, 3, HPC])
        ng = K.din("ng", [128, 128]); cst = K.din("cst", [128, 5, 128]); rope = K.din("rope", [Lp, 32])
        og_o = K.dout("og", [Lp, NG * HPC * 128], BF16)
        KT_o = K.dout("KT", [2, 128, Lp], BF16); V_o = K.dout("Vt", [Lp, 2 * 128], BF16); ikT_o = K.dout("ikT", [128, Lp], BF16)
        scr = gdn_scratch(K, Lp)
        dbgt = None
        if dbg:
            dbgt = (K.dout("dbg_gb", [128, nblk, 8]), K.dout("dbg_qT", [2, 128, Lp], BF16), K.dout("dbg_kT", [2, 128, Lp], BF16), K.dout("dbg_v", [Lp, 512], BF16))
        emit_gdn_all(K, Lp, NG, hfull, w_a, gpre, cw, hp, ng, cst, rope, og_o, KT_o, V_o, ikT_o, scr, dbgt)
    return nc


def gdn_scratch(K, Lp, n=2):
    return [(K.dtmp(f"qT_d{i}", [2, 128, Lp]), K.dtmp(f"kT_d{i}", [2, 128, Lp]), K.dtmp(f"k_d{i}", [Lp, 256]), K.dtmp(f"v_d{i}", [Lp, 512]), K.dtmp(f"sz_d{i}", [Lp, 512]))
            for i in range(n)]


def prep1(inp, Lp, core):
    f = np.float32
    x = inp['x'][0]; SEQ = x.shape[0]
    hfull = np.zeros((Lp, D), f)
    hfull[PADF:PADF + NMETA] = inp['meta_tokens']; hfull[PADF + NMETA:PADF + NMETA + SEQ] = x
    w_in = inp['w_in'][0]
    o = np.cumsum([0, 2048, 2048, 4096, 4096, 32, 32, 2048, 256, 256, 2048, 128, 16, 2048, 2048])
    gq, gk, gv, gz, gb, ga, aq, ak, av, iq, ik, iw, g1, g2 = [slice(o[i], o[i + 1]) for i in range(14)]
    c = core
    cols = np.concatenate([np.arange(o[0] + 256 * c, o[0] + 256 * c + 256), np.arange(o[1] + 256 * c, o[1] + 256 * c + 256),
                           np.arange(o[2] + 512 * c, o[2] + 512 * c + 512), np.arange(o[3] + 512 * c, o[3] + 512 * c + 512),
                           np.arange(o[4] + 4 * c, o[4] + 4 * c + 4), np.arange(o[5] + 4 * c, o[5] + 4 * c + 4),
                           np.arange(o[8], o[9]), np.arange(o[7], o[8]), np.arange(o[10], o[11])])
    w_a = np.ascontiguousarray(w_in[:, cols])
    gpre = np.ascontiguousarray(inp['mix_pre_g'][0].reshape(16, 128).T)
    cwf = inp['gdn_conv_w'][0]
    ccols = np.concatenate([np.arange(256 * c, 256 * c + 256), np.arange(2048 + 256 * c, 2048 + 256 * c + 256),
                            np.arange(4096 + 512 * c, 4096 + 512 * c + 512)])
    cw = np.ascontiguousarray(cwf[:, ccols].T.reshape(8, 128, 4).transpose(1, 0, 2))
    hp = np.zeros((128, 3, HPC), f)
    hp[:, 0, :] = inp['gdn_a_log'][0][4 * c:4 * c + 4][None, :]
    hp[:, 1, :] = inp['gdn_dt_bias'][0][4 * c:4 * c + 4][None, :]
    ng = np.ascontiguousarray(np.broadcast_to(inp['gdn_norm_g'][0][None, :], (128, 128))).astype(f)
    pos = np.maximum(np.arange(Lp) - PADF, 0)
    return {"hfull": hfull, "w_a": w_a[None], "gpre": gpre, "cw": cw[None], "hp": hp[None], "ng": ng, "cst": make_consts(), "rope": rope_table(pos)}


def emit_rope(P, xf, xb, r_t, r_m, nh):
    cosb = lambda: r_t[:, 0:16].unsqueeze(1).to_broadcast([128, nh, 16])
    sinb = lambda: r_t[:, 16:32].unsqueeze(1).to_broadcast([128, nh, 16])
    x1 = lambda: xf[:, 0:nh, 0:16]
    x2 = lambda: xf[:, 0:nh, 16:32]
    P.op('dve', lambda e: e.tensor_tensor(out=r_m[:, 0, 0:nh, :], in0=x1(), in1=cosb(), op=ALU.mult), reads=[xf, r_t], writes=[r_m])
    P.op('dve', lambda e: e.tensor_tensor(out=r_m[:, 1, 0:nh, :], in0=x2(), in1=sinb(), op=ALU.mult), reads=[xf, r_t, r_m], writes=[r_m])
    P.op('dve', lambda e: e.tensor_tensor(out=r_m[:, 2, 0:nh, :], in0=x2(), in1=cosb(), op=ALU.mult), reads=[xf, r_t, r_m], writes=[r_m])
    P.op('dve', lambda e: e.tensor_tensor(out=r_m[:, 3, 0:nh, :], in0=x1(), in1=sinb(), op=ALU.mult), reads=[xf, r_t, r_m], writes=[r_m])
    P.op('dve', lambda e: e.tensor_tensor(out=xb[:, 0:nh, 0:16], in0=r_m[:, 0, 0:nh, :], in1=r_m[:, 1, 0:nh, :], op=ALU.subtract), reads=[r_m, xb], writes=[xb])
    P.op('dve', lambda e: e.tensor_tensor(out=xb[:, 0:nh, 16:32], in0=r_m[:, 2, 0:nh, :], in1=r_m[:, 3, 0:nh, :], op=ALU.add), reads=[r_m, xb], writes=[xb])


def gdn_setup(K, nblk, cf, cb, epsc, psT, ps_s, gb_all, ng_s, qT_d, kT_d, k_d, v_d, sz_d, og_o, ogc0=0):
    P = K.P
    H = HPC
    S32 = K.sb([128, H, 128], F32, "S32"); Sb = K.sb([128, H, 128], BF16, "Sb")
    P.op('dve', lambda e: e.memset(S32[:], 0.0), writes=[S32])
    P.op('dve', lambda e: e.memset(Sb[:], 0.0), writes=[Sb])
    qT = [K.sb([128, 2, 128], BF16, f"g_qT{i}") for i in range(2)]
    kT = [K.sb([128, 2, 128], BF16, f"g_kT{i}") for i in range(2)]
    ktm = [K.sb([128, 2, 128], BF16, f"g_ktm{i}") for i in range(2)]
    vtm = [K.sb([128, H, 128], BF16, f"g_vtm{i}") for i in range(2)]
    szt = [K.sb([128, H, 128], BF16, f"g_sz{i}") for i in range(2)]
    gbc = K.sb([128, H, 128], F32, "g_gbc")
    Dt = K.sb([128, H, 128], F32, "g_Dt")
    grow = K.sb([128, H, 128], F32, "g_grow")
    ks = K.sb([128, H, 128], BF16, "g_ks")
    ksT = K.sb([128, H, 128], BF16, "g_ksT")
    Nn = [K.sb([128, H, 128], BF16, f"g_N{i}") for i in range(2)]
    NT = [K.sb([128, H, 128], BF16, f"g_NT{i}") for i in range(2)]
    Pb = K.sb([128, H, 128], BF16, "g_Pb")
    tmpR = K.sb([128, H, 128], F32, "g_tmpR"); tmpS = K.sb([128, H, 128], F32, "g_tmpS")
    vs = K.sb([128, H, 128], F32, "g_vs")
    Rt = K.sb([128, H, 128], BF16, "g_Rt")
    vnew = K.sb([128, H, 128], BF16, "g_vnew")
    attnT = K.sb([128, H, 128], BF16, "g_attnT")
    qgT = K.sb([128, H, 128], BF16, "g_qgT")
    kd = K.sb([128, H, 128], BF16, "g_kd")
    gz = K.sb([128, H, 128], F32, "g_gz")
    og = [K.sb([128, H, 128], BF16, f"g_og{i}") for i in range(2)]
    junk = K.sb([128, 128], BF16, "g_junk")
    ssq = K.sb([128, 2, H], F32, "g_ssq")
    ident4 = K.sb([128, H, 128], F32, "g_id4")
    offd4 = K.sb([128, H, 128], F32, "g_offd4")
    for h in range(H):
        P.op('dve', lambda e, h=h: e.tensor_copy(out=ident4[:, h, :], in_=cf[:, CI_ID, :]), reads=[cf, ident4], writes=[ident4])
        P.op('dve', lambda e, h=h: e.tensor_copy(out=offd4[:, h, :], in_=cf[:, CI_OFFD, :]), reads=[cf, offd4], writes=[offd4])
    psA = K.ps([128, H, 128], F32, "g_psA"); psB = K.ps([128, H, 128], F32, "g_psB"); psC = K.ps([128, H, 128], F32, "g_psC")

    SM = K.sb([128, nblk, 8, H], F32, "g_SM")
    gcn = K.sb([128, nblk, H], F32, "g_gcn")
    P.op('dve', lambda e: e.tensor_copy(out=gcn[:], in_=gb_all[:, :, 4:8]), reads=[gb_all], writes=[gcn])
    for b0 in range(0, nblk, 128):
        nb = min(128, nblk - b0)
        pav = lambda nb=nb: psA[:].rearrange("p h d -> p (h d)")[:, 0:nb * H].rearrange("p (b f) -> p b f", f=H)
        pbv = lambda nb=nb: psB[:].rearrange("p h d -> p (h d)")[:, 0:nb * H].rearrange("p (b f) -> p b f", f=H)
        smr = lambda r, b0=b0, nb=nb: SM[:, b0:b0 + nb, r, :]
        P.op('pe', lambda e, b0=b0, nb=nb: e.matmul(psA[:].rearrange("p h d -> p (h d)")[:, 0:nb * H], lhsT=cf[:, CI_TRIU, :],
                                                   rhs=gcn[:, b0:b0 + nb, :].rearrange("p b f -> p (b f)"), start=True, stop=True), reads=[cf, gcn], writes=[psA])
        P.op('pe', lambda e, b0=b0, nb=nb: e.matmul(psB[:].rearrange("p h d -> p (h d)")[:, 0:nb * H], lhsT=cf[:, CI_ONES, :],
                                                   rhs=gcn[:, b0:b0 + nb, :].rearrange("p b f -> p (b f)"), start=True, stop=True), reads=[cf, gcn], writes=[psB])
        P.op('dve', lambda e, pav=pav, smr=smr: e.tensor_copy(out=smr(0), in_=pav()), reads=[psA, SM], writes=[SM])
        P.op('act', lambda e, pav=pav, smr=smr: e.activation(out=smr(1), in_=pav(), func=AF.Exp), reads=[psA, SM], writes=[SM])
        P.op('act', lambda e, pbv=pbv, smr=smr: e.activation(out=smr(4), in_=pbv(), func=AF.Exp), reads=[psB, SM], writes=[SM])
        P.op('act', lambda e, smr=smr, b0=b0, nb=nb: e.activation(out=smr(2), in_=gb_all[:, b0:b0 + nb, 0:4], func=AF.Sqrt), reads=[gb_all, SM], writes=[SM])
        P.op('dve', lambda e, smr=smr: e.scalar_tensor_tensor(out=smr(3), in0=smr(2), scalar=-1.0, in1=smr(1), op0=ALU.mult, op1=ALU.mult), reads=[SM], writes=[SM])
        P.op('dve', lambda e, pbv=pbv, smr=smr: e.tensor_tensor(out=smr(7), in0=pbv(), in1=smr(0), op=ALU.subtract), reads=[psB, SM], writes=[SM])
        P.op('act', lambda e, smr=smr: e.activation(out=smr(5), in_=smr(7), func=AF.Exp), reads=[SM], writes=[SM])
        P.op('dve', lambda e, smr=smr: e.tensor_scalar(out=smr(6), in0=smr(0), scalar1=-1.0, scalar2=None, op0=ALU.mult), reads=[SM], writes=[SM])
    def chunk(c):
        r0 = c * 128
        q_T = qT[c % 2]; k_T = kT[c % 2]; k_t = ktm[c % 2]; v_t = vtm[c % 2]; s_z = szt[c % 2]; o_g = og[c % 2]
        P.op('sp', lambda e, q_T=q_T, r0=r0: e.dma_start(out=q_T[:], in_=qT_d.t[:, :, r0:r0 + 128].rearrange("h p t -> p h t")), reads=[qT_d], writes=[q_T], dma=True)
        P.op('sp', lambda e, k_T=k_T, r0=r0: e.dma_start(out=k_T[:], in_=kT_d.t[:, :, r0:r0 + 128].rearrange("h p t -> p h t")), reads=[kT_d], writes=[k_T], dma=True)
        P.op('sp', lambda e, k_t=k_t, r0=r0: e.dma_start(out=k_t[:], in_=k_d.t[r0:r0 + 128, :].rearrange("t (h d) -> t h d", h=2)), reads=[k_d], writes=[k_t], dma=True)
        P.op('sp', lambda e, v_t=v_t, r0=r0: e.dma_start(out=v_t[:], in_=v_d.t[r0:r0 + 128, :].rearrange("t (h d) -> t h d", h=H)), reads=[v_d], writes=[v_t], dma=True)
        P.op('sp', lambda e, s_z=s_z, r0=r0: e.dma_start(out=s_z[:], in_=sz_d.t[r0:r0 + 128, :].rearrange("t (h d) -> t h d", h=H)), reads=[sz_d], writes=[s_z], dma=True)
        beta = lambda: gb_all[:, c, 0:4]
        g = lambda: gb_all[:, c, 4:8]
        yield
        sm = SMView(SM, c)
        yield
        yield
        for h in range(H):
            P.op('dve', lambda e, h=h, c=c: e.tensor_scalar(out=gbc[:, h, :], in0=cf[:, CI_ONES, :], scalar1=gb_all[:, c, 4 + h:5 + h], scalar2=None, op0=ALU.mult),
                 reads=[cf, gb_all, gbc], writes=[gbc])
        yield
        for h in range(H):
            P.op('pe', lambda e, h=h: e.matmul(psA[:, h, :], lhsT=gbc[:, h, :], rhs=cf[:, CI_TRIU, :], start=True, stop=True), reads=[gbc, cf, psA], writes=[psA])
            P.op('pe', lambda e, h=h: e.matmul(psB[:, h, :], lhsT=gbc[:, h, :], rhs=cf[:, CI_TRIU, :], start=True, stop=False), reads=[gbc, cf, psB], writes=[psB])
            P.op('pe', lambda e, h=h: e.matmul(psB[:, h, :], lhsT=cf[:, CI_ID, :], rhs=cf[:, CI_NEGM, :], start=False, stop=True), reads=[cf, psB], writes=[psB])
        P.op('act', lambda e: e.activation(out=grow[:], in_=psA[:], func=AF.Exp), reads=[psA], writes=[grow])
        yield
        for h in range(H):
            P.op('act', lambda e, h=h: e.activation(out=Dt[:, h, :], in_=psB[:, h, :], func=AF.Exp, bias=sm[:, 6, h:h + 1]), reads=[psB, sm, Dt], writes=[Dt])
        yield
        yield
        P.op('dve', lambda e, v_t=v_t: e.tensor_tensor(out=vs[:], in0=v_t[:], in1=sm[:, 2, :].unsqueeze(2).to_broadcast([128, H, 128]), op=ALU.mult), reads=[v_t, sm], writes=[vs])
        for h in range(H):
            P.op('dve', lambda e, h=h, k_t=k_t: e.tensor_scalar(out=ks[:, h, :], in0=k_t[:, h // 2, :], scalar1=sm[:, 2, h:h + 1], scalar2=None, op0=ALU.mult),
                 reads=[k_t, sm, ks], writes=[ks])
            P.op('act', lambda e, h=h, k_t=k_t: e.activation(out=kd[:, h, :], in_=k_t[:, h // 2, :], func=AF.Copy, scale=sm[:, 5, h:h + 1]),
                 reads=[k_t, sm, kd], writes=[kd])
            P.op('dve', lambda e, h=h, q_T=q_T: e.tensor_tensor(out=qgT[:, h, :], in0=q_T[:, h // 2, :], in1=grow[:, h, :], op=ALU.mult), reads=[q_T, grow, qgT], writes=[qgT])
            P.op('pool', lambda e, h=h, s_z=s_z: e.tensor_tensor(out=gz[:, h, :], in0=s_z[:, h, :], in1=ng_s[:], op=ALU.mult), reads=[s_z, ng_s, gz], writes=[gz])
        yield
        for h in range(H):
            P.op('pe', lambda e, h=h: e.transpose(out=psT[:, h, :], in_=ks[:, h, :], identity=cb[:, CI_ID, :]), reads=[ks, cb, psT], writes=[psT])
        P.op('act', lambda e: e.activation(out=ksT[:], in_=psT[:], func=AF.Copy), reads=[psT], writes=[ksT])
        yield
        yield
        for h in range(H):
            P.op('pe', lambda e, h=h: e.matmul(psA[:, h, :], lhsT=ksT[:, h, :], rhs=ksT[:, h, :], start=True, stop=True), reads=[ksT, psA], writes=[psA])
        yield
        for hq in range(2):
            P.op('pe', lambda e, hq=hq, k_T=k_T, q_T=q_T: e.matmul(psC[:, hq, :], lhsT=k_T[:, hq, :], rhs=q_T[:, hq, :], start=True, stop=True), reads=[k_T, q_T, psC], writes=[psC])
        yield
        for h in range(H):
            P.op('dve', lambda e, h=h: e.tensor_tensor(out=attnT[:, h, :], in0=psC[:, h // 2, :], in1=Dt[:, h, :], op=ALU.mult), reads=[psC, Dt, attnT], writes=[attnT])
        P.op('dve', lambda e: e.tensor_tensor(out=Dt[:], in0=Dt[:], in1=offd4[:], op=ALU.mult), reads=[Dt, offd4, attnT], writes=[Dt])
        N0 = Nn[0]; NT0 = NT[0]
        P.op('dve', lambda e: e.scalar_tensor_tensor(out=N0[:], in0=psA[:], scalar=-1.0, in1=Dt[:], op0=ALU.mult, op1=ALU.mult), reads=[psA, Dt], writes=[N0])
        yield
        for h in range(H):
            P.op('pe', lambda e, h=h: e.transpose(out=psT[:, h, :], in_=N0[:, h, :], identity=cb[:, CI_ID, :]), reads=[N0, cb, psT], writes=[psT])
        P.op('act', lambda e: e.activation(out=NT0[:], in_=psT[:], func=AF.Copy), reads=[psT], writes=[NT0])
        P.op('dve', lambda e: e.tensor_tensor(out=Pb[:], in0=N0[:], in1=ident4[:], op=ALU.add), reads=[N0, ident4], writes=[Pb])
        cur = 0
        yield
        for step in range(1, 7):
            Nc = Nn[cur]; NTc = NT[cur]; Nx = Nn[1 - cur]; NTx = NT[1 - cur]
            for h in range(H):
                P.op('pe', lambda e, h=h, Nc=Nc, NTc=NTc: e.matmul(psB[:, h, :], lhsT=Nc[:, h, :], rhs=NTc[:, h, :], start=True, stop=True), reads=[Nc, NTc, psB], writes=[psB])
            P.op('act', lambda e, NTx=NTx: e.activation(out=NTx[:], in_=psB[:], func=AF.Copy), reads=[psB], writes=[NTx])
            if step < 6:
                for h in range(H):
                    P.op('pe', lambda e, h=h, Nc=Nc, NTc=NTc: e.matmul(psA[:, h, :], lhsT=NTc[:, h, :], rhs=Nc[:, h, :], start=True, stop=True), reads=[Nc, NTc, psA], writes=[psA])
                P.op('dve', lambda e, Nx=Nx: e.tensor_copy(out=Nx[:], in_=psA[:]), reads=[psA], writes=[Nx])
            for h in range(H):
                P.op('pe', lambda e, h=h, NTx=NTx: e.matmul(psC[:, h, :], lhsT=NTx[:, h, :], rhs=Pb[:, h, :], start=True, stop=True), reads=[NTx, Pb, psC], writes=[psC])
            P.op('dve', lambda e: e.tensor_tensor(out=Pb[:], in0=Pb[:], in1=psC[:], op=ALU.add), reads=[Pb, psC], writes=[Pb])
            cur = 1 - cur
        yield
        yield
        for h in range(H):
            P.op('pe', lambda e, h=h, k_T=k_T: e.matmul(psA[:, h, :], lhsT=k_T[:, h // 2, :], rhs=Sb[:, h, :], start=True, stop=True), reads=[k_T, Sb, psA], writes=[psA])
        yield
        P.op('dve', lambda e: e.tensor_tensor(out=tmpR[:], in0=psA[:], in1=sm[:, 3, :].unsqueeze(2).to_broadcast([128, H, 128]), op=ALU.mult), reads=[psA, sm], writes=[tmpR])
        P.op('dve', lambda e: e.tensor_tensor(out=Rt[:], in0=tmpR[:], in1=vs[:], op=ALU.add), reads=[tmpR, vs], writes=[Rt])
        yield
        for h in range(H):
            P.op('pe', lambda e, h=h: e.matmul(psB[:, h, :], lhsT=Pb[:, h, :], rhs=Rt[:, h, :], start=True, stop=True), reads=[Pb, Rt, psB], writes=[psB])
        yield
        P.op('dve', lambda e: e.tensor_tensor(out=vnew[:], in0=psB[:], in1=sm[:, 2, :].unsqueeze(2).to_broadcast([128, H, 128]), op=ALU.mult), reads=[psB, sm], writes=[vnew])
        yield
        for h in range(H):
            P.op('pe', lambda e, h=h: e.matmul(psC[:, h, :], lhsT=qgT[:, h, :], rhs=Sb[:, h, :], start=True, stop=False), reads=[qgT, Sb, psC], writes=[psC])
            P.op('pe', lambda e, h=h: e.matmul(psC[:, h, :], lhsT=attnT[:, h, :], rhs=vnew[:, h, :], start=False, stop=True), reads=[attnT, vnew, psC], writes=[psC])
        yield
        for h in range(H):
            P.op('pe', lambda e, h=h: e.matmul(psA[:, h, :], lhsT=kd[:, h, :], rhs=vnew[:, h, :], start=True, stop=True), reads=[kd, vnew, psA], writes=[psA])
        yield
        P.op('dve', lambda e: e.tensor_tensor(out=tmpS[:], in0=S32[:], in1=sm[:, 4, :].unsqueeze(2).to_broadcast([128, H, 128]), op=ALU.mult), reads=[S32, sm], writes=[tmpS])
        P.op('dve', lambda e: e.tensor_tensor(out=Sb[:], in0=tmpS[:], in1=psA[:], op=ALU.add), reads=[tmpS, psA], writes=[Sb])
        P.op('dve', lambda e: e.tensor_tensor(out=S32[:], in0=tmpS[:], in1=psA[:], op=ALU.add), reads=[tmpS, psA], writes=[S32])
        yield
        yield
        for h in range(H):
            P.op('act', lambda e, h=h: e.activation(out=junk[:], in_=psC[:, h, :], func=AF.Square, accum_out=ssq[:, 0, h:h + 1]), reads=[psC, junk, ssq], writes=[junk, ssq])
        P.op('act', lambda e: e.activation(out=ssq[:, 1, :], in_=ssq[:, 0, :], func=AF.Sqrt, scale=1.0 / 128, bias=epsc[:, 0:1]), reads=[ssq, epsc], writes=[ssq])
        P.op('dve', lambda e: e.reciprocal(out=ssq[:, 1, :], in_=ssq[:, 1, :]), reads=[ssq], writes=[ssq])
        yield
        for h in range(H):
            P.op('dve', lambda e, h=h, o_g=o_g: e.scalar_tensor_tensor(out=o_g[:, h, :], in0=psC[:, h, :], scalar=ssq[:, 1, h:h + 1], in1=gz[:, h, :], op0=ALU.mult, op1=ALU.mult),
                 reads=[psC, ssq, gz, o_g], writes=[o_g])
        P.op('sp', lambda e, o_g=o_g, r0=r0: e.dma_start(out=og_o[r0:r0 + 128, ogc0:ogc0 + HPC * 128], in_=o_g[:]), reads=[o_g], writes=[og_o], dma=True)

    return chunk


def emit_gdn_multi(K, nblk, cf, cb, epsc, ng_s, groups):
    psT = K.ps([128, HPC, 128], BF16, "g_psT")
    ps_s = K.ps([128, 2, HPC], F32, "g_pss")
    fns = [gdn_setup(K, nblk, cf, cb, epsc, psT, ps_s, gb, ng_s, *scr, og_o, c0) for (gb, scr, og_o, c0) in groups]
    for c in range(nblk):
        gens = [f(c) for f in fns]
        while gens:
            nxt = []
            for gen in gens:
                try:
                    next(gen); nxt.append(gen)
                except StopIteration:
                    pass
            gens = nxt


NWC = 2048 + 2048 + 16 + 2048 + 2048
BSTR = 126
A0 = 126
NIOTA = 1408


def blocks_for(SEQ):
    nb = -(-SEQ // BSTR)
    NS = -(-nb // NCORE)
    return nb, NS


def slot_geom(j, nblk):
    a_min = A0 + BSTR * (NCORE * j)
    a_max = A0 + BSTR * (NCORE * j + NCORE - 1)
    kd = min(a_min // 128, nblk)
    kb_end = min((a_max + 127) // 128 + 1, nblk)
    kd = min(kd, kb_end)
    return kd, kb_end


def build_prog2(Lp, NS, topk, dbg=False, fused=False):
    nblk = Lp // 128
    nc = bass.Bass("TRN2", target_bir_lowering=False)
    es = ExitStack()
    with es:
        K = Ctx(nc, es); P = K.P
        R = NS * 128
        hown = K.din("hown", [R, D])
        if not fused:
            ogown = K.din("ogown", [R, 4096], BF16)
            KT_i = K.din("KT", [2, 128, Lp], BF16); V_i = K.din("Vt", [Lp, 256], BF16); ikT_i = K.din("ikT", [128, Lp], BF16)
            ogsrc = ('own', ogown)
        else:
            NG = NCORE
            hfull = K.din("hfull", [Lp, D])
            w_a = K.din("w_a", [NG, D, NA]); cw = K.din("cw", [NG, 128, 8, 4]); hp = K.din("hp", [NG, 128, 3, HPC])
            ng = K.din("ng", [128, 128]); rope = K.din("rope", [Lp, 32])
            sel_i = K.din("sel", [128, 8, 128], BF16)
            og_d = K.dtmp("og_d", [Lp, 4096]); KT_i = K.dtmp("KT_s", [2, 128, Lp]); V_i = K.dtmp("Vt_s", [Lp, 256]); ikT_i = K.dtmp("ikT_s", [128, Lp])
        ropeo = K.din("ropeo", [R, 32])
        qrel_i = K.din("qrel", [128, NS])
        iota_i = K.din("iota", [128, NIOTA])
        cst = K.din("cst", [128, 5, 128])
        gpre = K.din("gpre", [128, 16]); g2pre = K.din("g2pre", [128, 16])
        gpost = K.din("gpost", [128, D]); g2post = K.din("g2post", [128, D])
        fcw = K.din("fcw", [128, 96, 3]); fcb = K.din("fcb", [128, 96])
        w_c = K.din("w_c", [D, NWC]); wbg = K.din("wbg", [4096, D]); wba = K.din("wba", [D, D]); wout = K.din("wout", [D, D])
        wup = K.din("wup", [D, 2 * NFF]); wdn = K.din("wdn", [NFF, D])
        out_o = K.dout("out", [NS, BSTR, D])
        wc_b = K.dtmp("wc_b", [128, 17, 16, 512]); wbg_b = K.dtmp("wbg_b", [128, 4, 32, 512]); wba_b = K.dtmp("wba_b", [128, 4, 16, 512])
        wout_b = K.dtmp("wout_b", [128, 4, 16, 512]); wup_b = K.dtmp("wup_b", [128, 24, 16, 512]); wdn_b = K.dtmp("wdn_b", [128, 4, 48, 512])
        if dbg:
            dbg_oatt = K.dout("dbg_oatt", [R, D], BF16); dbg_thr = K.dout("dbg_thr", [128, NS]); dbg_h1 = K.dout("dbg_h1", [R, D])
            dbg_q = K.dout("dbg_q", [R, D], BF16)

        if fused:
            cf, cb, epsc = emit_gdn_all(K, Lp, NG, hfull, w_a, gpre, cw, hp, ng, cst, rope, og_d, KT_i, V_i, ikT_i, gdn_scratch(K, Lp))
            sel_s = K.sb([128, 8, 128], BF16, "sel_s")
            P.op('sp', lambda e: e.dma_start(out=sel_s[:], in_=sel_i[:]), writes=[sel_s], dma=True)
            ogsrc = ('sel', og_d, sel_s, Lp)
        else:
            cf = K.sb([128, 5, 128], F32, "cf"); cb = K.sb([128, 5, 128], BF16, "cb")
            P.op('sp', lambda e: e.dma_start(out=cf[:], in_=cst[:]), writes=[cf], dma=True)
            P.op('dve', lambda e: e.tensor_copy(out=cb[:], in_=cf[:]), reads=[cf], writes=[cb])
            epsc = K.sb([128, 1], F32, "epsc")
            P.op('dve', lambda e: e.memset(epsc[:], EPS), writes=[epsc])
        gpre_s = K.sb([128, 16], F32); g2pre_s = K.sb([128, 16], F32); qrel_s = K.sb([128, NS], F32)
        fcw_s = K.sb([128, 96, 3], F32); fcb_s = K.sb([128, 96], F32)
        for dst, src in ((gpre_s, gpre), (g2pre_s, g2pre), (qrel_s, qrel_i), (fcw_s, fcw), (fcb_s, fcb)):
            P.op('sp', lambda e, dst=dst, src=src: e.dma_start(out=dst[:], in_=src[:]), writes=[dst], dma=True)

        sc0 = K.scope()
        st = [K.sb([128, 2048], F32, f"w0s{i}") for i in range(2)]
        sbt = [K.sb([128, 2048], BF16, f"w0b{i}") for i in range(2)]
        it = [0]
        def cast_w(src, dst, KC, N, gain, up=False):
            sv = src.t.rearrange("(kc p) n -> p kc n", p=128)
            for kc in range(KC):
                for n0 in range(0, N, 2048):
                    n1 = min(N, n0 + 2048); w = n1 - n0
                    s_ = st[it[0] % 2]; b_ = sbt[it[0] % 2]; it[0] += 1
                    P.op('sp', lambda e, s_=s_, kc=kc, n0=n0, n1=n1, w=w: e.dma_start(out=s_[:, 0:w], in_=sv[:, kc, n0:n1]), writes=[s_], dma=True)
                    if gain is None:
                        P.op('act', lambda e, s_=s_, b_=b_, w=w: e.activation(out=b_[:, 0:w], in_=s_[:, 0:w], func=AF.Copy), reads=[s_], writes=[b_])
                    else:
                        P.op('act', lambda e, s_=s_, b_=b_, w=w, kc=kc: e.activation(out=b_[:, 0:w], in_=s_[:, 0:w], func=AF.Copy, scale=gain[:, kc:kc + 1]),
                             reads=[s_, gain], writes=[b_])
                    if up:
                        half = n0 // NFF; g0 = (n0 % NFF) // 256
                        P.op('pool', lambda e, b_=b_, kc=kc, g0=g0, half=half: e.dma_start(out=dst[:, g0:g0 + 8, kc, half * 256:(half + 1) * 256],
                                                                                          in_=b_[:, 0:2048].rearrange("p (g c) -> p g c", c=256)), reads=[b_], writes=[dst], dma=True)
                    else:
                        nt = w // 512; rem = w % 512; t0_ = n0 // 512
                        if nt:
                            P.op('pool', lambda e, b_=b_, kc=kc, nt=nt, t0_=t0_: e.dma_start(out=dst[:, t0_:t0_ + nt, kc, :], in_=b_[:, 0:nt * 512].rearrange("p (g c) -> p g c", c=512)),
                                 reads=[b_], writes=[dst], dma=True)
                        if rem:
                            P.op('pool', lambda e, b_=b_, kc=kc, nt=nt, t0_=t0_, rem=rem: e.dma_start(out=dst[:, t0_ + nt, kc, 0:rem], in_=b_[:, nt * 512:nt * 512 + rem]),
                                 reads=[b_], writes=[dst], dma=True)
        cast_w(w_c, wc_b, 16, NWC, gpre_s)
        cast_w(wbg, wbg_b, 32, D, None); cast_w(wba, wba_b, 16, D, None); cast_w(wout, wout_b, 16, D, None)
        cast_w(wup, wup_b, 16, 2 * NFF, g2pre_s, up=True); cast_w(wdn, wdn_b, 48, D, None)
        P.emit(); K.unscope(); sc0.close()

        xo = K.sb([128, D], F32, "xo")
        QT = K.sb([128, 16, 128], BF16, "QT"); iqT = K.sb([128, 16, 128], BF16, "iqT")
        sgn = K.sb([128, 16], F32, "sgn"); aiw = K.sb([128, 16], F32, "aiw")
        sig = K.sb([128, 2, D], BF16, "sig")
        oatt = K.sb([128, D], BF16, "oatt")
        thr = K.sb([128, NS], F32, "thr")

        for j in range(NS):
            r0 = j * 128
            kd, kb_end = slot_geom(j, nblk)
            nk = kb_end * 128
            scW = K.scope()
            W = make_wpool(K)
            xs = K.sb([128, D], BF16, "p_xs"); ss = K.sb([128, 2], F32, "p_ss")
            uT = K.sb([128, 16, 128], BF16, "p_uT")
            tq = K.sb([128, 16, 128], F32, "p_tq"); tqb = K.sb([128, 16, 128], BF16, "p_tqb")
            r_t = K.sb([128, 32], F32, "p_rt"); r_m = K.sb([128, 4, 16, 16], F32, "p_rm")
            iwt = K.sb([128, 16], F32, "p_iw")
            P.op('sp', lambda e, r0=r0: e.dma_start(out=xo[:], in_=hown[r0:r0 + 128, :]), writes=[xo], dma=True)
            P.op('sp', lambda e, r0=r0: e.dma_start(out=r_t[:], in_=ropeo[r0:r0 + 128, :]), writes=[r_t], dma=True)
            emit_norm_T(P, W, cb, epsc, xo, xs, ss, uT)
            for ct in range(17):
                n0 = ct * 512 if ct < 8 else (8192 if ct == 8 else 4096 + (ct - 9) * 512)
                ncol = 16 if ct == 8 else 512
                pm = W.linear(uT, wc_b, 16, n0, ncol)
                if ct < 8:
                    hh = (ct % 4) * 4
                    if ct == 4:
                        finish_q(P, W, cb, tq, tqb, r_t, r_m, QT, 128 ** -0.5, None)
                    P.op('act', lambda e, pm=pm, hh=hh: e.activation(out=tq[:, hh:hh + 4, :], in_=pm[:, 0:512], func=AF.Copy), reads=[pm], writes=[tq])
                elif ct == 8:
                    P.op('act', lambda e, pm=pm: e.activation(out=iwt[:], in_=pm[:, 0:16], func=AF.Copy), reads=[pm], writes=[iwt])
                    P.op('act', lambda e: e.activation(out=aiw[:], in_=iwt[:], func=AF.Abs, scale=(16 ** -0.5) * (128 ** -0.5)), reads=[iwt], writes=[aiw])
                    P.op('dve', lambda e: e.tensor_scalar(out=sgn[:], in0=iwt[:], scalar1=0.0, scalar2=2.0, op0=ALU.is_gt, op1=ALU.mult), reads=[iwt], writes=[sgn])
                    P.op('dve', lambda e: e.tensor_scalar(out=sgn[:], in0=sgn[:], scalar1=-1.0, scalar2=None, op0=ALU.add), reads=[sgn], writes=[sgn])
                    finish_q(P, W, cb, tq, tqb, r_t, r_m, iqT, None, aiw)
                else:
                    gi = (ct - 9) // 4; c0 = ((ct - 9) % 4) * 512
                    P.op('act', lambda e, pm=pm, gi=gi, c0=c0: e.activation(out=sig[:, gi, c0:c0 + 512], in_=pm[:, 0:512], func=AF.Sigmoid), reads=[pm], writes=[sig])
            if dbg:
                P.op('sp', lambda e, r0=r0: e.dma_start(out=dbg_q[r0:r0 + 128, :], in_=tqb[:]), reads=[tqb], writes=[dbg_q], dma=True)
            P.emit(); K.unscope(); scW.close()

            scA = K.scope()
            emit_attention(K, cb, cf, j, kd, kb_end, topk, QT, iqT, sgn, qrel_s, iota_i, KT_i, V_i, ikT_i, oatt, thr)
            K.unscope(); scA.close()
            if dbg:
                P.op('sp', lambda e, r0=r0: e.dma_start(out=dbg_oatt[r0:r0 + 128, :], in_=oatt[:]), reads=[oatt], writes=[dbg_oatt], dma=True)

            scW = K.scope()
            W = make_wpool(K)
            emit_merge_ffn(K, W, cb, epsc, j, xo, ogsrc, oatt, sig, gpost, g2post, fcw_s, fcb_s,
                           wbg_b, wba_b, wout_b, wup_b, wdn_b, out_o, dbg_h1 if dbg else None)
            P.emit(); K.unscope(); scW.close()
        if dbg:
            P.op('sp', lambda e: e.dma_start(out=dbg_thr[:], in_=thr[:]), reads=[thr], writes=[dbg_thr], dma=True)
        P.emit()
    return nc


class WPool:
    pass


def make_wpool(K):
    P = K.P
    W = WPool()
    W.wt = [K.sb([128, 16, 512], BF16, f"wt{i}") for i in range(3)]
    W.ps_mm = [K.ps([128, 512], F32, f"w_psmm{i}") for i in range(3)]
    W.ps_tr = [K.ps([128, 4, 128], BF16, f"w_pstr{i}") for i in range(2)]
    W.nw = 0; W.nm = 0; W.nt = 0
    def next_tr():
        W.nt += 1
        return W.ps_tr[W.nt % 2]
    W.next_tr = next_tr
    def linear(xT, wd, KC, n0, ncol, pm=None, xoff=0):
        if pm is None:
            W.nm += 1; pm = W.ps_mm[W.nm % 3]
        for part in range(KC // 16):
            wt = W.wt[W.nw % 3]; W.nw += 1
            P.op('sp', lambda e, wt=wt, part=part: e.dma_start(out=wt[:, :, 0:ncol], in_=wd[:, n0 // 512, part * 16:(part + 1) * 16, 0:ncol]), reads=[wd], writes=[wt], dma=True)
            for kc in range(16):
                kk = part * 16 + kc
                P.op('pe', lambda e, wt=wt, kc=kc, kk=kk: e.matmul(pm[:, 0:ncol], lhsT=xT[:, xoff + kk, :], rhs=wt[:, kc, 0:ncol], start=(kk == 0), stop=(kk == KC - 1)),
                     reads=[xT, wt], writes=[pm])
        return pm
    W.linear = linear
    return W


def emit_norm_T(P, W, cb, epsc, x, xs, ss, uT):
    P.op('act', lambda e: e.activation(out=xs[:], in_=x[:], func=AF.Square, accum_out=ss[:, 0:1]), reads=[x], writes=[xs, ss])
    P.op('act', lambda e: e.activation(out=ss[:, 1:2], in_=ss[:, 0:1], func=AF.Sqrt, scale=1.0 / D, bias=epsc[:, 0:1]), reads=[ss, epsc], writes=[ss])
    P.op('dve', lambda e: e.reciprocal(out=ss[:, 1:2], in_=ss[:, 1:2]), reads=[ss], writes=[ss])
    P.op('dve', lambda e: e.tensor_scalar(out=xs[:], in0=x[:], scalar1=ss[:, 1:2], scalar2=None, op0=ALU.mult), reads=[x, ss], writes=[xs])
    emit_T(P, W, cb, lambda kc: xs[:, kc * 128:(kc + 1) * 128], xs, uT, 16)


def emit_T(P, W, cb, src_ap, src_tl, dstT, n):
    for g4 in range(0, n, 4):
        pt = W.next_tr()
        m = min(4, n - g4)
        for q in range(m):
            P.op('pe', lambda e, pt=pt, q=q, i=g4 + q: e.transpose(out=pt[:, q, :], in_=src_ap(i), identity=cb[:, CI_ID, :]), reads=[src_tl, cb], writes=[pt])
        if (g4 // 4) % 2:
            P.op('act', lambda e, pt=pt, g4=g4, m=m: e.activation(out=dstT[:, g4:g4 + m, :], in_=pt[:, 0:m, :], func=AF.Copy), reads=[pt], writes=[dstT])
        else:
            P.op('dve', lambda e, pt=pt, g4=g4, m=m: e.tensor_copy(out=dstT[:, g4:g4 + m, :], in_=pt[:, 0:m, :]), reads=[pt], writes=[dstT])


def finish_q(P, W, cb, tq, tqb, r_t, r_m, dstT, const_scale, row_scale):
    if row_scale is not None:
        P.op('dve', lambda e: e.tensor_tensor(out=tq[:], in0=tq[:], in1=row_scale[:].unsqueeze(2).to_broadcast([128, 16, 128]), op=ALU.mult), reads=[tq, row_scale], writes=[tq])
    else:
        P.op('dve', lambda e: e.tensor_scalar(out=tq[:], in0=tq[:], scalar1=const_scale, scalar2=None, op0=ALU.mult), reads=[tq], writes=[tq])
    P.op('act', lambda e: e.activation(out=tqb[:], in_=tq[:], func=AF.Copy), reads=[tq], writes=[tqb])
    emit_rope(P, tq, tqb, r_t, r_m, 16)
    emit_T(P, W, cb, lambda h: tqb[:, h, :], tqb, dstT, 16)


def emit_attention(K, cb, cf, j, kd, kb_end, topk, QT, iqT, sgn, qrel_s, iota_i, KT_i, V_i, ikT_i, oatt, thr):
    P = K.P
    nk = kb_end * 128
    sc = K.sb([128, nk], F32, "a_sc")
    maskT = K.sb([128, kb_end, 128], BF16, "a_maskT")
    JW = 4096
    junk = K.sb([128, JW], BF16, "a_junk")
    rl = [K.sb([128, 512], F32, f"a_rl{i}") for i in range(2)]
    ikb = [K.sb([128, 512], BF16, f"a_ik{i}") for i in range(2)]
    iot = K.sb([128, NIOTA], F32, "a_iota")
    P.op('sp', lambda e: e.dma_start(out=iot[:], in_=iota_i[:]), writes=[iot], dma=True)
    scp = K.scope()
    ps_i = [K.ps([128, 512], F32, f"a_psi{i}") for i in range(3)]
    ps_t = [K.ps([128, 4, 128], BF16, f"a_pst{i}") for i in range(2)]
    ni = 0
    for u0 in range(0, kb_end, 4):
        nb_ = min(4, kb_end - u0); w = nb_ * 128; k0 = u0 * 128
        ik = ikb[(u0 // 4) % 2]
        P.op('sp', lambda e, ik=ik, k0=k0, w=w: e.dma_start(out=ik[:, 0:w], in_=ikT_i[:, k0:k0 + w]), reads=[ikT_i], writes=[ik], dma=True)
        for h in range(16):
            pi = ps_i[ni % 3]; r_ = rl[ni % 2]; ni += 1
            P.op('pe', lambda e, pi=pi, ik=ik, h=h, w=w: e.matmul(pi[:, 0:w], lhsT=iqT[:, h, :], rhs=ik[:, 0:w], start=True, stop=True), reads=[iqT, ik], writes=[pi])
            P.op('act', lambda e, pi=pi, r_=r_, w=w: e.activation(out=r_[:, 0:w], in_=pi[:, 0:w], func=AF.Relu), reads=[pi], writes=[r_])
            if h == 0:
                P.op('dve', lambda e, r_=r_, k0=k0, w=w: e.tensor_scalar(out=sc[:, k0:k0 + w], in0=r_[:, 0:w], scalar1=sgn[:, 0:1], scalar2=None, op0=ALU.mult), reads=[r_, sgn], writes=[sc])
            else:
                P.op('dve', lambda e, r_=r_, k0=k0, w=w, h=h: e.scalar_tensor_tensor(out=sc[:, k0:k0 + w], in0=r_[:, 0:w], scalar=sgn[:, h:h + 1], in1=sc[:, k0:k0 + w], op0=ALU.mult, op1=ALU.add),
                     reads=[r_, sgn, sc], writes=[sc])
    P.op('dve', lambda e: e.memset(sc[:, 0:PADF], -1e30), reads=[sc], writes=[sc])
    wc = (kb_end - kd) * 128
    if wc > 0:
        assert wc <= NIOTA
        P.op('dve', lambda e: e.tensor_scalar(out=iot[:, 0:wc], in0=iot[:, 0:wc], scalar1=qrel_s[:, j:j + 1], scalar2=-1e30, op0=ALU.is_gt, op1=ALU.mult), reads=[iot, qrel_s], writes=[iot])
        P.op('dve', lambda e: e.tensor_tensor(out=sc[:, kd * 128:nk], in0=sc[:, kd * 128:nk], in1=iot[:, 0:wc], op=ALU.add), reads=[sc, iot], writes=[sc])
    lo = K.sb([128, 1], F32, "a_lo"); mid = K.sb([128, 1], F32, "a_mid"); cnt = K.sb([128, 8], F32, "a_cnt"); dl = K.sb([128, 1], F32, "a_dl")
    LO0, RANGE, NIT = -16.0, 64.0, 26
    P.op('dve', lambda e: e.memset(lo[:], LO0), writes=[lo])
    npc = -(-nk // JW)
    for it in range(NIT):
        hk = RANGE / (2 ** (it + 1))
        P.op('dve', lambda e, hk=hk: e.tensor_scalar(out=mid[:], in0=lo[:], scalar1=hk, scalar2=None, op0=ALU.add), reads=[lo], writes=[mid])
        for pc in range(npc):
            c0 = pc * JW; c1 = min(nk, c0 + JW)
            P.op('dve', lambda e, c0=c0, c1=c1, pc=pc: e.tensor_scalar(out=junk[:, 0:c1 - c0], in0=sc[:, c0:c1], scalar1=mid[:, 0:1], scalar2=0.0, op0=ALU.is_ge, op1=ALU.add,
                                                                     accum_out=cnt[:, pc:pc + 1]), reads=[sc, mid, junk], writes=[junk, cnt])
        if npc > 1:
            P.op('dve', lambda e: e.tensor_reduce(out=cnt[:, 7:8], in_=cnt[:, 0:npc], axis=mybir.AxisListType.X, op=ALU.add), reads=[cnt], writes=[cnt])
            cc = 7
        else:
            cc = 0
        P.op('dve', lambda e, hk=hk, cc=cc: e.tensor_scalar(out=dl[:], in0=cnt[:, cc:cc + 1], scalar1=topk - 0.5, scalar2=hk, op0=ALU.is_gt, op1=ALU.mult), reads=[cnt], writes=[dl])
        P.op('dve', lambda e: e.tensor_tensor(out=lo[:], in0=lo[:], in1=dl[:], op=ALU.add), reads=[lo, dl], writes=[lo])
    P.op('dve', lambda e: e.tensor_copy(out=thr[:, j:j + 1], in_=lo[:]), reads=[lo, thr], writes=[thr])
    mk = [K.sb([128, 512], BF16, f"a_mk{i}") for i in range(2)]
    for u0 in range(0, kb_end, 4):
        nb_ = min(4, kb_end - u0); w = nb_ * 128; k0 = u0 * 128
        m_ = mk[(u0 // 4) % 2]; pt = ps_t[(u0 // 4) % 2]
        P.op('dve', lambda e, m_=m_, k0=k0, w=w: e.tensor_scalar(out=m_[:, 0:w], in0=sc[:, k0:k0 + w], scalar1=lo[:, 0:1], scalar2=None, op0=ALU.is_ge), reads=[sc, lo], writes=[m_])
        for q in range(nb_):
            P.op('pe', lambda e, pt=pt, q=q, m_=m_: e.transpose(out=pt[:, q, :], in_=m_[:, q * 128:(q + 1) * 128], identity=cb[:, CI_ID, :]), reads=[m_, cb], writes=[pt])
        P.op('act', lambda e, pt=pt, u0=u0, nb_=nb_: e.activation(out=maskT[:, u0:u0 + nb_, :], in_=pt[:, 0:nb_, :], func=AF.Copy), reads=[pt], writes=[maskT])
    P.emit(); K.unscope(); scp.close()
    scp = K.scope()
    psO = [K.ps([128, 512], F32, f"a_pso{i}") for i in range(6)]
    psS = [K.ps([128, 4, 128], F32, f"a_pss{i}") for i in range(2)]
    Kb = [K.sb([128, 2, 128], BF16, f"a_K{i}") for i in range(3)]
    Vb = [K.sb([128, 2, 129], BF16, f"a_V{i}") for i in range(3)]
    eb = [K.sb([128, 4, 128], BF16, f"a_e{i}") for i in range(2)]
    pb = [K.sb([128, 4, 128], BF16, f"a_p{i}") for i in range(3)]
    rc = K.sb([128, 16], F32, "a_rc")
    for v_ in Vb:
        P.op('dve', lambda e, v_=v_: e.memset(v_[:, :, 128:129], 1.0), writes=[v_])
    ns = 0
    for kb in range(kb_end):
        k_ = Kb[kb % 3]; v_ = Vb[kb % 3]; k0 = kb * 128
        P.op('sp', lambda e, k_=k_, k0=k0: e.dma_start(out=k_[:], in_=KT_i.t[:, :, k0:k0 + 128].rearrange("h p t -> p h t")), reads=[KT_i], writes=[k_], dma=True)
        P.op('sp', lambda e, v_=v_, k0=k0: e.dma_start(out=v_[:, :, 0:128], in_=V_i.t[k0:k0 + 128, :].rearrange("t (h d) -> t h d", h=2)), reads=[V_i, v_], writes=[v_], dma=True)
        for g in range(2):
            for half in range(2):
                h0 = g * 8 + half * 4
                pS = psS[ns % 2]; e_ = eb[ns % 2]; p_ = pb[ns % 3]; ns += 1
                P.op('pe', lambda e, pS=pS, k_=k_, g=g, h0=h0: e.matmul(pS[:], lhsT=k_[:, g, :], rhs=QT[:, h0:h0 + 4, :], start=True, stop=True), reads=[k_, QT], writes=[pS])
                P.op('act', lambda e, pS=pS, e_=e_: e.activation(out=e_[:], in_=pS[:], func=AF.Exp), reads=[pS], writes=[e_])
                P.op('dve', lambda e, e_=e_, p_=p_, kb=kb: e.tensor_tensor(out=p_[:], in0=e_[:], in1=maskT[:, kb, :].unsqueeze(1).to_broadcast([128, 4, 128]), op=ALU.mult),
                     reads=[e_, maskT], writes=[p_])
                for hh in range(4):
                    hd = h0 + hh
                    po = psO[hd // 3]
                    P.op('pe', lambda e, po=po, hd=hd, p_=p_, hh=hh, v_=v_, g=g, kb=kb: e.matmul(po[:, (hd % 3) * 129:(hd % 3) * 129 + 129], lhsT=p_[:, hh, :], rhs=v_[:, g, :], start=(kb == 0 and hd % 3 == 0), stop=(kb == kb_end - 1 and (hd % 3 == 2 or hd == 15))),
                         reads=[p_, v_], writes=[po])
    for hd in range(16):
        po = psO[hd // 3]
        P.op('dve', lambda e, po=po, hd=hd: e.reciprocal(out=rc[:, hd:hd + 1], in_=po[:, (hd % 3) * 129 + 128:(hd % 3) * 129 + 129]), reads=[po, rc], writes=[rc])
        P.op('act', lambda e, po=po, hd=hd: e.activation(out=oatt[:, hd * 128:(hd + 1) * 128], in_=po[:, (hd % 3) * 129:(hd % 3) * 129 + 128], func=AF.Copy, scale=rc[:, hd:hd + 1]), reads=[po, rc, oatt], writes=[oatt])
    P.emit(); K.unscope(); scp.close()


def emit_merge_ffn(K, W, cb, epsc, j, xo, ogsrc, oatt, sig, gpost, g2post, fcw_s, fcb_s,
                   wbg_b, wba_b, wout_b, wup_b, wdn_b, out_o, dbg_h1):
    P = K.P
    r0 = j * 128
    if ogsrc[0] == 'own':
        ogb = K.sb([128, 4096], BF16, "m_og")
    ogT = K.sb([128, 32, 128], BF16, "m_ogT"); oaT = K.sb([128, 16, 128], BF16, "m_oaT")
    mg = K.sb([128, D], F32, "m_mg"); t1 = K.sb([128, 512], F32, "m_t1"); mgb = K.sb([128, D], BF16, "m_mgb"); mgT = K.sb([128, 16, 128], BF16, "m_mgT")
    ysb = K.sb([128, D], F32, "m_y"); gp = K.sb([128, D], F32, "m_gp")
    ssq = K.sb([128, 8], F32, "m_ssq"); junk = K.sb([128, 512], BF16, "m_junk")
    xs = K.sb([128, D], BF16, "m_xs"); ss = K.sb([128, 2], F32, "m_ss"); u2T = K.sb([128, 16, 128], BF16, "m_u2T")
    actT = K.sb([128, 48, 128], BF16, "m_actT")
    cv = [K.sb([128, 4, 126], F32, f"m_cv{i}") for i in range(2)]
    sg = [K.sb([128, 2, 126], F32, f"m_sg{i}") for i in range(2)]
    if ogsrc[0] == 'own':
        ogown = ogsrc[1]
        P.op('sp', lambda e: e.dma_start(out=ogb[:], in_=ogown[r0:r0 + 128, :]), writes=[ogb], dma=True)
    P.op('sp', lambda e: e.dma_start(out=gp[:], in_=gpost[:]), writes=[gp], dma=True)
    if ogsrc[0] == 'own':
        emit_T(P, W, cb, lambda i: ogb[:, i * 128:(i + 1) * 128], ogb, ogT, 32)
    else:
        _, og_d, sel_s, Lp_ = ogsrc
        win0 = A0 + BSTR * (NCORE * j)
        ogw = [K.sb([128, 8, 1024], BF16, f"m_ogw{i}") for i in range(2)]
        psg = W.ps_mm
        for fq in range(4):
            t_ = ogw[fq % 2]
            valid = []
            P.op('pool', lambda e, t_=t_: e.memset(t_[:], 0.0), writes=[t_])
            for kc in range(8):
                rlo = win0 + kc * 128
                nv = max(0, min(128, Lp_ - rlo))
                if nv == 0: continue
                valid.append(kc)
                P.op('sp', lambda e, t_=t_, kc=kc, rlo=rlo, nv=nv, fq=fq: e.dma_start(out=t_[0:nv, kc, :], in_=og_d[rlo:rlo + nv, fq * 1024:(fq + 1) * 1024]),
                     reads=[og_d, t_], writes=[t_], dma=True)
            for half in range(2):
                W.nm += 1; pm = psg[W.nm % 3]
                for q in range(4):
                    fc = half * 4 + q
                    for kc in valid:
                        P.op('pe', lambda e, pm=pm, q=q, fc=fc, kc=kc, t_=t_: e.matmul(pm[:, q * 128:(q + 1) * 128], lhsT=t_[:, kc, fc * 128:(fc + 1) * 128], rhs=sel_s[:, kc, :],
                                                                                  start=(kc == valid[0]), stop=(kc == valid[-1])), reads=[t_, sel_s], writes=[pm])
                i0 = fq * 8 + half * 4
                if valid:
                    P.op('act', lambda e, pm=pm, i0=i0: e.activation(out=ogT[:, i0:i0 + 4, :], in_=pm[:, 0:512], func=AF.Copy), reads=[pm], writes=[ogT])
                else:
                    P.op('pool', lambda e, i0=i0: e.memset(ogT[:, i0:i0 + 4, :], 0.0), writes=[ogT])
    emit_T(P, W, cb, lambda i: oatt[:, i * 128:(i + 1) * 128], oatt, oaT, 16)
    for ct in range(4):
        c0 = ct * 512
        pm = W.linear(ogT, wbg_b, 32, c0, 512)
        P.op('dve', lambda e, pm=pm, c0=c0: e.tensor_tensor(out=mg[:, c0:c0 + 512], in0=pm[:, 0:512], in1=sig[:, 0, c0:c0 + 512], op=ALU.mult), reads=[pm, sig, mg], writes=[mg])
        pm = W.linear(oaT, wba_b, 16, c0, 512)
        P.op('dve', lambda e, pm=pm, c0=c0: e.tensor_tensor(out=t1[:], in0=pm[:, 0:512], in1=sig[:, 1, c0:c0 + 512], op=ALU.mult), reads=[pm, sig], writes=[t1])
        P.op('pool', lambda e, c0=c0: e.tensor_tensor(out=mgb[:, c0:c0 + 512], in0=mg[:, c0:c0 + 512], in1=t1[:], op=ALU.add), reads=[mg, t1, mgb], writes=[mgb])
    emit_T(P, W, cb, lambda i: mgb[:, i * 128:(i + 1) * 128], mgb, mgT, 16)

    def post_norm_residual(src_T, wd, KC, gain_tl, res_in, res_out):
        for ct in range(4):
            c0 = ct * 512
            pm = W.linear(src_T, wd, KC, c0, 512)
            P.op('act', lambda e, pm=pm, c0=c0: e.activation(out=ysb[:, c0:c0 + 512], in_=pm[:, 0:512], func=AF.Copy), reads=[pm, ysb], writes=[ysb])
            P.op('act', lambda e, pm=pm, ct=ct: e.activation(out=junk[:], in_=pm[:, 0:512], func=AF.Square, accum_out=ssq[:, ct:ct + 1]), reads=[pm, junk, ssq], writes=[junk, ssq])
        P.op('dve', lambda e: e.tensor_reduce(out=ssq[:, 4:5], in_=ssq[:, 0:4], axis=mybir.AxisListType.X, op=ALU.add), reads=[ssq], writes=[ssq])
        P.op('act', lambda e: e.activation(out=ssq[:, 5:6], in_=ssq[:, 4:5], func=AF.Sqrt, scale=1.0 / D, bias=epsc[:, 0:1]), reads=[ssq, epsc], writes=[ssq])
        P.op('dve', lambda e: e.reciprocal(out=ssq[:, 5:6], in_=ssq[:, 5:6]), reads=[ssq], writes=[ssq])
        P.op('dve', lambda e: e.scalar_tensor_tensor(out=ysb[:], in0=ysb[:], scalar=ssq[:, 5:6], in1=gain_tl[:], op0=ALU.mult, op1=ALU.mult), reads=[ysb, ssq, gain_tl], writes=[ysb])
        P.op('pool', lambda e: e.tensor_tensor(out=res_out[:], in0=res_in[:], in1=ysb[:], op=ALU.add), reads=[res_in, ysb], writes=[res_out])

    post_norm_residual(mgT, wout_b, 16, gp, xo, xo)
    if dbg_h1 is not None:
        P.op('sp', lambda e: e.dma_start(out=dbg_h1[r0:r0 + 128, :], in_=xo[:]), reads=[xo], writes=[dbg_h1], dma=True)
    P.op('sp', lambda e: e.dma_start(out=gp[:], in_=g2post[:]), reads=[gp], writes=[gp], dma=True)
    emit_norm_T(P, W, cb, epsc, xo, xs, ss, u2T)
    P.op('pool', lambda e: e.memset(actT[:], 0.0), writes=[actT])
    psu = W.ps_mm
    nu = 0
    for g in range(24):
        wt = W.wt[W.nw % 3]; W.nw += 1
        P.op('sp', lambda e, wt=wt, g=g: e.dma_start(out=wt[:], in_=wup_b[:, g, :, :]), reads=[wup_b], writes=[wt], dma=True)
        pm = psu[nu % 3]; c_v = cv[nu % 2]; s_g = sg[nu % 2]; nu += 1
        for cc in range(4):
            for kc in range(16):
                P.op('pe', lambda e, pm=pm, wt=wt, cc=cc, kc=kc: e.matmul(pm[:, cc * 128:(cc + 1) * 128], lhsT=wt[:, kc, cc * 128:(cc + 1) * 128], rhs=u2T[:, kc, :], start=(kc == 0), stop=(kc == 15)),
                     reads=[wt, u2T], writes=[pm])
        for cc in range(4):
            ch = (2 * g + cc) if cc < 2 else (48 + 2 * g + cc - 2)
            P.op('dve', lambda e, pm=pm, c_v=c_v, cc=cc, ch=ch: e.tensor_scalar(out=c_v[:, cc, :], in0=pm[:, cc * 128:cc * 128 + 126], scalar1=fcw_s[:, ch, 0:1], scalar2=fcb_s[:, ch:ch + 1], op0=ALU.mult, op1=ALU.add),
                 reads=[pm, fcw_s, fcb_s, c_v], writes=[c_v])
            for tp in (1, 2):
                P.op('dve', lambda e, pm=pm, c_v=c_v, cc=cc, ch=ch, tp=tp: e.scalar_tensor_tensor(out=c_v[:, cc, :], in0=pm[:, cc * 128 + tp:cc * 128 + tp + 126], scalar=fcw_s[:, ch, tp:tp + 1], in1=c_v[:, cc, :], op0=ALU.mult, op1=ALU.add),
                     reads=[pm, fcw_s, c_v], writes=[c_v])
        P.op('act', lambda e, c_v=c_v, s_g=s_g: e.activation(out=s_g[:], in_=c_v[:, 0:2, :], func=AF.Silu), reads=[c_v], writes=[s_g])
        P.op('pool', lambda e, c_v=c_v, s_g=s_g, g=g: e.tensor_tensor(out=actT[:, 2 * g:2 * g + 2, 2:128], in0=s_g[:], in1=c_v[:, 2:4, :], op=ALU.mult), reads=[c_v, s_g, actT], writes=[actT])
    post_norm_residual(actT, wdn_b, 48, gp, xo, ysb)
    P.op('sp', lambda e: e.dma_start(out=out_o[j, :, :], in_=ysb[2:128, :]), reads=[ysb], writes=[out_o], dma=True)


def own_rows(core, NS):
    rows = np.zeros(NS * 128, np.int64)
    for j in range(NS):
        s_ = NCORE * j + core
        rows[j * 128:(j + 1) * 128] = A0 + BSTR * s_ + np.arange(128)
    return rows


def prep2_common(inp):
    f = np.float32
    o = np.cumsum([0, 2048, 2048, 4096, 4096, 32, 32, 2048, 256, 256, 2048, 128, 16, 2048, 2048])
    w_in = inp['w_in'][0]
    cols = np.concatenate([np.arange(o[6], o[7]), np.arange(o[9], o[10]), np.arange(o[12], o[13]), np.arange(o[13], o[14]), np.arange(o[11], o[12])])
    cm = {
        "w_c": np.ascontiguousarray(w_in[:, cols]),
        "wbg": np.ascontiguousarray(inp['w_branch_gdn'][0]), "wba": np.ascontiguousarray(inp['w_branch_att'][0]),
        "wout": np.ascontiguousarray(inp['w_out'][0]), "wup": np.ascontiguousarray(inp['w_up'][0]), "wdn": np.ascontiguousarray(inp['w_down'][0]),
        "gpre": np.ascontiguousarray(inp['mix_pre_g'][0].reshape(16, 128).T), "g2pre": np.ascontiguousarray(inp['ffn_pre_g'][0].reshape(16, 128).T),
        "gpost": np.ascontiguousarray(np.broadcast_to(inp['mix_post_g'][0][None, :], (128, D))).astype(f),
        "g2post": np.ascontiguousarray(np.broadcast_to(inp['ffn_post_g'][0][None, :], (128, D))).astype(f),
        "fcw": np.ascontiguousarray(inp['ffn_conv_w'][0].T.reshape(96, 128, 3).transpose(1, 0, 2)),
        "fcb": np.ascontiguousarray(inp['ffn_conv_b'][0].reshape(96, 128).T),
        "cst": make_consts(),
        "iota": np.ascontiguousarray(np.broadcast_to(np.arange(NIOTA, dtype=f)[None, :], (128, NIOTA))),
    }
    return cm


def prep2(cm, hfull, og_all, KT, Vt, ikT, Lp, NS, core):
    f = np.float32
    nblk = Lp // 128
    rows = own_rows(core, NS)
    ok = rows < Lp
    rc = np.minimum(rows, Lp - 1)
    hown = np.where(ok[:, None], hfull[rc], 0).astype(f)
    ogown = None
    if og_all is not None:
        ogown = og_all[rc].copy(); ogown[~ok] = 0
    pos = np.maximum(rows - PADF, 0)
    qrel = np.zeros((128, NS), f)
    for j in range(NS):
        kd, kb_end = slot_geom(j, nblk)
        qrel[:, j] = rows[j * 128:(j + 1) * 128] - kd * 128
    m = dict(cm)
    m.update({"hown": hown, "ogown": ogown, "KT": KT, "Vt": Vt, "ikT": ikT, "ropeo": rope_table(pos), "qrel": qrel})
    return m


def kernel(**inputs):
    inp = {k: np.asarray(v) for k, v in inputs.items()}
    return kernel_fused(inp)


def kernel_unfused(**inputs):
    inp = {k: np.asarray(v) for k, v in inputs.items()}
    x = inp['x']
    SEQ = x.shape[1]
    Lp = PADF + NMETA + SEQ
    assert Lp % 128 == 0
    L = NMETA + SEQ
    topk = min(256, L // 4)
    nb, NS = blocks_for(SEQ)
    cores = list(range(NCORE))
    nc1 = build_prog1(Lp)
    ims = [prep1(inp, Lp, c) for c in cores]
    hfull = ims[0]["hfull"]
    for m in ims[1:]:
        m["hfull"] = hfull
    r1 = run_bass_kernel_spmd(nc1, ims, core_ids=cores).results
    og_all = np.concatenate([np.asarray(r1[c]["og"]) for c in cores], axis=1)
    KT = np.asarray(r1[0]["KT"]); Vt = np.asarray(r1[0]["Vt"]); ikT = np.asarray(r1[0]["ikT"])
    del ims, r1
    nc2 = build_prog2(Lp, NS, topk)
    cm = prep2_common(inp)
    ims2 = [prep2(cm, hfull, og_all, KT, Vt, ikT, Lp, NS, c) for c in cores]
    r2 = run_bass_kernel_spmd(nc2, ims2, core_ids=cores).results
    out = np.zeros((1, SEQ, D), np.float32)
    for c in cores:
        o = np.asarray(r2[c]["out"])
        for j in range(NS):
            s_ = NCORE * j + c
            t_lo = BSTR * s_; t_hi = min(SEQ, t_lo + BSTR)
            if t_hi > t_lo:
                out[0, t_lo:t_hi] = o[j, :t_hi - t_lo]
    return out


def make_sel(core):
    import ml_dtypes
    sel = np.zeros((128, 8, 128), np.float32)
    for r in range(128):
        w = BSTR * core + r
        sel[w % 128, w // 128, r] = 1.0
    return sel.astype(ml_dtypes.bfloat16)


def kernel_fused(inp):
    x = inp['x']; SEQ = x.shape[1]
    Lp = PADF + NMETA + SEQ
    L = NMETA + SEQ
    topk = min(256, L // 4)
    nb, NS = blocks_for(SEQ)
    cores = list(range(NCORE))
    p1 = [prep1(inp, Lp, c) for c in cores]
    hfull = p1[0]["hfull"]
    w_a = np.concatenate([p["w_a"] for p in p1], 0); cw = np.concatenate([p["cw"] for p in p1], 0); hp = np.concatenate([p["hp"] for p in p1], 0)
    cm = prep2_common(inp)
    cm.update({"hfull": hfull, "w_a": w_a, "cw": cw, "hp": hp, "ng": p1[0]["ng"], "rope": p1[0]["rope"]})
    del p1
    ims = []
    for c in cores:
        m = prep2(cm, hfull, None, None, None, None, Lp, NS, c)
        for k in ("ogown", "KT", "Vt", "ikT"): m.pop(k)
        m["sel"] = make_sel(c)
        ims.append(m)
    nc = build_prog2(Lp, NS, topk, fused=True)
    r2 = run_bass_kernel_spmd(nc, ims, core_ids=cores).results
    out = np.zeros((1, SEQ, D), np.float32)
    for c in cores:
        o = np.asarray(r2[c]["out"])
        for j in range(NS):
            s_ = NCORE * j + c
            t_lo = BSTR * s_; t_hi = min(SEQ, t_lo + BSTR)
            if t_hi > t_lo:
                out[0, t_lo:t_hi] = o[j, :t_hi - t_lo]
    return out
```

```python
import numpy as np
import concourse.bass as bass
import concourse.mybir as mybir
from concourse.bass_utils import run_bass_kernel_spmd
from contextlib import ExitStack

F32 = mybir.dt.float32; BF16 = mybir.dt.bfloat16; I32 = mybir.dt.int32
AF = mybir.ActivationFunctionType; ALU = mybir.AluOpType

D = 2048
NMETA = 16
PADF = 112
EPS = 1e-6
NCORE = 8
HPC = 4
NFF = 6144
ROPE_THETA = 500000.0


class Buf:
    __slots__ = ('w', 'r', 'multi')
    def __init__(self, multi=False):
        self.w = {}; self.r = {}; self.multi = multi


class Tl:
    def __init__(self, t, multi=False):
        self.t = t; self.b = Buf(multi)
    def __getitem__(self, k):
        return self.t[k]


class SMView(Tl):
    def __init__(self, T, c):
        self.t = T.t; self.b = T.b; self.c = c
    def __getitem__(self, k):
        return self.t[(k[0], self.c) + tuple(k[1:])]


class Prog:
    ENG = ('pe', 'act', 'dve', 'pool', 'sp')
    def __init__(self, nc, es, n_dma=12):
        self.nc = nc; self.es = es
        self.ops = {e: [] for e in self.ENG}
        self.cnt = {e: 0 for e in self.ENG}
        self.sem = {e: es.enter_context(nc.semaphore('s_' + e)) for e in ('pe', 'act', 'dve', 'pool')}
        self.seen = {e: {} for e in self.ENG}
        self.dsem = {q: [[es.enter_context(nc.semaphore(f'd_{q}{i}')), 0] for i in range(n_dma)] for q in ('sp', 'pool')}
        self.drr = {q: 0 for q in ('sp', 'pool')}
        self.nops = 0

    def op(self, eng, fn, reads=(), writes=(), dma=False):
        deps = {}
        def add(ev):
            s, v = ev
            k = id(s)
            if k not in deps or deps[k][1] < v: deps[k] = (s, v)
        for t in reads:
            b = t.b if isinstance(t, Tl) else t
            for ev in b.w.values(): add(ev)
        for t in writes:
            b = t.b if isinstance(t, Tl) else t
            if not b.multi:
                for ev in b.w.values(): add(ev)
                for ev in b.r.values(): add(ev)
        if dma:
            slots = self.dsem[eng]
            slot = slots[self.drr[eng] % len(slots)]; self.drr[eng] += 1
            if slot[1] > 0: add((slot[0], slot[1]))
            slot[1] += 16
            ev = (slot[0], slot[1]); inc = 16
        else:
            self.cnt[eng] += 1
            ev = (self.sem[eng], self.cnt[eng]); inc = 1
        waits = []
        seen = self.seen[eng]
        own = id(self.sem['pe']) if eng == 'pe' else None
        for k, (s, v) in deps.items():
            if k == own: continue
            if seen.get(k, 0) >= v: continue
            seen[k] = v; waits.append((s, v))
        for t in writes:
            b = t.b if isinstance(t, Tl) else t
            if b.multi:
                b.w[id(ev[0])] = ev
            else:
                b.w = {id(ev[0]): ev}; b.r = {}
        for t in reads:
            b = t.b if isinstance(t, Tl) else t
            if not b.multi:
                b.r[id(ev[0])] = ev
        self.ops[eng].append((waits, fn, ev, inc))
        self.nops += 1
        return ev

    def emit(self):
        nc = self.nc
        fin = []
        for e in ('pe', 'act', 'dve', 'pool'):
            if self.cnt[e]: fin.append((self.sem[e], self.cnt[e]))
        for q in self.dsem:
            for s, v in self.dsem[q]:
                if v: fin.append((s, v))
        bar = getattr(self, 'barrier', [])
        def run(name, e):
            for s, v in bar: e.wait_ge(s, v)
            for waits, fn, ev, inc in self.ops[name]:
                for (s, v) in waits: e.wait_ge(s, v)
                ins = fn(e)
                ins.then_inc(ev[0], inc)
            if name == 'sp':
                for s, v in fin: e.wait_ge(s, v)
            self.ops[name] = []
        self.barrier = fin
        with nc.Block() as block:
            @block.tensor
            def _(e): run('pe', e)
            @block.scalar
            def _(e): run('act', e)
            @block.vector
            def _(e): run('dve', e)
            @block.gpsimd
            def _(e): run('pool', e)
            @block.sync
            def _(e): run('sp', e)


class Ctx:
    def __init__(self, nc, es):
        self.nc = nc; self.es = es; self.P = Prog(nc, es)
        self._n = 0
    def scope(self):
        st = ExitStack()
        if not hasattr(self, '_stk'): self._stk = []
        self._stk.append(self.es); self.es = st
        return st
    def unscope(self):
        self.es = self._stk.pop()
    def sb(self, shape, dt, name=None):
        self._n += 1
        return Tl(self.es.enter_context(self.nc.sbuf_tensor(f"{name or 'sb'}_{self._n}", list(shape), dt)))
    def ps(self, shape, dt=F32, name=None):
        self._n += 1
        return Tl(self.es.enter_context(self.nc.psum_tensor(f"{name or 'ps'}_{self._n}", list(shape), dt)))
    def din(self, name, shape, dt=F32):
        return Tl(self.nc.dram_tensor(name, list(shape), dt, kind="ExternalInput").ap(), multi=True)
    def dout(self, name, shape, dt=F32):
        return Tl(self.nc.dram_tensor(name, list(shape), dt, kind="ExternalOutput").ap(), multi=True)
    def dtmp(self, name, shape, dt=BF16):
        return Tl(self.nc.dram_tensor(name, list(shape), dt, kind="Internal").ap(), multi=True)


CI_ID, CI_TRIU, CI_NEGM, CI_OFFD, CI_ONES = 0, 1, 2, 3, 4
def make_consts():
    c = np.zeros((128, 5, 128), np.float32)
    j = np.arange(128)[:, None]; i = np.arange(128)[None, :]
    c[:, CI_ID] = (i == j)
    c[:, CI_TRIU] = (j <= i)
    c[:, CI_NEGM] = np.where(i >= j, 0.0, -1e9)
    c[:, CI_OFFD] = (i != j)
    c[:, CI_ONES] = 1.0
    return c


def rope_table(pos):
    half = 16
    inv = ROPE_THETA ** (-np.arange(half, dtype=np.float32) / half)
    ang = pos.astype(np.float32)[:, None] * inv[None, :].astype(np.float32)
    return np.concatenate([np.cos(ang), np.sin(ang)], axis=1).astype(np.float32)


NA_FM = 1024
NA_TM = 512 + 8 + 256 + 256 + 128
NA = NA_FM + NA_TM


def emit_gdn_all(K, Lp, NG, hfull, w_a, gpre, cw, hp, ng, cst, rope, og_o, KT_o, V_o, ikT_o, scr, dbgt=None):
    P = K.P; nblk = Lp // 128; dbg = dbgt is not None
    if dbg: dbg_gb, dbg_q, dbg_k, dbg_v = dbgt
    if True:
        cf = K.sb([128, 5, 128], F32, "cf")
        P.op('sp', lambda e: e.dma_start(out=cf[:], in_=cst[:]), writes=[cf], dma=True)
        epsc = K.sb([128, 1], F32, "epsc")
        P.op('dve', lambda e: e.memset(epsc[:], EPS), writes=[epsc])
        cb = K.sb([128, 5, 128], BF16, "cb")
        P.op('dve', lambda e: e.tensor_copy(out=cb[:], in_=cf[:]), reads=[cf], writes=[cb])
        gpre_s = K.sb([128, 16], F32); cw_s = K.sb([128, 8, 4], F32); hp_s = K.sb([128, 3, HPC], F32); ng_s = K.sb([128, 128], F32)
        for dst, src in ((gpre_s, gpre), (ng_s, ng)):
            P.op('sp', lambda e, dst=dst, src=src: e.dma_start(out=dst[:], in_=src[:]), writes=[dst], dma=True)
        nea = K.sb([128, HPC], F32)
        ut_d = K.dtmp("ut_d", [128, 16, Lp]) if NG > 1 else None
        NPAR = min(2, NG)
        gb_alls = [K.sb([128, nblk, 8], F32, f"gb_all{i}") for i in range(NPAR)]
        pend = []

        for g in range(NG):
            gb_all = gb_alls[g % NPAR]
            qT_d, kT_d, k_d, v_d, sz_d = scr[g % NPAR]
            P.op('sp', lambda e, g=g: e.dma_start(out=cw_s[:], in_=cw[g]), writes=[cw_s], dma=True)
            P.op('sp', lambda e, g=g: e.dma_start(out=hp_s[:], in_=hp[g]), writes=[hp_s], dma=True)
            P.op('act', lambda e: e.activation(out=nea[:], in_=hp_s[:, 0, :], func=AF.Exp), reads=[hp_s], writes=[nea])
            P.op('dve', lambda e: e.tensor_scalar(out=nea[:], in0=nea[:], scalar1=-1.0, scalar2=None, op0=ALU.mult), reads=[nea], writes=[nea])
            scA = K.scope()
            wA = K.sb([128, 16, NA], BF16, "wA")
            wst = [K.sb([128, NA], F32, f"wst{i}") for i in range(1)]
            w_a_v = w_a.t[g].rearrange("(kc p) n -> p kc n", p=128)
            for kc in range(16):
                st = wst[0]
                P.op('sp', lambda e, st=st, kc=kc: e.dma_start(out=st[:], in_=w_a_v[:, kc, :]), writes=[st], dma=True)
                P.op('act', lambda e, st=st, kc=kc: e.activation(out=wA[:, kc, :], in_=st[:], func=AF.Copy, scale=gpre_s[:, kc:kc + 1]),
                     reads=[st, gpre_s], writes=[wA])

            TB = 512
            xf = [K.sb([128, D], F32, f"xf{i}") for i in range(2)]
            xs = [K.sb([128, D], BF16, f"xs{i}") for i in range(2)]
            ss = [K.sb([128, 2], F32, f"ss{i}") for i in range(2)]
            uTs = [K.sb([128, 16, TB], BF16, f"uT{i}") for i in range(2)]
            pre = K.sb([128, 8, 3 + TB], F32, "pre")
            pre_b = [Buf() for _ in range(8)]
            P.op('dve', lambda e: e.memset(pre[:], 0.0), writes=pre_b)
            cv = [K.sb([128, TB], F32, f"cv{i}") for i in range(2)]
            sl = [K.sb([128, TB], F32, f"sl{i}") for i in range(2)]
            sq = [K.sb([128, TB], BF16, f"sq{i}") for i in range(2)]
            rrs = [K.sb([128, TB], F32, f"rr{i}") for i in range(2)]
            fmTs = [K.sb([128, 8, TB], BF16, f"fmT{i}") for i in range(2)]
            tm_kv = [K.sb([128, 6, 128], BF16, f"tmkv{i}") for i in range(2)]
            ztm = [K.sb([128, 512], BF16, f"ztm{i}") for i in range(2)]
            bars = [K.sb([128, 4, 8], F32, f"bar{i}") for i in range(2)]
            vst = [K.sb([128, 256], BF16, f"vst{i}") for i in range(2)]
            kif = [K.sb([128, 3, 128], F32, f"kif{i}") for i in range(2)]
            kib = [K.sb([128, 3, 128], BF16, f"kib{i}") for i in range(2)]
            rt = [K.sb([128, 32], F32, f"rt{i}") for i in range(2)]
            rtmp = [K.sb([128, 4, 3, 16], F32, f"rtmp{i}") for i in range(2)]
            kiT = [K.sb([128, 3, 128], BF16, f"kiT{i}") for i in range(2)]
            ps_tr = [K.ps([128, 4, 128], BF16, f"pstr{i}") for i in range(2)]
            ps_mm = [K.ps([128, 512], F32, f"psmm{i}") for i in range(3)]
            ps_n = K.ps([128, 512], F32, "psn")
            nmm = [0]
            def next_mm():
                nmm[0] += 1
                return ps_mm[nmm[0] % 3]
            ntr = [0]
            def next_tr():
                ntr[0] += 1
                return ps_tr[ntr[0] % 2]

            nsb = (Lp + TB - 1) // TB
            blk = 0
            for sbi in range(nsb):
                t0 = sbi * TB
                n_sub = min(4, (Lp - t0) // 128)
                TBn = n_sub * 128
                uT = uTs[sbi % 2]; fmT = fmTs[sbi % 2]; bar = bars[sbi % 2]
                if g > 0:
                    if sbi == 0:
                        P.op('sp', lambda e, uT=uT, t0=t0, TBn=TBn: e.dma_start(out=uT[:, :, 0:TBn], in_=ut_d[:, :, t0:t0 + TBn]), reads=[ut_d], writes=[uT], dma=True)
                    if sbi + 1 < nsb:
                        t1_ = (sbi + 1) * TB; TB1 = min(4, (Lp - t1_) // 128) * 128; uT1 = uTs[(sbi + 1) % 2]
                        P.op('sp', lambda e, uT1=uT1, t1_=t1_, TB1=TB1: e.dma_start(out=uT1[:, :, 0:TB1], in_=ut_d[:, :, t1_:t1_ + TB1]), reads=[ut_d], writes=[uT1], dma=True)
                for j in range(n_sub if g == 0 else 0):
                    b = sbi * 4 + j
                    x_f = xf[b % 2]; x_s = xs[b % 2]; s_s = ss[b % 2]
                    P.op('sp', lambda e, x_f=x_f, b=b: e.dma_start(out=x_f[:], in_=hfull[b * 128:(b + 1) * 128, :]), writes=[x_f], dma=True)
                    P.op('act', lambda e, x_f=x_f, s_s=s_s, x_s=x_s: e.activation(out=x_s[:], in_=x_f[:], func=AF.Square, accum_out=s_s[:, 0:1]),
                         reads=[x_f], writes=[x_s, s_s])
                    P.op('act', lambda e, s_s=s_s: e.activation(out=s_s[:, 1:2], in_=s_s[:, 0:1], func=AF.Sqrt, scale=1.0 / D, bias=epsc[:, 0:1]),
                         reads=[s_s, epsc], writes=[s_s])
                    P.op('dve', lambda e, s_s=s_s: e.reciprocal(out=s_s[:, 1:2], in_=s_s[:, 1:2]), reads=[s_s], writes=[s_s])
                    P.op('dve', lambda e, x_f=x_f, x_s=x_s, s_s=s_s: e.tensor_scalar(out=x_s[:], in0=x_f[:], scalar1=s_s[:, 1:2], scalar2=None, op0=ALU.mult),
                         reads=[x_f, s_s], writes=[x_s])
                    for g4 in range(4):
                        pt = next_tr()
                        for q in range(4):
                            kc = g4 * 4 + q
                            P.op('pe', lambda e, pt=pt, q=q, kc=kc, x_s=x_s: e.transpose(out=pt[:, q, :], in_=x_s[:, kc * 128:(kc + 1) * 128], identity=cb[:, CI_ID, :]),
                                 reads=[x_s, cb], writes=[pt])
                        P.op('act' if g4 % 2 else 'dve',
                             (lambda e, uT=uT, pt=pt, g4=g4, j=j: e.activation(out=uT[:, g4 * 4:(g4 + 1) * 4, j * 128:(j + 1) * 128], in_=pt[:], func=AF.Copy)) if g4 % 2 else
                             (lambda e, uT=uT, pt=pt, g4=g4, j=j: e.tensor_copy(out=uT[:, g4 * 4:(g4 + 1) * 4, j * 128:(j + 1) * 128], in_=pt[:])),
                             reads=[pt], writes=[uT])
                if g == 0 and NG > 1:
                    P.op('sp', lambda e, uT=uT, t0=t0, TBn=TBn: e.dma_start(out=ut_d[:, :, t0:t0 + TBn], in_=uT[:, :, 0:TBn]), reads=[uT], writes=[ut_d], dma=True)
                pending = []
                for ch in range(8):
                    rr = rrs[ch % 2]; pre_c = pre_b[ch]
                    pm = next_mm()
                    for kc in range(16):
                        P.op('pe', lambda e, uT=uT, pm=pm, kc=kc, ch=ch, TBn=TBn: e.matmul(pm[:, 0:TBn], lhsT=wA[:, kc, ch * 128:(ch + 1) * 128], rhs=uT[:, kc, 0:TBn],
                                                                                  start=(kc == 0), stop=(kc == 15)),
                             reads=[wA, uT], writes=[pm])
                    P.op('act', lambda e, pm=pm, ch=ch, TBn=TBn: e.activation(out=pre[:, ch, 3:3 + TBn], in_=pm[:, 0:TBn], func=AF.Copy), reads=[pm], writes=[pre_c])
                    while pending: pending.pop(0)()
                    c_v = cv[ch % 2]; s_l = sl[ch % 2]; s_q = sq[ch % 2]
                    P.op('dve', lambda e, c_v=c_v, ch=ch, TBn=TBn: e.tensor_scalar(out=c_v[:, 0:TBn], in0=pre[:, ch, 0:TBn], scalar1=cw_s[:, ch, 0:1], scalar2=None, op0=ALU.mult),
                         reads=[pre_c, cw_s], writes=[c_v])
                    for tp in range(1, 4):
                        P.op('dve', lambda e, c_v=c_v, ch=ch, tp=tp, TBn=TBn: e.scalar_tensor_tensor(out=c_v[:, 0:TBn], in0=pre[:, ch, tp:tp + TBn], scalar=cw_s[:, ch, tp:tp + 1],
                                                                                                  in1=c_v[:, 0:TBn], op0=ALU.mult, op1=ALU.add),
                             reads=[pre_c, cw_s, c_v], writes=[c_v])
                    P.op('pool', lambda e, ch=ch, TBn=TBn: e.tensor_copy(out=pre[:, ch, 0:3], in_=pre[:, ch, TBn:TBn + 3]), reads=[pre_c], writes=[pre_c])
                    if ch >= 4:
                        P.op('act', lambda e, fmT=fmT, c_v=c_v, ch=ch, TBn=TBn: e.activation(out=fmT[:, ch, 0:TBn], in_=c_v[:, 0:TBn], func=AF.Silu), reads=[c_v], writes=[fmT])
                    else:
                        P.op('act', lambda e, c_v=c_v, s_l=s_l, TBn=TBn: e.activation(out=s_l[:, 0:TBn], in_=c_v[:, 0:TBn], func=AF.Silu), reads=[c_v], writes=[s_l])
                        def l2tail(ch=ch, s_l=s_l, s_q=s_q, TBn=TBn, rr=rr, fmT=fmT):
                            P.op('pool', lambda e, s_l=s_l, s_q=s_q, TBn=TBn: e.tensor_tensor(out=s_q[:, 0:TBn], in0=s_l[:, 0:TBn], in1=s_l[:, 0:TBn], op=ALU.mult),
                                 reads=[s_l], writes=[s_q])
                            P.op('pe', lambda e, s_q=s_q, TBn=TBn: e.matmul(ps_n[:, 0:TBn], lhsT=cb[:, CI_ONES, :], rhs=s_q[:, 0:TBn], start=True, stop=True),
                                 reads=[s_q, cb], writes=[ps_n])
                            P.op('act', lambda e, rr=rr, TBn=TBn: e.activation(out=rr[:, 0:TBn], in_=ps_n[:, 0:TBn], func=AF.Sqrt, bias=epsc[:, 0:1]), reads=[ps_n, epsc], writes=[rr])
                            P.op('dve', lambda e, rr=rr, TBn=TBn: e.reciprocal(out=rr[:, 0:TBn], in_=rr[:, 0:TBn]), reads=[rr], writes=[rr])
                            sc = (128 ** -0.5) if ch < 2 else 1.0
                            P.op('dve', lambda e, rr=rr, fmT=fmT, s_l=s_l, ch=ch, sc=sc, TBn=TBn: e.scalar_tensor_tensor(out=fmT[:, ch, 0:TBn], in0=s_l[:, 0:TBn], scalar=sc, in1=rr[:, 0:TBn],
                                                                                                      op0=ALU.mult, op1=ALU.mult),
                                 reads=[s_l, rr], writes=[fmT])
                        pending.append(l2tail)
                while pending: pending.pop(0)()
                for hq in range(2):
                    P.op('sp', lambda e, fmT=fmT, hq=hq, t0=t0, TBn=TBn: e.dma_start(out=qT_d[hq, :, t0:t0 + TBn], in_=fmT[:, hq, 0:TBn]), reads=[fmT], writes=[qT_d], dma=True)
                    P.op('sp', lambda e, fmT=fmT, hq=hq, t0=t0, TBn=TBn: e.dma_start(out=kT_d[hq, :, t0:t0 + TBn], in_=fmT[:, 2 + hq, 0:TBn]), reads=[fmT], writes=[kT_d], dma=True)
                if dbg:
                    for hq in range(2):
                        P.op('sp', lambda e, fmT=fmT, hq=hq, t0=t0, TBn=TBn: e.dma_start(out=dbg_q[hq, :, t0:t0 + TBn], in_=fmT[:, hq, 0:TBn]), reads=[fmT], writes=[dbg_q], dma=True)
                        P.op('sp', lambda e, fmT=fmT, hq=hq, t0=t0, TBn=TBn: e.dma_start(out=dbg_k[hq, :, t0:t0 + TBn], in_=fmT[:, 2 + hq, 0:TBn]), reads=[fmT], writes=[dbg_k], dma=True)
                for j in range(n_sub):
                    b = sbi * 4 + j
                    r0 = b * 128
                    pm = next_mm()
                    for kc in range(16):
                        P.op('pe', lambda e, uT=uT, pm=pm, kc=kc, j=j: e.matmul(pm[:, 0:512], lhsT=uT[:, kc, j * 128:(j + 1) * 128], rhs=wA[:, kc, NA_FM:NA_FM + 512],
                                                                       start=(kc == 0), stop=(kc == 15)), reads=[wA, uT], writes=[pm])
                    z_t = ztm[b % 2]
                    P.op('act', lambda e, pm=pm, z_t=z_t: e.activation(out=z_t[:], in_=pm[:, 0:512], func=AF.Silu), reads=[pm], writes=[z_t])
                    P.op('sp', lambda e, z_t=z_t, r0=r0: e.dma_start(out=sz_d[r0:r0 + 128, :], in_=z_t[:]), reads=[z_t], writes=[sz_d], dma=True)
                    pm = next_mm()
                    c0 = NA_FM + 512
                    nba = 264 if g == 0 else 8
                    for kc in range(16):
                        P.op('pe', lambda e, uT=uT, pm=pm, kc=kc, j=j, c0=c0, nba=nba: e.matmul(pm[:, 0:nba], lhsT=uT[:, kc, j * 128:(j + 1) * 128], rhs=wA[:, kc, c0:c0 + nba],
                                                                              start=(kc == 0), stop=(kc == 15)), reads=[wA, uT], writes=[pm])
                    v_s = vst[b % 2]
                    P.op('dve', lambda e, pm=pm, j=j, bar=bar: e.tensor_copy(out=bar[:, j, :], in_=pm[:, 0:8]), reads=[pm, bar], writes=[bar])
                    if g > 0: continue
                    P.op('act', lambda e, pm=pm, v_s=v_s: e.activation(out=v_s[:], in_=pm[:, 8:264], func=AF.Copy), reads=[pm], writes=[v_s])
                    if g == 0: P.op('sp', lambda e, v_s=v_s, r0=r0: e.dma_start(out=V_o[r0:r0 + 128, :], in_=v_s[:]), reads=[v_s], writes=[V_o], dma=True)
                    pm = next_mm()
                    c0 = NA_FM + 512 + 264
                    for kc in range(16):
                        P.op('pe', lambda e, uT=uT, pm=pm, kc=kc, j=j, c0=c0: e.matmul(pm[:, 0:384], lhsT=uT[:, kc, j * 128:(j + 1) * 128], rhs=wA[:, kc, c0:c0 + 384],
                                                                              start=(kc == 0), stop=(kc == 15)), reads=[wA, uT], writes=[pm])
                    k_f = kif[b % 2]; k_b = kib[b % 2]; r_t = rt[b % 2]; r_m = rtmp[b % 2]; k_T = kiT[b % 2]
                    P.op('sp', lambda e, r_t=r_t, r0=r0: e.dma_start(out=r_t[:], in_=rope[r0:r0 + 128, :]), writes=[r_t], dma=True)
                    P.op('act', lambda e, pm=pm, k_f=k_f: e.activation(out=k_f[:], in_=pm[:, 0:384], func=AF.Copy), reads=[pm], writes=[k_f])
                    P.op('act', lambda e, k_f=k_f, k_b=k_b: e.activation(out=k_b[:], in_=k_f[:], func=AF.Copy), reads=[k_f], writes=[k_b])
                    emit_rope(P, k_f, k_b, r_t, r_m, 3)
                    pt = next_tr()
                    for q in range(3):
                        P.op('pe', lambda e, pt=pt, q=q, k_b=k_b: e.transpose(out=pt[:, q, :], in_=k_b[:, q, :], identity=cb[:, CI_ID, :]), reads=[k_b, cb], writes=[pt])
                    P.op('act', lambda e, pt=pt, k_T=k_T: e.activation(out=k_T[:], in_=pt[:, 0:3, :], func=AF.Copy), reads=[pt], writes=[k_T])
                    for q in range(2 if g == 0 else 0):
                        P.op('sp', lambda e, k_T=k_T, q=q, r0=r0: e.dma_start(out=KT_o[q, :, r0:r0 + 128], in_=k_T[:, q, :]), reads=[k_T], writes=[KT_o], dma=True)
                    if g == 0: P.op('sp', lambda e, k_T=k_T, r0=r0: e.dma_start(out=ikT_o[:, r0:r0 + 128], in_=k_T[:, 2, :]), reads=[k_T], writes=[ikT_o], dma=True)
                b0 = sbi * 4
                P.op('act', lambda e, bar=bar, n_sub=n_sub: e.activation(out=bar[:, 0:n_sub, 0:4], in_=bar[:, 0:n_sub, 0:4], func=AF.Exp, scale=-1.0), reads=[bar], writes=[bar])
                P.op('dve', lambda e, bar=bar, n_sub=n_sub: e.tensor_tensor(out=bar[:, 0:n_sub, 4:8], in0=bar[:, 0:n_sub, 4:8], in1=hp_s[:, 1, :].unsqueeze(1).to_broadcast([128, n_sub, 4]), op=ALU.add),
                     reads=[bar, hp_s], writes=[bar])
                P.op('act', lambda e, bar=bar, n_sub=n_sub: e.activation(out=bar[:, 0:n_sub, 4:8], in_=bar[:, 0:n_sub, 4:8], func=AF.Exp), reads=[bar], writes=[bar])
                P.op('dve', lambda e, bar=bar, n_sub=n_sub: e.tensor_scalar(out=bar[:, 0:n_sub, :], in0=bar[:, 0:n_sub, :], scalar1=1.0, scalar2=None, op0=ALU.add), reads=[bar], writes=[bar])
                P.op('act', lambda e, bar=bar, n_sub=n_sub: e.activation(out=bar[:, 0:n_sub, 4:8], in_=bar[:, 0:n_sub, 4:8], func=AF.Ln), reads=[bar], writes=[bar])
                P.op('dve', lambda e, bar=bar, n_sub=n_sub, b0=b0: e.reciprocal(out=gb_all[:, b0:b0 + n_sub, 0:4], in_=bar[:, 0:n_sub, 0:4]), reads=[bar], writes=[gb_all])
                P.op('dve', lambda e, bar=bar, n_sub=n_sub, b0=b0: e.tensor_tensor(out=gb_all[:, b0:b0 + n_sub, 4:8], in0=bar[:, 0:n_sub, 4:8], in1=nea[:].unsqueeze(1).to_broadcast([128, n_sub, 4]), op=ALU.mult),
                     reads=[bar, nea, gb_all], writes=[gb_all])
                for j in range(n_sub):
                    b = sbi * 4 + j
                    r0 = b * 128
                    tk = tm_kv[b % 2]
                    pt = next_tr()
                    for q in range(4):
                        P.op('pe', lambda e, fmT=fmT, pt=pt, q=q, j=j: e.transpose(out=pt[:, q, :], in_=fmT[:, 4 + q, j * 128:(j + 1) * 128], identity=cb[:, CI_ID, :]),
                             reads=[fmT, cb], writes=[pt])
                    P.op('act', lambda e, pt=pt, tk=tk: e.activation(out=tk[:, 2:6, :], in_=pt[:], func=AF.Copy), reads=[pt], writes=[tk])
                    pt = next_tr()
                    for q in range(2):
                        P.op('pe', lambda e, fmT=fmT, pt=pt, q=q, j=j: e.transpose(out=pt[:, q, :], in_=fmT[:, 2 + q, j * 128:(j + 1) * 128], identity=cb[:, CI_ID, :]),
                             reads=[fmT, cb], writes=[pt])
                    P.op('dve', lambda e, pt=pt, tk=tk: e.tensor_copy(out=tk[:, 0:2, :], in_=pt[:, 0:2, :]), reads=[pt], writes=[tk])
                    P.op('sp', lambda e, tk=tk, r0=r0: e.dma_start(out=k_d[r0:r0 + 128, :], in_=tk[:, 0:2, :]), reads=[tk], writes=[k_d], dma=True)
                    P.op('sp', lambda e, tk=tk, r0=r0: e.dma_start(out=v_d[r0:r0 + 128, :], in_=tk[:, 2:6, :]), reads=[tk], writes=[v_d], dma=True)
                    if dbg:
                        P.op('sp', lambda e, tk=tk, r0=r0: e.dma_start(out=dbg_v[r0:r0 + 128, :], in_=tk[:, 2:6, :]), reads=[tk], writes=[dbg_v], dma=True)
            if dbg:
                P.op('sp', lambda e: e.dma_start(out=dbg_gb[:], in_=gb_all[:]), reads=[gb_all], writes=[dbg_gb], dma=True)

            P.emit()
            K.unscope(); scA.close()
            pend.append((gb_all, scr[g % NPAR], og_o, g * HPC * 128))
            if len(pend) == NPAR or g == NG - 1:
                scB = K.scope()
                emit_gdn_multi(K, nblk, cf, cb, epsc, ng_s, pend)
                P.emit()
                K.unscope(); scB.close()
                pend = []
    return cf, cb, epsc


def build_prog1(Lp, dbg=False, NG=1):
    nblk = Lp // 128
    nc = bass.Bass("TRN2", target_bir_lowering=False)
    es = ExitStack()
    with es:
        K = Ctx(nc, es); P = K.P
        hfull = K.din("hfull", [Lp, D])
        w_a = K.din("w_a", [NG, D, NA]); gpre = K.din("gpre", [128, 16]); cw = K.din("cw", [NG, 128, 8, 4]); hp = K.din("hp", [NG, 128, 3, HPC])
        ng = K.din("ng", [128, 128]); cst = K.din("cst", [128, 5, 128]); rope = K.din("rope", [Lp, 32])
        og_o = K.dout("og", [Lp, NG * HPC * 128], BF16)
        KT_o = K.dout("KT", [2, 128, Lp], BF16); V_o = K.dout("Vt", [Lp, 2 * 128], BF16); ikT_o = K.dout("ikT", [128, Lp], BF16)
        scr = gdn_scratch(K, Lp)
        dbgt = None
        if dbg:
            dbgt = (K.dout("dbg_gb", [128, nblk, 8]), K.dout("dbg_qT", [2, 128, Lp], BF16), K.dout("dbg_kT", [2, 128, Lp], BF16), K.dout("dbg_v", [Lp, 512], BF16))
        emit_gdn_all(K, Lp, NG, hfull, w_a, gpre, cw, hp, ng, cst, rope, og_o, KT_o, V_o, ikT_o, scr, dbgt)
    return nc


def gdn_scratch(K, Lp, n=2):
    return [(K.dtmp(f"qT_d{i}", [2, 128, Lp]), K.dtmp(f"kT_d{i}", [2, 128, Lp]), K.dtmp(f"k_d{i}", [Lp, 256]), K.dtmp(f"v_d{i}", [Lp, 512]), K.dtmp(f"sz_d{i}", [Lp, 512]))
            for i in range(n)]


def prep1(inp, Lp, core):
    f = np.float32
    x = inp['x'][0]; SEQ = x.shape[0]
    hfull = np.zeros((Lp, D), f)
    hfull[PADF:PADF + NMETA] = inp['meta_tokens']; hfull[PADF + NMETA:PADF + NMETA + SEQ] = x
    w_in = inp['w_in'][0]
    o = np.cumsum([0, 2048, 2048, 4096, 4096, 32, 32, 2048, 256, 256, 2048, 128, 16, 2048, 2048])
    gq, gk, gv, gz, gb, ga, aq, ak, av, iq, ik, iw, g1, g2 = [slice(o[i], o[i + 1]) for i in range(14)]
    c = core
    cols = np.concatenate([np.arange(o[0] + 256 * c, o[0] + 256 * c + 256), np.arange(o[1] + 256 * c, o[1] + 256 * c + 256),
                           np.arange(o[2] + 512 * c, o[2] + 512 * c + 512), np.arange(o[3] + 512 * c, o[3] + 512 * c + 512),
                           np.arange(o[4] + 4 * c, o[4] + 4 * c + 4), np.arange(o[5] + 4 * c, o[5] + 4 * c + 4),
                           np.arange(o[8], o[9]), np.arange(o[7], o[8]), np.arange(o[10], o[11])])
    w_a = np.ascontiguousarray(w_in[:, cols])
    gpre = np.ascontiguousarray(inp['mix_pre_g'][0].reshape(16, 128).T)
    cwf = inp['gdn_conv_w'][0]
    ccols = np.concatenate([np.arange(256 * c, 256 * c + 256), np.arange(2048 + 256 * c, 2048 + 256 * c + 256),
                            np.arange(4096 + 512 * c, 4096 + 512 * c + 512)])
    cw = np.ascontiguousarray(cwf[:, ccols].T.reshape(8, 128, 4).transpose(1, 0, 2))
    hp = np.zeros((128, 3, HPC), f)
    hp[:, 0, :] = inp['gdn_a_log'][0][4 * c:4 * c + 4][None, :]
    hp[:, 1, :] = inp['gdn_dt_bias'][0][4 * c:4 * c + 4][None, :]
    ng = np.ascontiguousarray(np.broadcast_to(inp['gdn_norm_g'][0][None, :], (128, 128))).astype(f)
    pos = np.maximum(np.arange(Lp) - PADF, 0)
    return {"hfull": hfull, "w_a": w_a[None], "gpre": gpre, "cw": cw[None], "hp": hp[None], "ng": ng, "cst": make_consts(), "rope": rope_table(pos)}


def emit_rope(P, xf, xb, r_t, r_m, nh):
    cosb = lambda: r_t[:, 0:16].unsqueeze(1).to_broadcast([128, nh, 16])
    sinb = lambda: r_t[:, 16:32].unsqueeze(1).to_broadcast([128, nh, 16])
    x1 = lambda: xf[:, 0:nh, 0:16]
    x2 = lambda: xf[:, 0:nh, 16:32]
    P.op('dve', lambda e: e.tensor_tensor(out=r_m[:, 0, 0:nh, :], in0=x1(), in1=cosb(), op=ALU.mult), reads=[xf, r_t], writes=[r_m])
    P.op('dve', lambda e: e.tensor_tensor(out=r_m[:, 1, 0:nh, :], in0=x2(), in1=sinb(), op=ALU.mult), reads=[xf, r_t, r_m], writes=[r_m])
    P.op('dve', lambda e: e.tensor_tensor(out=r_m[:, 2, 0:nh, :], in0=x2(), in1=cosb(), op=ALU.mult), reads=[xf, r_t, r_m], writes=[r_m])
    P.op('dve', lambda e: e.tensor_tensor(out=r_m[:, 3, 0:nh, :], in0=x1(), in1=sinb(), op=ALU.mult), reads=[xf, r_t, r_m], writes=[r_m])
    P.op('dve', lambda e: e.tensor_tensor(out=xb[:, 0:nh, 0:16], in0=r_m[:, 0, 0:nh, :], in1=r_m[:, 1, 0:nh, :], op=ALU.subtract), reads=[r_m, xb], writes=[xb])
    P.op('dve', lambda e: e.tensor_tensor(out=xb[:, 0:nh, 16:32], in0=r_m[:, 2, 0:nh, :], in1=r_m[:, 3, 0:nh, :], op=ALU.add), reads=[r_m, xb], writes=[xb])


def gdn_setup(K, nblk, cf, cb, epsc, psT, ps_s, gb_all, ng_s, qT_d, kT_d, k_d, v_d, sz_d, og_o, ogc0=0):
    P = K.P
    H = HPC
    S32 = K.sb([128, H, 128], F32, "S32"); Sb = K.sb([128, H, 128], BF16, "Sb")
    P.op('dve', lambda e: e.memset(S32[:], 0.0), writes=[S32])
    P.op('dve', lambda e: e.memset(Sb[:], 0.0), writes=[Sb])
    qT = [K.sb([128, 2, 128], BF16, f"g_qT{i}") for i in range(2)]
    kT = [K.sb([128, 2, 128], BF16, f"g_kT{i}") for i in range(2)]
    ktm = [K.sb([128, 2, 128], BF16, f"g_ktm{i}") for i in range(2)]
    vtm = [K.sb([128, H, 128], BF16, f"g_vtm{i}") for i in range(2)]
    szt = [K.sb([128, H, 128], BF16, f"g_sz{i}") for i in range(2)]
    gbc = K.sb([128, H, 128], F32, "g_gbc")
    Dt = K.sb([128, H, 128], F32, "g_Dt")
    grow = K.sb([128, H, 128], F32, "g_grow")
    ks = K.sb([128, H, 128], BF16, "g_ks")
    ksT = K.sb([128, H, 128], BF16, "g_ksT")
    Nn = [K.sb([128, H, 128], BF16, f"g_N{i}") for i in range(2)]
    NT = [K.sb([128, H, 128], BF16, f"g_NT{i}") for i in range(2)]
    Pb = K.sb([128, H, 128], BF16, "g_Pb")
    tmpO = K.sb([128, H, 128], F32, "g_tmpO")
    tmpR = K.sb([128, H, 128], F32, "g_tmpR"); tmpS = K.sb([128, H, 128], F32, "g_tmpS")
    vs = K.sb([128, H, 128], F32, "g_vs")
    Rt = K.sb([128, H, 128], BF16, "g_Rt")
    vnew = K.sb([128, H, 128], BF16, "g_vnew")
    attnT = K.sb([128, H, 128], BF16, "g_attnT")
    qgT = K.sb([128, H, 128], BF16, "g_qgT")
    kd = K.sb([128, H, 128], BF16, "g_kd")
    gz = K.sb([128, H, 128], F32, "g_gz")
    og = [K.sb([128, H, 128], BF16, f"g_og{i}") for i in range(2)]
    junk = K.sb([128, 128], BF16, "g_junk")
    ssq = K.sb([128, 2, H], F32, "g_ssq")
    ident4 = K.sb([128, H, 128], F32, "g_id4")
    offd4 = K.sb([128, H, 128], F32, "g_offd4")
    for h in range(H):
        P.op('dve', lambda e, h=h: e.tensor_copy(out=ident4[:, h, :], in_=cf[:, CI_ID, :]), reads=[cf, ident4], writes=[ident4])
        P.op('dve', lambda e, h=h: e.tensor_copy(out=offd4[:, h, :], in_=cf[:, CI_OFFD, :]), reads=[cf, offd4], writes=[offd4])
    psA = K.ps([128, H, 128], F32, "g_psA"); psB = K.ps([128, H, 128], F32, "g_psB"); psC = K.ps([128, H, 128], F32, "g_psC")

    SM = K.sb([128, nblk, 8, H], F32, "g_SM")
    gcn = K.sb([128, nblk, H], F32, "g_gcn")
    P.op('dve', lambda e: e.tensor_copy(out=gcn[:], in_=gb_all[:, :, 4:8]), reads=[gb_all], writes=[gcn])
    for b0 in range(0, nblk, 128):
        nb = min(128, nblk - b0)
        pav = lambda nb=nb: psA[:].rearrange("p h d -> p (h d)")[:, 0:nb * H].rearrange("p (b f) -> p b f", f=H)
        pbv = lambda nb=nb: psB[:].rearrange("p h d -> p (h d)")[:, 0:nb * H].rearrange("p (b f) -> p b f", f=H)
        smr = lambda r, b0=b0, nb=nb: SM[:, b0:b0 + nb, r, :]
        P.op('pe', lambda e, b0=b0, nb=nb: e.matmul(psA[:].rearrange("p h d -> p (h d)")[:, 0:nb * H], lhsT=cf[:, CI_TRIU, :],
                                                   rhs=gcn[:, b0:b0 + nb, :].rearrange("p b f -> p (b f)"), start=True, stop=True), reads=[cf, gcn], writes=[psA])
        P.op('pe', lambda e, b0=b0, nb=nb: e.matmul(psB[:].rearrange("p h d -> p (h d)")[:, 0:nb * H], lhsT=cf[:, CI_ONES, :],
                                                   rhs=gcn[:, b0:b0 + nb, :].rearrange("p b f -> p (b f)"), start=True, stop=True), reads=[cf, gcn], writes=[psB])
        P.op('dve', lambda e, pav=pav, smr=smr: e.tensor_copy(out=smr(0), in_=pav()), reads=[psA, SM], writes=[SM])
        P.op('act', lambda e, pav=pav, smr=smr: e.activation(out=smr(1), in_=pav(), func=AF.Exp), reads=[psA, SM], writes=[SM])
        P.op('act', lambda e, pbv=pbv, smr=smr: e.activation(out=smr(4), in_=pbv(), func=AF.Exp), reads=[psB, SM], writes=[SM])
        P.op('act', lambda e, smr=smr, b0=b0, nb=nb: e.activation(out=smr(2), in_=gb_all[:, b0:b0 + nb, 0:4], func=AF.Sqrt), reads=[gb_all, SM], writes=[SM])
        P.op('dve', lambda e, smr=smr: e.scalar_tensor_tensor(out=smr(3), in0=smr(2), scalar=-1.0, in1=smr(1), op0=ALU.mult, op1=ALU.mult), reads=[SM], writes=[SM])
        P.op('dve', lambda e, pbv=pbv, smr=smr: e.tensor_tensor(out=smr(7), in0=pbv(), in1=smr(0), op=ALU.subtract), reads=[psB, SM], writes=[SM])
        P.op('act', lambda e, smr=smr: e.activation(out=smr(5), in_=smr(7), func=AF.Exp), reads=[SM], writes=[SM])
        P.op('dve', lambda e, smr=smr: e.tensor_scalar(out=smr(6), in0=smr(0), scalar1=-1.0, scalar2=None, op0=ALU.mult), reads=[SM], writes=[SM])
    def chunk(c):
        r0 = c * 128
        q_T = qT[c % 2]; k_T = kT[c % 2]; k_t = ktm[c % 2]; v_t = vtm[c % 2]; s_z = szt[c % 2]; o_g = og[c % 2]
        P.op('sp', lambda e, q_T=q_T, r0=r0: e.dma_start(out=q_T[:], in_=qT_d.t[:, :, r0:r0 + 128].rearrange("h p t -> p h t")), reads=[qT_d], writes=[q_T], dma=True)
        P.op('sp', lambda e, k_T=k_T, r0=r0: e.dma_start(out=k_T[:], in_=kT_d.t[:, :, r0:r0 + 128].rearrange("h p t -> p h t")), reads=[kT_d], writes=[k_T], dma=True)
        P.op('sp', lambda e, k_t=k_t, r0=r0: e.dma_start(out=k_t[:], in_=k_d.t[r0:r0 + 128, :].rearrange("t (h d) -> t h d", h=2)), reads=[k_d], writes=[k_t], dma=True)
        P.op('sp', lambda e, v_t=v_t, r0=r0: e.dma_start(out=v_t[:], in_=v_d.t[r0:r0 + 128, :].rearrange("t (h d) -> t h d", h=H)), reads=[v_d], writes=[v_t], dma=True)
        P.op('sp', lambda e, s_z=s_z, r0=r0: e.dma_start(out=s_z[:], in_=sz_d.t[r0:r0 + 128, :].rearrange("t (h d) -> t h d", h=H)), reads=[sz_d], writes=[s_z], dma=True)
        beta = lambda: gb_all[:, c, 0:4]
        g = lambda: gb_all[:, c, 4:8]
        yield
        sm = SMView(SM, c)
        yield
        yield
        P.op('dve', lambda e, c=c: e.tensor_copy(out=gbc[:], in_=gcn[:, c, :].unsqueeze(2).to_broadcast([128, H, 128])), reads=[gcn], writes=[gbc])
        yield
        for h in range(H):
            P.op('pe', lambda e, h=h: e.matmul(psA[:, h, :], lhsT=gbc[:, h, :], rhs=cf[:, CI_TRIU, :], start=True, stop=True), reads=[gbc, cf, psA], writes=[psA])
            P.op('pe', lambda e, h=h: e.matmul(psB[:, h, :], lhsT=gbc[:, h, :], rhs=cf[:, CI_TRIU, :], start=True, stop=False), reads=[gbc, cf, psB], writes=[psB])
            P.op('pe', lambda e, h=h: e.matmul(psB[:, h, :], lhsT=cf[:, CI_ID, :], rhs=cf[:, CI_NEGM, :], start=False, stop=True), reads=[cf, psB], writes=[psB])
        P.op('act', lambda e: e.activation(out=grow[:], in_=psA[:], func=AF.Exp), reads=[psA], writes=[grow])
        yield
        for h in range(H):
            P.op('act', lambda e, h=h: e.activation(out=Dt[:, h, :], in_=psB[:, h, :], func=AF.Exp, bias=sm[:, 6, h:h + 1]), reads=[psB, sm, Dt], writes=[Dt])
        yield
        yield
        P.op('dve', lambda e, v_t=v_t: e.tensor_tensor(out=vs[:], in0=v_t[:], in1=sm[:, 2, :].unsqueeze(2).to_broadcast([128, H, 128]), op=ALU.mult), reads=[v_t, sm], writes=[vs])
        for h in range(H):
            P.op('dve', lambda e, h=h, k_t=k_t: e.tensor_scalar(out=ks[:, h, :], in0=k_t[:, h // 2, :], scalar1=sm[:, 2, h:h + 1], scalar2=None, op0=ALU.mult),
                 reads=[k_t, sm, ks], writes=[ks])
            P.op('act', lambda e, h=h, k_t=k_t: e.activation(out=kd[:, h, :], in_=k_t[:, h // 2, :], func=AF.Copy, scale=sm[:, 5, h:h + 1]),
                 reads=[k_t, sm, kd], writes=[kd])
            P.op('dve', lambda e, h=h, q_T=q_T: e.tensor_tensor(out=qgT[:, h, :], in0=q_T[:, h // 2, :], in1=grow[:, h, :], op=ALU.mult), reads=[q_T, grow, qgT], writes=[qgT])
            P.op('pool', lambda e, h=h, s_z=s_z: e.tensor_tensor(out=gz[:, h, :], in0=s_z[:, h, :], in1=ng_s[:], op=ALU.mult), reads=[s_z, ng_s, gz], writes=[gz])
        yield
        for h in range(H):
            P.op('pe', lambda e, h=h: e.transpose(out=psT[:, h, :], in_=ks[:, h, :], identity=cb[:, CI_ID, :]), reads=[ks, cb, psT], writes=[psT])
        P.op('act', lambda e: e.activation(out=ksT[:], in_=psT[:], func=AF.Copy), reads=[psT], writes=[ksT])
        yield
        yield
        for h in range(H):
            P.op('pe', lambda e, h=h: e.matmul(psA[:, h, :], lhsT=ksT[:, h, :], rhs=ksT[:, h, :], start=True, stop=True), reads=[ksT, psA], writes=[psA])
        yield
        for hq in range(2):
            P.op('pe', lambda e, hq=hq, k_T=k_T, q_T=q_T: e.matmul(psC[:, hq, :], lhsT=k_T[:, hq, :], rhs=q_T[:, hq, :], start=True, stop=True), reads=[k_T, q_T, psC], writes=[psC])
        yield
        for h in range(H):
            P.op('dve', lambda e, h=h: e.tensor_tensor(out=attnT[:, h, :], in0=psC[:, h // 2, :], in1=Dt[:, h, :], op=ALU.mult), reads=[psC, Dt, attnT], writes=[attnT])
        P.op('dve', lambda e: e.tensor_tensor(out=Dt[:], in0=Dt[:], in1=offd4[:], op=ALU.mult), reads=[Dt, offd4, attnT], writes=[Dt])
        N0 = Nn[0]; NT0 = NT[0]
        P.op('dve', lambda e: e.scalar_tensor_tensor(out=N0[:], in0=psA[:], scalar=-1.0, in1=Dt[:], op0=ALU.mult, op1=ALU.mult), reads=[psA, Dt], writes=[N0])
        yield
        for h in range(H):
            P.op('pe', lambda e, h=h: e.transpose(out=psT[:, h, :], in_=N0[:, h, :], identity=cb[:, CI_ID, :]), reads=[N0, cb, psT], writes=[psT])
        P.op('act', lambda e: e.activation(out=NT0[:], in_=psT[:], func=AF.Copy), reads=[psT], writes=[NT0])
        P.op('dve', lambda e: e.tensor_tensor(out=Pb[:], in0=N0[:], in1=ident4[:], op=ALU.add), reads=[N0, ident4], writes=[Pb])
        cur = 0
        yield
        for step in range(1, 7):
            Nc = Nn[cur]; NTc = NT[cur]; Nx = Nn[1 - cur]; NTx = NT[1 - cur]
            for h in range(H):
                P.op('pe', lambda e, h=h, Nc=Nc, NTc=NTc: e.matmul(psB[:, h, :], lhsT=Nc[:, h, :], rhs=NTc[:, h, :], start=True, stop=True), reads=[Nc, NTc, psB], writes=[psB])
            P.op('act', lambda e, NTx=NTx: e.activation(out=NTx[:], in_=psB[:], func=AF.Copy), reads=[psB], writes=[NTx])
            if step < 6:
                for h in range(H):
                    P.op('pe', lambda e, h=h, Nc=Nc, NTc=NTc: e.matmul(psA[:, h, :], lhsT=NTc[:, h, :], rhs=Nc[:, h, :], start=True, stop=True), reads=[Nc, NTc, psA], writes=[psA])
                P.op('dve', lambda e, Nx=Nx: e.tensor_copy(out=Nx[:], in_=psA[:]), reads=[psA], writes=[Nx])
            for h in range(H):
                P.op('pe', lambda e, h=h, NTx=NTx: e.matmul(psC[:, h, :], lhsT=NTx[:, h, :], rhs=Pb[:, h, :], start=True, stop=True), reads=[NTx, Pb, psC], writes=[psC])
            P.op('dve', lambda e: e.tensor_tensor(out=Pb[:], in0=Pb[:], in1=psC[:], op=ALU.add), reads=[Pb, psC], writes=[Pb])
            cur = 1 - cur
        yield
        yield
        for h in range(H):
            P.op('pe', lambda e, h=h, k_T=k_T: e.matmul(psA[:, h, :], lhsT=k_T[:, h // 2, :], rhs=Sb[:, h, :], start=True, stop=True), reads=[k_T, Sb, psA], writes=[psA])
        yield
        P.op('dve', lambda e: e.tensor_tensor(out=tmpR[:], in0=psA[:], in1=sm[:, 3, :].unsqueeze(2).to_broadcast([128, H, 128]), op=ALU.mult), reads=[psA, sm], writes=[tmpR])
        P.op('dve', lambda e: e.tensor_tensor(out=Rt[:], in0=tmpR[:], in1=vs[:], op=ALU.add), reads=[tmpR, vs], writes=[Rt])
        yield
        for h in range(H):
            P.op('pe', lambda e, h=h: e.matmul(psB[:, h, :], lhsT=Pb[:, h, :], rhs=Rt[:, h, :], start=True, stop=True), reads=[Pb, Rt, psB], writes=[psB])
        yield
        P.op('dve', lambda e: e.tensor_tensor(out=vnew[:], in0=psB[:], in1=sm[:, 2, :].unsqueeze(2).to_broadcast([128, H, 128]), op=ALU.mult), reads=[psB, sm], writes=[vnew])
        yield
        for h in range(H):
            P.op('pe', lambda e, h=h: e.matmul(psC[:, h, :], lhsT=qgT[:, h, :], rhs=Sb[:, h, :], start=True, stop=False), reads=[qgT, Sb, psC], writes=[psC])
            P.op('pe', lambda e, h=h: e.matmul(psC[:, h, :], lhsT=attnT[:, h, :], rhs=vnew[:, h, :], start=False, stop=True), reads=[attnT, vnew, psC], writes=[psC])
        yield
        for h in range(H):
            P.op('pe', lambda e, h=h: e.matmul(psA[:, h, :], lhsT=kd[:, h, :], rhs=vnew[:, h, :], start=True, stop=True), reads=[kd, vnew, psA], writes=[psA])
        yield
        P.op('dve', lambda e: e.tensor_tensor(out=tmpS[:], in0=S32[:], in1=sm[:, 4, :].unsqueeze(2).to_broadcast([128, H, 128]), op=ALU.mult), reads=[S32, sm], writes=[tmpS])
        P.op('dve', lambda e: e.tensor_tensor(out=Sb[:], in0=tmpS[:], in1=psA[:], op=ALU.add), reads=[tmpS, psA], writes=[Sb])
        P.op('dve', lambda e: e.tensor_tensor(out=S32[:], in0=tmpS[:], in1=psA[:], op=ALU.add), reads=[tmpS, psA], writes=[S32])
        yield
        yield
        for h in range(H):
            P.op('act', lambda e, h=h: e.activation(out=junk[:], in_=psC[:, h, :], func=AF.Square, accum_out=ssq[:, 0, h:h + 1]), reads=[psC, junk, ssq], writes=[junk, ssq])
        P.op('act', lambda e: e.activation(out=ssq[:, 1, :], in_=ssq[:, 0, :], func=AF.Sqrt, scale=1.0 / 128, bias=epsc[:, 0:1]), reads=[ssq, epsc], writes=[ssq])
        P.op('dve', lambda e: e.reciprocal(out=ssq[:, 1, :], in_=ssq[:, 1, :]), reads=[ssq], writes=[ssq])
        yield
        P.op('dve', lambda e: e.tensor_tensor(out=tmpO[:], in0=psC[:], in1=ssq[:, 1, :].unsqueeze(2).to_broadcast([128, H, 128]), op=ALU.mult), reads=[psC, ssq], writes=[tmpO])
        P.op('dve', lambda e, o_g=o_g: e.tensor_tensor(out=o_g[:], in0=tmpO[:], in1=gz[:], op=ALU.mult), reads=[tmpO, gz], writes=[o_g])
        P.op('sp', lambda e, o_g=o_g, r0=r0: e.dma_start(out=og_o[r0:r0 + 128, ogc0:ogc0 + HPC * 128], in_=o_g[:]), reads=[o_g], writes=[og_o], dma=True)

    return chunk


def emit_gdn_multi(K, nblk, cf, cb, epsc, ng_s, groups):
    psT = K.ps([128, HPC, 128], BF16, "g_psT")
    ps_s = K.ps([128, 2, HPC], F32, "g_pss")
    fns = [gdn_setup(K, nblk, cf, cb, epsc, psT, ps_s, gb, ng_s, *scr, og_o, c0) for (gb, scr, og_o, c0) in groups]
    for c in range(nblk):
        gens = [f(c) for f in fns]
        while gens:
            nxt = []
            for gen in gens:
                try:
                    next(gen); nxt.append(gen)
                except StopIteration:
                    pass
            gens = nxt


NWC = 2048 + 2048 + 16 + 2048 + 2048
BSTR = 126
A0 = 126
NIOTA = 1408


def blocks_for(SEQ):
    nb = -(-SEQ // BSTR)
    NS = -(-nb // NCORE)
    return nb, NS


def slot_geom(j, nblk):
    a_min = A0 + BSTR * (NCORE * j)
    a_max = A0 + BSTR * (NCORE * j + NCORE - 1)
    kd = min(a_min // 128, nblk)
    kb_end = min((a_max + 127) // 128 + 1, nblk)
    kd = min(kd, kb_end)
    return kd, kb_end


def build_prog2(Lp, NS, topk, dbg=False, fused=False):
    nblk = Lp // 128
    nc = bass.Bass("TRN2", target_bir_lowering=False)
    es = ExitStack()
    with es:
        K = Ctx(nc, es); P = K.P
        R = NS * 128
        hown = K.din("hown", [R, D])
        if not fused:
            ogown = K.din("ogown", [R, 4096], BF16)
            KT_i = K.din("KT", [2, 128, Lp], BF16); V_i = K.din("Vt", [Lp, 256], BF16); ikT_i = K.din("ikT", [128, Lp], BF16)
            ogsrc = ('own', ogown)
        else:
            NG = NCORE
            hfull = K.din("hfull", [Lp, D])
            w_a = K.din("w_a", [NG, D, NA]); cw = K.din("cw", [NG, 128, 8, 4]); hp = K.din("hp", [NG, 128, 3, HPC])
            ng = K.din("ng", [128, 128]); rope = K.din("rope", [Lp, 32])
            sel_i = K.din("sel", [128, 8, 128], BF16)
            og_d = K.dtmp("og_d", [Lp, 4096]); KT_i = K.dtmp("KT_s", [2, 128, Lp]); V_i = K.dtmp("Vt_s", [Lp, 256]); ikT_i = K.dtmp("ikT_s", [128, Lp])
        ropeo = K.din("ropeo", [R, 32])
        qrel_i = K.din("qrel", [128, NS])
        iota_i = K.din("iota", [128, NIOTA])
        cst = K.din("cst", [128, 5, 128])
        gpre = K.din("gpre", [128, 16]); g2pre = K.din("g2pre", [128, 16])
        gpost = K.din("gpost", [128, D]); g2post = K.din("g2post", [128, D])
        fcw = K.din("fcw", [128, 96, 3]); fcb = K.din("fcb", [128, 96])
        w_c = K.din("w_c", [D, NWC]); wbg = K.din("wbg", [4096, D]); wba = K.din("wba", [D, D]); wout = K.din("wout", [D, D])
        wup = K.din("wup", [D, 2 * NFF]); wdn = K.din("wdn", [NFF, D])
        out_o = K.dout("out", [NS, BSTR, D])
        wc_b = K.dtmp("wc_b", [128, 17, 16, 512]); wbg_b = K.dtmp("wbg_b", [128, 4, 32, 512]); wba_b = K.dtmp("wba_b", [128, 4, 16, 512])
        wout_b = K.dtmp("wout_b", [128, 4, 16, 512]); wup_b = K.dtmp("wup_b", [128, 24, 16, 512]); wdn_b = K.dtmp("wdn_b", [128, 4, 48, 512])
        if dbg:
            dbg_oatt = K.dout("dbg_oatt", [R, D], BF16); dbg_thr = K.dout("dbg_thr", [128, NS]); dbg_h1 = K.dout("dbg_h1", [R, D])
            dbg_q = K.dout("dbg_q", [R, D], BF16)

        if fused:
            cf, cb, epsc = emit_gdn_all(K, Lp, NG, hfull, w_a, gpre, cw, hp, ng, cst, rope, og_d, KT_i, V_i, ikT_i, gdn_scratch(K, Lp))
            sel_s = K.sb([128, 8, 128], BF16, "sel_s")
            P.op('sp', lambda e: e.dma_start(out=sel_s[:], in_=sel_i[:]), writes=[sel_s], dma=True)
            ogsrc = ('sel', og_d, sel_s, Lp)
        else:
            cf = K.sb([128, 5, 128], F32, "cf"); cb = K.sb([128, 5, 128], BF16, "cb")
            P.op('sp', lambda e: e.dma_start(out=cf[:], in_=cst[:]), writes=[cf], dma=True)
            P.op('dve', lambda e: e.tensor_copy(out=cb[:], in_=cf[:]), reads=[cf], writes=[cb])
            epsc = K.sb([128, 1], F32, "epsc")
            P.op('dve', lambda e: e.memset(epsc[:], EPS), writes=[epsc])
        gpre_s = K.sb([128, 16], F32); g2pre_s = K.sb([128, 16], F32); qrel_s = K.sb([128, NS], F32)
        fcw_s = K.sb([128, 96, 3], F32); fcb_s = K.sb([128, 96], F32)
        for dst, src in ((gpre_s, gpre), (g2pre_s, g2pre), (qrel_s, qrel_i), (fcw_s, fcw), (fcb_s, fcb)):
            P.op('sp', lambda e, dst=dst, src=src: e.dma_start(out=dst[:], in_=src[:]), writes=[dst], dma=True)

        sc0 = K.scope()
        st = [K.sb([128, 2048], F32, f"w0s{i}") for i in range(2)]
        sbt = [K.sb([128, 2048], BF16, f"w0b{i}") for i in range(2)]
        it = [0]
        def cast_w(src, dst, KC, N, gain, up=False):
            sv = src.t.rearrange("(kc p) n -> p kc n", p=128)
            for kc in range(KC):
                for n0 in range(0, N, 2048):
                    n1 = min(N, n0 + 2048); w = n1 - n0
                    s_ = st[it[0] % 2]; b_ = sbt[it[0] % 2]; it[0] += 1
                    P.op('sp', lambda e, s_=s_, kc=kc, n0=n0, n1=n1, w=w: e.dma_start(out=s_[:, 0:w], in_=sv[:, kc, n0:n1]), writes=[s_], dma=True)
                    if gain is None:
                        P.op('act', lambda e, s_=s_, b_=b_, w=w: e.activation(out=b_[:, 0:w], in_=s_[:, 0:w], func=AF.Copy), reads=[s_], writes=[b_])
                    else:
                        P.op('act', lambda e, s_=s_, b_=b_, w=w, kc=kc: e.activation(out=b_[:, 0:w], in_=s_[:, 0:w], func=AF.Copy, scale=gain[:, kc:kc + 1]),
                             reads=[s_, gain], writes=[b_])
                    if up:
                        half = n0 // NFF; g0 = (n0 % NFF) // 256
                        P.op('pool', lambda e, b_=b_, kc=kc, g0=g0, half=half: e.dma_start(out=dst[:, g0:g0 + 8, kc, half * 256:(half + 1) * 256],
                                                                                          in_=b_[:, 0:2048].rearrange("p (g c) -> p g c", c=256)), reads=[b_], writes=[dst], dma=True)
                    else:
                        nt = w // 512; rem = w % 512; t0_ = n0 // 512
                        if nt:
                            P.op('pool', lambda e, b_=b_, kc=kc, nt=nt, t0_=t0_: e.dma_start(out=dst[:, t0_:t0_ + nt, kc, :], in_=b_[:, 0:nt * 512].rearrange("p (g c) -> p g c", c=512)),
                                 reads=[b_], writes=[dst], dma=True)
                        if rem:
                            P.op('pool', lambda e, b_=b_, kc=kc, nt=nt, t0_=t0_, rem=rem: e.dma_start(out=dst[:, t0_ + nt, kc, 0:rem], in_=b_[:, nt * 512:nt * 512 + rem]),
                                 reads=[b_], writes=[dst], dma=True)
        cast_w(w_c, wc_b, 16, NWC, gpre_s)
        cast_w(wbg, wbg_b, 32, D, None); cast_w(wba, wba_b, 16, D, None); cast_w(wout, wout_b, 16, D, None)
        cast_w(wup, wup_b, 16, 2 * NFF, g2pre_s, up=True); cast_w(wdn, wdn_b, 48, D, None)
        P.emit(); K.unscope(); sc0.close()

        xo = K.sb([128, D], F32, "xo")
        QT = K.sb([128, 16, 128], BF16, "QT"); iqT = K.sb([128, 16, 128], BF16, "iqT")
        sgn = K.sb([128, 16], F32, "sgn"); aiw = K.sb([128, 16], F32, "aiw")
        sig = K.sb([128, 2, D], BF16, "sig")
        oatt = K.sb([128, D], BF16, "oatt")
        thr = K.sb([128, NS], F32, "thr")

        for j in range(NS):
            r0 = j * 128
            kd, kb_end = slot_geom(j, nblk)
            nk = kb_end * 128
            scW = K.scope()
            W = make_wpool(K)
            xs = K.sb([128, D], BF16, "p_xs"); ss = K.sb([128, 2], F32, "p_ss")
            uT = K.sb([128, 16, 128], BF16, "p_uT")
            tq = K.sb([128, 16, 128], F32, "p_tq"); tqb = K.sb([128, 16, 128], BF16, "p_tqb")
            r_t = K.sb([128, 32], F32, "p_rt"); r_m = K.sb([128, 4, 16, 16], F32, "p_rm")
            iwt = K.sb([128, 16], F32, "p_iw")
            P.op('sp', lambda e, r0=r0: e.dma_start(out=xo[:], in_=hown[r0:r0 + 128, :]), writes=[xo], dma=True)
            P.op('sp', lambda e, r0=r0: e.dma_start(out=r_t[:], in_=ropeo[r0:r0 + 128, :]), writes=[r_t], dma=True)
            emit_norm_T(P, W, cb, epsc, xo, xs, ss, uT)
            for ct in range(17):
                n0 = ct * 512 if ct < 8 else (8192 if ct == 8 else 4096 + (ct - 9) * 512)
                ncol = 16 if ct == 8 else 512
                pm = W.linear(uT, wc_b, 16, n0, ncol)
                if ct < 8:
                    hh = (ct % 4) * 4
                    if ct == 4:
                        finish_q(P, W, cb, tq, tqb, r_t, r_m, QT, 128 ** -0.5, None)
                    P.op('act', lambda e, pm=pm, hh=hh: e.activation(out=tq[:, hh:hh + 4, :], in_=pm[:, 0:512], func=AF.Copy), reads=[pm], writes=[tq])
                elif ct == 8:
                    P.op('act', lambda e, pm=pm: e.activation(out=iwt[:], in_=pm[:, 0:16], func=AF.Copy), reads=[pm], writes=[iwt])
                    P.op('act', lambda e: e.activation(out=aiw[:], in_=iwt[:], func=AF.Abs, scale=(16 ** -0.5) * (128 ** -0.5)), reads=[iwt], writes=[aiw])
                    P.op('dve', lambda e: e.tensor_scalar(out=sgn[:], in0=iwt[:], scalar1=0.0, scalar2=2.0, op0=ALU.is_gt, op1=ALU.mult), reads=[iwt], writes=[sgn])
                    P.op('dve', lambda e: e.tensor_scalar(out=sgn[:], in0=sgn[:], scalar1=-1.0, scalar2=None, op0=ALU.add), reads=[sgn], writes=[sgn])
                    finish_q(P, W, cb, tq, tqb, r_t, r_m, iqT, None, aiw)
                else:
                    gi = (ct - 9) // 4; c0 = ((ct - 9) % 4) * 512
                    P.op('act', lambda e, pm=pm, gi=gi, c0=c0: e.activation(out=sig[:, gi, c0:c0 + 512], in_=pm[:, 0:512], func=AF.Sigmoid), reads=[pm], writes=[sig])
            if dbg:
                P.op('sp', lambda e, r0=r0: e.dma_start(out=dbg_q[r0:r0 + 128, :], in_=tqb[:]), reads=[tqb], writes=[dbg_q], dma=True)
            P.emit(); K.unscope(); scW.close()

            scA = K.scope()
            emit_attention(K, cb, cf, j, kd, kb_end, topk, QT, iqT, sgn, qrel_s, iota_i, KT_i, V_i, ikT_i, oatt, thr)
            K.unscope(); scA.close()
            if dbg:
                P.op('sp', lambda e, r0=r0: e.dma_start(out=dbg_oatt[r0:r0 + 128, :], in_=oatt[:]), reads=[oatt], writes=[dbg_oatt], dma=True)

            scW = K.scope()
            W = make_wpool(K)
            emit_merge_ffn(K, W, cb, epsc, j, xo, ogsrc, oatt, sig, gpost, g2post, fcw_s, fcb_s,
                           wbg_b, wba_b, wout_b, wup_b, wdn_b, out_o, dbg_h1 if dbg else None)
            P.emit(); K.unscope(); scW.close()
        if dbg:
            P.op('sp', lambda e: e.dma_start(out=dbg_thr[:], in_=thr[:]), reads=[thr], writes=[dbg_thr], dma=True)
        P.emit()
    return nc


class WPool:
    pass


def make_wpool(K):
    P = K.P
    W = WPool()
    W.wt = [K.sb([128, 16, 512], BF16, f"wt{i}") for i in range(3)]
    W.ps_mm = [K.ps([128, 512], F32, f"w_psmm{i}") for i in range(3)]
    W.ps_tr = [K.ps([128, 4, 128], BF16, f"w_pstr{i}") for i in range(2)]
    W.nw = 0; W.nm = 0; W.nt = 0
    def next_tr():
        W.nt += 1
        return W.ps_tr[W.nt % 2]
    W.next_tr = next_tr
    def linear(xT, wd, KC, n0, ncol, pm=None, xoff=0):
        if pm is None:
            W.nm += 1; pm = W.ps_mm[W.nm % 3]
        for part in range(KC // 16):
            wt = W.wt[W.nw % 3]; W.nw += 1
            P.op('sp', lambda e, wt=wt, part=part: e.dma_start(out=wt[:, :, 0:ncol], in_=wd[:, n0 // 512, part * 16:(part + 1) * 16, 0:ncol]), reads=[wd], writes=[wt], dma=True)
            for kc in range(16):
                kk = part * 16 + kc
                P.op('pe', lambda e, wt=wt, kc=kc, kk=kk: e.matmul(pm[:, 0:ncol], lhsT=xT[:, xoff + kk, :], rhs=wt[:, kc, 0:ncol], start=(kk == 0), stop=(kk == KC - 1)),
                     reads=[xT, wt], writes=[pm])
        return pm
    W.linear = linear
    return W


def emit_norm_T(P, W, cb, epsc, x, xs, ss, uT):
    P.op('act', lambda e: e.activation(out=xs[:], in_=x[:], func=AF.Square, accum_out=ss[:, 0:1]), reads=[x], writes=[xs, ss])
    P.op('act', lambda e: e.activation(out=ss[:, 1:2], in_=ss[:, 0:1], func=AF.Sqrt, scale=1.0 / D, bias=epsc[:, 0:1]), reads=[ss, epsc], writes=[ss])
    P.op('dve', lambda e: e.reciprocal(out=ss[:, 1:2], in_=ss[:, 1:2]), reads=[ss], writes=[ss])
    P.op('dve', lambda e: e.tensor_scalar(out=xs[:], in0=x[:], scalar1=ss[:, 1:2], scalar2=None, op0=ALU.mult), reads=[x, ss], writes=[xs])
    emit_T(P, W, cb, lambda kc: xs[:, kc * 128:(kc + 1) * 128], xs, uT, 16)


def emit_T(P, W, cb, src_ap, src_tl, dstT, n):
    for g4 in range(0, n, 4):
        pt = W.next_tr()
        m = min(4, n - g4)
        for q in range(m):
            P.op('pe', lambda e, pt=pt, q=q, i=g4 + q: e.transpose(out=pt[:, q, :], in_=src_ap(i), identity=cb[:, CI_ID, :]), reads=[src_tl, cb], writes=[pt])
        if (g4 // 4) % 2:
            P.op('act', lambda e, pt=pt, g4=g4, m=m: e.activation(out=dstT[:, g4:g4 + m, :], in_=pt[:, 0:m, :], func=AF.Copy), reads=[pt], writes=[dstT])
        else:
            P.op('dve', lambda e, pt=pt, g4=g4, m=m: e.tensor_copy(out=dstT[:, g4:g4 + m, :], in_=pt[:, 0:m, :]), reads=[pt], writes=[dstT])


def finish_q(P, W, cb, tq, tqb, r_t, r_m, dstT, const_scale, row_scale):
    if row_scale is not None:
        P.op('dve', lambda e: e.tensor_tensor(out=tq[:], in0=tq[:], in1=row_scale[:].unsqueeze(2).to_broadcast([128, 16, 128]), op=ALU.mult), reads=[tq, row_scale], writes=[tq])
    else:
        P.op('dve', lambda e: e.tensor_scalar(out=tq[:], in0=tq[:], scalar1=const_scale, scalar2=None, op0=ALU.mult), reads=[tq], writes=[tq])
    P.op('act', lambda e: e.activation(out=tqb[:], in_=tq[:], func=AF.Copy), reads=[tq], writes=[tqb])
    emit_rope(P, tq, tqb, r_t, r_m, 16)
    emit_T(P, W, cb, lambda h: tqb[:, h, :], tqb, dstT, 16)


def emit_attention(K, cb, cf, j, kd, kb_end, topk, QT, iqT, sgn, qrel_s, iota_i, KT_i, V_i, ikT_i, oatt, thr):
    P = K.P
    nk = kb_end * 128
    sc = K.sb([128, nk], F32, "a_sc")
    maskT = K.sb([128, kb_end, 128], BF16, "a_maskT")
    JW = 4096
    junk = K.sb([128, JW], BF16, "a_junk")
    rl = [K.sb([128, 512], F32, f"a_rl{i}") for i in range(2)]
    ikb = [K.sb([128, 512], BF16, f"a_ik{i}") for i in range(2)]
    iot = K.sb([128, NIOTA], F32, "a_iota")
    P.op('sp', lambda e: e.dma_start(out=iot[:], in_=iota_i[:]), writes=[iot], dma=True)
    scp = K.scope()
    ps_i = [K.ps([128, 512], F32, f"a_psi{i}") for i in range(3)]
    ps_t = [K.ps([128, 4, 128], BF16, f"a_pst{i}") for i in range(2)]
    ni = 0
    for u0 in range(0, kb_end, 4):
        nb_ = min(4, kb_end - u0); w = nb_ * 128; k0 = u0 * 128
        ik = ikb[(u0 // 4) % 2]
        P.op('sp', lambda e, ik=ik, k0=k0, w=w: e.dma_start(out=ik[:, 0:w], in_=ikT_i[:, k0:k0 + w]), reads=[ikT_i], writes=[ik], dma=True)
        for h in range(16):
            pi = ps_i[ni % 3]; r_ = rl[ni % 2]; ni += 1
            P.op('pe', lambda e, pi=pi, ik=ik, h=h, w=w: e.matmul(pi[:, 0:w], lhsT=iqT[:, h, :], rhs=ik[:, 0:w], start=True, stop=True), reads=[iqT, ik], writes=[pi])
            P.op('act', lambda e, pi=pi, r_=r_, w=w: e.activation(out=r_[:, 0:w], in_=pi[:, 0:w], func=AF.Relu), reads=[pi], writes=[r_])
            if h == 0:
                P.op('dve', lambda e, r_=r_, k0=k0, w=w: e.tensor_scalar(out=sc[:, k0:k0 + w], in0=r_[:, 0:w], scalar1=sgn[:, 0:1], scalar2=None, op0=ALU.mult), reads=[r_, sgn], writes=[sc])
            else:
                P.op('dve', lambda e, r_=r_, k0=k0, w=w, h=h: e.scalar_tensor_tensor(out=sc[:, k0:k0 + w], in0=r_[:, 0:w], scalar=sgn[:, h:h + 1], in1=sc[:, k0:k0 + w], op0=ALU.mult, op1=ALU.add),
                     reads=[r_, sgn, sc], writes=[sc])
    P.op('dve', lambda e: e.memset(sc[:, 0:PADF], -1e30), reads=[sc], writes=[sc])
    wc = (kb_end - kd) * 128
    if wc > 0:
        assert wc <= NIOTA
        P.op('dve', lambda e: e.tensor_scalar(out=iot[:, 0:wc], in0=iot[:, 0:wc], scalar1=qrel_s[:, j:j + 1], scalar2=-1e30, op0=ALU.is_gt, op1=ALU.mult), reads=[iot, qrel_s], writes=[iot])
        P.op('dve', lambda e: e.tensor_tensor(out=sc[:, kd * 128:nk], in0=sc[:, kd * 128:nk], in1=iot[:, 0:wc], op=ALU.add), reads=[sc, iot], writes=[sc])
    lo = K.sb([128, 1], F32, "a_lo"); mid = K.sb([128, 1], F32, "a_mid"); cnt = K.sb([128, 8], F32, "a_cnt"); dl = K.sb([128, 1], F32, "a_dl")
    LO0, RANGE, NIT = -16.0, 64.0, 26
    P.op('dve', lambda e: e.memset(lo[:], LO0), writes=[lo])
    npc = -(-nk // JW)
    for it in range(NIT):
        hk = RANGE / (2 ** (it + 1))
        P.op('dve', lambda e, hk=hk: e.tensor_scalar(out=mid[:], in0=lo[:], scalar1=hk, scalar2=None, op0=ALU.add), reads=[lo], writes=[mid])
        for pc in range(npc):
            c0 = pc * JW; c1 = min(nk, c0 + JW)
            P.op('dve', lambda e, c0=c0, c1=c1, pc=pc: e.tensor_scalar(out=junk[:, 0:c1 - c0], in0=sc[:, c0:c1], scalar1=mid[:, 0:1], scalar2=0.0, op0=ALU.is_ge, op1=ALU.add,
                                                                     accum_out=cnt[:, pc:pc + 1]), reads=[sc, mid, junk], writes=[junk, cnt])
        if npc > 1:
            P.op('dve', lambda e: e.tensor_reduce(out=cnt[:, 7:8], in_=cnt[:, 0:npc], axis=mybir.AxisListType.X, op=ALU.add), reads=[cnt], writes=[cnt])
            cc = 7
        else:
            cc = 0
        P.op('dve', lambda e, hk=hk, cc=cc: e.tensor_scalar(out=dl[:], in0=cnt[:, cc:cc + 1], scalar1=topk - 0.5, scalar2=hk, op0=ALU.is_gt, op1=ALU.mult), reads=[cnt], writes=[dl])
        P.op('dve', lambda e: e.tensor_tensor(out=lo[:], in0=lo[:], in1=dl[:], op=ALU.add), reads=[lo, dl], writes=[lo])
    P.op('dve', lambda e: e.tensor_copy(out=thr[:, j:j + 1], in_=lo[:]), reads=[lo, thr], writes=[thr])
    mk = [K.sb([128, 512], BF16, f"a_mk{i}") for i in range(2)]
    for u0 in range(0, kb_end, 4):
        nb_ = min(4, kb_end - u0); w = nb_ * 128; k0 = u0 * 128
        m_ = mk[(u0 // 4) % 2]; pt = ps_t[(u0 // 4) % 2]
        P.op('dve', lambda e, m_=m_, k0=k0, w=w: e.tensor_scalar(out=m_[:, 0:w], in0=sc[:, k0:k0 + w], scalar1=lo[:, 0:1], scalar2=None, op0=ALU.is_ge), reads=[sc, lo], writes=[m_])
        for q in range(nb_):
            P.op('pe', lambda e, pt=pt, q=q, m_=m_: e.transpose(out=pt[:, q, :], in_=m_[:, q * 128:(q + 1) * 128], identity=cb[:, CI_ID, :]), reads=[m_, cb], writes=[pt])
        P.op('act', lambda e, pt=pt, u0=u0, nb_=nb_: e.activation(out=maskT[:, u0:u0 + nb_, :], in_=pt[:, 0:nb_, :], func=AF.Copy), reads=[pt], writes=[maskT])
    P.emit(); K.unscope(); scp.close()
    scp = K.scope()
    psO = [K.ps([128, 512], F32, f"a_pso{i}") for i in range(6)]
    psS = [K.ps([128, 4, 128], F32, f"a_pss{i}") for i in range(2)]
    Kb = [K.sb([128, 2, 128], BF16, f"a_K{i}") for i in range(3)]
    Vb = [K.sb([128, 2, 129], BF16, f"a_V{i}") for i in range(3)]
    eb = [K.sb([128, 4, 128], BF16, f"a_e{i}") for i in range(2)]
    pb = [K.sb([128, 4, 128], BF16, f"a_p{i}") for i in range(3)]
    rc = K.sb([128, 16], F32, "a_rc")
    for v_ in Vb:
        P.op('dve', lambda e, v_=v_: e.memset(v_[:, :, 128:129], 1.0), writes=[v_])
    ns = 0
    for kb in range(kb_end):
        k_ = Kb[kb % 3]; v_ = Vb[kb % 3]; k0 = kb * 128
        P.op('sp', lambda e, k_=k_, k0=k0: e.dma_start(out=k_[:], in_=KT_i.t[:, :, k0:k0 + 128].rearrange("h p t -> p h t")), reads=[KT_i], writes=[k_], dma=True)
        P.op('sp', lambda e, v_=v_, k0=k0: e.dma_start(out=v_[:, :, 0:128], in_=V_i.t[k0:k0 + 128, :].rearrange("t (h d) -> t h d", h=2)), reads=[V_i, v_], writes=[v_], dma=True)
        for g in range(2):
            for half in range(2):
                h0 = g * 8 + half * 4
                pS = psS[ns % 2]; e_ = eb[ns % 2]; p_ = pb[ns % 3]; ns += 1
                P.op('pe', lambda e, pS=pS, k_=k_, g=g, h0=h0: e.matmul(pS[:], lhsT=k_[:, g, :], rhs=QT[:, h0:h0 + 4, :], start=True, stop=True), reads=[k_, QT], writes=[pS])
                P.op('act', lambda e, pS=pS, e_=e_: e.activation(out=e_[:], in_=pS[:], func=AF.Exp), reads=[pS], writes=[e_])
                P.op('dve', lambda e, e_=e_, p_=p_, kb=kb: e.tensor_tensor(out=p_[:], in0=e_[:], in1=maskT[:, kb, :].unsqueeze(1).to_broadcast([128, 4, 128]), op=ALU.mult),
                     reads=[e_, maskT], writes=[p_])
                for hh in range(4):
                    hd = h0 + hh
                    po = psO[hd // 3]
                    P.op('pe', lambda e, po=po, hd=hd, p_=p_, hh=hh, v_=v_, g=g, kb=kb: e.matmul(po[:, (hd % 3) * 129:(hd % 3) * 129 + 129], lhsT=p_[:, hh, :], rhs=v_[:, g, :], start=(kb == 0 and hd % 3 == 0), stop=(kb == kb_end - 1 and (hd % 3 == 2 or hd == 15))),
                         reads=[p_, v_], writes=[po])
    for hd in range(16):
        po = psO[hd // 3]
        P.op('dve', lambda e, po=po, hd=hd: e.reciprocal(out=rc[:, hd:hd + 1], in_=po[:, (hd % 3) * 129 + 128:(hd % 3) * 129 + 129]), reads=[po, rc], writes=[rc])
        P.op('act', lambda e, po=po, hd=hd: e.activation(out=oatt[:, hd * 128:(hd + 1) * 128], in_=po[:, (hd % 3) * 129:(hd % 3) * 129 + 128], func=AF.Copy, scale=rc[:, hd:hd + 1]), reads=[po, rc, oatt], writes=[oatt])
    P.emit(); K.unscope(); scp.close()


def emit_merge_ffn(K, W, cb, epsc, j, xo, ogsrc, oatt, sig, gpost, g2post, fcw_s, fcb_s,
                   wbg_b, wba_b, wout_b, wup_b, wdn_b, out_o, dbg_h1):
    P = K.P
    r0 = j * 128
    if ogsrc[0] == 'own':
        ogb = K.sb([128, 4096], BF16, "m_og")
    ogT = K.sb([128, 32, 128], BF16, "m_ogT"); oaT = K.sb([128, 16, 128], BF16, "m_oaT")
    mg = K.sb([128, D], F32, "m_mg"); t1 = K.sb([128, 512], F32, "m_t1"); mgb = K.sb([128, D], BF16, "m_mgb"); mgT = K.sb([128, 16, 128], BF16, "m_mgT")
    ysb = K.sb([128, D], F32, "m_y"); gp = K.sb([128, D], F32, "m_gp")
    ssq = K.sb([128, 8], F32, "m_ssq"); junk = K.sb([128, 512], BF16, "m_junk")
    xs = K.sb([128, D], BF16, "m_xs"); ss = K.sb([128, 2], F32, "m_ss"); u2T = K.sb([128, 16, 128], BF16, "m_u2T")
    actT = K.sb([128, 48, 128], BF16, "m_actT")
    cv = [K.sb([128, 4, 126], F32, f"m_cv{i}") for i in range(2)]
    sg = [K.sb([128, 2, 126], F32, f"m_sg{i}") for i in range(2)]
    if ogsrc[0] == 'own':
        ogown = ogsrc[1]
        P.op('sp', lambda e: e.dma_start(out=ogb[:], in_=ogown[r0:r0 + 128, :]), writes=[ogb], dma=True)
    P.op('sp', lambda e: e.dma_start(out=gp[:], in_=gpost[:]), writes=[gp], dma=True)
    if ogsrc[0] == 'own':
        emit_T(P, W, cb, lambda i: ogb[:, i * 128:(i + 1) * 128], ogb, ogT, 32)
    else:
        _, og_d, sel_s, Lp_ = ogsrc
        win0 = A0 + BSTR * (NCORE * j)
        ogw = [K.sb([128, 8, 1024], BF16, f"m_ogw{i}") for i in range(2)]
        psg = W.ps_mm
        for fq in range(4):
            t_ = ogw[fq % 2]
            valid = []
            P.op('pool', lambda e, t_=t_: e.memset(t_[:], 0.0), writes=[t_])
            for kc in range(8):
                rlo = win0 + kc * 128
                nv = max(0, min(128, Lp_ - rlo))
                if nv == 0: continue
                valid.append(kc)
                P.op('sp', lambda e, t_=t_, kc=kc, rlo=rlo, nv=nv, fq=fq: e.dma_start(out=t_[0:nv, kc, :], in_=og_d[rlo:rlo + nv, fq * 1024:(fq + 1) * 1024]),
                     reads=[og_d, t_], writes=[t_], dma=True)
            for half in range(2):
                W.nm += 1; pm = psg[W.nm % 3]
                for q in range(4):
                    fc = half * 4 + q
                    for kc in valid:
                        P.op('pe', lambda e, pm=pm, q=q, fc=fc, kc=kc, t_=t_: e.matmul(pm[:, q * 128:(q + 1) * 128], lhsT=t_[:, kc, fc * 128:(fc + 1) * 128], rhs=sel_s[:, kc, :],
                                                                                  start=(kc == valid[0]), stop=(kc == valid[-1])), reads=[t_, sel_s], writes=[pm])
                i0 = fq * 8 + half * 4
                if valid:
                    P.op('act', lambda e, pm=pm, i0=i0: e.activation(out=ogT[:, i0:i0 + 4, :], in_=pm[:, 0:512], func=AF.Copy), reads=[pm], writes=[ogT])
                else:
                    P.op('pool', lambda e, i0=i0: e.memset(ogT[:, i0:i0 + 4, :], 0.0), writes=[ogT])
    emit_T(P, W, cb, lambda i: oatt[:, i * 128:(i + 1) * 128], oatt, oaT, 16)
    for ct in range(4):
        c0 = ct * 512
        pm = W.linear(ogT, wbg_b, 32, c0, 512)
        P.op('dve', lambda e, pm=pm, c0=c0: e.tensor_tensor(out=mg[:, c0:c0 + 512], in0=pm[:, 0:512], in1=sig[:, 0, c0:c0 + 512], op=ALU.mult), reads=[pm, sig, mg], writes=[mg])
        pm = W.linear(oaT, wba_b, 16, c0, 512)
        P.op('dve', lambda e, pm=pm, c0=c0: e.tensor_tensor(out=t1[:], in0=pm[:, 0:512], in1=sig[:, 1, c0:c0 + 512], op=ALU.mult), reads=[pm, sig], writes=[t1])
        P.op('pool', lambda e, c0=c0: e.tensor_tensor(out=mgb[:, c0:c0 + 512], in0=mg[:, c0:c0 + 512], in1=t1[:], op=ALU.add), reads=[mg, t1, mgb], writes=[mgb])
    emit_T(P, W, cb, lambda i: mgb[:, i * 128:(i + 1) * 128], mgb, mgT, 16)

    def post_norm_residual(src_T, wd, KC, gain_tl, res_in, res_out):
        for ct in range(4):
            c0 = ct * 512
            pm = W.linear(src_T, wd, KC, c0, 512)
            P.op('act', lambda e, pm=pm, c0=c0: e.activation(out=ysb[:, c0:c0 + 512], in_=pm[:, 0:512], func=AF.Copy), reads=[pm, ysb], writes=[ysb])
            P.op('act', lambda e, pm=pm, ct=ct: e.activation(out=junk[:], in_=pm[:, 0:512], func=AF.Square, accum_out=ssq[:, ct:ct + 1]), reads=[pm, junk, ssq], writes=[junk, ssq])
        P.op('dve', lambda e: e.tensor_reduce(out=ssq[:, 4:5], in_=ssq[:, 0:4], axis=mybir.AxisListType.X, op=ALU.add), reads=[ssq], writes=[ssq])
        P.op('act', lambda e: e.activation(out=ssq[:, 5:6], in_=ssq[:, 4:5], func=AF.Sqrt, scale=1.0 / D, bias=epsc[:, 0:1]), reads=[ssq, epsc], writes=[ssq])
        P.op('dve', lambda e: e.reciprocal(out=ssq[:, 5:6], in_=ssq[:, 5:6]), reads=[ssq], writes=[ssq])
        P.op('dve', lambda e: e.scalar_tensor_tensor(out=ysb[:], in0=ysb[:], scalar=ssq[:, 5:6], in1=gain_tl[:], op0=ALU.mult, op1=ALU.mult), reads=[ysb, ssq, gain_tl], writes=[ysb])
        P.op('pool', lambda e: e.tensor_tensor(out=res_out[:], in0=res_in[:], in1=ysb[:], op=ALU.add), reads=[res_in, ysb], writes=[res_out])

    post_norm_residual(mgT, wout_b, 16, gp, xo, xo)
    if dbg_h1 is not None:
        P.op('sp', lambda e: e.dma_start(out=dbg_h1[r0:r0 + 128, :], in_=xo[:]), reads=[xo], writes=[dbg_h1], dma=True)
    P.op('sp', lambda e: e.dma_start(out=gp[:], in_=g2post[:]), reads=[gp], writes=[gp], dma=True)
    emit_norm_T(P, W, cb, epsc, xo, xs, ss, u2T)
    P.op('pool', lambda e: e.memset(actT[:], 0.0), writes=[actT])
    psu = W.ps_mm
    nu = 0
    for g in range(24):
        wt = W.wt[W.nw % 3]; W.nw += 1
        P.op('sp', lambda e, wt=wt, g=g: e.dma_start(out=wt[:], in_=wup_b[:, g, :, :]), reads=[wup_b], writes=[wt], dma=True)
        pm = psu[nu % 3]; c_v = cv[nu % 2]; s_g = sg[nu % 2]; nu += 1
        for cc in range(4):
            for kc in range(16):
                P.op('pe', lambda e, pm=pm, wt=wt, cc=cc, kc=kc: e.matmul(pm[:, cc * 128:(cc + 1) * 128], lhsT=wt[:, kc, cc * 128:(cc + 1) * 128], rhs=u2T[:, kc, :], start=(kc == 0), stop=(kc == 15)),
                     reads=[wt, u2T], writes=[pm])
        for cc in range(4):
            ch = (2 * g + cc) if cc < 2 else (48 + 2 * g + cc - 2)
            P.op('dve', lambda e, pm=pm, c_v=c_v, cc=cc, ch=ch: e.tensor_scalar(out=c_v[:, cc, :], in0=pm[:, cc * 128:cc * 128 + 126], scalar1=fcw_s[:, ch, 0:1], scalar2=fcb_s[:, ch:ch + 1], op0=ALU.mult, op1=ALU.add),
                 reads=[pm, fcw_s, fcb_s, c_v], writes=[c_v])
            for tp in (1, 2):
                P.op('dve', lambda e, pm=pm, c_v=c_v, cc=cc, ch=ch, tp=tp: e.scalar_tensor_tensor(out=c_v[:, cc, :], in0=pm[:, cc * 128 + tp:cc * 128 + tp + 126], scalar=fcw_s[:, ch, tp:tp + 1], in1=c_v[:, cc, :], op0=ALU.mult, op1=ALU.add),
                     reads=[pm, fcw_s, c_v], writes=[c_v])
        P.op('act', lambda e, c_v=c_v, s_g=s_g: e.activation(out=s_g[:], in_=c_v[:, 0:2, :], func=AF.Silu), reads=[c_v], writes=[s_g])
        P.op('pool', lambda e, c_v=c_v, s_g=s_g, g=g: e.tensor_tensor(out=actT[:, 2 * g:2 * g + 2, 2:128], in0=s_g[:], in1=c_v[:, 2:4, :], op=ALU.mult), reads=[c_v, s_g, actT], writes=[actT])
    post_norm_residual(actT, wdn_b, 48, gp, xo, ysb)
    P.op('sp', lambda e: e.dma_start(out=out_o[j, :, :], in_=ysb[2:128, :]), reads=[ysb], writes=[out_o], dma=True)


def own_rows(core, NS):
    rows = np.zeros(NS * 128, np.int64)
    for j in range(NS):
        s_ = NCORE * j + core
        rows[j * 128:(j + 1) * 128] = A0 + BSTR * s_ + np.arange(128)
    return rows


def prep2_common(inp):
    f = np.float32
    o = np.cumsum([0, 2048, 2048, 4096, 4096, 32, 32, 2048, 256, 256, 2048, 128, 16, 2048, 2048])
    w_in = inp['w_in'][0]
    cols = np.concatenate([np.arange(o[6], o[7]), np.arange(o[9], o[10]), np.arange(o[12], o[13]), np.arange(o[13], o[14]), np.arange(o[11], o[12])])
    cm = {
        "w_c": np.ascontiguousarray(w_in[:, cols]),
        "wbg": np.ascontiguousarray(inp['w_branch_gdn'][0]), "wba": np.ascontiguousarray(inp['w_branch_att'][0]),
        "wout": np.ascontiguousarray(inp['w_out'][0]), "wup": np.ascontiguousarray(inp['w_up'][0]), "wdn": np.ascontiguousarray(inp['w_down'][0]),
        "gpre": np.ascontiguousarray(inp['mix_pre_g'][0].reshape(16, 128).T), "g2pre": np.ascontiguousarray(inp['ffn_pre_g'][0].reshape(16, 128).T),
        "gpost": np.ascontiguousarray(np.broadcast_to(inp['mix_post_g'][0][None, :], (128, D))).astype(f),
        "g2post": np.ascontiguousarray(np.broadcast_to(inp['ffn_post_g'][0][None, :], (128, D))).astype(f),
        "fcw": np.ascontiguousarray(inp['ffn_conv_w'][0].T.reshape(96, 128, 3).transpose(1, 0, 2)),
        "fcb": np.ascontiguousarray(inp['ffn_conv_b'][0].reshape(96, 128).T),
        "cst": make_consts(),
        "iota": np.ascontiguousarray(np.broadcast_to(np.arange(NIOTA, dtype=f)[None, :], (128, NIOTA))),
    }
    return cm


def prep2(cm, hfull, og_all, KT, Vt, ikT, Lp, NS, core):
    f = np.float32
    nblk = Lp // 128
    rows = own_rows(core, NS)
    ok = rows < Lp
    rc = np.minimum(rows, Lp - 1)
    hown = np.where(ok[:, None], hfull[rc], 0).astype(f)
    ogown = None
    if og_all is not None:
        ogown = og_all[rc].copy(); ogown[~ok] = 0
    pos = np.maximum(rows - PADF, 0)
    qrel = np.zeros((128, NS), f)
    for j in range(NS):
        kd, kb_end = slot_geom(j, nblk)
        qrel[:, j] = rows[j * 128:(j + 1) * 128] - kd * 128
    m = dict(cm)
    m.update({"hown": hown, "ogown": ogown, "KT": KT, "Vt": Vt, "ikT": ikT, "ropeo": rope_table(pos), "qrel": qrel})
    return m


def kernel(**inputs):
    inp = {k: np.asarray(v) for k, v in inputs.items()}
    return kernel_fused(inp)


def kernel_unfused(**inputs):
    inp = {k: np.asarray(v) for k, v in inputs.items()}
    x = inp['x']
    SEQ = x.shape[1]
    Lp = PADF + NMETA + SEQ
    assert Lp % 128 == 0
    L = NMETA + SEQ
    topk = min(256, L // 4)
    nb, NS = blocks_for(SEQ)
    cores = list(range(NCORE))
    nc1 = build_prog1(Lp)
    ims = [prep1(inp, Lp, c) for c in cores]
    hfull = ims[0]["hfull"]
    for m in ims[1:]:
        m["hfull"] = hfull
    r1 = run_bass_kernel_spmd(nc1, ims, core_ids=cores).results
    og_all = np.concatenate([np.asarray(r1[c]["og"]) for c in cores], axis=1)
    KT = np.asarray(r1[0]["KT"]); Vt = np.asarray(r1[0]["Vt"]); ikT = np.asarray(r1[0]["ikT"])
    del ims, r1
    nc2 = build_prog2(Lp, NS, topk)
    cm = prep2_common(inp)
    ims2 = [prep2(cm, hfull, og_all, KT, Vt, ikT, Lp, NS, c) for c in cores]
    r2 = run_bass_kernel_spmd(nc2, ims2, core_ids=cores).results
    out = np.zeros((1, SEQ, D), np.float32)
    for c in cores:
        o = np.asarray(r2[c]["out"])
        for j in range(NS):
            s_ = NCORE * j + c
            t_lo = BSTR * s_; t_hi = min(SEQ, t_lo + BSTR)
            if t_hi > t_lo:
                out[0, t_lo:t_hi] = o[j, :t_hi - t_lo]
    return out


def make_sel(core):
    import ml_dtypes
    sel = np.zeros((128, 8, 128), np.float32)
    for r in range(128):
        w = BSTR * core + r
        sel[w % 128, w // 128, r] = 1.0
    return sel.astype(ml_dtypes.bfloat16)


def kernel_fused(inp):
    x = inp['x']; SEQ = x.shape[1]
    Lp = PADF + NMETA + SEQ
    L = NMETA + SEQ
    topk = min(256, L // 4)
    nb, NS = blocks_for(SEQ)
    cores = list(range(NCORE))
    p1 = [prep1(inp, Lp, c) for c in cores]
    hfull = p1[0]["hfull"]
    w_a = np.concatenate([p["w_a"] for p in p1], 0); cw = np.concatenate([p["cw"] for p in p1], 0); hp = np.concatenate([p["hp"] for p in p1], 0)
    cm = prep2_common(inp)
    cm.update({"hfull": hfull, "w_a": w_a, "cw": cw, "hp": hp, "ng": p1[0]["ng"], "rope": p1[0]["rope"]})
    del p1
    ims = []
    for c in cores:
        m = prep2(cm, hfull, None, None, None, None, Lp, NS, c)
        for k in ("ogown", "KT", "Vt", "ikT"): m.pop(k)
        m["sel"] = make_sel(c)
        ims.append(m)
    nc = build_prog2(Lp, NS, topk, fused=True)
    r2 = run_bass_kernel_spmd(nc, ims, core_ids=cores).results
    out = np.zeros((1, SEQ, D), np.float32)
    for c in cores:
        o = np.asarray(r2[c]["out"])
        for j in range(NS):
            s_ = NCORE * j + c
            t_lo = BSTR * s_; t_hi = min(SEQ, t_lo + BSTR)
            if t_hi > t_lo:
                out[0, t_lo:t_hi] = o[j, :t_hi - t_lo]
    return out
```
